# Optimizing a Trainium2 kernel written in Bass

```python
import math
import jax, jax.numpy as jnp
from jax import lax
import numpy as np

D_MODEL = 1024
BATCH = 2
SEQ = 8192
DEPTH = 2

N_BRANCH = 4
A_HEADS = 4
A_HEAD_DIM = 128
A_WIDTH = A_HEADS * A_HEAD_DIM
IDX_HEADS = 4
IDX_DIM = 64
TOPK_MAX = 256
CB_WIDTH = 512
CB_CONV = 31
CC_WIDTH = 512
CC_CONV = 3
SB_HEADS = 4
SB_HEAD_DIM = 128
SB_WIDTH = SB_HEADS * SB_HEAD_DIM
D_FF = 2816
FFN_CONV = 3
ROPE_THETA = 500000.0
ROT_FRACTION_DIV = 4
BLOCK_Q = 128
NORM_EPS = 1e-6

SPLIT_SIZES = (A_WIDTH, A_WIDTH, A_WIDTH, IDX_HEADS * IDX_DIM, IDX_DIM, IDX_HEADS,
               2 * CB_WIDTH, 3 * CC_WIDTH, 3 * SB_WIDTH, N_BRANCH * D_MODEL)
N_IN = sum(SPLIT_SIZES)

kernel_name = "hybrid_gated_dsa_conformer_shortconv_stickbreak"


def _split_offsets(sizes):
    offs, acc = [], 0
    for s in sizes[:-1]:
        acc += s
        offs.append(acc)
    return offs


def rms_norm(x, g):
    xf = x.astype(jnp.float32)
    y = xf * lax.rsqrt(jnp.mean(xf * xf, axis=-1, keepdims=True) + NORM_EPS)
    return (y * g.astype(jnp.float32)).astype(x.dtype)


def layer_norm(x, g, b):
    xf = x.astype(jnp.float32)
    mu = jnp.mean(xf, axis=-1, keepdims=True)
    xc = xf - mu
    y = xc * lax.rsqrt(jnp.mean(xc * xc, axis=-1, keepdims=True) + NORM_EPS)
    return (y * g.astype(jnp.float32) + b.astype(jnp.float32)).astype(x.dtype)


def partial_rope(x, pos):
    d = x.shape[-1]
    rot = d // ROT_FRACTION_DIV
    half = rot // 2
    inv_freq = ROPE_THETA ** (-(jnp.arange(half, dtype=jnp.float32) * 2.0) / rot)
    ang = pos.astype(jnp.float32)[..., None] * inv_freq
    cos = jnp.cos(ang)[:, :, None, :]
    sin = jnp.sin(ang)[:, :, None, :]
    xf = x.astype(jnp.float32)
    x1, x2 = xf[..., :half], xf[..., half:rot]
    out = jnp.concatenate([x1 * cos - x2 * sin, x2 * cos + x1 * sin, xf[..., rot:]], axis=-1)
    return out.astype(x.dtype)


def causal_dwconv(x, w, b=None):
    width = w.shape[0]
    y = lax.conv_general_dilated(
        x, w.astype(x.dtype)[:, None, :], window_strides=(1,), padding=[(width - 1, 0)],
        dimension_numbers=('NWC', 'WIO', 'NWC'), feature_group_count=x.shape[-1])
    if b is not None:
        y = y + b.astype(x.dtype)
    return y


def _to_blocks(a, nb):
    return jnp.moveaxis(a.reshape((a.shape[0], nb, BLOCK_Q) + a.shape[2:]), 1, 0)


def _from_blocks(a):
    a = jnp.moveaxis(a, 0, 1)
    return a.reshape(a.shape[0], -1, a.shape[-2] * a.shape[-1])


def dsa_attention(q, k, v, q_idx, k_idx, w_idx):
    bsz, seq = q.shape[0], q.shape[1]
    nb = seq // BLOCK_Q
    k_sel = min(TOPK_MAX, seq // 4)
    kv = jnp.stack([k, v], axis=2)
    s_idx = jnp.arange(seq)
    k_idx_f = k_idx.astype(jnp.float32)

    def block(args):
        qb, qib, wb, tb = args
        rel = jax.nn.relu(jnp.einsum('bthd,bsd->bths', qib.astype(jnp.float32), k_idx_f) * IDX_DIM ** -0.5)
        score = jnp.einsum('bths,bth->bts', rel, wb.astype(jnp.float32)) * IDX_HEADS ** -0.5
        causal = s_idx[None, :] <= tb[:, None]
        score = jnp.where(causal[None], score, -jnp.inf)
        _, sel = lax.top_k(score, k_sel)
        valid = sel <= tb[None, :, None]
        kv_sel = jax.vmap(lambda a, i: a[i])(kv, sel)
        logits = jnp.einsum('bthd,btkhd->bthk', qb.astype(jnp.float32),
                            kv_sel[:, :, :, 0].astype(jnp.float32)) * A_HEAD_DIM ** -0.5
        logits = jnp.where(valid[:, :, None, :], logits, -jnp.inf)
        p = jax.nn.softmax(logits, axis=-1)
        out = jnp.einsum('bthk,btkhd->bthd', p, kv_sel[:, :, :, 1].astype(jnp.float32))
        return out.astype(qb.dtype)

    t_blocks = jnp.arange(seq).reshape(nb, BLOCK_Q)
    out = lax.map(block, (_to_blocks(q, nb), _to_blocks(q_idx, nb), _to_blocks(w_idx, nb), t_blocks))
    return _from_blocks(out)


def stick_breaking_attention(q, k, v):
    seq = q.shape[1]
    nb = seq // BLOCK_Q
    s_idx = jnp.arange(seq)
    k_f = k.astype(jnp.float32)
    v_f = v.astype(jnp.float32)

    def block(args):
        qb, tb = args
        z = jnp.einsum('bthd,bshd->bhts', qb.astype(jnp.float32), k_f) * SB_HEAD_DIM ** -0.5
        mask = (s_idx[None, :] < tb[:, None])[None, None]
        log_keep = jnp.where(mask, jax.nn.log_sigmoid(-z), 0.0)
        between = lax.cumsum(log_keep, axis=3, reverse=True) - log_keep
        a = jnp.where(mask, jnp.exp(jax.nn.log_sigmoid(z) + between), 0.0)
        return jnp.einsum('bhts,bshd->bthd', a, v_f).astype(qb.dtype)

    t_blocks = jnp.arange(seq).reshape(nb, BLOCK_Q)
    out = lax.map(block, (_to_blocks(q, nb), t_blocks))
    return _from_blocks(out)


def setup_inputs(seed: int = 0) -> dict:
    key = jax.random.key(seed)
    ks = jax.random.split(key, 24)
    f32 = jnp.float32

    def nrm(k, shape, scale):
        return jax.random.normal(k, shape, f32) * scale

    def gain(k, shape):
        return 1.0 + 0.05 * jax.random.normal(k, shape, f32)

    L, D = DEPTH, D_MODEL
    return {
        "x": jax.random.normal(ks[0], (BATCH, SEQ, D), f32),
        "positions": jnp.broadcast_to(jnp.arange(SEQ, dtype=jnp.int32), (BATCH, SEQ)),
        "g_mix": gain(ks[1], (L, D)),
        "w_in": nrm(ks[2], (L, D, N_IN), D ** -0.5),
        "b_gate": nrm(ks[3], (L, N_BRANCH * D), 0.1),
        "g_qa": gain(ks[4], (L, A_HEAD_DIM)),
        "g_ka": gain(ks[5], (L, A_HEAD_DIM)),
        "g_kidx": gain(ks[6], (L, IDX_DIM)),
        "w_oa": nrm(ks[7], (L, A_WIDTH, D), A_WIDTH ** -0.5),
        "cb_conv_w": nrm(ks[8], (L, CB_CONV, CB_WIDTH), CB_CONV ** -0.5),
        "cb_conv_b": nrm(ks[9], (L, CB_WIDTH), 0.02),
        "cb_ln_g": gain(ks[10], (L, CB_WIDTH)),
        "cb_ln_b": nrm(ks[11], (L, CB_WIDTH), 0.02),
        "w_ob": nrm(ks[12], (L, CB_WIDTH, D), CB_WIDTH ** -0.5),
        "cc_conv_w": nrm(ks[13], (L, CC_CONV, CC_WIDTH), CC_CONV ** -0.5),
        "w_oc": nrm(ks[14], (L, CC_WIDTH, D), CC_WIDTH ** -0.5),
        "w_od": nrm(ks[15], (L, SB_WIDTH, D), SB_WIDTH ** -0.5),
        "w_merge": nrm(ks[16], (L, D, D), D ** -0.5),
        "g_ffn": gain(ks[17], (L, D)),
        "w_ffn_gate": nrm(ks[18], (L, D, D_FF), D ** -0.5),
        "w_ffn_up": nrm(ks[19], (L, D, D_FF), D ** -0.5),
        "ffn_conv_w": nrm(ks[20], (L, FFN_CONV, D_FF), FFN_CONV ** -0.5),
        "w_ffn_down": nrm(ks[21], (L, D_FF, D), D_FF ** -0.5),
    }


def reference(x, positions, g_mix, w_in, b_gate, g_qa, g_ka, g_kidx, w_oa,
              cb_conv_w, cb_conv_b, cb_ln_g, cb_ln_b, w_ob, cc_conv_w, w_oc, w_od,
              w_merge, g_ffn, w_ffn_gate, w_ffn_up, ffn_conv_w, w_ffn_down):
    bsz, seq, dm = x.shape
    offs = _split_offsets(SPLIT_SIZES)
    for l in range(DEPTH):
        h = rms_norm(x, g_mix[l])
        proj = h @ w_in[l]
        aq, ak, av, iq, ik, iw, glu_in, cc_in, sb_in, gate_in = jnp.split(proj, offs, axis=-1)

        qa = partial_rope(rms_norm(aq.reshape(bsz, seq, A_HEADS, A_HEAD_DIM), g_qa[l]), positions)
        ka = partial_rope(rms_norm(ak.reshape(bsz, seq, A_HEADS, A_HEAD_DIM), g_ka[l]), positions)
        va = av.reshape(bsz, seq, A_HEADS, A_HEAD_DIM)
        qi = partial_rope(iq.reshape(bsz, seq, IDX_HEADS, IDX_DIM), positions)
        ki = partial_rope(rms_norm(ik, g_kidx[l])[:, :, None, :], positions)[:, :, 0]
        y_a = dsa_attention(qa, ka, va, qi, ki, iw) @ w_oa[l]

        glu_a, glu_b = jnp.split(glu_in, 2, axis=-1)
        u = causal_dwconv(glu_a * jax.nn.sigmoid(glu_b), cb_conv_w[l], cb_conv_b[l])
        y_b = jax.nn.silu(layer_norm(u, cb_ln_g[l], cb_ln_b[l])) @ w_ob[l]

        gb, gc, xc = jnp.split(cc_in, 3, axis=-1)
        y_c = (gb * causal_dwconv(gc * xc, cc_conv_w[l])) @ w_oc[l]

        sq, sk, sv = jnp.split(sb_in, 3, axis=-1)
        shp = (bsz, seq, SB_HEADS, SB_HEAD_DIM)
        y_d = stick_breaking_attention(sq.reshape(shp), sk.reshape(shp), sv.reshape(shp)) @ w_od[l]

        gates = jax.nn.sigmoid(gate_in + b_gate[l]).reshape(bsz, seq, N_BRANCH, dm)
        merged = (gates[:, :, 0] * y_a + gates[:, :, 1] * y_b
                  + gates[:, :, 2] * y_c + gates[:, :, 3] * y_d)
        x = x + merged @ w_merge[l]

        h2 = rms_norm(x, g_ffn[l])
        gt = causal_dwconv(h2 @ w_ffn_gate[l], ffn_conv_w[l])
        x = x + (jax.nn.silu(gt) * (h2 @ w_ffn_up[l])) @ w_ffn_down[l]
    return x
```

```python
import math
import contextlib
import numpy as np
from concourse.bass_utils import run_bass_kernel_spmd
import numpy as np
import concourse.bass as bass
import concourse.mybir as mybir

F32 = mybir.dt.float32
BF16 = mybir.dt.bfloat16
I32 = mybir.dt.int32
ALU = mybir.AluOpType
AF = mybir.ActivationFunctionType
AX = mybir.AxisListType

SEM_EPOCH = 30000


class Prog:
    ENGS = ('pe', 'act', 'dve', 'pool', 'sp')

    def __init__(self, nc, n_dma_sems=6):
        self.nc = nc
        self.ins = []
        self.stream = {e: [] for e in self.ENGS}
        self.last_w = {}
        self.readers = {}
        self.n_dma_sems = n_dma_sems
        self.dma_rr = {e: 0 for e in self.ENGS}
        self.dma_slot_last = {}

    def _deps(self, eng, reads, writes, is_dma):
        deps = set()
        for k in reads:
            w = self.last_w.get(k)
            if w is not None:
                deps.add(w)
        for k in writes:
            w = self.last_w.get(k)
            if w is not None:
                deps.add(w)
            for r in self.readers.get(k, ()):
                deps.add(r)
        out = []
        for d in deps:
            di = self.ins[d]
            if (not is_dma) and (not di['dma']) and di['eng'] == eng:
                if eng == 'pe':
                    continue
            out.append(d)
        return out

    def _commit(self, iid, reads, writes):
        for k in reads:
            self.readers.setdefault(k, []).append(iid)
        for k in writes:
            self.last_w[k] = iid
            self.readers[k] = []

    def op(self, eng, fn, reads=(), writes=()):
        reads = list(reads); writes = list(writes)
        deps = self._deps(eng, reads, writes, False)
        iid = len(self.ins)
        self.ins.append(dict(eng=eng, fn=fn, deps=deps, dma=False, signal=False))
        self.stream[eng].append(iid)
        self._commit(iid, reads, writes)
        return iid

    def dma(self, eng, out, in_, reads=(), writes=(), **kw):
        reads = list(reads); writes = list(writes)
        deps = self._deps(eng, reads, writes, True)
        slot = self.dma_rr[eng] % self.n_dma_sems
        self.dma_rr[eng] += 1
        prev = self.dma_slot_last.get((eng, slot))
        if prev is not None and prev not in deps:
            deps.append(prev)
        iid = len(self.ins)
        self.ins.append(dict(eng=eng, fn=None, deps=deps, dma=True, signal=True,
                             slot=slot, out=out, in_=in_, kw=kw))
        self.dma_slot_last[(eng, slot)] = iid
        self.stream[eng].append(iid)
        self._commit(iid, reads, writes)
        return iid

    def cc(self, eng, src, dst, reads=(), writes=()):
        reads = list(reads); writes = list(writes)
        deps = self._deps(eng, reads, writes, True)
        n = self.dma_rr.get((eng, 'cc'), 0)
        self.dma_rr[(eng, 'cc')] = n + 1
        slot = 100 + n % 4
        prev = self.dma_slot_last.get((eng, slot))
        if prev is not None and prev not in deps:
            deps.append(prev)
        iid = len(self.ins)
        self.ins.append(dict(eng=eng, fn=None, deps=deps, dma=True, signal=True, slot=slot, cc=(src, dst), inc=1))
        self.dma_slot_last[(eng, slot)] = iid
        self.stream[eng].append(iid)
        self._commit(iid, reads, writes)
        return iid

    def emit(self, final_wait_eng='sp'):
        nc = self.nc
        ins = self.ins
        pos = {}
        for e in self.ENGS:
            for p, iid in enumerate(self.stream[e]):
                pos[iid] = p
        def chan(i):
            d = ins[i]
            return ('d', d['eng'], d['slot']) if d['dma'] else ('c', d['eng'])
        need = {}
        for e in self.ENGS:
            waited = {}
            for iid in self.stream[e]:
                lst = []
                for d in sorted(ins[iid]['deps'], key=lambda x: -pos[x]):
                    c = chan(d)
                    if waited.get(c, -1) >= pos[d]:
                        continue
                    waited[c] = pos[d]
                    ins[d]['signal'] = True
                    lst.append(d)
                need[iid] = lst
        final = list(self.dma_slot_last.values())
        semcount = {}
        sems = {}
        stack = []

        def get_sem(key):
            if key not in sems:
                s = nc.alloc_semaphore(name=nc.make_name("s_" + "_".join(str(k) for k in key), True))
                sems[key] = s
            return sems[key]

        for e in self.ENGS:
            ccount = 0
            dcount = {}
            for iid in self.stream[e]:
                d = ins[iid]
                if d['dma']:
                    sl = d['slot']
                    dcount[sl] = dcount.get(sl, 0) + 16
                    d['semkey'] = ('d', e, sl, dcount[sl] // (SEM_EPOCH * 16 + 16))
                    d['semval'] = dcount[sl] - d['semkey'][3] * (SEM_EPOCH * 16 + 16) if False else None
                elif d['signal']:
                    ccount += 1
                    ep = (ccount - 1) // SEM_EPOCH
                    d['semkey'] = ('c', e, ep)
                    d['semval'] = ccount - ep * SEM_EPOCH
            dc2 = {}
            for iid in self.stream[e]:
                d = ins[iid]
                if d['dma']:
                    sl = d['slot']
                    n = dc2.get(sl, 0) + 1
                    dc2[sl] = n
                    ep = (n - 1) // 2000
                    d['semkey'] = ('d', e, sl, ep)
                    d['semval'] = d.get('inc', 16) * (n - ep * 2000)
        engobj = {'pe': 'tensor', 'act': 'scalar', 'dve': 'vector', 'pool': 'gpsimd', 'sp': 'sync'}
        self.n_waits = 0

        def run_stream(e, eng):
            for iid in self.stream[e]:
                d = ins[iid]
                for dep in need[iid]:
                    dd = ins[dep]
                    eng.wait_ge(get_sem(dd['semkey']), dd['semval'])
                    self.n_waits += 1
                if d['dma'] and 'cc' in d:
                    eng.collective_compute("AllGather", mybir.AluOpType.bypass, replica_groups=[[0, 1, 2, 3], [4, 5, 6, 7]],
                                           ins=[d['cc'][0].ap().opt()], outs=[d['cc'][1].ap().opt()]).then_inc(get_sem(d['semkey']))
                elif d['dma']:
                    eng.dma_start(out=d['out'], in_=d['in_'], **d['kw']).then_inc(
                        get_sem(d['semkey']), 16)
                else:
                    r = d['fn'](eng)
                    if d['signal']:
                        r.then_inc(get_sem(d['semkey']), 1)
            if e == final_wait_eng:
                for f in final:
                    dd = ins[f]
                    eng.wait_ge(get_sem(dd['semkey']), dd['semval'])

        for e in self.ENGS:
            for iid in self.stream[e]:
                d = ins[iid]
                if d['dma'] or d['signal']:
                    get_sem(d['semkey'])
        with nc.Block() as block:
            for e in self.ENGS:
                if not self.stream[e] and e != final_wait_eng:
                    continue
                deco = getattr(block, engobj[e])
                deco(lambda eng, e=e: run_stream(e, eng))
        nc.clear_and_free_semaphores(list(sems.values()))
        nc.all_engine_barrier()
        return len(sems)


T = 2048
D = 1024
N_IN = 10052
EPS = 1e-6
PI = math.pi
SINK = 0.999999

O_AQ, O_AK, O_AV, O_IQ, O_IK, O_IW, O_GLU, O_CC, O_SQ, O_SK, O_SV, O_GATE = (
    0, 512, 1024, 1536, 1792, 1856, 1860, 2884, 4420, 4932, 5444, 5956)
R_QA, R_QI, R_SQ, NR_Q = 0, 512, 768, 1280
R_KI, R_KA, R_SK, NR_K = 0, 64, 576, 1088
R_GLU, R_CC, R_GATE, NR_F32 = 0, 1024, 2560, 6656


def l1_items():
    items = []
    for h in range(4):
        items.append(('qkA', O_AQ + 128 * h, 128, R_QA + 128 * h, 0))
    for h in range(4):
        items.append(('qkA', O_AK + 128 * h, 128, R_KA + 128 * h, 1))
    items.append(('tmv', O_AV, 256, 0, 0))
    items.append(('tmv', O_AV + 256, 256, 256, 0))
    items.append(('qi', O_IQ, 128, R_QI, 0))
    items.append(('qi', O_IQ + 128, 128, R_QI + 128, 0))
    items.append(('ki', O_IK, 64, R_KI, 0))
    items.append(('iw', O_IW, 4, 0, 0))
    for j in range(8):
        items.append(('f32', O_GLU + 128 * j, 128, R_GLU + 128 * j, 0))
    for j in range(12):
        items.append(('f32', O_CC + 128 * j, 128, R_CC + 128 * j, 0))
    for j in range(4):
        items.append(('bf', O_SQ + 128 * j, 128, R_SQ + 128 * j, 0))
    for j in range(4):
        items.append(('bf', O_SK + 128 * j, 128, R_SK + 128 * j, 0))
    items.append(('tmv', O_SV, 256, 512, 0))
    items.append(('tmv', O_SV + 256, 256, 768, 0))
    for j in range(32):
        items.append(('f32', O_GATE + 128 * j, 128, R_GATE + 128 * j, 0))
    groups = []
    cur = []
    for it in items:
        if cur and (it[1] + it[2] - cur[0][1]) > 256:
            groups.append(cur); cur = []
        cur.append(it)
    if cur:
        groups.append(cur)
    return groups


def l1_consts():
    half = 16
    invA = (500000.0 ** (-(np.arange(half, dtype=np.float32) * 2.0) / 32)).astype(np.float32)
    invI = (500000.0 ** (-(np.arange(8, dtype=np.float32) * 2.0) / 16)).astype(np.float32)
    cst = np.zeros((128, 8), np.float32)
    for p in range(32):
        cst[p, 0] = invA[p % 16]
    for p in range(128):
        if p % 64 < 16:
            cst[p, 1] = invI[p % 8]
    cst[:, 3] = 0.0
    cst[:, 4] = 0.5 * PI * SINK
    RmA = np.zeros((128, 128), np.float32)
    for m in range(16):
        RmA[m + 16, m] = -1.0
        RmA[m, m + 16] = 1.0
    RmI = np.zeros((128, 128), np.float32)
    for b in (0, 64):
        for m in range(8):
            RmI[b + m + 8, b + m] = -1.0
            RmI[b + m, b + m + 8] = 1.0
    return cst, RmA, RmI


def build_l1(nc, IO, pfx):
    xT, pos, w, gmix, gq, cst, rmA, rmI = IO['xT'], IO['pos'], IO['w'], IO['gmix'], IO['gq'], IO['cst'], IO['rmA'], IO['rmI']
    o_q, o_k, o_f32, o_v, o_iw, tail1 = IO['o_q'], IO['o_k'], IO['o_f32'], IO['o_v'], IO['o_iw'], IO['tail1']

    P = Prog(nc)
    import contextlib
    es = contextlib.ExitStack()

    def sb(name, shape, dt):
        return es.enter_context(nc.sbuf_tensor(pfx + name, shape, dt))

    def pst(name, shape, dt=F32):
        return es.enter_context(nc.psum_tensor(pfx + name, shape, dt))

    with es:
        hT = sb("hT", [128, 8, T], BF16)
        xs = [sb(f"xs{i}", [128, 8, 256], F32) for i in range(2)]
        sqs = sb("sqs", [128, 8, 256], F32)
        rs = sb("rs", [128, 256], F32)
        gm = sb("gm", [128, 8], F32)
        gqt = sb("gqt", [128, 4], F32)
        cs = sb("cs", [128, 8], F32)
        rA = sb("rA", [128, 128], F32)
        rI = sb("rI", [128, 128], F32)
        ones = sb("ones", [128, 128], F32)
        posi = sb("posi", [128, T], I32)
        posf = sb("posf", [128, T], F32)
        ang = sb("ang", [128, T], F32)
        posf2 = sb("posf2", [128, T], F32)
        posi2 = sb("posi2", [128, T], I32)
        cosA = sb("cosA", [128, T], F32)
        sinA = sb("sinA", [128, T], F32)
        cosI = sb("cosI", [128, T], F32)
        sinI = sb("sinI", [128, T], F32)
        wf = [sb(f"wf{i}", [128, 8, 256], F32) for i in range(2)]
        wb = [sb(f"wb{i}", [128, 8, 256], BF16) for i in range(2)]
        of32 = [sb(f"of{i}", [128, T], F32) for i in range(2)]
        obf = [sb(f"ob{i}", [128, T], BF16) for i in range(2)]
        ov = [sb(f"ov{i}", [128, 16, 256], BF16) for i in range(2)]
        oiw = sb("oiw", [128, 16, 4], F32)
        raw = sb("raw", [128, 512], F32)
        sq = sb("sq", [128, 512], F32)
        rstd = sb("rstd", [128, 512], F32)
        nrm = sb("nrm", [128, 512], F32)
        t1 = sb("t1", [128, 512], F32)
        t2 = sb("t2", [128, 512], F32)
        psm = [pst(f"psm{i}", [128, 512]) for i in range(4)]
        psn = pst("psn", [128, 512])
        psr = pst("psr", [128, 512])

        P.dma('sp', gm[:], gmix, writes=['gm'])
        P.dma('sp', gqt[:], gq, writes=['gqt'])
        P.dma('sp', cs[:], cst, writes=['cs'])
        P.dma('sp', rA[:], rmA, writes=['rA'])
        P.dma('sp', rI[:], rmI, writes=['rI'])
        P.dma('sp', posi[:], pos, writes=['posi'])
        P.op('dve', lambda e: e.memset(ones[:], 1.0), writes=['ones'])
        P.op('dve', lambda e: e.tensor_copy(posf[:], posi[:]), reads=['posi'], writes=['posf'])
        for j, (ct, st) in enumerate(((cosA, sinA), (cosI, sinI))):
            cn, sn = f"cos{j}", f"sin{j}"
            P.op('dve', lambda e, j=j: e.tensor_scalar(ang[:], posf[:], cs[:, j:j + 1], None, op0=ALU.mult),
                 reads=['posf', 'cs'], writes=['ang'])
            for (tt_, tn, shift, bcol) in ((st, sn, 0.0, 3), (ct, cn, 0.5 * PI, 4)):
                P.op('dve', lambda e, shift=shift: e.tensor_scalar(posf2[:], ang[:], 1.0 / (2 * PI), 0.5 + shift / (2 * PI),
                                                                   op0=ALU.mult, op1=ALU.add),
                     reads=['ang'], writes=['posf2'])
                P.op('dve', lambda e: e.tensor_copy(posi2[:], posf2[:]), reads=['posf2'], writes=['posi2'])
                P.op('dve', lambda e: e.tensor_copy(posf2[:], posi2[:]), reads=['posi2'], writes=['posf2'])
                P.op('dve', lambda e, tt_=tt_: e.scalar_tensor_tensor(tt_[:], posf2[:], -2 * PI, ang[:], op0=ALU.mult, op1=ALU.add),
                     reads=['posf2', 'ang'], writes=[tn])
                P.op('dve', lambda e, tt_=tt_, shift=shift: e.tensor_scalar(posf2[:], tt_[:], -PI - shift, 2 * PI,
                                                                          op0=ALU.is_lt, op1=ALU.mult),
                     reads=[tn], writes=['posf2'])
                P.op('dve', lambda e, tt_=tt_: e.tensor_tensor(tt_[:], tt_[:], posf2[:], op=ALU.add),
                     reads=[tn, 'posf2'], writes=[tn])
                P.op('act', lambda e, tt_=tt_, bcol=bcol: e.activation(tt_[:], tt_[:], AF.Sin, bias=cs[:, bcol:bcol + 1], scale=SINK),
                     reads=[tn, 'cs'], writes=[tn])

        xTv = xT.rearrange("(c p) t -> p c t", p=128)
        for tg in range(8):
            xb = xs[tg % 2]
            xk = f"xs{tg % 2}"
            P.dma('sp', xb[:], xTv[:, :, tg * 256:(tg + 1) * 256], writes=[xk])
            P.op('act', lambda e, xb=xb: e.activation(sqs[:], xb[:], AF.Square), reads=[xk], writes=['sqs'])
            for c in range(8):
                P.op('pe', lambda e, c=c: e.matmul(psn[:, 0:256], lhsT=ones[:], rhs=sqs[:, c, :],
                                                   start=(c == 0), stop=(c == 7)),
                     reads=['ones', 'sqs'], writes=['psn'])
            P.op('dve', lambda e: e.tensor_scalar(rs[:], psn[:, 0:256], 1.0 / D, EPS, op0=ALU.mult, op1=ALU.add),
                 reads=['psn'], writes=['rs'])
            P.op('act', lambda e: e.activation(rs[:], rs[:], AF.Sqrt), reads=['rs'], writes=['rs'])
            P.op('dve', lambda e: e.reciprocal(rs[:], rs[:]), reads=['rs'], writes=['rs'])
            for c in range(8):
                P.op('dve', lambda e, c=c, xb=xb, tg=tg: e.scalar_tensor_tensor(
                    hT[:, c, tg * 256:(tg + 1) * 256], xb[:, c, :], gm[:, c:c + 1], rs[:],
                    op0=ALU.mult, op1=ALU.mult),
                     reads=[xk, 'gm', 'rs'], writes=[f'hT{tg}'])
        hkeys = [f'hT{tg}' for tg in range(8)]

        wv = w.rearrange("(c p) n -> p c n", p=128)
        groups = l1_items()
        cnt = dict(ps=0, of=0, ob=0, ov=0)
        for gi, grp in enumerate(groups):
            c0 = grp[0][1]
            c1 = grp[-1][1] + grp[-1][2]
            nco = c1 - c0
            b = gi % 2
            P.dma('sp', wf[b][:, :, 0:nco], wv[:, :, c0:c1], writes=[f'wf{b}'])
            ceng = 'pool' if gi % 2 == 0 else 'dve'
            P.op(ceng, lambda e, b=b, nco=nco: e.tensor_copy(wb[b][:, :, 0:nco], wf[b][:, :, 0:nco]),
                 reads=[f'wf{b}'], writes=[f'wb{b}'])
            for (kind, col, ncol, orow, aux) in grp:
                lo = col - c0
                if kind in ('tmv', 'iw'):
                    if kind == 'tmv':
                        ob_ = ov[cnt['ov'] % 2]; okey = f"ov{cnt['ov'] % 2}"; cnt['ov'] += 1
                    else:
                        ob_ = oiw; okey = 'oiw'
                    for tt in range(16):
                        pi = cnt['ps'] % 4; cnt['ps'] += 1
                        for c in range(8):
                            P.op('pe', lambda e, pi=pi, c=c, tt=tt, lo=lo, ncol=ncol, b=b: e.matmul(
                                psm[pi][:, 0:ncol], lhsT=hT[:, c, tt * 128:(tt + 1) * 128],
                                rhs=wb[b][:, c, lo:lo + ncol], start=(c == 0), stop=(c == 7)),
                                 reads=hkeys + [f'wb{b}'], writes=[f'psm{pi}'])
                        ee = 'act' if tt % 2 == 0 else 'dve'
                        if ee == 'act':
                            P.op('act', lambda e, pi=pi, tt=tt, ncol=ncol, ob_=ob_: e.copy(ob_[:, tt, 0:ncol], psm[pi][:, 0:ncol]),
                                 reads=[f'psm{pi}'], writes=[okey])
                        else:
                            P.op('dve', lambda e, pi=pi, tt=tt, ncol=ncol, ob_=ob_: e.tensor_copy(ob_[:, tt, 0:ncol], psm[pi][:, 0:ncol]),
                                 reads=[f'psm{pi}'], writes=[okey])
                    if kind == 'tmv':
                        for q in range(8):
                            P.dma('act', o_v[q].rearrange("(tt p) c -> p tt c", p=128)[:, :, orow:orow + 256], ob_[:, 2 * q:2 * q + 2, :], reads=[okey])
                    else:
                        P.dma('act', o_iw.rearrange("(tt p) c -> p tt c", p=128), ob_[:], reads=[okey])
                    continue
                if kind == 'f32':
                    ob_ = of32[cnt['of'] % 2]; okey = f"of{cnt['of'] % 2}"; cnt['of'] += 1
                else:
                    ob_ = obf[cnt['ob'] % 2]; okey = f"ob{cnt['ob'] % 2}"; cnt['ob'] += 1
                for g in range(4):
                    pi = cnt['ps'] % 4; cnt['ps'] += 1
                    ts_ = slice(g * 512, (g + 1) * 512)
                    for c in range(8):
                        P.op('pe', lambda e, pi=pi, c=c, lo=lo, ncol=ncol, b=b, ts_=ts_: e.matmul(
                            psm[pi][0:ncol, :], lhsT=wb[b][:, c, lo:lo + ncol], rhs=hT[:, c, ts_],
                            start=(c == 0), stop=(c == 7)),
                             reads=hkeys + [f'wb{b}'], writes=[f'psm{pi}'])
                    pk = f'psm{pi}'
                    if kind in ('f32', 'bf'):
                        if g % 2 == 0:
                            P.op('act', lambda e, pi=pi, ts_=ts_, ob_=ob_: e.copy(ob_[:, ts_], psm[pi][:]),
                                 reads=[pk], writes=[okey])
                        else:
                            P.op('dve', lambda e, pi=pi, ts_=ts_, ob_=ob_: e.tensor_copy(ob_[:, ts_], psm[pi][:]),
                                 reads=[pk], writes=[okey])
                        continue
                    np_ = ncol
                    if kind in ('qkA', 'ki'):
                        gcol = aux if kind == 'qkA' else 2
                        P.op('act', lambda e, pi=pi, np_=np_: e.copy(raw[0:np_, :], psm[pi][0:np_, :]), reads=[pk], writes=['raw'])
                        P.op('act', lambda e, pi=pi, np_=np_: e.activation(sq[0:np_, :], psm[pi][0:np_, :], AF.Square),
                             reads=[pk], writes=['sq'])
                        P.op('pe', lambda e, np_=np_: e.matmul(psn[0:np_, :], lhsT=ones[0:np_, 0:np_], rhs=sq[0:np_, :],
                                                              start=True, stop=True),
                             reads=['ones', 'sq'], writes=['psn'])
                        P.op('dve', lambda e, np_=np_: e.tensor_scalar(rstd[0:np_, :], psn[0:np_, :], 1.0 / np_, EPS,
                                                                      op0=ALU.mult, op1=ALU.add),
                             reads=['psn'], writes=['rstd'])
                        P.op('act', lambda e, np_=np_: e.activation(rstd[0:np_, :], rstd[0:np_, :], AF.Sqrt),
                             reads=['rstd'], writes=['rstd'])
                        P.op('dve', lambda e, np_=np_: e.reciprocal(rstd[0:np_, :], rstd[0:np_, :]),
                             reads=['rstd'], writes=['rstd'])
                        P.op('dve', lambda e, np_=np_, gcol=gcol: e.scalar_tensor_tensor(
                            nrm[0:np_, :], raw[0:np_, :], gqt[0:np_, gcol:gcol + 1], rstd[0:np_, :],
                            op0=ALU.mult, op1=ALU.mult), reads=['raw', 'gqt', 'rstd'], writes=['nrm'])
                        src = nrm
                    else:
                        P.op('act', lambda e, pi=pi: e.copy(nrm[:], psm[pi][:]), reads=[pk], writes=['nrm'])
                        src = nrm
                    if kind == 'qkA':
                        rp = 32; R = rA; ct, st, cn, sn = cosA, sinA, 'cos0', 'sin0'
                    else:
                        rp = np_; R = rI; ct, st, cn, sn = cosI, sinI, 'cos1', 'sin1'
                    P.op('pe', lambda e, rp=rp, R=R: e.matmul(psr[0:rp, :], lhsT=R[0:rp, 0:rp], rhs=nrm[0:rp, :],
                                                             start=True, stop=True),
                         reads=['nrm', 'rA', 'rI'], writes=['psr'])
                    P.op('pool', lambda e, rp=rp, ct=ct, ts_=ts_: e.tensor_tensor(t1[0:rp, :], nrm[0:rp, :], ct[0:rp, ts_], op=ALU.mult),
                         reads=['nrm', cn], writes=['t1'])
                    P.op('dve', lambda e, rp=rp, st=st, ts_=ts_: e.tensor_tensor(t2[0:rp, :], psr[0:rp, :], st[0:rp, ts_], op=ALU.mult),
                         reads=['psr', sn], writes=['t2'])
                    P.op('pool', lambda e, rp=rp, ob_=ob_, ts_=ts_: e.tensor_tensor(ob_[0:rp, ts_], t1[0:rp, :], t2[0:rp, :], op=ALU.add),
                         reads=['t1', 't2'], writes=[okey])
                    if rp < np_:
                        P.op('act', lambda e, ob_=ob_, ts_=ts_: e.copy(ob_[32:64, ts_], nrm[32:64, :]),
                             reads=['nrm'], writes=[okey])
                        P.op('act', lambda e, ob_=ob_, ts_=ts_: e.copy(ob_[64:128, ts_], nrm[64:128, :]),
                             reads=['nrm'], writes=[okey])
                oq_ = 'act'
                if kind == 'f32':
                    P.dma(oq_, o_f32[orow:orow + ncol, :], ob_[0:ncol, :], reads=[okey], writes=['o_f32'])
                else:
                    if (kind == 'ki' or (kind == 'qkA' and aux == 1) or (kind == 'bf' and col >= O_SK)):
                        P.dma('act', o_k[orow][0:ncol, :], ob_[0:ncol, :], reads=[okey])
                    else:
                        P.dma('act', o_q[orow:orow + ncol, :], ob_[0:ncol, :], reads=[okey])
        for (r0, t0) in ((R_GLU, 0), (R_CC + 512, 1024)):
            for q in range(4):
                P.dma('sp', tail1[(t0 + q * 256) // 256].rearrange("r (b k) -> r b k", k=32),
                      o_f32[r0 + q * 256:r0 + (q + 1) * 256, :].rearrange("r (b t) -> r b t", t=128)[:, :, 96:128],
                      reads=['o_f32'])
        ns = P.emit()
        print("L1: instrs", len(P.ins), "sems", ns, "waits", P.n_waits)


S = 8192
NQ = 16
NIT = 18
NEG = -1.0e30
MNEG = -30000.0
SCALE = 128 ** -0.5


def l2_masks(j):
    neg = np.zeros((128, 4, 128), np.float32)
    md = np.zeros((128, 4, 4, 128), np.float32)
    tp = np.arange(128)[:, None]
    sp = np.arange(128)[None, :]
    for m in range(4):
        if m < j:
            neg[:, m, :] = 0.0
            md[:, m, :, :] = 1.0
        elif m == j:
            neg[:, m, :] = np.where(sp <= tp, 0.0, NEG)
            md[:, m, :, :] = (np.arange(128)[:, None] < np.arange(128)[None, :]).astype(np.float32)[:, None, :]
        else:
            neg[:, m, :] = NEG
            md[:, m, :, :] = 0.0
    return neg.reshape(128, 512), md


def l2_consts():
    su = (np.arange(128)[:, None] > np.arange(128)[None, :]).astype(np.float32)
    ident = np.eye(128, dtype=np.float32)
    pw = (2.0 ** -(np.arange(NIT, dtype=np.float32) + 1.0))[None, :].repeat(128, 0).astype(np.float32)
    return su, ident, pw


def build_l2(nc, IO, pfx, do_a=True, do_d=True, nq=NQ):
    G_k, G_v, o_q, o_iw = IO['G_k'], IO['G_v'], IO['o_q'], IO['o_iw']
    negm, mdm, su_d, id_d, pw_d = IO['negm'], IO['mdm'], IO['su'], IO['ident'], IO['pw']
    yaT, ydT = IO['yaT'], IO['ydT']
    Gv4 = [g.rearrange("(r i p) n -> p r i n", r=4, p=128) for g in G_v]

    def load_nat(P, eng, dst2d, row0, nrows, key):
        dv = dst2d.rearrange("p (i r t) -> p i r t", r=4, t=128)
        for r in range(4):
            P.dma(eng if r % 2 == 0 else ('pool' if eng == 'sp' else 'sp'), dv[:, :, r, :],
                  G_k[row0][r * nrows:(r + 1) * nrows, :].rearrange("p (i t) -> p i t", t=128), reads=[f'Gk{row0}'], writes=[key])

    P = Prog(nc)
    es = contextlib.ExitStack()

    def sb(name, shape, dt):
        return es.enter_context(nc.sbuf_tensor(pfx + name, shape, dt))

    def pst(name, shape, dt=F32):
        return es.enter_context(nc.psum_tensor(pfx + name, shape, dt))

    with es:
        KT = sb("KT", [128, 4, S], BF16)
        kit = sb("kit", [64, S], BF16)
        score = sb("score", [128, S], F32)
        mneg = sb("mneg", [128, S], BF16)
        junk = sb("junk", [128, S], BF16)
        vt = [sb(f"vt{i}", [128, 4, 512], BF16) for i in range(3)]
        qblk = [sb(f"qblk{i}", [128, 4, 128], BF16) for i in range(2)]
        qiblk = [sb(f"qiblk{i}", [64, 4, 128], BF16) for i in range(2)]
        iwt = sb("iwt", [128, NQ, 4], F32)
        absw = sb("absw", [128, NQ, 4], F32)
        sgnw = sb("sgnw", [128, NQ, 4], F32)
        negt = sb("negt", [128, 512], F32)
        mdt = sb("mdt", [128, 4, 4, 128], F32)
        sut = sb("sut", [128, 128], F32)
        idf = sb("idf", [128, 128], F32)
        idb = sb("idb", [128, 128], BF16)
        pwt = sb("pwt", [128, NIT], F32)
        ones_f = sb("ones_f", [128, 128], F32)
        ones_b = sb("ones_b", [128, 128], BF16)
        rh = [sb(f"rh{i}", [128, 512], F32) for i in range(2)]
        st = sb("st", [128, 16], F32)
        wk = sb("wk", [128, NIT], F32)
        PT = [sb(f"PT{i}", [128, 512], BF16) for i in range(2)]
        rec = sb("rec", [128, 512], F32)
        oat = [sb(f"oat{i}", [128, 512], F32) for i in range(2)]
        e_t = [sb(f"e_t{i}", [128, 512], F32) for i in range(2)]
        sp_t = [sb(f"sp_t{i}", [128, 512], F32) for i in range(3)]
        u_t = [sb(f"u_t{i}", [128, 512], F32) for i in range(2)]
        R_t = [sb(f"R_t{i}", [128, 512], F32) for i in range(2)]
        aT = [sb(f"aT{i}", [128, 512], BF16) for i in range(2)]
        ps = [pst(f"ps{i}", [128, 512]) for i in range(8)]

        for (src_t, dst_t, key) in IO['cc_l2']:
            P.cc('pool', src_t, dst_t, writes=[key])
        P.dma('sp', iwt[:], o_iw.rearrange("(i t) h -> t i h", t=128), writes=['iwt'])
        P.dma('sp', negt[:], negm, writes=['negt'])
        P.dma('sp', mdt[:], mdm, writes=['mdt'])
        P.dma('sp', sut[:], su_d, writes=['sut'])
        P.dma('sp', idf[:], id_d, writes=['idf'])
        P.dma('sp', pwt[:], pw_d, writes=['pwt'])
        P.op('dve', lambda e: e.memset(ones_f[:], 1.0), writes=['ones_f'])
        P.op('dve', lambda e: e.memset(ones_b[:], 1.0), writes=['ones_b'])
        P.op('dve', lambda e: e.tensor_copy(idb[:], idf[:]), reads=['idf'], writes=['idb'])
        P.op('act', lambda e: e.activation(absw[:], iwt[:], AF.Abs), reads=['iwt'], writes=['absw'])
        P.op('act', lambda e: e.activation(sgnw[:], iwt[:], AF.Sign), reads=['iwt'], writes=['sgnw'])

        hsl = [slice(h * 128, (h + 1) * 128) for h in range(4)]

        if do_d:
            for h in range(4):
                load_nat(P, 'sp', KT[:, h, :], 576 + h * 128, 128, f'KT{h}')
            vcnt = 0
            for i in range(nq):
                NK = 4 * i + 4
                NG = i + 1
                qb_ = qblk[i % 2]; qbk = f'qblk{i % 2}'
                P.dma('sp', qb_[:], o_q[768:1280, i * 128:(i + 1) * 128].rearrange("(h d) t -> d h t", d=128), writes=[qbk])
                chs = list(range(NK - 1, -1, -1))
                vbs = {}

                def s1(k, ch):
                    nonlocal vcnt
                    kg, cc = ch // 4, ch % 4
                    if cc == 3:
                        vb = vt[vcnt % 3]; vk = f'vt{vcnt % 3}'; vcnt += 1
                        P.dma('pool', vb[:], Gv4[kg // 2][:, :, kg % 2, 512:1024], reads=[f'Gv{kg // 2}'], writes=[vk])
                        vbs[kg] = (vb, vk)
                    cs_ = slice(ch * 128, (ch + 1) * 128)
                    pz = ps[k % 2]; pzk = f'ps{k % 2}'
                    et = e_t[k % 2]; ek = f'e_t{k % 2}'
                    spt = sp_t[k % 3]; spk = f'sp{k % 3}'
                    for h in range(4):
                        P.op('pe', lambda e, pz=pz, h=h, cs_=cs_, qb_=qb_: e.matmul(pz[:, hsl[h]], lhsT=KT[:, h, cs_], rhs=qb_[:, h, :], start=True, stop=True),
                             reads=[f'KT{h}', qbk], writes=[pzk])
                    P.op('act', lambda e, pz=pz, et=et: e.activation(et[:], pz[:], AF.Exp, scale=SCALE), reads=[pzk], writes=[ek])
                    P.op('act', lambda e, spt=spt, et=et: e.activation(spt[:], et[:], AF.Ln, bias=ones_f[:, 0:1]), reads=[ek, 'ones_f'], writes=[spk])
                    if kg == NG - 1:
                        P.op('pool', lambda e, spt=spt, cc=cc: e.tensor_tensor(
                            spt[:], spt[:], mdt[:, cc, :, :].rearrange("p h t -> p (h t)"), op=ALU.mult), reads=[spk, 'mdt'], writes=[spk])

                def s2(k, ch):
                    first = (k == 0)
                    pz = ps[k % 2]; pzk = f'ps{k % 2}'
                    pB = ps[2 + k % 2]; pBk = f'ps{2 + k % 2}'
                    spt = sp_t[k % 3]; spk = f'sp{k % 3}'
                    ut = u_t[k % 2]; uk = f'u_t{k % 2}'
                    Rp, Rpk = R_t[k % 2], f'R_t{k % 2}'
                    Rn, Rnk = R_t[(k + 1) % 2], f'R_t{(k + 1) % 2}'
                    P.op('pe', lambda e, pB=pB, spt=spt, first=first: e.matmul(pB[:], lhsT=sut[:], rhs=spt[:], start=True, stop=first),
                         reads=['sut', spk], writes=[pBk])
                    if not first:
                        P.op('pe', lambda e, pB=pB, Rp=Rp: e.matmul(pB[:], lhsT=ones_f[:], rhs=Rp[:], start=False, stop=True),
                             reads=['ones_f', Rpk], writes=[pBk])
                    if first:
                        P.op('pool', lambda e, spt=spt, Rn=Rn: e.tensor_copy(Rn[:], spt[:]), reads=[spk], writes=[Rnk])
                    elif k + 1 < NK:
                        P.op('pool', lambda e, spt=spt, Rn=Rn, Rp=Rp: e.tensor_tensor(Rn[:], Rp[:], spt[:], op=ALU.add), reads=[spk, Rpk], writes=[Rnk])
                    P.op('dve', lambda e, pz=pz, spt=spt, ut=ut: e.scalar_tensor_tensor(ut[:], pz[:], SCALE, spt[:], op0=ALU.mult, op1=ALU.subtract),
                         reads=[pzk, spk], writes=[uk])
                    P.op('dve', lambda e, pB=pB, ut=ut: e.tensor_tensor(ut[:], ut[:], pB[:], op=ALU.subtract), reads=[uk, pBk], writes=[uk])

                def s3(k, ch):
                    first = (k == 0)
                    kg, cc = ch // 4, ch % 4
                    ut = u_t[k % 2]; uk = f'u_t{k % 2}'
                    at = aT[k % 2]; atk = f'aT{k % 2}'
                    vb, vk = vbs[kg]
                    P.op('act', lambda e, at=at, ut=ut: e.activation(at[:], ut[:], AF.Exp), reads=[uk], writes=[atk])
                    if kg == NG - 1:
                        P.op('pool', lambda e, at=at, cc=cc: e.tensor_tensor(
                            at[:], at[:], mdt[:, cc, :, :].rearrange("p h t -> p (h t)"), op=ALU.mult), reads=[atk, 'mdt'], writes=[atk])
                    for h in range(4):
                        P.op('pe', lambda e, at=at, h=h, vb=vb, cc=cc, first=first, ch=ch: e.matmul(
                            ps[4 + h][:, 0:128], lhsT=vb[:, cc, hsl[h]], rhs=at[:, hsl[h]], start=first, stop=(ch == 0)),
                             reads=[atk, vk], writes=[f'ps{4 + h}'])

                for n in range(NK + 2):
                    if n < NK:
                        s1(n, chs[n])
                    if 0 <= n - 1 < NK:
                        s2(n - 1, chs[n - 1])
                    if 0 <= n - 2 < NK:
                        s3(n - 2, chs[n - 2])
                ot = oat[i % 2]; otk = f'oat{i % 2}'
                for h in range(4):
                    if h % 2 == 0:
                        P.op('dve', lambda e, h=h, ot=ot: e.tensor_copy(ot[:, hsl[h]], ps[4 + h][:, 0:128]), reads=[f'ps{4 + h}'], writes=[otk])
                    else:
                        P.op('act', lambda e, h=h, ot=ot: e.copy(ot[:, hsl[h]], ps[4 + h][:, 0:128]), reads=[f'ps{4 + h}'], writes=[otk])
                P.dma('sp', ydT[:, i * 128:(i + 1) * 128].rearrange("(h d) t -> d h t", d=128), ot[:].rearrange("p (h t) -> p h t", t=128), reads=[otk])
        if do_a:
            load_nat(P, 'sp', kit[:, :], 0, 64, 'kit')
            for h in range(4):
                load_nat(P, 'sp', KT[:, h, :], 64 + h * 128, 128, f'KT{h}')
            vstate = dict(cnt=0)

            def a_score(i):
                NG = i + 1
                Sc = 512 * NG
                qb_ = qblk[i % 2]; qbk = f'qblk{i % 2}'
                qib = qiblk[i % 2]; qik = f'qiblk{i % 2}'
                P.dma('sp', qb_[:], o_q[0:512, i * 128:(i + 1) * 128].rearrange("(h d) t -> d h t", d=128), writes=[qbk])
                P.dma('sp', qib[:], o_q[512:768, i * 128:(i + 1) * 128].rearrange("(h d) t -> d h t", d=64), writes=[qik])
                for kg in range(NG):
                    ks = slice(kg * 512, (kg + 1) * 512)
                    for h in range(4):
                        pb = ps[h % 2]; pk = f'ps{h % 2}'
                        P.op('pe', lambda e, pb=pb, h=h, ks=ks, qib=qib: e.matmul(pb[:], lhsT=qib[:, h, :], rhs=kit[:, ks], start=True, stop=True),
                             reads=[qik, 'kit'], writes=[pk])
                        r_ = rh[h % 2]; rk = f'rh{h % 2}'
                        P.op('act', lambda e, pb=pb, r_=r_, i=i, h=h: e.activation(r_[:], pb[:], AF.Relu, scale=absw[:, i, h:h + 1]),
                             reads=[pk, 'absw'], writes=[rk])
                        if h == 0:
                            P.op('dve', lambda e, r_=r_, ks=ks, i=i, h=h: e.tensor_scalar(score[:, ks], r_[:], sgnw[:, i, h:h + 1], None, op0=ALU.mult),
                                 reads=[rk, 'sgnw'], writes=['score'])
                        else:
                            P.op('dve', lambda e, r_=r_, ks=ks, i=i, h=h: e.scalar_tensor_tensor(
                                score[:, ks], r_[:], sgnw[:, i, h:h + 1], score[:, ks], op0=ALU.mult, op1=ALU.add),
                                 reads=[rk, 'sgnw', 'score'], writes=['score'])
                P.op('dve', lambda e, Sc=Sc: e.tensor_reduce(st[:, 1:2], score[:, 0:Sc], axis=AX.X, op=ALU.max), reads=['score'], writes=['st_hi'])
                P.op('dve', lambda e, Sc=Sc: e.tensor_reduce(st[:, 0:1], score[:, 0:Sc], axis=AX.X, op=ALU.min), reads=['score'], writes=['st_lo'])
                P.op('pool', lambda e, Sc=Sc: e.tensor_tensor(score[:, Sc - 512:Sc], score[:, Sc - 512:Sc], negt[:], op=ALU.add),
                     reads=['score', 'negt'], writes=['score'])

            def a_bisect(i):
                Sc = 512 * (i + 1)
                P.op('dve', lambda e: e.tensor_tensor(st[:, 2:3], st[:, 1:2], st[:, 0:1], op=ALU.subtract), reads=['st_hi', 'st_lo'], writes=['st_W'])
                P.op('dve', lambda e: e.tensor_scalar(wk[:], pwt[:], st[:, 2:3], None, op0=ALU.mult), reads=['pwt', 'st_W'], writes=['wk'])
                P.op('dve', lambda e: e.tensor_tensor(st[:, 3:4], st[:, 0:1], wk[:, 0:1], op=ALU.add), reads=['st_lo', 'wk'], writes=['st_mid'])
                yield
                Sa = max(64, int(round(0.56 * Sc / 64.0)) * 64)
                for k in range(NIT):
                    P.op('act', lambda e, Sa=Sa: e.activation(junk[:, 0:Sa], score[:, 0:Sa], AF.Sign, bias=st[:, 3:4], scale=-1.0,
                                                              accum_out=st[:, 4:5]),
                         reads=['score', 'st_mid'], writes=['junkA', 'st_cntA'])
                    P.op('dve', lambda e, Sa=Sa, Sc=Sc: e.tensor_scalar(junk[:, Sa:Sc], score[:, Sa:Sc], st[:, 3:4], None,
                                                                        op0=ALU.is_ge, op1=ALU.add, accum_out=st[:, 6:7]),
                         reads=['score', 'st_mid'], writes=['junkD', 'st_cntD'])
                    P.op('dve', lambda e: e.scalar_tensor_tensor(st[:, 7:8], st[:, 4:5], -0.5, st[:, 6:7], op0=ALU.mult, op1=ALU.add),
                         reads=['st_cntA', 'st_cntD'], writes=['st_tmp1'])
                    P.op('dve', lambda e, k=k, Sa=Sa: e.scalar_tensor_tensor(st[:, 5:6], st[:, 7:8], 255.5 - 0.5 * Sa, wk[:, k:k + 1],
                                                                             op0=ALU.is_ge, op1=ALU.mult),
                         reads=['st_tmp1', 'wk'], writes=['st_tmp'])
                    P.op('dve', lambda e: e.tensor_tensor(st[:, 0:1], st[:, 0:1], st[:, 5:6], op=ALU.add), reads=['st_lo', 'st_tmp'], writes=['st_lo'])
                    if k + 1 < NIT:
                        P.op('dve', lambda e, k=k: e.tensor_tensor(st[:, 3:4], st[:, 0:1], wk[:, k + 1:k + 2], op=ALU.add),
                             reads=['st_lo', 'wk'], writes=['st_mid'])
                    yield

            def a_mask(i):
                Sc = 512 * (i + 1)
                P.op('dve', lambda e, Sc=Sc: e.tensor_scalar(mneg[:, 0:Sc], score[:, 0:Sc], st[:, 0:1], MNEG, op0=ALU.is_lt, op1=ALU.mult),
                     reads=['score', 'st_lo'], writes=['mneg'])

            def a_attn(i):
                NK = 4 * i + 4
                qb_ = qblk[i % 2]; qbk = f'qblk{i % 2}'
                vbs = {}

                def s1(ch):
                    if ch % 4 == 0:
                        kg = ch // 4
                        vb = vt[vstate['cnt'] % 3]; vk = f"vt{vstate['cnt'] % 3}"; vstate['cnt'] += 1
                        P.dma('pool', vb[:], Gv4[kg // 2][:, :, kg % 2, 0:512], reads=[f'Gv{kg // 2}'], writes=[vk])
                        vbs[kg] = (vb, vk)
                    cs_ = slice(ch * 128, (ch + 1) * 128)
                    pl = ps[ch % 2]; plk = f'ps{ch % 2}'
                    for h in range(4):
                        P.op('pe', lambda e, pl=pl, h=h, cs_=cs_: e.matmul(pl[:, hsl[h]], lhsT=KT[:, h, cs_], rhs=qb_[:, h, :], start=True, stop=False),
                             reads=[f'KT{h}', qbk], writes=[plk])
                        P.op('pe', lambda e, pl=pl, h=h, cs_=cs_: e.matmul(pl[:, hsl[h]], lhsT=mneg[:, cs_], rhs=idb[:], start=False, stop=True),
                             reads=['mneg', 'idb'], writes=[plk])

                def s2(ch):
                    pl = ps[ch % 2]; plk = f'ps{ch % 2}'
                    pt = PT[ch % 2]; ptk = f'PT{ch % 2}'
                    vb, vk = vbs[ch // 4]
                    cc = ch % 4
                    P.op('act', lambda e, pl=pl, pt=pt: e.activation(pt[:], pl[:], AF.Exp, scale=SCALE), reads=[plk], writes=[ptk])
                    P.op('pe', lambda e, pt=pt, ch=ch: e.matmul(ps[2][:, :], lhsT=ones_b[:, :], rhs=pt[:], start=(ch == 0), stop=(ch == NK - 1)),
                         reads=[ptk, 'ones_b'], writes=['ps2'])
                    for h in range(4):
                        P.op('pe', lambda e, pt=pt, h=h, ch=ch, vb=vb, cc=cc: e.matmul(
                            ps[3 + h][:, 0:128], lhsT=vb[:, cc, hsl[h]], rhs=pt[:, hsl[h]], start=(ch == 0), stop=(ch == NK - 1)),
                             reads=[ptk, vk], writes=[f'ps{3 + h}'])

                for n in range(NK + 1):
                    if n < NK:
                        s1(n)
                    if n >= 1:
                        s2(n - 1)
                    yield
                P.op('dve', lambda e: e.reciprocal(rec[:], ps[2][:, :]), reads=['ps2'], writes=['rec'])
                ot = oat[i % 2]; otk = f'oat{i % 2}'
                for h in range(4):
                    P.op('dve', lambda e, h=h, ot=ot: e.tensor_tensor(ot[:, hsl[h]], ps[3 + h][:, 0:128], rec[:, hsl[h]], op=ALU.mult),
                         reads=[f'ps{3 + h}', 'rec'], writes=[otk])
                P.dma('sp', yaT[:, i * 128:(i + 1) * 128].rearrange("(h d) t -> d h t", d=128), ot[:].rearrange("p (h t) -> p h t", t=128), reads=[otk])

            a_score(0)
            for _ in a_bisect(0):
                pass
            a_mask(0)
            for i in range(nq):
                ga = a_attn(i)
                n_att = 4 * i + 5
                if i + 1 < nq:
                    a_score(i + 1)
                    gb = a_bisect(i + 1)
                    n_bis = NIT + 1
                    done_b = 0
                    for s_ in range(n_att):
                        next(ga)
                        tgt = ((s_ + 1) * n_bis) // n_att
                        while done_b < tgt:
                            next(gb); done_b += 1
                    for _ in gb:
                        pass
                for _ in ga:
                    pass
                if i + 1 < nq:
                    a_mask(i + 1)

        ns = P.emit()
        print("L2: instrs", len(P.ins), "sems", ns, "waits", P.n_waits)

T = 2048
HB = 32
EPS = 1e-6
DFF = 2816
NFF = 22


def _blend(P, out_ap, cands, ckeys, selt, okey):
    P.op('dve', lambda e: e.tensor_scalar(out_ap, cands[0], selt[:, 0:1], None, op0=ALU.mult),
         reads=[ckeys[0], 'selt'], writes=[okey])
    for c in range(1, 4):
        P.op('dve', lambda e, c=c: e.scalar_tensor_tensor(out_ap, cands[c], selt[:, c:c + 1], out_ap, op0=ALU.mult, op1=ALU.add),
             reads=[ckeys[c], 'selt', okey], writes=[okey])


def build_l3a(nc, IO, pfx):
    o_f32, GT1, sel_d, yaT, ydT, xT = IO['o_f32'], IO['GT1'], IO['sel'], IO['yaT'], IO['ydT'], IO['xT']
    wo, wm, sm, xmT, tail2 = IO['wo'], IO['wm'], IO['sm3a'], IO['xm'], IO['tail2']
    gateT = o_f32[2560:6656, :]
    P = Prog(nc)
    es = contextlib.ExitStack()
    sb = lambda name, shape, dt: es.enter_context(nc.sbuf_tensor(pfx + name, shape, dt))
    pst = lambda name, shape, dt=F32: es.enter_context(nc.psum_tensor(pfx + name, shape, dt))
    W = 512
    with es:
        wob = [sb(f"wob{i}", [128, 4, 1024], BF16) for i in range(4)]
        wmb = sb("wmb", [128, 8, 1024], BF16)
        wst = sb("wst", [128, 4, 1024], F32)
        smt = sb("smt", [128, 192], F32)
        selt = sb("selt", [128, 4], F32)
        ones = sb("ones", [128, 128], F32)
        hh = [sb(f"hh{i}", [128, 4, 4, 160], F32) for i in range(2)]
        cand = [sb(f"cand{i}", [128, 4, 4, 32], F32) for i in range(4)]
        acc = sb("acc", [128, 4, W], F32)
        xc = sb("xc", [128, 4, W], F32)
        sq = sb("sq", [128, 4, W], F32)
        gbb = sb("gbb", [128, 4, W], F32)
        brs = [[sb(f"br{i}_{p}", [128, 4, W], BF16) for i in range(4)] for p in range(2)]
        rln = sb("rln", [128, W], F32)
        gt_ = [sb(f"gt{i}", [128, W], F32) for i in range(2)]
        mg = sb("mg", [128, W], F32)
        mgb = sb("mgb", [128, 8, W], BF16)
        xt = sb("xt", [128, 8, W], F32)
        ot = sb("ot", [128, 8, W], F32)
        ps = [pst(f"ps{i}", [128, 512]) for i in range(8)]

        P.dma('sp', smt[:], sm, writes=['smt'])
        P.dma('sp', selt[:], sel_d, writes=['selt'])
        P.op('dve', lambda e: e.memset(ones[:], 1.0 / 512), writes=['ones'])
        for i in range(4):
            P.dma('sp', wst[:], wo[i].rearrange("(c p) n -> p c n", p=128), writes=['wst'])
            P.op('pool' if i % 2 else 'dve', lambda e, i=i: e.tensor_copy(wob[i][:], wst[:]), reads=['wst'], writes=[f'wob{i}'])
        for hf in range(2):
            P.dma('sp', wst[:], wm[hf * 512:(hf + 1) * 512, :].rearrange("(c p) n -> p c n", p=128), writes=['wst'])
            P.op('pool' if hf else 'dve', lambda e, hf=hf: e.tensor_copy(wmb[:, hf * 4:(hf + 1) * 4, :], wst[:]), reads=['wst'], writes=['wmb'])

        def load_haloed(dst, dkey, row_main, row_tail, tl):
            u0 = tl * W
            for ch in range(4):
                P.dma('sp' if ch % 2 == 0 else 'act', dst[:, ch, :, 32:160],
                      o_f32[row_main + ch * 128:row_main + (ch + 1) * 128, u0:u0 + W].rearrange("p (b t) -> p b t", t=128),
                      writes=[dkey])
            q0 = row_tail // 256
            for r in range(4):
                if r == 3 and tl == 0:
                    P.op('pool', lambda e: e.memset(cand[3][:], 0.0), writes=['cand3'])
                for hf in range(2):
                    src = GT1[q0 + hf][r * 256:(r + 1) * 256, :]
                    if r < 3:
                        P.dma('sp' if (r + hf) % 2 == 0 else 'act', cand[r][:, 2 * hf:2 * hf + 2, :, :],
                              src[:, tl * 128:(tl + 1) * 128].rearrange("(c p) (b k) -> p c b k", p=128, k=32), writes=[f'cand{r}'])
                    elif tl == 0:
                        P.dma('act', cand[3][:, 2 * hf:2 * hf + 2, 1:4, :],
                              src[:, 0:96].rearrange("(c p) (b k) -> p c b k", p=128, k=32), writes=['cand3'])
                    else:
                        P.dma('act', cand[3][:, 2 * hf:2 * hf + 2, :, :],
                              src[:, tl * 128 - 32:tl * 128 + 96].rearrange("(c p) (b k) -> p c b k", p=128, k=32), writes=['cand3'])
            _blend(P, dst[:].rearrange("p c b k -> p (c b) k")[:, :, 0:32], [c_[:].rearrange("p c b k -> p (c b) k") for c_ in cand],
                   [f'cand{r}' for r in range(4)], selt, dkey)

        def stage_x(tl):
            u0 = tl * W
            br = brs[tl % 2]
            bk = [f'br{i}_{tl % 2}' for i in range(4)]
            ga, gb_ = hh[0], hh[1]
            load_haloed(ga, 'hh0', 0, 0, tl)
            load_haloed(gb_, 'hh1', 512, 512, tl)
            P.op('act', lambda e: e.activation(gb_[:], gb_[:], AF.Sigmoid), reads=['hh1'], writes=['hh1'])
            P.op('pool', lambda e: e.tensor_tensor(ga[:], ga[:], gb_[:], op=ALU.mult), reads=['hh0', 'hh1'], writes=['hh0'])
            for ch in range(4):
                ak = f'acc{ch}'
                av_ = acc[:, ch, :].rearrange("p (b t) -> p b t", t=128)
                P.op('dve', lambda e, ch=ch, av_=av_: e.tensor_scalar(av_, ga[:, ch, :, 2:130], smt[:, 32 + ch * 31:33 + ch * 31],
                                                                    smt[:, 156 + ch:157 + ch], op0=ALU.mult, op1=ALU.add),
                     reads=['hh0', 'smt'], writes=[ak])
                for k in range(1, 31):
                    P.op('dve', lambda e, ch=ch, k=k, av_=av_: e.scalar_tensor_tensor(
                        av_, ga[:, ch, :, 2 + k:130 + k], smt[:, 32 + ch * 31 + k:33 + ch * 31 + k], av_,
                        op0=ALU.mult, op1=ALU.add), reads=['hh0', 'smt', ak], writes=[ak])
            aks = [f'acc{ch}' for ch in range(4)]
            for ch in range(4):
                P.op('pe', lambda e, ch=ch: e.matmul(ps[0][:], lhsT=ones[:], rhs=acc[:, ch, :], start=(ch == 0), stop=(ch == 3)),
                     reads=['ones'] + aks, writes=['ps0'])
            for ch in range(4):
                P.op('dve', lambda e, ch=ch: e.tensor_tensor(xc[:, ch, :], acc[:, ch, :], ps[0][:], op=ALU.subtract),
                     reads=aks + ['ps0'], writes=['xc'])
            P.op('act', lambda e: e.activation(sq[:], xc[:], AF.Square), reads=['xc'], writes=['sq'])
            for ch in range(4):
                P.op('pe', lambda e, ch=ch: e.matmul(ps[1][:], lhsT=ones[:], rhs=sq[:, ch, :], start=(ch == 0), stop=(ch == 3)),
                     reads=['ones', 'sq'], writes=['ps1'])
            P.op('dve', lambda e: e.tensor_scalar(rln[:], ps[1][:], 1.0, EPS, op0=ALU.mult, op1=ALU.add), reads=['ps1'], writes=['rln'])
            P.op('act', lambda e: e.activation(rln[:], rln[:], AF.Sqrt), reads=['rln'], writes=['rln'])
            P.op('dve', lambda e: e.reciprocal(rln[:], rln[:]), reads=['rln'], writes=['rln'])
            for ch in range(4):
                P.op('dve', lambda e, ch=ch: e.tensor_tensor(xc[:, ch, :], xc[:, ch, :], rln[:], op=ALU.mult),
                     reads=['xc', 'rln'], writes=['xc'])
                P.op('dve', lambda e, ch=ch: e.tensor_scalar(xc[:, ch, :], xc[:, ch, :], smt[:, 160 + ch:161 + ch],
                                                             smt[:, 164 + ch:165 + ch], op0=ALU.mult, op1=ALU.add),
                     reads=['xc', 'smt'], writes=['xc'])
            P.op('act', lambda e: e.activation(br[1][:], xc[:], AF.Silu), reads=['xc'], writes=[bk[1]])
            gc, xcc = hh[0], hh[1]
            load_haloed(gc, 'hh0', 1024 + 512, 1024, tl)
            load_haloed(xcc, 'hh1', 1024 + 1024, 1536, tl)
            P.dma('sp', gbb[:], o_f32[1024:1536, u0:u0 + W].rearrange("(c p) t -> p c t", p=128), writes=['gbb'])
            P.op('pool', lambda e: e.tensor_tensor(gc[:], gc[:], xcc[:], op=ALU.mult), reads=['hh0', 'hh1'], writes=['hh0'])
            for ch in range(4):
                ak = f'acc{ch}'
                av_ = acc[:, ch, :].rearrange("p (b t) -> p b t", t=128)
                P.op('dve', lambda e, ch=ch, av_=av_: e.tensor_scalar(av_, gc[:, ch, :, 30:158], smt[:, 168 + ch * 3:169 + ch * 3],
                                                                    None, op0=ALU.mult), reads=['hh0', 'smt'], writes=[ak])
                for k in (1, 2):
                    P.op('dve', lambda e, ch=ch, k=k, av_=av_: e.scalar_tensor_tensor(
                        av_, gc[:, ch, :, 30 + k:158 + k], smt[:, 168 + ch * 3 + k:169 + ch * 3 + k], av_,
                        op0=ALU.mult, op1=ALU.add), reads=['hh0', 'smt', ak], writes=[ak])
                P.op('pool', lambda e, ch=ch: e.tensor_tensor(br[2][:, ch, :], acc[:, ch, :], gbb[:, ch, :], op=ALU.mult),
                     reads=[ak, 'gbb'], writes=[bk[2]])
            P.dma('sp', xc[:], yaT[:, u0:u0 + W].rearrange("(c p) t -> p c t", p=128), writes=['xc'])
            P.op('act', lambda e: e.copy(br[0][:], xc[:]), reads=['xc'], writes=[bk[0]])
            P.dma('act', sq[:], ydT[:, u0:u0 + W].rearrange("(c p) t -> p c t", p=128), writes=['sq'])
            P.op('act', lambda e: e.copy(br[3][:], sq[:]), reads=['sq'], writes=[bk[3]])
        def stage_y(tl):
            u0 = tl * W
            br = brs[tl % 2]
            bk = [f'br{i}_{tl % 2}' for i in range(4)]
            P.dma('sp', xt[:], xT[:, u0:u0 + W].rearrange("(c p) t -> p c t", p=128), writes=['xt'])
            gi = 0
            for oc in range(8):
                ocs = slice(oc * 128, (oc + 1) * 128)
                for i in range(4):
                    pb = ps[2 + (gi % 4)]; pk = f'ps{2 + gi % 4}'
                    g_ = gt_[gi % 2]; gk = f'gt{gi % 2}'; gi += 1
                    P.dma('act' if gi % 2 else 'sp', g_[:], gateT[i * 1024 + oc * 128:i * 1024 + (oc + 1) * 128, u0:u0 + W], writes=[gk])
                    P.op('act', lambda e, g_=g_, i=i, oc=oc: e.activation(g_[:], g_[:], AF.Sigmoid, bias=smt[:, i * 8 + oc:i * 8 + oc + 1]),
                         reads=[gk, 'smt'], writes=[gk])
                    for kc in range(4):
                        P.op('pe', lambda e, pb=pb, i=i, kc=kc, ocs=ocs: e.matmul(pb[:], lhsT=wob[i][:, kc, ocs], rhs=br[i][:, kc, :],
                                                                                 start=(kc == 0), stop=(kc == 3)),
                             reads=[f'wob{i}', bk[i]], writes=[pk])
                    if i == 0:
                        P.op('dve', lambda e, pb=pb, g_=g_: e.tensor_tensor(mg[:], pb[:], g_[:], op=ALU.mult), reads=[pk, gk], writes=['mg'])
                    else:
                        P.op('dve', lambda e, pb=pb, g_=g_: e.tensor_tensor(g_[:], pb[:], g_[:], op=ALU.mult), reads=[pk, gk], writes=[gk])
                        if i < 3:
                            P.op('pool', lambda e, g_=g_: e.tensor_tensor(mg[:], mg[:], g_[:], op=ALU.add), reads=['mg', gk], writes=['mg'])
                        else:
                            P.op('pool', lambda e, g_=g_, oc=oc: e.tensor_tensor(mgb[:, oc, :], mg[:], g_[:], op=ALU.add),
                                 reads=['mg', gk], writes=[f'mgb{oc}'])
            mks = [f'mgb{oc}' for oc in range(8)]
            for oc in range(8):
                ocs = slice(oc * 128, (oc + 1) * 128)
                pb = ps[6 + oc % 2]; pk = f'ps{6 + oc % 2}'
                for kc in range(8):
                    P.op('pe', lambda e, pb=pb, kc=kc, ocs=ocs: e.matmul(pb[:], lhsT=wmb[:, kc, ocs], rhs=mgb[:, kc, :],
                                                                       start=(kc == 0), stop=(kc == 7)),
                         reads=['wmb'] + mks, writes=[pk])
                P.op('dve', lambda e, pb=pb, oc=oc: e.tensor_tensor(ot[:, oc, :], pb[:], xt[:, oc, :], op=ALU.add),
                     reads=[pk, 'xt'], writes=['ot'])
            P.dma('sp', xmT[:, u0:u0 + W].rearrange("(c p) t -> p c t", p=128), ot[:], reads=['ot'], writes=['xm'])
        stage_x(0)
        for tl in range(4):
            if tl + 1 < 4:
                stage_x(tl + 1)
            stage_y(tl)
        for q in range(4):
            P.dma('sp', tail2[q * 256:(q + 1) * 256, :].rearrange("r (b k) -> r b k", k=2),
                  xmT[q * 256:(q + 1) * 256, :].rearrange("r (b t) -> r b t", t=128)[:, :, 126:128], reads=['xm'])
        ns = P.emit()
        print("L3a: instrs", len(P.ins), "sems", ns, "waits", P.n_waits)


def build_l3b(nc, IO, pfx):
    xmT, GT2, sel_d, wg, wu, wd, sm, xoT = IO['xm'], IO['GT2'], IO['sel'], IO['wg'], IO['wu'], IO['wd'], IO['sm3b'], IO['xo']
    TT = 1024
    NB = 8
    NC_ = NB * 130
    P = Prog(nc)
    es = contextlib.ExitStack()
    sb = lambda name, shape, dt: es.enter_context(nc.sbuf_tensor(pfx + name, shape, dt))
    pst = lambda name, shape, dt=F32: es.enter_context(nc.psum_tensor(pfx + name, shape, dt))
    with es:
        smt = sb("smt", [128, 80], F32)
        selt = sb("selt", [128, 4], F32)
        ones = sb("ones", [128, 128], F32)
        xm = sb("xm", [128, 8, NB, 130], F32)
        cand = [sb(f"cand{i}", [128, 8, NB, 2], F32) for i in range(4)]
        sqt = sb("sqt", [128, NC_], F32)
        rs = sb("rs", [128, NC_], F32)
        h2 = sb("h2", [128, 8, NB, 130], BF16)
        prod = sb("prod", [128, NFF, TT], BF16)
        wgs = [sb(f"wgs{i}", [128, 8, 128], F32) for i in range(2)]
        wus = [sb(f"wus{i}", [128, 8, 128], F32) for i in range(2)]
        wgb = [sb(f"wgb{i}", [128, 8, 128], BF16) for i in range(2)]
        wub = [sb(f"wub{i}", [128, 8, 128], BF16) for i in range(2)]
        gtl = [sb(f"gtl{i}", [128, NB, 130], F32) for i in range(2)]
        av = [sb(f"av{i}", [128, TT], F32) for i in range(2)]
        wds = [sb(f"wds{i}", [128, 512], F32) for i in range(2)]
        wdb = [sb(f"wdb{i}", [128, 512], BF16) for i in range(2)]
        ot = [sb(f"ot{i}", [128, 512], F32) for i in range(2)]
        ps = [pst(f"ps{i}", [128, 512]) for i in range(8)]
        P.dma('sp', smt[:], sm, writes=['smt'])
        P.dma('sp', selt[:], sel_d, writes=['selt'])
        P.op('dve', lambda e: e.memset(ones[:], 1.0 / 1024), writes=['ones'])
        wgv = wg.rearrange("(c p) n -> p c n", p=128)
        wuv = wu.rearrange("(c p) n -> p c n", p=128)
        xmf = xm[:].rearrange("p c b k -> p c (b k)")
        h2f = h2[:].rearrange("p c b k -> p c (b k)")
        for tl in range(2):
            u0 = tl * TT
            for c in range(8):
                P.dma('sp' if c % 2 == 0 else 'act', xm[:, c, :, 2:130],
                      xmT[c * 128:(c + 1) * 128, u0:u0 + TT].rearrange("p (b t) -> p b t", t=128), writes=['xm'])
            for r in range(3):
                P.dma('sp', cand[r][:], GT2[r * 1024:(r + 1) * 1024, tl * 16:(tl + 1) * 16].rearrange("(c p) (b k) -> p c b k", p=128, k=2),
                      writes=[f'cand{r}'])
            if tl == 0:
                P.op('pool', lambda e: e.memset(cand[3][:], 0.0), writes=['cand3'])
                P.dma('act', cand[3][:, :, 1:8, :], GT2[3 * 1024:4 * 1024, 0:14].rearrange("(c p) (b k) -> p c b k", p=128, k=2), writes=['cand3'])
            else:
                P.dma('act', cand[3][:], GT2[3 * 1024:4 * 1024, 14:30].rearrange("(c p) (b k) -> p c b k", p=128, k=2), writes=['cand3'])
            _blend(P, xm[:].rearrange("p c b k -> p (c b) k")[:, :, 0:2], [c_[:].rearrange("p c b k -> p (c b) k") for c_ in cand],
                   [f'cand{r}' for r in range(4)], selt, 'xm')
            col_groups = [(0, 512), (512, 1024), (1024, NC_)]
            for (a, b) in col_groups:
                n = b - a
                for c in range(8):
                    P.op('act', lambda e, c=c, a=a, b=b: e.activation(sqt[:, a:b], xmf[:, c, a:b], AF.Square), reads=['xm'], writes=['sqt'])
                    P.op('pe', lambda e, c=c, a=a, b=b, n=n: e.matmul(ps[0][:, 0:n], lhsT=ones[:], rhs=sqt[:, a:b], start=(c == 0), stop=(c == 7)),
                         reads=['ones', 'sqt'], writes=['ps0'])
                P.op('dve', lambda e, a=a, b=b, n=n: e.tensor_scalar(rs[:, a:b], ps[0][:, 0:n], 1.0, EPS, op0=ALU.mult, op1=ALU.add),
                     reads=['ps0'], writes=['rs'])
            P.op('act', lambda e: e.activation(rs[:], rs[:], AF.Sqrt), reads=['rs'], writes=['rs'])
            P.op('dve', lambda e: e.reciprocal(rs[:], rs[:]), reads=['rs'], writes=['rs'])
            for c in range(8):
                P.op('dve', lambda e, c=c: e.scalar_tensor_tensor(h2f[:, c, :], xmf[:, c, :], smt[:, c:c + 1], rs[:],
                                                                 op0=ALU.mult, op1=ALU.mult),
                     reads=['xm', 'smt', 'rs'], writes=[f'h2_{c}'])
            hk = [f'h2_{c}' for c in range(8)]
            for f in range(NFF):
                b = f % 2
                fs = slice(f * 128, (f + 1) * 128)
                P.dma('sp', wgs[b][:], wgv[:, :, fs], writes=[f'wgs{b}'])
                P.dma('act', wus[b][:], wuv[:, :, fs], writes=[f'wus{b}'])
                P.op('dve', lambda e, b=b: e.tensor_copy(wgb[b][:], wgs[b][:]), reads=[f'wgs{b}'], writes=[f'wgb{b}'])
                P.op('pool', lambda e, b=b: e.tensor_copy(wub[b][:], wus[b][:]), reads=[f'wus{b}'], writes=[f'wub{b}'])
                g_ = gtl[b]; gk = f'gtl{b}'
                gf = g_[:].rearrange("p b k -> p (b k)")
                a_ = av[b]; ak = f'av{b}'
                a3 = a_[:].rearrange("p (b t) -> p b t", t=128)
                for gi_, (a, bb) in enumerate(col_groups):
                    n = bb - a
                    pb = ps[1 + gi_]; pk = f'ps{1 + gi_}'
                    for c in range(8):
                        P.op('pe', lambda e, pb=pb, c=c, a=a, bb=bb, n=n, b=b: e.matmul(pb[:, 0:n], lhsT=wgb[b][:, c, :], rhs=h2f[:, c, a:bb],
                                                                                     start=(c == 0), stop=(c == 7)),
                             reads=[f'wgb{b}'] + hk, writes=[pk])
                    P.op('act', lambda e, pb=pb, a=a, bb=bb, n=n, gf=gf: e.copy(gf[:, a:bb], pb[:, 0:n]), reads=[pk], writes=[gk])
                P.op('dve', lambda e, f=f, g_=g_, a3=a3: e.tensor_scalar(a3, g_[:, :, 0:128], smt[:, 8 + 3 * f:9 + 3 * f], None, op0=ALU.mult),
                     reads=[gk, 'smt'], writes=[ak])
                for k in (1, 2):
                    P.op('dve', lambda e, f=f, g_=g_, a3=a3, k=k: e.scalar_tensor_tensor(
                        a3, g_[:, :, k:k + 128], smt[:, 8 + 3 * f + k:9 + 3 * f + k], a3, op0=ALU.mult, op1=ALU.add),
                         reads=[gk, 'smt', ak], writes=[ak])
                P.op('act', lambda e, a_=a_: e.activation(a_[:], a_[:], AF.Silu), reads=[ak], writes=[ak])
                for gi_ in range(2):
                    pb = ps[4 + gi_]; pk = f'ps{4 + gi_}'
                    for c in range(8):
                        P.op('pe', lambda e, pb=pb, c=c, gi_=gi_, b=b: e.matmul(pb[:], lhsT=wub[b][:, c, :], rhs=h2[:, c, gi_ * 4:(gi_ + 1) * 4, 2:130],
                                                                              start=(c == 0), stop=(c == 7)),
                             reads=[f'wub{b}'] + hk, writes=[pk])
                    P.op('dve', lambda e, pb=pb, gi_=gi_, f=f, a_=a_: e.tensor_tensor(
                        prod[:, f, gi_ * 512:(gi_ + 1) * 512], a_[:, gi_ * 512:(gi_ + 1) * 512], pb[:], op=ALU.mult), reads=[pk, ak], writes=[f'prod{f}'])
            pks = [f'prod{f}' for f in range(NFF)]
            di = 0
            for og in range(2):
                for th in range(2):
                    tsl = slice(th * 512, (th + 1) * 512)
                    for f in range(NFF):
                        b = di % 2; di += 1
                        P.dma('sp' if di % 2 else 'act', wds[b][:], wd[f * 128:(f + 1) * 128, og * 512:(og + 1) * 512], writes=[f'wds{b}'])
                        P.op('pool' if di % 2 else 'dve', lambda e, b=b: e.tensor_copy(wdb[b][:], wds[b][:]), reads=[f'wds{b}'], writes=[f'wdb{b}'])
                        for o4 in range(4):
                            P.op('pe', lambda e, o4=o4, b=b, f=f, tsl=tsl: e.matmul(ps[o4][:], lhsT=wdb[b][:, o4 * 128:(o4 + 1) * 128], rhs=prod[:, f, tsl],
                                                                                  start=(f == 0), stop=(f == NFF - 1)),
                                 reads=[f'wdb{b}'] + pks, writes=[f'ps{o4}'])
                    for o4 in range(4):
                        oc = og * 4 + o4
                        o_ = ot[o4 % 2]; ok = f'ot{o4 % 2}'
                        P.op('dve', lambda e, o4=o4, oc=oc, o_=o_, th=th: e.tensor_tensor(
                            o_[:].rearrange("p (b t) -> p b t", t=128), ps[o4][:].rearrange("p (b t) -> p b t", t=128),
                            xm[:, oc, th * 4:(th + 1) * 4, 2:130], op=ALU.add),
                             reads=[f'ps{o4}', 'xm'], writes=[ok])
                        P.dma('sp', xoT[oc * 128:(oc + 1) * 128, u0 + th * 512:u0 + (th + 1) * 512], o_[:], reads=[ok])
        ns = P.emit()
        print("L3b: instrs", len(P.ins), "sems", ns, "waits", P.n_waits)


RG = [[0, 1, 2, 3], [4, 5, 6, 7]]


def _allgather(nc, pairs):
    s = nc.alloc_semaphore(name=nc.make_name("cc_sem", True))
    with nc.Block() as block:
        @block.gpsimd
        def _(g):
            for (src, dst) in pairs:
                g.collective_compute("AllGather", mybir.AluOpType.bypass, replica_groups=RG,
                                     ins=[src.ap().opt()], outs=[dst.ap().opt()]).then_inc(s)
            g.wait_ge(s, len(pairs))
    nc.clear_and_free_semaphores([s])
    nc.all_engine_barrier()


def build_fused(nlayers=2, debug=False):
    nc = bass.Bass("TRN2", target_bir_lowering=False)
    ext = lambda name, shape, dt: nc.dram_tensor(name, shape, dt, kind="ExternalInput").ap()
    xT = ext("xT", [1024, 2048], F32)
    pos = ext("pos", [128, 2048], I32)
    w_in = ext("w_in", [2, 1024, 10052], F32)
    gmix = ext("gmix", [2, 128, 8], F32)
    gq = ext("gq", [2, 128, 4], F32)
    cst = ext("cst", [128, 8], F32)
    rmA = ext("rmA", [128, 128], F32)
    rmI = ext("rmI", [128, 128], F32)
    negm = ext("negm", [128, 512], F32)
    mdm = ext("mdm", [128, 4, 4, 128], F32)
    su = ext("su", [128, 128], F32)
    ident = ext("ident", [128, 128], F32)
    pw = ext("pw", [128, NIT], F32)
    sel = ext("sel", [128, 4], F32)
    wo = ext("wo", [2, 4, 512, 1024], F32)
    wm = ext("wm", [2, 1024, 1024], F32)
    sm3a = ext("sm3a", [2, 128, 192], F32)
    wg = ext("wg", [2, 1024, 2816], F32)
    wu = ext("wu", [2, 1024, 2816], F32)
    wd = ext("wd", [2, 2816, 1024], F32)
    sm3b = ext("sm3b", [2, 128, 80], F32)
    out = nc.dram_tensor("out", [1024, 2048], F32, kind="ExternalOutput").ap()
    dk = dict(kind="ExternalOutput") if debug else {}
    o_q = nc.dram_tensor("o_q", [1280, 2048], BF16, **dk)
    o_f32 = nc.dram_tensor("o_f32", [6656, 2048], F32, **dk)
    o_iw = nc.dram_tensor("o_iw", [2048, 4], F32, **dk)
    krows = [0] + [64 + 128 * h for h in range(4)] + [576 + 128 * h for h in range(4)]
    o_k = {r0: nc.dram_tensor(f"o_k{r0}", [64 if r0 == 0 else 128, 2048], BF16) for r0 in krows}
    G_k = {r0: nc.dram_tensor(f"G_k{r0}", [4 * (64 if r0 == 0 else 128), 2048], BF16) for r0 in krows}
    o_v = [nc.dram_tensor(f"o_v{q}", [256, 1024], BF16) for q in range(8)]
    G_v = [nc.dram_tensor(f"G_v{q}", [4 * 256, 1024], BF16) for q in range(8)]
    tail1 = [nc.dram_tensor(f"tail1_{q}", [256, 512], F32) for q in range(8)]
    GT1 = [nc.dram_tensor(f"GT1_{q}", [4 * 256, 512], F32) for q in range(8)]
    yaT = nc.dram_tensor("yaT", [512, 2048], F32, **dk)
    ydT = nc.dram_tensor("ydT", [512, 2048], F32, **dk)
    xm = nc.dram_tensor("xm", [1024, 2048], F32, **dk)
    tail2 = nc.dram_tensor("tail2", [1024, 32], F32)
    GT2 = nc.dram_tensor("GT2", [4 * 1024, 32], F32)
    xo0 = nc.dram_tensor("xo0", [1024, 2048], F32)
    for l in range(nlayers):
        x_in = xT if l == 0 else xo0.ap()
        IOD = dict(xT=x_in, pos=pos, w=w_in[l], gmix=gmix[l], gq=gq[l], cst=cst, rmA=rmA, rmI=rmI,
                 o_q=o_q.ap(), o_k={k: v.ap() for k, v in o_k.items()}, o_f32=o_f32.ap(), o_v=[v.ap() for v in o_v], o_iw=o_iw.ap(), tail1=[v.ap() for v in tail1],
                 G_k={k: v.ap() for k, v in G_k.items()}, G_v=[v.ap() for v in G_v], GT1=[v.ap() for v in GT1], negm=negm, mdm=mdm, su=su, ident=ident, pw=pw, sel=sel,
                 yaT=yaT.ap(), ydT=ydT.ap(), wo=wo[l], wm=wm[l], sm3a=sm3a[l], xm=xm.ap(), tail2=tail2.ap(), GT2=GT2.ap(),
                 wg=wg[l], wu=wu[l], wd=wd[l], sm3b=sm3b[l], xo=(xo0.ap() if l < nlayers - 1 else out))
        IOD['cc_l2'] = ([(o_k[r0], G_k[r0], f'Gk{r0}') for r0 in krows[5:]] + [(o_v[q], G_v[q], f'Gv{q}') for q in range(8)]
                        + [(o_k[r0], G_k[r0], f'Gk{r0}') for r0 in krows[:5]] + [(tail1[q], GT1[q], f'GT1_{q}') for q in range(8)])
        build_l1(nc, IOD, f"a{l}_")
        build_l2(nc, IOD, f"b{l}_")
        build_l3a(nc, IOD, f"c{l}_")
        _allgather(nc, [(tail2, GT2)])
        build_l3b(nc, IOD, f"d{l}_")
    return nc


_NC = {}


def _stripe(a, j):
    return a.reshape((64, 128) + a.shape[1:])[j::4].reshape((2048,) + a.shape[1:])


def kernel(_nlayers=2, _debug=False, **inputs):
    inp = {k: np.asarray(v) for k, v in inputs.items()}
    if 'nc' not in _NC:
        _NC['nc'] = build_fused(_nlayers, _debug)
    nc = _NC['nc']
    cst, RmA, RmI = l1_consts()
    su, ident, pw = l2_consts()
    gq = np.zeros((2, 128, 4), np.float32)
    gq[:, :, 0] = inp['g_qa']; gq[:, :, 1] = inp['g_ka']; gq[:, :64, 2] = inp['g_kidx']
    gmix = np.ascontiguousarray(inp['g_mix'].reshape(2, 8, 128).transpose(0, 2, 1))
    sm3a = np.zeros((2, 128, 192), np.float32)
    sm3b = np.zeros((2, 128, 80), np.float32)
    for l in range(2):
        sm3a[l, :, 0:32] = inp['b_gate'][l].reshape(32, 128).T
        sm3a[l, :, 32:156] = inp['cb_conv_w'][l].T.reshape(4, 128, 31).transpose(1, 0, 2).reshape(128, 124)
        sm3a[l, :, 156:160] = inp['cb_conv_b'][l].reshape(4, 128).T
        sm3a[l, :, 160:164] = inp['cb_ln_g'][l].reshape(4, 128).T
        sm3a[l, :, 164:168] = inp['cb_ln_b'][l].reshape(4, 128).T
        sm3a[l, :, 168:180] = inp['cc_conv_w'][l].T.reshape(4, 128, 3).transpose(1, 0, 2).reshape(128, 12)
        sm3b[l, :, 0:8] = inp['g_ffn'][l].reshape(8, 128).T
        sm3b[l, :, 8:74] = inp['ffn_conv_w'][l].T.reshape(22, 128, 3).transpose(1, 0, 2).reshape(128, 66)
    wo = np.ascontiguousarray(np.stack([inp['w_oa'], inp['w_ob'], inp['w_oc'], inp['w_od']], axis=1))
    shared = dict(w_in=np.ascontiguousarray(inp['w_in']), gmix=gmix, gq=gq, cst=cst, rmA=RmA, rmI=RmI, su=su, ident=ident, pw=pw,
                  wo=wo, wm=np.ascontiguousarray(inp['w_merge']), sm3a=sm3a, wg=np.ascontiguousarray(inp['w_ffn_gate']),
                  wu=np.ascontiguousarray(inp['w_ffn_up']), wd=np.ascontiguousarray(inp['w_ffn_down']), sm3b=sm3b)
    maps = []
    for c in range(8):
        b, j = c // 4, c % 4
        neg, md = l2_masks(j)
        sel = np.zeros((128, 4), np.float32)
        sel[:, (j - 1) % 4] = 1.0
        m = dict(shared)
        m.update(xT=np.ascontiguousarray(_stripe(inp['x'][b], j).T),
                 pos=np.ascontiguousarray(np.broadcast_to(_stripe(inp['positions'][b], j)[None, :], (128, 2048)).astype(np.int32)),
                 negm=neg, mdm=md, sel=sel)
        maps.append(m)
    res = run_bass_kernel_spmd(nc, maps, core_ids=list(range(8)))
    if _debug:
        _NC['res'] = res.results
    out = np.zeros((2, 64, 128, 1024), np.float32)
    for c in range(8):
        b, j = c // 4, c % 4
        out[b, j::4] = np.asarray(res.results[c]['out']).T.reshape(16, 128, 1024)
    return out.reshape(2, 8192, 1024)
```

```python
import math
import contextlib
import numpy as np
from concourse.bass_utils import run_bass_kernel_spmd
import numpy as np
import concourse.bass as bass
import concourse.mybir as mybir

F32 = mybir.dt.float32
BF16 = mybir.dt.bfloat16
I32 = mybir.dt.int32
ALU = mybir.AluOpType
AF = mybir.ActivationFunctionType
AX = mybir.AxisListType

SEM_EPOCH = 30000


class Prog:
    ENGS = ('pe', 'act', 'dve', 'pool', 'sp')

    def __init__(self, nc, n_dma_sems=6):
        self.nc = nc
        self.ins = []
        self.stream = {e: [] for e in self.ENGS}
        self.last_w = {}
        self.readers = {}
        self.n_dma_sems = n_dma_sems
        self.dma_rr = {e: 0 for e in self.ENGS}
        self.dma_slot_last = {}

    def _deps(self, eng, reads, writes, is_dma):
        deps = set()
        for k in reads:
            w = self.last_w.get(k)
            if w is not None:
                deps.add(w)
        for k in writes:
            w = self.last_w.get(k)
            if w is not None:
                deps.add(w)
            for r in self.readers.get(k, ()):
                deps.add(r)
        out = []
        for d in deps:
            di = self.ins[d]
            if (not is_dma) and (not di['dma']) and di['eng'] == eng:
                if eng == 'pe':
                    continue
            out.append(d)
        return out

    def _commit(self, iid, reads, writes):
        for k in reads:
            self.readers.setdefault(k, []).append(iid)
        for k in writes:
            self.last_w[k] = iid
            self.readers[k] = []

    def op(self, eng, fn, reads=(), writes=()):
        reads = list(reads); writes = list(writes)
        deps = self._deps(eng, reads, writes, False)
        iid = len(self.ins)
        self.ins.append(dict(eng=eng, fn=fn, deps=deps, dma=False, signal=False))
        self.stream[eng].append(iid)
        self._commit(iid, reads, writes)
        return iid

    def dma(self, eng, out, in_, reads=(), writes=(), **kw):
        reads = list(reads); writes = list(writes)
        deps = self._deps(eng, reads, writes, True)
        slot = self.dma_rr[eng] % self.n_dma_sems
        self.dma_rr[eng] += 1
        prev = self.dma_slot_last.get((eng, slot))
        if prev is not None and prev not in deps:
            deps.append(prev)
        iid = len(self.ins)
        self.ins.append(dict(eng=eng, fn=None, deps=deps, dma=True, signal=True,
                             slot=slot, out=out, in_=in_, kw=kw))
        self.dma_slot_last[(eng, slot)] = iid
        self.stream[eng].append(iid)
        self._commit(iid, reads, writes)
        return iid

    def cc(self, eng, src, dst, reads=(), writes=()):
        reads = list(reads); writes = list(writes)
        deps = self._deps(eng, reads, writes, True)
        n = self.dma_rr.get((eng, 'cc'), 0)
        self.dma_rr[(eng, 'cc')] = n + 1
        slot = 100 + n % 4
        prev = self.dma_slot_last.get((eng, slot))
        if prev is not None and prev not in deps:
            deps.append(prev)
        iid = len(self.ins)
        self.ins.append(dict(eng=eng, fn=None, deps=deps, dma=True, signal=True, slot=slot, cc=(src, dst), inc=1))
        self.dma_slot_last[(eng, slot)] = iid
        self.stream[eng].append(iid)
        self._commit(iid, reads, writes)
        return iid

    def emit(self, final_wait_eng='sp'):
        nc = self.nc
        ins = self.ins
        pos = {}
        for e in self.ENGS:
            for p, iid in enumerate(self.stream[e]):
                pos[iid] = p
        def chan(i):
            d = ins[i]
            return ('d', d['eng'], d['slot']) if d['dma'] else ('c', d['eng'])
        need = {}
        for e in self.ENGS:
            waited = {}
            for iid in self.stream[e]:
                lst = []
                for d in sorted(ins[iid]['deps'], key=lambda x: -pos[x]):
                    c = chan(d)
                    if waited.get(c, -1) >= pos[d]:
                        continue
                    waited[c] = pos[d]
                    ins[d]['signal'] = True
                    lst.append(d)
                need[iid] = lst
        final = list(self.dma_slot_last.values())
        semcount = {}
        sems = {}
        stack = []

        def get_sem(key):
            if key not in sems:
                s = nc.alloc_semaphore(name=nc.make_name("s_" + "_".join(str(k) for k in key), True))
                sems[key] = s
            return sems[key]

        for e in self.ENGS:
            ccount = 0
            dcount = {}
            for iid in self.stream[e]:
                d = ins[iid]
                if d['dma']:
                    sl = d['slot']
                    dcount[sl] = dcount.get(sl, 0) + 16
                    d['semkey'] = ('d', e, sl, dcount[sl] // (SEM_EPOCH * 16 + 16))
                    d['semval'] = dcount[sl] - d['semkey'][3] * (SEM_EPOCH * 16 + 16) if False else None
                elif d['signal']:
                    ccount += 1
                    ep = (ccount - 1) // SEM_EPOCH
                    d['semkey'] = ('c', e, ep)
                    d['semval'] = ccount - ep * SEM_EPOCH
            dc2 = {}
            for iid in self.stream[e]:
                d = ins[iid]
                if d['dma']:
                    sl = d['slot']
                    n = dc2.get(sl, 0) + 1
                    dc2[sl] = n
                    ep = (n - 1) // 2000
                    d['semkey'] = ('d', e, sl, ep)
                    d['semval'] = d.get('inc', 16) * (n - ep * 2000)
        engobj = {'pe': 'tensor', 'act': 'scalar', 'dve': 'vector', 'pool': 'gpsimd', 'sp': 'sync'}
        self.n_waits = 0

        def run_stream(e, eng):
            for iid in self.stream[e]:
                d = ins[iid]
                for dep in need[iid]:
                    dd = ins[dep]
                    eng.wait_ge(get_sem(dd['semkey']), dd['semval'])
                    self.n_waits += 1
                if d['dma'] and 'cc' in d:
                    eng.collective_compute("AllGather", mybir.AluOpType.bypass, replica_groups=[[0, 1, 2, 3], [4, 5, 6, 7]],
                                           ins=[d['cc'][0].ap().opt()], outs=[d['cc'][1].ap().opt()]).then_inc(get_sem(d['semkey']))
                elif d['dma']:
                    eng.dma_start(out=d['out'], in_=d['in_'], **d['kw']).then_inc(
                        get_sem(d['semkey']), 16)
                else:
                    r = d['fn'](eng)
                    if d['signal']:
                        r.then_inc(get_sem(d['semkey']), 1)
            if e == final_wait_eng:
                for f in final:
                    dd = ins[f]
                    eng.wait_ge(get_sem(dd['semkey']), dd['semval'])

        for e in self.ENGS:
            for iid in self.stream[e]:
                d = ins[iid]
                if d['dma'] or d['signal']:
                    get_sem(d['semkey'])
        with nc.Block() as block:
            for e in self.ENGS:
                if not self.stream[e] and e != final_wait_eng:
                    continue
                deco = getattr(block, engobj[e])
                deco(lambda eng, e=e: run_stream(e, eng))
        nc.clear_and_free_semaphores(list(sems.values()))
        nc.all_engine_barrier()
        return len(sems)


T = 2048
D = 1024
N_IN = 10052
EPS = 1e-6
PI = math.pi
SINK = 0.999999

O_AQ, O_AK, O_AV, O_IQ, O_IK, O_IW, O_GLU, O_CC, O_SQ, O_SK, O_SV, O_GATE = (
    0, 512, 1024, 1536, 1792, 1856, 1860, 2884, 4420, 4932, 5444, 5956)
R_QA, R_QI, R_SQ, NR_Q = 0, 512, 768, 1280
R_KI, R_KA, R_SK, NR_K = 0, 64, 576, 1088
R_GLU, R_CC, R_GATE, NR_F32 = 0, 1024, 2560, 6656


def l1_items():
    items = []
    for h in range(4):
        items.append(('qkA', O_AQ + 128 * h, 128, R_QA + 128 * h, 0))
    for h in range(4):
        items.append(('qkA', O_AK + 128 * h, 128, R_KA + 128 * h, 1))
    items.append(('tmv', O_AV, 256, 0, 0))
    items.append(('tmv', O_AV + 256, 256, 256, 0))
    items.append(('qi', O_IQ, 128, R_QI, 0))
    items.append(('qi', O_IQ + 128, 128, R_QI + 128, 0))
    items.append(('ki', O_IK, 64, R_KI, 0))
    items.append(('iw', O_IW, 4, 0, 0))
    for j in range(8):
        items.append(('f32', O_GLU + 128 * j, 128, R_GLU + 128 * j, 0))
    for j in range(12):
        items.append(('f32', O_CC + 128 * j, 128, R_CC + 128 * j, 0))
    for j in range(4):
        items.append(('bf', O_SQ + 128 * j, 128, R_SQ + 128 * j, 0))
    for j in range(4):
        items.append(('bf', O_SK + 128 * j, 128, R_SK + 128 * j, 0))
    items.append(('tmv', O_SV, 256, 512, 0))
    items.append(('tmv', O_SV + 256, 256, 768, 0))
    for j in range(32):
        items.append(('f32', O_GATE + 128 * j, 128, R_GATE + 128 * j, 0))
    groups = []
    cur = []
    for it in items:
        if cur and (it[1] + it[2] - cur[0][1]) > 256:
            groups.append(cur); cur = []
        cur.append(it)
    if cur:
        groups.append(cur)
    return groups


def l1_consts():
    half = 16
    invA = (500000.0 ** (-(np.arange(half, dtype=np.float32) * 2.0) / 32)).astype(np.float32)
    invI = (500000.0 ** (-(np.arange(8, dtype=np.float32) * 2.0) / 16)).astype(np.float32)
    cst = np.zeros((128, 8), np.float32)
    for p in range(32):
        cst[p, 0] = invA[p % 16]
    for p in range(128):
        if p % 64 < 16:
            cst[p, 1] = invI[p % 8]
    cst[:, 3] = 0.0
    cst[:, 4] = 0.5 * PI * SINK
    RmA = np.zeros((128, 128), np.float32)
    for m in range(16):
        RmA[m + 16, m] = -1.0
        RmA[m, m + 16] = 1.0
    RmI = np.zeros((128, 128), np.float32)
    for b in (0, 64):
        for m in range(8):
            RmI[b + m + 8, b + m] = -1.0
            RmI[b + m, b + m + 8] = 1.0
    return cst, RmA, RmI


def build_l1(nc, IO, pfx):
    xT, pos, w, gmix, gq, cst, rmA, rmI = IO['xT'], IO['pos'], IO['w'], IO['gmix'], IO['gq'], IO['cst'], IO['rmA'], IO['rmI']
    o_q, o_k, o_f32, o_v, o_iw, tail1 = IO['o_q'], IO['o_k'], IO['o_f32'], IO['o_v'], IO['o_iw'], IO['tail1']

    P = Prog(nc)
    import contextlib
    es = contextlib.ExitStack()

    def sb(name, shape, dt):
        return es.enter_context(nc.sbuf_tensor(pfx + name, shape, dt))

    def pst(name, shape, dt=F32):
        return es.enter_context(nc.psum_tensor(pfx + name, shape, dt))

    with es:
        hT = sb("hT", [128, 8, T], BF16)
        xs = [sb(f"xs{i}", [128, 8, 256], F32) for i in range(2)]
        sqs = sb("sqs", [128, 8, 256], F32)
        rs = sb("rs", [128, 256], F32)
        gm = sb("gm", [128, 8], F32)
        gqt = sb("gqt", [128, 4], F32)
        cs = sb("cs", [128, 8], F32)
        rA = sb("rA", [128, 128], F32)
        rI = sb("rI", [128, 128], F32)
        ones = sb("ones", [128, 128], F32)
        posi = sb("posi", [128, T], I32)
        posf = sb("posf", [128, T], F32)
        ang = sb("ang", [128, T], F32)
        posf2 = sb("posf2", [128, T], F32)
        posi2 = sb("posi2", [128, T], I32)
        cosA = sb("cosA", [128, T], F32)
        sinA = sb("sinA", [128, T], F32)
        cosI = sb("cosI", [128, T], F32)
        sinI = sb("sinI", [128, T], F32)
        wf = [sb(f"wf{i}", [128, 8, 256], F32) for i in range(2)]
        wb = [sb(f"wb{i}", [128, 8, 256], BF16) for i in range(2)]
        of32 = [sb(f"of{i}", [128, T], F32) for i in range(2)]
        obf = [sb(f"ob{i}", [128, T], BF16) for i in range(2)]
        ov = [sb(f"ov{i}", [128, 16, 256], BF16) for i in range(2)]
        oiw = sb("oiw", [128, 16, 4], F32)
        raw = sb("raw", [128, 512], F32)
        sq = sb("sq", [128, 512], F32)
        rstd = sb("rstd", [128, 512], F32)
        nrm = sb("nrm", [128, 512], F32)
        t1 = sb("t1", [128, 512], F32)
        t2 = sb("t2", [128, 512], F32)
        psm = [pst(f"psm{i}", [128, 512]) for i in range(4)]
        psn = pst("psn", [128, 512])
        psr = pst("psr", [128, 512])

        P.dma('sp', gm[:], gmix, writes=['gm'])
        P.dma('sp', gqt[:], gq, writes=['gqt'])
        P.dma('sp', cs[:], cst, writes=['cs'])
        P.dma('sp', rA[:], rmA, writes=['rA'])
        P.dma('sp', rI[:], rmI, writes=['rI'])
        P.dma('sp', posi[:], pos, writes=['posi'])
        P.op('dve', lambda e: e.memset(ones[:], 1.0), writes=['ones'])
        P.op('dve', lambda e: e.tensor_copy(posf[:], posi[:]), reads=['posi'], writes=['posf'])
        for j, (ct, st) in enumerate(((cosA, sinA), (cosI, sinI))):
            cn, sn = f"cos{j}", f"sin{j}"
            P.op('dve', lambda e, j=j: e.tensor_scalar(ang[:], posf[:], cs[:, j:j + 1], None, op0=ALU.mult),
                 reads=['posf', 'cs'], writes=['ang'])
            for (tt_, tn, shift, bcol) in ((st, sn, 0.0, 3), (ct, cn, 0.5 * PI, 4)):
                P.op('dve', lambda e, shift=shift: e.tensor_scalar(posf2[:], ang[:], 1.0 / (2 * PI), 0.5 + shift / (2 * PI),
                                                                   op0=ALU.mult, op1=ALU.add),
                     reads=['ang'], writes=['posf2'])
                P.op('dve', lambda e: e.tensor_copy(posi2[:], posf2[:]), reads=['posf2'], writes=['posi2'])
                P.op('dve', lambda e: e.tensor_copy(posf2[:], posi2[:]), reads=['posi2'], writes=['posf2'])
                P.op('dve', lambda e, tt_=tt_: e.scalar_tensor_tensor(tt_[:], posf2[:], -2 * PI, ang[:], op0=ALU.mult, op1=ALU.add),
                     reads=['posf2', 'ang'], writes=[tn])
                P.op('dve', lambda e, tt_=tt_, shift=shift: e.tensor_scalar(posf2[:], tt_[:], -PI - shift, 2 * PI,
                                                                          op0=ALU.is_lt, op1=ALU.mult),
                     reads=[tn], writes=['posf2'])
                P.op('dve', lambda e, tt_=tt_: e.tensor_tensor(tt_[:], tt_[:], posf2[:], op=ALU.add),
                     reads=[tn, 'posf2'], writes=[tn])
                P.op('act', lambda e, tt_=tt_, bcol=bcol: e.activation(tt_[:], tt_[:], AF.Sin, bias=cs[:, bcol:bcol + 1], scale=SINK),
                     reads=[tn, 'cs'], writes=[tn])

        xTv = xT.rearrange("(c p) t -> p c t", p=128)
        for tg in range(8):
            xb = xs[tg % 2]
            xk = f"xs{tg % 2}"
            P.dma('sp', xb[:], xTv[:, :, tg * 256:(tg + 1) * 256], writes=[xk])
            P.op('act', lambda e, xb=xb: e.activation(sqs[:], xb[:], AF.Square), reads=[xk], writes=['sqs'])
            for c in range(8):
                P.op('pe', lambda e, c=c: e.matmul(psn[:, 0:256], lhsT=ones[:], rhs=sqs[:, c, :],
                                                   start=(c == 0), stop=(c == 7)),
                     reads=['ones', 'sqs'], writes=['psn'])
            P.op('dve', lambda e: e.tensor_scalar(rs[:], psn[:, 0:256], 1.0 / D, EPS, op0=ALU.mult, op1=ALU.add),
                 reads=['psn'], writes=['rs'])
            P.op('act', lambda e: e.activation(rs[:], rs[:], AF.Sqrt), reads=['rs'], writes=['rs'])
            P.op('dve', lambda e: e.reciprocal(rs[:], rs[:]), reads=['rs'], writes=['rs'])
            for c in range(8):
                P.op('dve', lambda e, c=c, xb=xb, tg=tg: e.scalar_tensor_tensor(
                    hT[:, c, tg * 256:(tg + 1) * 256], xb[:, c, :], gm[:, c:c + 1], rs[:],
                    op0=ALU.mult, op1=ALU.mult),
                     reads=[xk, 'gm', 'rs'], writes=[f'hT{tg}'])
        hkeys = [f'hT{tg}' for tg in range(8)]

        wv = w.rearrange("(c p) n -> p c n", p=128)
        groups = l1_items()
        cnt = dict(ps=0, of=0, ob=0, ov=0)
        for gi, grp in enumerate(groups):
            c0 = grp[0][1]
            c1 = grp[-1][1] + grp[-1][2]
            nco = c1 - c0
            b = gi % 2
            P.dma('sp', wf[b][:, :, 0:nco], wv[:, :, c0:c1], writes=[f'wf{b}'])
            ceng = 'pool' if gi % 2 == 0 else 'dve'
            P.op(ceng, lambda e, b=b, nco=nco: e.tensor_copy(wb[b][:, :, 0:nco], wf[b][:, :, 0:nco]),
                 reads=[f'wf{b}'], writes=[f'wb{b}'])
            for (kind, col, ncol, orow, aux) in grp:
                lo = col - c0
                if kind in ('tmv', 'iw'):
                    if kind == 'tmv':
                        ob_ = ov[cnt['ov'] % 2]; okey = f"ov{cnt['ov'] % 2}"; cnt['ov'] += 1
                    else:
                        ob_ = oiw; okey = 'oiw'
                    for tt in range(16):
                        pi = cnt['ps'] % 4; cnt['ps'] += 1
                        for c in range(8):
                            P.op('pe', lambda e, pi=pi, c=c, tt=tt, lo=lo, ncol=ncol, b=b: e.matmul(
                                psm[pi][:, 0:ncol], lhsT=hT[:, c, tt * 128:(tt + 1) * 128],
                                rhs=wb[b][:, c, lo:lo + ncol], start=(c == 0), stop=(c == 7)),
                                 reads=hkeys + [f'wb{b}'], writes=[f'psm{pi}'])
                        ee = 'act' if tt % 2 == 0 else 'dve'
                        if ee == 'act':
                            P.op('act', lambda e, pi=pi, tt=tt, ncol=ncol, ob_=ob_: e.copy(ob_[:, tt, 0:ncol], psm[pi][:, 0:ncol]),
                                 reads=[f'psm{pi}'], writes=[okey])
                        else:
                            P.op('dve', lambda e, pi=pi, tt=tt, ncol=ncol, ob_=ob_: e.tensor_copy(ob_[:, tt, 0:ncol], psm[pi][:, 0:ncol]),
                                 reads=[f'psm{pi}'], writes=[okey])
                    if kind == 'tmv':
                        for q in range(8):
                            P.dma('act', o_v[q].rearrange("(tt p) c -> p tt c", p=128)[:, :, orow:orow + 256], ob_[:, 2 * q:2 * q + 2, :], reads=[okey])
                    else:
                        P.dma('act', o_iw.rearrange("(tt p) c -> p tt c", p=128), ob_[:], reads=[okey])
                    continue
                if kind == 'f32':
                    ob_ = of32[cnt['of'] % 2]; okey = f"of{cnt['of'] % 2}"; cnt['of'] += 1
                else:
                    ob_ = obf[cnt['ob'] % 2]; okey = f"ob{cnt['ob'] % 2}"; cnt['ob'] += 1
                for g in range(4):
                    pi = cnt['ps'] % 4; cnt['ps'] += 1
                    ts_ = slice(g * 512, (g + 1) * 512)
                    for c in range(8):
                        P.op('pe', lambda e, pi=pi, c=c, lo=lo, ncol=ncol, b=b, ts_=ts_: e.matmul(
                            psm[pi][0:ncol, :], lhsT=wb[b][:, c, lo:lo + ncol], rhs=hT[:, c, ts_],
                            start=(c == 0), stop=(c == 7)),
                             reads=hkeys + [f'wb{b}'], writes=[f'psm{pi}'])
                    pk = f'psm{pi}'
                    if kind in ('f32', 'bf'):
                        if g % 2 == 0:
                            P.op('act', lambda e, pi=pi, ts_=ts_, ob_=ob_: e.copy(ob_[:, ts_], psm[pi][:]),
                                 reads=[pk], writes=[okey])
                        else:
                            P.op('dve', lambda e, pi=pi, ts_=ts_, ob_=ob_: e.tensor_copy(ob_[:, ts_], psm[pi][:]),
                                 reads=[pk], writes=[okey])
                        continue
                    np_ = ncol
                    if kind in ('qkA', 'ki'):
                        gcol = aux if kind == 'qkA' else 2
                        P.op('act', lambda e, pi=pi, np_=np_: e.copy(raw[0:np_, :], psm[pi][0:np_, :]), reads=[pk], writes=['raw'])
                        P.op('act', lambda e, pi=pi, np_=np_: e.activation(sq[0:np_, :], psm[pi][0:np_, :], AF.Square),
                             reads=[pk], writes=['sq'])
                        P.op('pe', lambda e, np_=np_: e.matmul(psn[0:np_, :], lhsT=ones[0:np_, 0:np_], rhs=sq[0:np_, :],
                                                              start=True, stop=True),
                             reads=['ones', 'sq'], writes=['psn'])
                        P.op('dve', lambda e, np_=np_: e.tensor_scalar(rstd[0:np_, :], psn[0:np_, :], 1.0 / np_, EPS,
                                                                      op0=ALU.mult, op1=ALU.add),
                             reads=['psn'], writes=['rstd'])
                        P.op('act', lambda e, np_=np_: e.activation(rstd[0:np_, :], rstd[0:np_, :], AF.Sqrt),
                             reads=['rstd'], writes=['rstd'])
                        P.op('dve', lambda e, np_=np_: e.reciprocal(rstd[0:np_, :], rstd[0:np_, :]),
                             reads=['rstd'], writes=['rstd'])
                        P.op('dve', lambda e, np_=np_, gcol=gcol: e.scalar_tensor_tensor(
                            nrm[0:np_, :], raw[0:np_, :], gqt[0:np_, gcol:gcol + 1], rstd[0:np_, :],
                            op0=ALU.mult, op1=ALU.mult), reads=['raw', 'gqt', 'rstd'], writes=['nrm'])
                        src = nrm
                    else:
                        P.op('act', lambda e, pi=pi: e.copy(nrm[:], psm[pi][:]), reads=[pk], writes=['nrm'])
                        src = nrm
                    if kind == 'qkA':
                        rp = 32; R = rA; ct, st, cn, sn = cosA, sinA, 'cos0', 'sin0'
                    else:
                        rp = np_; R = rI; ct, st, cn, sn = cosI, sinI, 'cos1', 'sin1'
                    P.op('pe', lambda e, rp=rp, R=R: e.matmul(psr[0:rp, :], lhsT=R[0:rp, 0:rp], rhs=nrm[0:rp, :],
                                                             start=True, stop=True),
                         reads=['nrm', 'rA', 'rI'], writes=['psr'])
                    P.op('pool', lambda e, rp=rp, ct=ct, ts_=ts_: e.tensor_tensor(t1[0:rp, :], nrm[0:rp, :], ct[0:rp, ts_], op=ALU.mult),
                         reads=['nrm', cn], writes=['t1'])
                    P.op('dve', lambda e, rp=rp, st=st, ts_=ts_: e.tensor_tensor(t2[0:rp, :], psr[0:rp, :], st[0:rp, ts_], op=ALU.mult),
                         reads=['psr', sn], writes=['t2'])
                    P.op('pool', lambda e, rp=rp, ob_=ob_, ts_=ts_: e.tensor_tensor(ob_[0:rp, ts_], t1[0:rp, :], t2[0:rp, :], op=ALU.add),
                         reads=['t1', 't2'], writes=[okey])
                    if rp < np_:
                        P.op('act', lambda e, ob_=ob_, ts_=ts_: e.copy(ob_[32:64, ts_], nrm[32:64, :]),
                             reads=['nrm'], writes=[okey])
                        P.op('act', lambda e, ob_=ob_, ts_=ts_: e.copy(ob_[64:128, ts_], nrm[64:128, :]),
                             reads=['nrm'], writes=[okey])
                oq_ = 'act'
                if kind == 'f32':
                    P.dma(oq_, o_f32[orow:orow + ncol, :], ob_[0:ncol, :], reads=[okey], writes=['o_f32'])
                else:
                    if (kind == 'ki' or (kind == 'qkA' and aux == 1) or (kind == 'bf' and col >= O_SK)):
                        P.dma('act', o_k[orow][0:ncol, :], ob_[0:ncol, :], reads=[okey])
                    else:
                        P.dma('act', o_q[orow:orow + ncol, :], ob_[0:ncol, :], reads=[okey])
        for (r0, t0) in ((R_GLU, 0), (R_CC + 512, 1024)):
            for q in range(4):
                P.dma('sp', tail1[(t0 + q * 256) // 256].rearrange("r (b k) -> r b k", k=32),
                      o_f32[r0 + q * 256:r0 + (q + 1) * 256, :].rearrange("r (b t) -> r b t", t=128)[:, :, 96:128],
                      reads=['o_f32'])
        ns = P.emit()
        print("L1: instrs", len(P.ins), "sems", ns, "waits", P.n_waits)


S = 8192
NQ = 16
NIT = 18
NEG = -1.0e30
MNEG = -30000.0
SCALE = 128 ** -0.5


def l2_masks(j):
    neg = np.zeros((128, 4, 128), np.float32)
    md = np.zeros((128, 4, 4, 128), np.float32)
    tp = np.arange(128)[:, None]
    sp = np.arange(128)[None, :]
    for m in range(4):
        if m < j:
            neg[:, m, :] = 0.0
            md[:, m, :, :] = 1.0
        elif m == j:
            neg[:, m, :] = np.where(sp <= tp, 0.0, NEG)
            md[:, m, :, :] = (np.arange(128)[:, None] < np.arange(128)[None, :]).astype(np.float32)[:, None, :]
        else:
            neg[:, m, :] = NEG
            md[:, m, :, :] = 0.0
    return neg.reshape(128, 512), md


def l2_consts():
    su = (np.arange(128)[:, None] > np.arange(128)[None, :]).astype(np.float32)
    ident = np.eye(128, dtype=np.float32)
    pw = (2.0 ** -(np.arange(NIT, dtype=np.float32) + 1.0))[None, :].repeat(128, 0).astype(np.float32)
    return su, ident, pw


def build_l2(nc, IO, pfx, do_a=True, do_d=True, nq=NQ):
    G_k, G_v, o_q, o_iw = IO['G_k'], IO['G_v'], IO['o_q'], IO['o_iw']
    negm, mdm, su_d, id_d, pw_d = IO['negm'], IO['mdm'], IO['su'], IO['ident'], IO['pw']
    yaT, ydT = IO['yaT'], IO['ydT']
    Gv4 = [g.rearrange("(r i p) n -> p r i n", r=4, p=128) for g in G_v]

    def load_nat(P, eng, dst2d, row0, nrows, key):
        dv = dst2d.rearrange("p (i r t) -> p i r t", r=4, t=128)
        for r in range(4):
            P.dma(eng if r % 2 == 0 else ('pool' if eng == 'sp' else 'sp'), dv[:, :, r, :],
                  G_k[row0][r * nrows:(r + 1) * nrows, :].rearrange("p (i t) -> p i t", t=128), reads=[f'Gk{row0}'], writes=[key])

    P = Prog(nc)
    es = contextlib.ExitStack()

    def sb(name, shape, dt):
        return es.enter_context(nc.sbuf_tensor(pfx + name, shape, dt))

    def pst(name, shape, dt=F32):
        return es.enter_context(nc.psum_tensor(pfx + name, shape, dt))

    with es:
        KT = sb("KT", [128, 4, S], BF16)
        kit = sb("kit", [64, S], BF16)
        score = sb("score", [128, S], F32)
        mneg = sb("mneg", [128, S], BF16)
        junk = sb("junk", [128, S], BF16)
        vt = [sb(f"vt{i}", [128, 4, 512], BF16) for i in range(3)]
        qblk = [sb(f"qblk{i}", [128, 4, 128], BF16) for i in range(2)]
        qiblk = [sb(f"qiblk{i}", [64, 4, 128], BF16) for i in range(2)]
        iwt = sb("iwt", [128, NQ, 4], F32)
        absw = sb("absw", [128, NQ, 4], F32)
        sgnw = sb("sgnw", [128, NQ, 4], F32)
        negt = sb("negt", [128, 512], F32)
        mdt = sb("mdt", [128, 4, 4, 128], F32)
        sut = sb("sut", [128, 128], F32)
        idf = sb("idf", [128, 128], F32)
        idb = sb("idb", [128, 128], BF16)
        pwt = sb("pwt", [128, NIT], F32)
        ones_f = sb("ones_f", [128, 128], F32)
        ones_b = sb("ones_b", [128, 128], BF16)
        rh = [sb(f"rh{i}", [128, 512], F32) for i in range(2)]
        st = sb("st", [128, 16], F32)
        wk = sb("wk", [128, NIT], F32)
        PT = [sb(f"PT{i}", [128, 512], BF16) for i in range(2)]
        rec = sb("rec", [128, 512], F32)
        oat = [sb(f"oat{i}", [128, 512], F32) for i in range(2)]
        e_t = [sb(f"e_t{i}", [128, 512], F32) for i in range(2)]
        sp_t = [sb(f"sp_t{i}", [128, 512], F32) for i in range(3)]
        u_t = [sb(f"u_t{i}", [128, 512], F32) for i in range(2)]
        R_t = [sb(f"R_t{i}", [128, 512], F32) for i in range(2)]
        aT = [sb(f"aT{i}", [128, 512], BF16) for i in range(2)]
        ps = [pst(f"ps{i}", [128, 512]) for i in range(8)]

        for (src_t, dst_t, key) in IO['cc_l2']:
            P.cc('pool', src_t, dst_t, writes=[key])
        P.dma('sp', iwt[:], o_iw.rearrange("(i t) h -> t i h", t=128), writes=['iwt'])
        P.dma('sp', negt[:], negm, writes=['negt'])
        P.dma('sp', mdt[:], mdm, writes=['mdt'])
        P.dma('sp', sut[:], su_d, writes=['sut'])
        P.dma('sp', idf[:], id_d, writes=['idf'])
        P.dma('sp', pwt[:], pw_d, writes=['pwt'])
        P.op('dve', lambda e: e.memset(ones_f[:], 1.0), writes=['ones_f'])
        P.op('dve', lambda e: e.memset(ones_b[:], 1.0), writes=['ones_b'])
        P.op('dve', lambda e: e.tensor_copy(idb[:], idf[:]), reads=['idf'], writes=['idb'])
        P.op('act', lambda e: e.activation(absw[:], iwt[:], AF.Abs), reads=['iwt'], writes=['absw'])
        P.op('act', lambda e: e.activation(sgnw[:], iwt[:], AF.Sign), reads=['iwt'], writes=['sgnw'])

        hsl = [slice(h * 128, (h + 1) * 128) for h in range(4)]

        if do_d:
            for h in range(4):
                load_nat(P, 'sp', KT[:, h, :], 576 + h * 128, 128, f'KT{h}')
            vcnt = 0
            for i in range(nq):
                NK = 4 * i + 4
                NG = i + 1
                qb_ = qblk[i % 2]; qbk = f'qblk{i % 2}'
                P.dma('sp', qb_[:], o_q[768:1280, i * 128:(i + 1) * 128].rearrange("(h d) t -> d h t", d=128), writes=[qbk])
                chs = list(range(NK - 1, -1, -1))
                vbs = {}

                def s1(k, ch):
                    nonlocal vcnt
                    kg, cc = ch // 4, ch % 4
                    if cc == 3:
                        vb = vt[vcnt % 3]; vk = f'vt{vcnt % 3}'; vcnt += 1
                        P.dma('sp', vb[:], Gv4[kg // 2][:, :, kg % 2, 512:1024], reads=[f'Gv{kg // 2}'], writes=[vk])
                        vbs[kg] = (vb, vk)
                    cs_ = slice(ch * 128, (ch + 1) * 128)
                    pz = ps[k % 2]; pzk = f'ps{k % 2}'
                    et = e_t[k % 2]; ek = f'e_t{k % 2}'
                    spt = sp_t[k % 3]; spk = f'sp{k % 3}'
                    for h in range(4):
                        P.op('pe', lambda e, pz=pz, h=h, cs_=cs_, qb_=qb_: e.matmul(pz[:, hsl[h]], lhsT=KT[:, h, cs_], rhs=qb_[:, h, :], start=True, stop=True),
                             reads=[f'KT{h}', qbk], writes=[pzk])
                    P.op('act', lambda e, pz=pz, et=et: e.activation(et[:], pz[:], AF.Exp, scale=SCALE), reads=[pzk], writes=[ek])
                    P.op('act', lambda e, spt=spt, et=et: e.activation(spt[:], et[:], AF.Ln, bias=ones_f[:, 0:1]), reads=[ek, 'ones_f'], writes=[spk])
                    if kg == NG - 1:
                        P.op('dve', lambda e, spt=spt, cc=cc: e.tensor_tensor(
                            spt[:], spt[:], mdt[:, cc, :, :].rearrange("p h t -> p (h t)"), op=ALU.mult), reads=[spk, 'mdt'], writes=[spk])

                def s2(k, ch):
                    first = (k == 0)
                    pz = ps[k % 2]; pzk = f'ps{k % 2}'
                    pB = ps[2 + k % 2]; pBk = f'ps{2 + k % 2}'
                    spt = sp_t[k % 3]; spk = f'sp{k % 3}'
                    ut = u_t[k % 2]; uk = f'u_t{k % 2}'
                    Rp, Rpk = R_t[k % 2], f'R_t{k % 2}'
                    Rn, Rnk = R_t[(k + 1) % 2], f'R_t{(k + 1) % 2}'
                    P.op('pe', lambda e, pB=pB, spt=spt, first=first: e.matmul(pB[:], lhsT=sut[:], rhs=spt[:], start=True, stop=first),
                         reads=['sut', spk], writes=[pBk])
                    if not first:
                        P.op('pe', lambda e, pB=pB, Rp=Rp: e.matmul(pB[:], lhsT=ones_f[:], rhs=Rp[:], start=False, stop=True),
                             reads=['ones_f', Rpk], writes=[pBk])
                    if first:
                        P.op('dve', lambda e, spt=spt, Rn=Rn: e.tensor_copy(Rn[:], spt[:]), reads=[spk], writes=[Rnk])
                    elif k + 1 < NK:
                        P.op('dve', lambda e, spt=spt, Rn=Rn, Rp=Rp: e.tensor_tensor(Rn[:], Rp[:], spt[:], op=ALU.add), reads=[spk, Rpk], writes=[Rnk])
                    P.op('dve', lambda e, pz=pz, spt=spt, ut=ut: e.scalar_tensor_tensor(ut[:], pz[:], SCALE, spt[:], op0=ALU.mult, op1=ALU.subtract),
                         reads=[pzk, spk], writes=[uk])
                    P.op('dve', lambda e, pB=pB, ut=ut: e.tensor_tensor(ut[:], ut[:], pB[:], op=ALU.subtract), reads=[uk, pBk], writes=[uk])

                def s3(k, ch):
                    first = (k == 0)
                    kg, cc = ch // 4, ch % 4
                    ut = u_t[k % 2]; uk = f'u_t{k % 2}'
                    at = aT[k % 2]; atk = f'aT{k % 2}'
                    vb, vk = vbs[kg]
                    P.op('act', lambda e, at=at, ut=ut: e.activation(at[:], ut[:], AF.Exp), reads=[uk], writes=[atk])
                    if kg == NG - 1:
                        P.op('dve', lambda e, at=at, cc=cc: e.tensor_tensor(
                            at[:], at[:], mdt[:, cc, :, :].rearrange("p h t -> p (h t)"), op=ALU.mult), reads=[atk, 'mdt'], writes=[atk])
                    for h in range(4):
                        P.op('pe', lambda e, at=at, h=h, vb=vb, cc=cc, first=first, ch=ch: e.matmul(
                            ps[4 + h][:, 0:128], lhsT=vb[:, cc, hsl[h]], rhs=at[:, hsl[h]], start=first, stop=(ch == 0)),
                             reads=[atk, vk], writes=[f'ps{4 + h}'])

                for n in range(NK + 2):
                    if n < NK:
                        s1(n, chs[n])
                    if 0 <= n - 1 < NK:
                        s2(n - 1, chs[n - 1])
                    if 0 <= n - 2 < NK:
                        s3(n - 2, chs[n - 2])
                ot = oat[i % 2]; otk = f'oat{i % 2}'
                for h in range(4):
                    if h % 2 == 0:
                        P.op('dve', lambda e, h=h, ot=ot: e.tensor_copy(ot[:, hsl[h]], ps[4 + h][:, 0:128]), reads=[f'ps{4 + h}'], writes=[otk])
                    else:
                        P.op('act', lambda e, h=h, ot=ot: e.copy(ot[:, hsl[h]], ps[4 + h][:, 0:128]), reads=[f'ps{4 + h}'], writes=[otk])
                P.dma('sp', ydT[:, i * 128:(i + 1) * 128].rearrange("(h d) t -> d h t", d=128), ot[:].rearrange("p (h t) -> p h t", t=128), reads=[otk])
        if do_a:
            load_nat(P, 'sp', kit[:, :], 0, 64, 'kit')
            for h in range(4):
                load_nat(P, 'sp', KT[:, h, :], 64 + h * 128, 128, f'KT{h}')
            vstate = dict(cnt=0)

            def a_score(i):
                NG = i + 1
                Sc = 512 * NG
                qb_ = qblk[i % 2]; qbk = f'qblk{i % 2}'
                qib = qiblk[i % 2]; qik = f'qiblk{i % 2}'
                P.dma('sp', qb_[:], o_q[0:512, i * 128:(i + 1) * 128].rearrange("(h d) t -> d h t", d=128), writes=[qbk])
                P.dma('sp', qib[:], o_q[512:768, i * 128:(i + 1) * 128].rearrange("(h d) t -> d h t", d=64), writes=[qik])
                for kg in range(NG):
                    ks = slice(kg * 512, (kg + 1) * 512)
                    for h in range(4):
                        pb = ps[h % 2]; pk = f'ps{h % 2}'
                        P.op('pe', lambda e, pb=pb, h=h, ks=ks, qib=qib: e.matmul(pb[:], lhsT=qib[:, h, :], rhs=kit[:, ks], start=True, stop=True),
                             reads=[qik, 'kit'], writes=[pk])
                        r_ = rh[h % 2]; rk = f'rh{h % 2}'
                        P.op('act', lambda e, pb=pb, r_=r_, i=i, h=h: e.activation(r_[:], pb[:], AF.Relu, scale=absw[:, i, h:h + 1]),
                             reads=[pk, 'absw'], writes=[rk])
                        if h == 0:
                            P.op('dve', lambda e, r_=r_, ks=ks, i=i, h=h: e.tensor_scalar(score[:, ks], r_[:], sgnw[:, i, h:h + 1], None, op0=ALU.mult),
                                 reads=[rk, 'sgnw'], writes=['score'])
                        else:
                            P.op('dve', lambda e, r_=r_, ks=ks, i=i, h=h: e.scalar_tensor_tensor(
                                score[:, ks], r_[:], sgnw[:, i, h:h + 1], score[:, ks], op0=ALU.mult, op1=ALU.add),
                                 reads=[rk, 'sgnw', 'score'], writes=['score'])
                P.op('dve', lambda e, Sc=Sc: e.tensor_reduce(st[:, 1:2], score[:, 0:Sc], axis=AX.X, op=ALU.max), reads=['score'], writes=['st_hi'])
                P.op('dve', lambda e, Sc=Sc: e.tensor_reduce(st[:, 0:1], score[:, 0:Sc], axis=AX.X, op=ALU.min), reads=['score'], writes=['st_lo'])
                P.op('pool', lambda e, Sc=Sc: e.tensor_tensor(score[:, Sc - 512:Sc], score[:, Sc - 512:Sc], negt[:], op=ALU.add),
                     reads=['score', 'negt'], writes=['score'])

            def a_bisect(i):
                Sc = 512 * (i + 1)
                P.op('dve', lambda e: e.tensor_tensor(st[:, 2:3], st[:, 1:2], st[:, 0:1], op=ALU.subtract), reads=['st_hi', 'st_lo'], writes=['st_W'])
                P.op('dve', lambda e: e.tensor_scalar(wk[:], pwt[:], st[:, 2:3], None, op0=ALU.mult), reads=['pwt', 'st_W'], writes=['wk'])
                P.op('dve', lambda e: e.tensor_tensor(st[:, 3:4], st[:, 0:1], wk[:, 0:1], op=ALU.add), reads=['st_lo', 'wk'], writes=['st_mid'])
                yield
                Sa = max(64, int(round(0.56 * Sc / 64.0)) * 64)
                for k in range(NIT):
                    P.op('act', lambda e, Sa=Sa: e.activation(junk[:, 0:Sa], score[:, 0:Sa], AF.Sign, bias=st[:, 3:4], scale=-1.0,
                                                              accum_out=st[:, 4:5]),
                         reads=['score', 'st_mid'], writes=['junkA', 'st_cntA'])
                    P.op('dve', lambda e, Sa=Sa, Sc=Sc: e.tensor_scalar(junk[:, Sa:Sc], score[:, Sa:Sc], st[:, 3:4], None,
                                                                        op0=ALU.is_ge, op1=ALU.add, accum_out=st[:, 6:7]),
                         reads=['score', 'st_mid'], writes=['junkD', 'st_cntD'])
                    P.op('dve', lambda e: e.scalar_tensor_tensor(st[:, 7:8], st[:, 4:5], -0.5, st[:, 6:7], op0=ALU.mult, op1=ALU.add),
                         reads=['st_cntA', 'st_cntD'], writes=['st_tmp1'])
                    P.op('dve', lambda e, k=k, Sa=Sa: e.scalar_tensor_tensor(st[:, 5:6], st[:, 7:8], 255.5 - 0.5 * Sa, wk[:, k:k + 1],
                                                                             op0=ALU.is_ge, op1=ALU.mult),
                         reads=['st_tmp1', 'wk'], writes=['st_tmp'])
                    P.op('dve', lambda e: e.tensor_tensor(st[:, 0:1], st[:, 0:1], st[:, 5:6], op=ALU.add), reads=['st_lo', 'st_tmp'], writes=['st_lo'])
                    if k + 1 < NIT:
                        P.op('dve', lambda e, k=k: e.tensor_tensor(st[:, 3:4], st[:, 0:1], wk[:, k + 1:k + 2], op=ALU.add),
                             reads=['st_lo', 'wk'], writes=['st_mid'])
                    yield

            def a_mask(i):
                Sc = 512 * (i + 1)
                P.op('dve', lambda e, Sc=Sc: e.tensor_scalar(mneg[:, 0:Sc], score[:, 0:Sc], st[:, 0:1], MNEG, op0=ALU.is_lt, op1=ALU.mult),
                     reads=['score', 'st_lo'], writes=['mneg'])

            def a_attn(i):
                NK = 4 * i + 4
                qb_ = qblk[i % 2]; qbk = f'qblk{i % 2}'
                vbs = {}

                def s1(ch):
                    if ch % 4 == 0:
                        kg = ch // 4
                        vb = vt[vstate['cnt'] % 3]; vk = f"vt{vstate['cnt'] % 3}"; vstate['cnt'] += 1
                        P.dma('pool', vb[:], Gv4[kg // 2][:, :, kg % 2, 0:512], reads=[f'Gv{kg // 2}'], writes=[vk])
                        vbs[kg] = (vb, vk)
                    cs_ = slice(ch * 128, (ch + 1) * 128)
                    pl = ps[ch % 2]; plk = f'ps{ch % 2}'
                    for h in range(4):
                        P.op('pe', lambda e, pl=pl, h=h, cs_=cs_: e.matmul(pl[:, hsl[h]], lhsT=KT[:, h, cs_], rhs=qb_[:, h, :], start=True, stop=False),
                             reads=[f'KT{h}', qbk], writes=[plk])
                        P.op('pe', lambda e, pl=pl, h=h, cs_=cs_: e.matmul(pl[:, hsl[h]], lhsT=mneg[:, cs_], rhs=idb[:], start=False, stop=True),
                             reads=['mneg', 'idb'], writes=[plk])

                def s2(ch):
                    pl = ps[ch % 2]; plk = f'ps{ch % 2}'
                    pt = PT[ch % 2]; ptk = f'PT{ch % 2}'
                    vb, vk = vbs[ch // 4]
                    cc = ch % 4
                    P.op('act', lambda e, pl=pl, pt=pt: e.activation(pt[:], pl[:], AF.Exp, scale=SCALE), reads=[plk], writes=[ptk])
                    P.op('pe', lambda e, pt=pt, ch=ch: e.matmul(ps[2][:, :], lhsT=ones_b[:, :], rhs=pt[:], start=(ch == 0), stop=(ch == NK - 1)),
                         reads=[ptk, 'ones_b'], writes=['ps2'])
                    for h in range(4):
                        P.op('pe', lambda e, pt=pt, h=h, ch=ch, vb=vb, cc=cc: e.matmul(
                            ps[3 + h][:, 0:128], lhsT=vb[:, cc, hsl[h]], rhs=pt[:, hsl[h]], start=(ch == 0), stop=(ch == NK - 1)),
                             reads=[ptk, vk], writes=[f'ps{3 + h}'])

                for n in range(NK + 1):
                    if n < NK:
                        s1(n)
                    if n >= 1:
                        s2(n - 1)
                    yield
                P.op('dve', lambda e: e.reciprocal(rec[:], ps[2][:, :]), reads=['ps2'], writes=['rec'])
                ot = oat[i % 2]; otk = f'oat{i % 2}'
                for h in range(4):
                    P.op('dve', lambda e, h=h, ot=ot: e.tensor_tensor(ot[:, hsl[h]], ps[3 + h][:, 0:128], rec[:, hsl[h]], op=ALU.mult),
                         reads=[f'ps{3 + h}', 'rec'], writes=[otk])
                P.dma('sp', yaT[:, i * 128:(i + 1) * 128].rearrange("(h d) t -> d h t", d=128), ot[:].rearrange("p (h t) -> p h t", t=128), reads=[otk])

            a_score(0)
            for _ in a_bisect(0):
                pass
            a_mask(0)
            for i in range(nq):
                ga = a_attn(i)
                n_att = 4 * i + 5
                if i + 1 < nq:
                    a_score(i + 1)
                    gb = a_bisect(i + 1)
                    n_bis = NIT + 1
                    done_b = 0
                    for s_ in range(n_att):
                        next(ga)
                        tgt = ((s_ + 1) * n_bis) // n_att
                        while done_b < tgt:
                            next(gb); done_b += 1
                    for _ in gb:
                        pass
                for _ in ga:
                    pass
                if i + 1 < nq:
                    a_mask(i + 1)

        ns = P.emit()
        print("L2: instrs", len(P.ins), "sems", ns, "waits", P.n_waits)

T = 2048
HB = 32
EPS = 1e-6
DFF = 2816
NFF = 22


def _blend(P, out_ap, cands, ckeys, selt, okey):
    P.op('dve', lambda e: e.tensor_scalar(out_ap, cands[0], selt[:, 0:1], None, op0=ALU.mult),
         reads=[ckeys[0], 'selt'], writes=[okey])
    for c in range(1, 4):
        P.op('dve', lambda e, c=c: e.scalar_tensor_tensor(out_ap, cands[c], selt[:, c:c + 1], out_ap, op0=ALU.mult, op1=ALU.add),
             reads=[ckeys[c], 'selt', okey], writes=[okey])


def build_l3a(nc, IO, pfx):
    o_f32, GT1, sel_d, yaT, ydT, xT = IO['o_f32'], IO['GT1'], IO['sel'], IO['yaT'], IO['ydT'], IO['xT']
    wo, wm, sm, xmT, tail2 = IO['wo'], IO['wm'], IO['sm3a'], IO['xm'], IO['tail2']
    gateT = o_f32[2560:6656, :]
    P = Prog(nc)
    es = contextlib.ExitStack()
    sb = lambda name, shape, dt: es.enter_context(nc.sbuf_tensor(pfx + name, shape, dt))
    pst = lambda name, shape, dt=F32: es.enter_context(nc.psum_tensor(pfx + name, shape, dt))
    W = 512
    with es:
        wob = [sb(f"wob{i}", [128, 4, 1024], BF16) for i in range(4)]
        wmb = sb("wmb", [128, 8, 1024], BF16)
        wst = sb("wst", [128, 4, 1024], F32)
        smt = sb("smt", [128, 192], F32)
        selt = sb("selt", [128, 4], F32)
        ones = sb("ones", [128, 128], F32)
        hh = [sb(f"hh{i}", [128, 4, 4, 160], F32) for i in range(2)]
        cand = [sb(f"cand{i}", [128, 4, 4, 32], F32) for i in range(4)]
        acc = sb("acc", [128, 4, W], F32)
        xc = sb("xc", [128, 4, W], F32)
        sq = sb("sq", [128, 4, W], F32)
        gbb = sb("gbb", [128, 4, W], F32)
        brs = [[sb(f"br{i}_{p}", [128, 4, W], BF16) for i in range(4)] for p in range(2)]
        rln = sb("rln", [128, W], F32)
        gt_ = [sb(f"gt{i}", [128, W], F32) for i in range(2)]
        mg = sb("mg", [128, W], F32)
        mgb = sb("mgb", [128, 8, W], BF16)
        xt = sb("xt", [128, 8, W], F32)
        ot = sb("ot", [128, 8, W], F32)
        ps = [pst(f"ps{i}", [128, 512]) for i in range(8)]

        P.dma('sp', smt[:], sm, writes=['smt'])
        P.dma('sp', selt[:], sel_d, writes=['selt'])
        P.op('dve', lambda e: e.memset(ones[:], 1.0 / 512), writes=['ones'])
        for i in range(4):
            P.dma('sp', wst[:], wo[i].rearrange("(c p) n -> p c n", p=128), writes=['wst'])
            P.op('pool' if i % 2 else 'dve', lambda e, i=i: e.tensor_copy(wob[i][:], wst[:]), reads=['wst'], writes=[f'wob{i}'])
        for hf in range(2):
            P.dma('sp', wst[:], wm[hf * 512:(hf + 1) * 512, :].rearrange("(c p) n -> p c n", p=128), writes=['wst'])
            P.op('pool' if hf else 'dve', lambda e, hf=hf: e.tensor_copy(wmb[:, hf * 4:(hf + 1) * 4, :], wst[:]), reads=['wst'], writes=['wmb'])

        def load_haloed(dst, dkey, row_main, row_tail, tl):
            u0 = tl * W
            for ch in range(4):
                P.dma('sp' if ch % 2 == 0 else 'act', dst[:, ch, :, 32:160],
                      o_f32[row_main + ch * 128:row_main + (ch + 1) * 128, u0:u0 + W].rearrange("p (b t) -> p b t", t=128),
                      writes=[dkey])
            q0 = row_tail // 256
            for r in range(4):
                if r == 3 and tl == 0:
                    P.op('pool', lambda e: e.memset(cand[3][:], 0.0), writes=['cand3'])
                for hf in range(2):
                    src = GT1[q0 + hf][r * 256:(r + 1) * 256, :]
                    if r < 3:
                        P.dma('sp' if (r + hf) % 2 == 0 else 'act', cand[r][:, 2 * hf:2 * hf + 2, :, :],
                              src[:, tl * 128:(tl + 1) * 128].rearrange("(c p) (b k) -> p c b k", p=128, k=32), writes=[f'cand{r}'])
                    elif tl == 0:
                        P.dma('act', cand[3][:, 2 * hf:2 * hf + 2, 1:4, :],
                              src[:, 0:96].rearrange("(c p) (b k) -> p c b k", p=128, k=32), writes=['cand3'])
                    else:
                        P.dma('act', cand[3][:, 2 * hf:2 * hf + 2, :, :],
                              src[:, tl * 128 - 32:tl * 128 + 96].rearrange("(c p) (b k) -> p c b k", p=128, k=32), writes=['cand3'])
            _blend(P, dst[:].rearrange("p c b k -> p (c b) k")[:, :, 0:32], [c_[:].rearrange("p c b k -> p (c b) k") for c_ in cand],
                   [f'cand{r}' for r in range(4)], selt, dkey)

        def stage_x(tl):
            u0 = tl * W
            br = brs[tl % 2]
            bk = [f'br{i}_{tl % 2}' for i in range(4)]
            ga, gb_ = hh[0], hh[1]
            load_haloed(ga, 'hh0', 0, 0, tl)
            load_haloed(gb_, 'hh1', 512, 512, tl)
            P.op('act', lambda e: e.activation(gb_[:], gb_[:], AF.Sigmoid), reads=['hh1'], writes=['hh1'])
            P.op('pool', lambda e: e.tensor_tensor(ga[:], ga[:], gb_[:], op=ALU.mult), reads=['hh0', 'hh1'], writes=['hh0'])
            for ch in range(4):
                ak = f'acc{ch}'
                av_ = acc[:, ch, :].rearrange("p (b t) -> p b t", t=128)
                P.op('dve', lambda e, ch=ch, av_=av_: e.tensor_scalar(av_, ga[:, ch, :, 2:130], smt[:, 32 + ch * 31:33 + ch * 31],
                                                                    smt[:, 156 + ch:157 + ch], op0=ALU.mult, op1=ALU.add),
                     reads=['hh0', 'smt'], writes=[ak])
                for k in range(1, 31):
                    P.op('dve', lambda e, ch=ch, k=k, av_=av_: e.scalar_tensor_tensor(
                        av_, ga[:, ch, :, 2 + k:130 + k], smt[:, 32 + ch * 31 + k:33 + ch * 31 + k], av_,
                        op0=ALU.mult, op1=ALU.add), reads=['hh0', 'smt', ak], writes=[ak])
            aks = [f'acc{ch}' for ch in range(4)]
            for ch in range(4):
                P.op('pe', lambda e, ch=ch: e.matmul(ps[0][:], lhsT=ones[:], rhs=acc[:, ch, :], start=(ch == 0), stop=(ch == 3)),
                     reads=['ones'] + aks, writes=['ps0'])
            for ch in range(4):
                P.op('dve', lambda e, ch=ch: e.tensor_tensor(xc[:, ch, :], acc[:, ch, :], ps[0][:], op=ALU.subtract),
                     reads=aks + ['ps0'], writes=['xc'])
            P.op('act', lambda e: e.activation(sq[:], xc[:], AF.Square), reads=['xc'], writes=['sq'])
            for ch in range(4):
                P.op('pe', lambda e, ch=ch: e.matmul(ps[1][:], lhsT=ones[:], rhs=sq[:, ch, :], start=(ch == 0), stop=(ch == 3)),
                     reads=['ones', 'sq'], writes=['ps1'])
            P.op('dve', lambda e: e.tensor_scalar(rln[:], ps[1][:], 1.0, EPS, op0=ALU.mult, op1=ALU.add), reads=['ps1'], writes=['rln'])
            P.op('act', lambda e: e.activation(rln[:], rln[:], AF.Sqrt), reads=['rln'], writes=['rln'])
            P.op('dve', lambda e: e.reciprocal(rln[:], rln[:]), reads=['rln'], writes=['rln'])
            for ch in range(4):
                P.op('dve', lambda e, ch=ch: e.tensor_tensor(xc[:, ch, :], xc[:, ch, :], rln[:], op=ALU.mult),
                     reads=['xc', 'rln'], writes=['xc'])
                P.op('dve', lambda e, ch=ch: e.tensor_scalar(xc[:, ch, :], xc[:, ch, :], smt[:, 160 + ch:161 + ch],
                                                             smt[:, 164 + ch:165 + ch], op0=ALU.mult, op1=ALU.add),
                     reads=['xc', 'smt'], writes=['xc'])
            P.op('act', lambda e: e.activation(br[1][:], xc[:], AF.Silu), reads=['xc'], writes=[bk[1]])
            gc, xcc = hh[0], hh[1]
            load_haloed(gc, 'hh0', 1024 + 512, 1024, tl)
            load_haloed(xcc, 'hh1', 1024 + 1024, 1536, tl)
            P.dma('sp', gbb[:], o_f32[1024:1536, u0:u0 + W].rearrange("(c p) t -> p c t", p=128), writes=['gbb'])
            P.op('pool', lambda e: e.tensor_tensor(gc[:], gc[:], xcc[:], op=ALU.mult), reads=['hh0', 'hh1'], writes=['hh0'])
            for ch in range(4):
                ak = f'acc{ch}'
                av_ = acc[:, ch, :].rearrange("p (b t) -> p b t", t=128)
                P.op('dve', lambda e, ch=ch, av_=av_: e.tensor_scalar(av_, gc[:, ch, :, 30:158], smt[:, 168 + ch * 3:169 + ch * 3],
                                                                    None, op0=ALU.mult), reads=['hh0', 'smt'], writes=[ak])
                for k in (1, 2):
                    P.op('dve', lambda e, ch=ch, k=k, av_=av_: e.scalar_tensor_tensor(
                        av_, gc[:, ch, :, 30 + k:158 + k], smt[:, 168 + ch * 3 + k:169 + ch * 3 + k], av_,
                        op0=ALU.mult, op1=ALU.add), reads=['hh0', 'smt', ak], writes=[ak])
                P.op('pool', lambda e, ch=ch: e.tensor_tensor(br[2][:, ch, :], acc[:, ch, :], gbb[:, ch, :], op=ALU.mult),
                     reads=[ak, 'gbb'], writes=[bk[2]])
            P.dma('sp', xc[:], yaT[:, u0:u0 + W].rearrange("(c p) t -> p c t", p=128), writes=['xc'])
            P.op('act', lambda e: e.copy(br[0][:], xc[:]), reads=['xc'], writes=[bk[0]])
            P.dma('act', sq[:], ydT[:, u0:u0 + W].rearrange("(c p) t -> p c t", p=128), writes=['sq'])
            P.op('act', lambda e: e.copy(br[3][:], sq[:]), reads=['sq'], writes=[bk[3]])
        def stage_y(tl):
            u0 = tl * W
            br = brs[tl % 2]
            bk = [f'br{i}_{tl % 2}' for i in range(4)]
            P.dma('sp', xt[:], xT[:, u0:u0 + W].rearrange("(c p) t -> p c t", p=128), writes=['xt'])
            gi = 0
            for oc in range(8):
                ocs = slice(oc * 128, (oc + 1) * 128)
                for i in range(4):
                    pb = ps[2 + (gi % 4)]; pk = f'ps{2 + gi % 4}'
                    g_ = gt_[gi % 2]; gk = f'gt{gi % 2}'; gi += 1
                    P.dma('act' if gi % 2 else 'sp', g_[:], gateT[i * 1024 + oc * 128:i * 1024 + (oc + 1) * 128, u0:u0 + W], writes=[gk])
                    P.op('act', lambda e, g_=g_, i=i, oc=oc: e.activation(g_[:], g_[:], AF.Sigmoid, bias=smt[:, i * 8 + oc:i * 8 + oc + 1]),
                         reads=[gk, 'smt'], writes=[gk])
                    for kc in range(4):
                        P.op('pe', lambda e, pb=pb, i=i, kc=kc, ocs=ocs: e.matmul(pb[:], lhsT=wob[i][:, kc, ocs], rhs=br[i][:, kc, :],
                                                                                 start=(kc == 0), stop=(kc == 3)),
                             reads=[f'wob{i}', bk[i]], writes=[pk])
                    if i == 0:
                        P.op('dve', lambda e, pb=pb, g_=g_: e.tensor_tensor(mg[:], pb[:], g_[:], op=ALU.mult), reads=[pk, gk], writes=['mg'])
                    else:
                        P.op('dve', lambda e, pb=pb, g_=g_: e.tensor_tensor(g_[:], pb[:], g_[:], op=ALU.mult), reads=[pk, gk], writes=[gk])
                        if i < 3:
                            P.op('pool', lambda e, g_=g_: e.tensor_tensor(mg[:], mg[:], g_[:], op=ALU.add), reads=['mg', gk], writes=['mg'])
                        else:
                            P.op('pool', lambda e, g_=g_, oc=oc: e.tensor_tensor(mgb[:, oc, :], mg[:], g_[:], op=ALU.add),
                                 reads=['mg', gk], writes=[f'mgb{oc}'])
            mks = [f'mgb{oc}' for oc in range(8)]
            for oc in range(8):
                ocs = slice(oc * 128, (oc + 1) * 128)
                pb = ps[6 + oc % 2]; pk = f'ps{6 + oc % 2}'
                for kc in range(8):
                    P.op('pe', lambda e, pb=pb, kc=kc, ocs=ocs: e.matmul(pb[:], lhsT=wmb[:, kc, ocs], rhs=mgb[:, kc, :],
                                                                       start=(kc == 0), stop=(kc == 7)),
                         reads=['wmb'] + mks, writes=[pk])
                P.op('dve', lambda e, pb=pb, oc=oc: e.tensor_tensor(ot[:, oc, :], pb[:], xt[:, oc, :], op=ALU.add),
                     reads=[pk, 'xt'], writes=['ot'])
            P.dma('sp', xmT[:, u0:u0 + W].rearrange("(c p) t -> p c t", p=128), ot[:], reads=['ot'], writes=['xm'])
        stage_x(0)
        for tl in range(4):
            if tl + 1 < 4:
                stage_x(tl + 1)
            stage_y(tl)
        for q in range(4):
            P.dma('sp', tail2[q * 256:(q + 1) * 256, :].rearrange("r (b k) -> r b k", k=2),
                  xmT[q * 256:(q + 1) * 256, :].rearrange("r (b t) -> r b t", t=128)[:, :, 126:128], reads=['xm'])
        ns = P.emit()
        print("L3a: instrs", len(P.ins), "sems", ns, "waits", P.n_waits)


def build_l3b(nc, IO, pfx):
    xmT, GT2, sel_d, wg, wu, wd, sm, xoT = IO['xm'], IO['GT2'], IO['sel'], IO['wg'], IO['wu'], IO['wd'], IO['sm3b'], IO['xo']
    TT = 1024
    NB = 8
    NC_ = NB * 130
    P = Prog(nc)
    es = contextlib.ExitStack()
    sb = lambda name, shape, dt: es.enter_context(nc.sbuf_tensor(pfx + name, shape, dt))
    pst = lambda name, shape, dt=F32: es.enter_context(nc.psum_tensor(pfx + name, shape, dt))
    with es:
        smt = sb("smt", [128, 80], F32)
        selt = sb("selt", [128, 4], F32)
        ones = sb("ones", [128, 128], F32)
        xm = sb("xm", [128, 8, NB, 130], F32)
        cand = [sb(f"cand{i}", [128, 8, NB, 2], F32) for i in range(4)]
        sqt = sb("sqt", [128, NC_], F32)
        rs = sb("rs", [128, NC_], F32)
        h2 = sb("h2", [128, 8, NB, 130], BF16)
        prod = sb("prod", [128, NFF, TT], BF16)
        wgs = [sb(f"wgs{i}", [128, 8, 128], F32) for i in range(2)]
        wus = [sb(f"wus{i}", [128, 8, 128], F32) for i in range(2)]
        wgb = [sb(f"wgb{i}", [128, 8, 128], BF16) for i in range(2)]
        wub = [sb(f"wub{i}", [128, 8, 128], BF16) for i in range(2)]
        gtl = [sb(f"gtl{i}", [128, NB, 130], F32) for i in range(2)]
        av = [sb(f"av{i}", [128, TT], F32) for i in range(2)]
        wds = [sb(f"wds{i}", [128, 512], F32) for i in range(2)]
        wdb = [sb(f"wdb{i}", [128, 512], BF16) for i in range(2)]
        ot = [sb(f"ot{i}", [128, 512], F32) for i in range(2)]
        ps = [pst(f"ps{i}", [128, 512]) for i in range(8)]
        P.dma('sp', smt[:], sm, writes=['smt'])
        P.dma('sp', selt[:], sel_d, writes=['selt'])
        P.op('dve', lambda e: e.memset(ones[:], 1.0 / 1024), writes=['ones'])
        wgv = wg.rearrange("(c p) n -> p c n", p=128)
        wuv = wu.rearrange("(c p) n -> p c n", p=128)
        xmf = xm[:].rearrange("p c b k -> p c (b k)")
        h2f = h2[:].rearrange("p c b k -> p c (b k)")
        for tl in range(2):
            u0 = tl * TT
            for c in range(8):
                P.dma('sp' if c % 2 == 0 else 'act', xm[:, c, :, 2:130],
                      xmT[c * 128:(c + 1) * 128, u0:u0 + TT].rearrange("p (b t) -> p b t", t=128), writes=['xm'])
            for r in range(3):
                P.dma('sp', cand[r][:], GT2[r * 1024:(r + 1) * 1024, tl * 16:(tl + 1) * 16].rearrange("(c p) (b k) -> p c b k", p=128, k=2),
                      writes=[f'cand{r}'])
            if tl == 0:
                P.op('pool', lambda e: e.memset(cand[3][:], 0.0), writes=['cand3'])
                P.dma('act', cand[3][:, :, 1:8, :], GT2[3 * 1024:4 * 1024, 0:14].rearrange("(c p) (b k) -> p c b k", p=128, k=2), writes=['cand3'])
            else:
                P.dma('act', cand[3][:], GT2[3 * 1024:4 * 1024, 14:30].rearrange("(c p) (b k) -> p c b k", p=128, k=2), writes=['cand3'])
            _blend(P, xm[:].rearrange("p c b k -> p (c b) k")[:, :, 0:2], [c_[:].rearrange("p c b k -> p (c b) k") for c_ in cand],
                   [f'cand{r}' for r in range(4)], selt, 'xm')
            col_groups = [(0, 512), (512, 1024), (1024, NC_)]
            for (a, b) in col_groups:
                n = b - a
                for c in range(8):
                    P.op('act', lambda e, c=c, a=a, b=b: e.activation(sqt[:, a:b], xmf[:, c, a:b], AF.Square), reads=['xm'], writes=['sqt'])
                    P.op('pe', lambda e, c=c, a=a, b=b, n=n: e.matmul(ps[0][:, 0:n], lhsT=ones[:], rhs=sqt[:, a:b], start=(c == 0), stop=(c == 7)),
                         reads=['ones', 'sqt'], writes=['ps0'])
                P.op('dve', lambda e, a=a, b=b, n=n: e.tensor_scalar(rs[:, a:b], ps[0][:, 0:n], 1.0, EPS, op0=ALU.mult, op1=ALU.add),
                     reads=['ps0'], writes=['rs'])
            P.op('act', lambda e: e.activation(rs[:], rs[:], AF.Sqrt), reads=['rs'], writes=['rs'])
            P.op('dve', lambda e: e.reciprocal(rs[:], rs[:]), reads=['rs'], writes=['rs'])
            for c in range(8):
                P.op('dve', lambda e, c=c: e.scalar_tensor_tensor(h2f[:, c, :], xmf[:, c, :], smt[:, c:c + 1], rs[:],
                                                                 op0=ALU.mult, op1=ALU.mult),
                     reads=['xm', 'smt', 'rs'], writes=[f'h2_{c}'])
            hk = [f'h2_{c}' for c in range(8)]
            for f in range(NFF):
                b = f % 2
                fs = slice(f * 128, (f + 1) * 128)
                P.dma('sp', wgs[b][:], wgv[:, :, fs], writes=[f'wgs{b}'])
                P.dma('act', wus[b][:], wuv[:, :, fs], writes=[f'wus{b}'])
                P.op('dve', lambda e, b=b: e.tensor_copy(wgb[b][:], wgs[b][:]), reads=[f'wgs{b}'], writes=[f'wgb{b}'])
                P.op('pool', lambda e, b=b: e.tensor_copy(wub[b][:], wus[b][:]), reads=[f'wus{b}'], writes=[f'wub{b}'])
                g_ = gtl[b]; gk = f'gtl{b}'
                gf = g_[:].rearrange("p b k -> p (b k)")
                a_ = av[b]; ak = f'av{b}'
                a3 = a_[:].rearrange("p (b t) -> p b t", t=128)
                for gi_, (a, bb) in enumerate(col_groups):
                    n = bb - a
                    pb = ps[1 + gi_]; pk = f'ps{1 + gi_}'
                    for c in range(8):
                        P.op('pe', lambda e, pb=pb, c=c, a=a, bb=bb, n=n, b=b: e.matmul(pb[:, 0:n], lhsT=wgb[b][:, c, :], rhs=h2f[:, c, a:bb],
                                                                                     start=(c == 0), stop=(c == 7)),
                             reads=[f'wgb{b}'] + hk, writes=[pk])
                    P.op('act', lambda e, pb=pb, a=a, bb=bb, n=n, gf=gf: e.copy(gf[:, a:bb], pb[:, 0:n]), reads=[pk], writes=[gk])
                P.op('dve', lambda e, f=f, g_=g_, a3=a3: e.tensor_scalar(a3, g_[:, :, 0:128], smt[:, 8 + 3 * f:9 + 3 * f], None, op0=ALU.mult),
                     reads=[gk, 'smt'], writes=[ak])
                for k in (1, 2):
                    P.op('dve', lambda e, f=f, g_=g_, a3=a3, k=k: e.scalar_tensor_tensor(
                        a3, g_[:, :, k:k + 128], smt[:, 8 + 3 * f + k:9 + 3 * f + k], a3, op0=ALU.mult, op1=ALU.add),
                         reads=[gk, 'smt', ak], writes=[ak])
                P.op('act', lambda e, a_=a_: e.activation(a_[:], a_[:], AF.Silu), reads=[ak], writes=[ak])
                for gi_ in range(2):
                    pb = ps[4 + gi_]; pk = f'ps{4 + gi_}'
                    for c in range(8):
                        P.op('pe', lambda e, pb=pb, c=c, gi_=gi_, b=b: e.matmul(pb[:], lhsT=wub[b][:, c, :], rhs=h2[:, c, gi_ * 4:(gi_ + 1) * 4, 2:130],
                                                                              start=(c == 0), stop=(c == 7)),
                             reads=[f'wub{b}'] + hk, writes=[pk])
                    P.op('dve', lambda e, pb=pb, gi_=gi_, f=f, a_=a_: e.tensor_tensor(
                        prod[:, f, gi_ * 512:(gi_ + 1) * 512], a_[:, gi_ * 512:(gi_ + 1) * 512], pb[:], op=ALU.mult), reads=[pk, ak], writes=[f'prod{f}'])
            pks = [f'prod{f}' for f in range(NFF)]
            di = 0
            for og in range(2):
                for th in range(2):
                    tsl = slice(th * 512, (th + 1) * 512)
                    for f in range(NFF):
                        b = di % 2; di += 1
                        P.dma('sp' if di % 2 else 'act', wds[b][:], wd[f * 128:(f + 1) * 128, og * 512:(og + 1) * 512], writes=[f'wds{b}'])
                        P.op('pool' if di % 2 else 'dve', lambda e, b=b: e.tensor_copy(wdb[b][:], wds[b][:]), reads=[f'wds{b}'], writes=[f'wdb{b}'])
                        for o4 in range(4):
                            P.op('pe', lambda e, o4=o4, b=b, f=f, tsl=tsl: e.matmul(ps[o4][:], lhsT=wdb[b][:, o4 * 128:(o4 + 1) * 128], rhs=prod[:, f, tsl],
                                                                                  start=(f == 0), stop=(f == NFF - 1)),
                                 reads=[f'wdb{b}'] + pks, writes=[f'ps{o4}'])
                    for o4 in range(4):
                        oc = og * 4 + o4
                        o_ = ot[o4 % 2]; ok = f'ot{o4 % 2}'
                        P.op('dve', lambda e, o4=o4, oc=oc, o_=o_, th=th: e.tensor_tensor(
                            o_[:].rearrange("p (b t) -> p b t", t=128), ps[o4][:].rearrange("p (b t) -> p b t", t=128),
                            xm[:, oc, th * 4:(th + 1) * 4, 2:130], op=ALU.add),
                             reads=[f'ps{o4}', 'xm'], writes=[ok])
                        P.dma('sp', xoT[oc * 128:(oc + 1) * 128, u0 + th * 512:u0 + (th + 1) * 512], o_[:], reads=[ok])
        ns = P.emit()
        print("L3b: instrs", len(P.ins), "sems", ns, "waits", P.n_waits)


RG = [[0, 1, 2, 3], [4, 5, 6, 7]]


def _allgather(nc, pairs):
    s = nc.alloc_semaphore(name=nc.make_name("cc_sem", True))
    with nc.Block() as block:
        @block.gpsimd
        def _(g):
            for (src, dst) in pairs:
                g.collective_compute("AllGather", mybir.AluOpType.bypass, replica_groups=RG,
                                     ins=[src.ap().opt()], outs=[dst.ap().opt()]).then_inc(s)
            g.wait_ge(s, len(pairs))
    nc.clear_and_free_semaphores([s])
    nc.all_engine_barrier()


def build_fused(nlayers=2, debug=False):
    nc = bass.Bass("TRN2", target_bir_lowering=False)
    ext = lambda name, shape, dt: nc.dram_tensor(name, shape, dt, kind="ExternalInput").ap()
    xT = ext("xT", [1024, 2048], F32)
    pos = ext("pos", [128, 2048], I32)
    w_in = ext("w_in", [2, 1024, 10052], F32)
    gmix = ext("gmix", [2, 128, 8], F32)
    gq = ext("gq", [2, 128, 4], F32)
    cst = ext("cst", [128, 8], F32)
    rmA = ext("rmA", [128, 128], F32)
    rmI = ext("rmI", [128, 128], F32)
    negm = ext("negm", [128, 512], F32)
    mdm = ext("mdm", [128, 4, 4, 128], F32)
    su = ext("su", [128, 128], F32)
    ident = ext("ident", [128, 128], F32)
    pw = ext("pw", [128, NIT], F32)
    sel = ext("sel", [128, 4], F32)
    wo = ext("wo", [2, 4, 512, 1024], F32)
    wm = ext("wm", [2, 1024, 1024], F32)
    sm3a = ext("sm3a", [2, 128, 192], F32)
    wg = ext("wg", [2, 1024, 2816], F32)
    wu = ext("wu", [2, 1024, 2816], F32)
    wd = ext("wd", [2, 2816, 1024], F32)
    sm3b = ext("sm3b", [2, 128, 80], F32)
    out = nc.dram_tensor("out", [1024, 2048], F32, kind="ExternalOutput").ap()
    dk = dict(kind="ExternalOutput") if debug else {}
    o_q = nc.dram_tensor("o_q", [1280, 2048], BF16, **dk)
    o_f32 = nc.dram_tensor("o_f32", [6656, 2048], F32, **dk)
    o_iw = nc.dram_tensor("o_iw", [2048, 4], F32, **dk)
    krows = [0] + [64 + 128 * h for h in range(4)] + [576 + 128 * h for h in range(4)]
    o_k = {r0: nc.dram_tensor(f"o_k{r0}", [64 if r0 == 0 else 128, 2048], BF16) for r0 in krows}
    G_k = {r0: nc.dram_tensor(f"G_k{r0}", [4 * (64 if r0 == 0 else 128), 2048], BF16) for r0 in krows}
    o_v = [nc.dram_tensor(f"o_v{q}", [256, 1024], BF16) for q in range(8)]
    G_v = [nc.dram_tensor(f"G_v{q}", [4 * 256, 1024], BF16) for q in range(8)]
    tail1 = [nc.dram_tensor(f"tail1_{q}", [256, 512], F32) for q in range(8)]
    GT1 = [nc.dram_tensor(f"GT1_{q}", [4 * 256, 512], F32) for q in range(8)]
    yaT = nc.dram_tensor("yaT", [512, 2048], F32, **dk)
    ydT = nc.dram_tensor("ydT", [512, 2048], F32, **dk)
    xm = nc.dram_tensor("xm", [1024, 2048], F32, **dk)
    tail2 = nc.dram_tensor("tail2", [1024, 32], F32)
    GT2 = nc.dram_tensor("GT2", [4 * 1024, 32], F32)
    xo0 = nc.dram_tensor("xo0", [1024, 2048], F32)
    for l in range(nlayers):
        x_in = xT if l == 0 else xo0.ap()
        IOD = dict(xT=x_in, pos=pos, w=w_in[l], gmix=gmix[l], gq=gq[l], cst=cst, rmA=rmA, rmI=rmI,
                 o_q=o_q.ap(), o_k={k: v.ap() for k, v in o_k.items()}, o_f32=o_f32.ap(), o_v=[v.ap() for v in o_v], o_iw=o_iw.ap(), tail1=[v.ap() for v in tail1],
                 G_k={k: v.ap() for k, v in G_k.items()}, G_v=[v.ap() for v in G_v], GT1=[v.ap() for v in GT1], negm=negm, mdm=mdm, su=su, ident=ident, pw=pw, sel=sel,
                 yaT=yaT.ap(), ydT=ydT.ap(), wo=wo[l], wm=wm[l], sm3a=sm3a[l], xm=xm.ap(), tail2=tail2.ap(), GT2=GT2.ap(),
                 wg=wg[l], wu=wu[l], wd=wd[l], sm3b=sm3b[l], xo=(xo0.ap() if l < nlayers - 1 else out))
        IOD['cc_l2'] = ([(o_k[r0], G_k[r0], f'Gk{r0}') for r0 in krows[5:]] + [(o_v[q], G_v[q], f'Gv{q}') for q in range(8)]
                        + [(o_k[r0], G_k[r0], f'Gk{r0}') for r0 in krows[:5]] + [(tail1[q], GT1[q], f'GT1_{q}') for q in range(8)])
        build_l1(nc, IOD, f"a{l}_")
        build_l2(nc, IOD, f"b{l}_")
        build_l3a(nc, IOD, f"c{l}_")
        _allgather(nc, [(tail2, GT2)])
        build_l3b(nc, IOD, f"d{l}_")
    return nc


_NC = {}


def _stripe(a, j):
    return a.reshape((64, 128) + a.shape[1:])[j::4].reshape((2048,) + a.shape[1:])


def kernel(_nlayers=2, _debug=False, **inputs):
    inp = {k: np.asarray(v) for k, v in inputs.items()}
    if 'nc' not in _NC:
        _NC['nc'] = build_fused(_nlayers, _debug)
    nc = _NC['nc']
    cst, RmA, RmI = l1_consts()
    su, ident, pw = l2_consts()
    gq = np.zeros((2, 128, 4), np.float32)
    gq[:, :, 0] = inp['g_qa']; gq[:, :, 1] = inp['g_ka']; gq[:, :64, 2] = inp['g_kidx']
    gmix = np.ascontiguousarray(inp['g_mix'].reshape(2, 8, 128).transpose(0, 2, 1))
    sm3a = np.zeros((2, 128, 192), np.float32)
    sm3b = np.zeros((2, 128, 80), np.float32)
    for l in range(2):
        sm3a[l, :, 0:32] = inp['b_gate'][l].reshape(32, 128).T
        sm3a[l, :, 32:156] = inp['cb_conv_w'][l].T.reshape(4, 128, 31).transpose(1, 0, 2).reshape(128, 124)
        sm3a[l, :, 156:160] = inp['cb_conv_b'][l].reshape(4, 128).T
        sm3a[l, :, 160:164] = inp['cb_ln_g'][l].reshape(4, 128).T
        sm3a[l, :, 164:168] = inp['cb_ln_b'][l].reshape(4, 128).T
        sm3a[l, :, 168:180] = inp['cc_conv_w'][l].T.reshape(4, 128, 3).transpose(1, 0, 2).reshape(128, 12)
        sm3b[l, :, 0:8] = inp['g_ffn'][l].reshape(8, 128).T
        sm3b[l, :, 8:74] = inp['ffn_conv_w'][l].T.reshape(22, 128, 3).transpose(1, 0, 2).reshape(128, 66)
    wo = np.ascontiguousarray(np.stack([inp['w_oa'], inp['w_ob'], inp['w_oc'], inp['w_od']], axis=1))
    shared = dict(w_in=np.ascontiguousarray(inp['w_in']), gmix=gmix, gq=gq, cst=cst, rmA=RmA, rmI=RmI, su=su, ident=ident, pw=pw,
                  wo=wo, wm=np.ascontiguousarray(inp['w_merge']), sm3a=sm3a, wg=np.ascontiguousarray(inp['w_ffn_gate']),
                  wu=np.ascontiguousarray(inp['w_ffn_up']), wd=np.ascontiguousarray(inp['w_ffn_down']), sm3b=sm3b)
    maps = []
    for c in range(8):
        b, j = c // 4, c % 4
        neg, md = l2_masks(j)
        sel = np.zeros((128, 4), np.float32)
        sel[:, (j - 1) % 4] = 1.0
        m = dict(shared)
        m.update(xT=np.ascontiguousarray(_stripe(inp['x'][b], j).T),
                 pos=np.ascontiguousarray(np.broadcast_to(_stripe(inp['positions'][b], j)[None, :], (128, 2048)).astype(np.int32)),
                 negm=neg, mdm=md, sel=sel)
        maps.append(m)
    res = run_bass_kernel_spmd(nc, maps, core_ids=list(range(8)))
    if _debug:
        _NC['res'] = res.results
    out = np.zeros((2, 64, 128, 1024), np.float32)
    for c in range(8):
        b, j = c // 4, c % 4
        out[b, j::4] = np.asarray(res.results[c]['out']).T.reshape(16, 128, 1024)
    return out.reshape(2, 8192, 1024)
```

```python
import math
import contextlib
import numpy as np
from concourse.bass_utils import run_bass_kernel_spmd
import numpy as np
import concourse.bass as bass
import concourse.mybir as mybir

F32 = mybir.dt.float32
BF16 = mybir.dt.bfloat16
I32 = mybir.dt.int32
ALU = mybir.AluOpType
AF = mybir.ActivationFunctionType
AX = mybir.AxisListType

SEM_EPOCH = 30000


class Prog:
    ENGS = ('pe', 'act', 'dve', 'pool', 'sp')

    def __init__(self, nc, n_dma_sems=6):
        self.nc = nc
        self.ins = []
        self.stream = {e: [] for e in self.ENGS}
        self.last_w = {}
        self.readers = {}
        self.n_dma_sems = n_dma_sems
        self.dma_rr = {e: 0 for e in self.ENGS}
        self.dma_slot_last = {}

    def _deps(self, eng, reads, writes, is_dma):
        deps = set()
        for k in reads:
            w = self.last_w.get(k)
            if w is not None:
                deps.add(w)
        for k in writes:
            w = self.last_w.get(k)
            if w is not None:
                deps.add(w)
            for r in self.readers.get(k, ()):
                deps.add(r)
        out = []
        for d in deps:
            di = self.ins[d]
            if (not is_dma) and (not di['dma']) and di['eng'] == eng:
                if eng == 'pe':
                    continue
            out.append(d)
        return out

    def _commit(self, iid, reads, writes):
        for k in reads:
            self.readers.setdefault(k, []).append(iid)
        for k in writes:
            self.last_w[k] = iid
            self.readers[k] = []

    def op(self, eng, fn, reads=(), writes=()):
        reads = list(reads); writes = list(writes)
        deps = self._deps(eng, reads, writes, False)
        iid = len(self.ins)
        self.ins.append(dict(eng=eng, fn=fn, deps=deps, dma=False, signal=False))
        self.stream[eng].append(iid)
        self._commit(iid, reads, writes)
        return iid

    def dma(self, eng, out, in_, reads=(), writes=(), **kw):
        reads = list(reads); writes = list(writes)
        deps = self._deps(eng, reads, writes, True)
        slot = self.dma_rr[eng] % self.n_dma_sems
        self.dma_rr[eng] += 1
        prev = self.dma_slot_last.get((eng, slot))
        if prev is not None and prev not in deps:
            deps.append(prev)
        iid = len(self.ins)
        self.ins.append(dict(eng=eng, fn=None, deps=deps, dma=True, signal=True,
                             slot=slot, out=out, in_=in_, kw=kw))
        self.dma_slot_last[(eng, slot)] = iid
        self.stream[eng].append(iid)
        self._commit(iid, reads, writes)
        return iid

    def cc(self, eng, src, dst, reads=(), writes=()):
        reads = list(reads); writes = list(writes)
        deps = self._deps(eng, reads, writes, True)
        n = self.dma_rr.get((eng, 'cc'), 0)
        self.dma_rr[(eng, 'cc')] = n + 1
        slot = 100 + n % 4
        prev = self.dma_slot_last.get((eng, slot))
        if prev is not None and prev not in deps:
            deps.append(prev)
        iid = len(self.ins)
        self.ins.append(dict(eng=eng, fn=None, deps=deps, dma=True, signal=True, slot=slot, cc=(src, dst), inc=1))
        self.dma_slot_last[(eng, slot)] = iid
        self.stream[eng].append(iid)
        self._commit(iid, reads, writes)
        return iid

    def emit(self, final_wait_eng='sp'):
        nc = self.nc
        ins = self.ins
        pos = {}
        for e in self.ENGS:
            for p, iid in enumerate(self.stream[e]):
                pos[iid] = p
        def chan(i):
            d = ins[i]
            return ('d', d['eng'], d['slot']) if d['dma'] else ('c', d['eng'])
        need = {}
        for e in self.ENGS:
            waited = {}
            for iid in self.stream[e]:
                lst = []
                for d in sorted(ins[iid]['deps'], key=lambda x: -pos[x]):
                    c = chan(d)
                    if waited.get(c, -1) >= pos[d]:
                        continue
                    waited[c] = pos[d]
                    ins[d]['signal'] = True
                    lst.append(d)
                need[iid] = lst
        final = list(self.dma_slot_last.values())
        semcount = {}
        sems = {}
        stack = []

        def get_sem(key):
            if key not in sems:
                s = nc.alloc_semaphore(name=nc.make_name("s_" + "_".join(str(k) for k in key), True))
                sems[key] = s
            return sems[key]

        for e in self.ENGS:
            ccount = 0
            dcount = {}
            for iid in self.stream[e]:
                d = ins[iid]
                if d['dma']:
                    sl = d['slot']
                    dcount[sl] = dcount.get(sl, 0) + 16
                    d['semkey'] = ('d', e, sl, dcount[sl] // (SEM_EPOCH * 16 + 16))
                    d['semval'] = dcount[sl] - d['semkey'][3] * (SEM_EPOCH * 16 + 16) if False else None
                elif d['signal']:
                    ccount += 1
                    ep = (ccount - 1) // SEM_EPOCH
                    d['semkey'] = ('c', e, ep)
                    d['semval'] = ccount - ep * SEM_EPOCH
            dc2 = {}
            for iid in self.stream[e]:
                d = ins[iid]
                if d['dma']:
                    sl = d['slot']
                    n = dc2.get(sl, 0) + 1
                    dc2[sl] = n
                    ep = (n - 1) // 2000
                    d['semkey'] = ('d', e, sl, ep)
                    d['semval'] = d.get('inc', 16) * (n - ep * 2000)
        engobj = {'pe': 'tensor', 'act': 'scalar', 'dve': 'vector', 'pool': 'gpsimd', 'sp': 'sync'}
        self.n_waits = 0

        def run_stream(e, eng):
            for iid in self.stream[e]:
                d = ins[iid]
                for dep in need[iid]:
                    dd = ins[dep]
                    eng.wait_ge(get_sem(dd['semkey']), dd['semval'])
                    self.n_waits += 1
                if d['dma'] and 'cc' in d:
                    eng.collective_compute("AllGather", mybir.AluOpType.bypass, replica_groups=[[0, 1, 2, 3], [4, 5, 6, 7]],
                                           ins=[d['cc'][0].ap().opt()], outs=[d['cc'][1].ap().opt()]).then_inc(get_sem(d['semkey']))
                elif d['dma']:
                    eng.dma_start(out=d['out'], in_=d['in_'], **d['kw']).then_inc(
                        get_sem(d['semkey']), 16)
                else:
                    r = d['fn'](eng)
                    if d['signal']:
                        r.then_inc(get_sem(d['semkey']), 1)
            if e == final_wait_eng:
                for f in final:
                    dd = ins[f]
                    eng.wait_ge(get_sem(dd['semkey']), dd['semval'])

        for e in self.ENGS:
            for iid in self.stream[e]:
                d = ins[iid]
                if d['dma'] or d['signal']:
                    get_sem(d['semkey'])
        with nc.Block() as block:
            for e in self.ENGS:
                if not self.stream[e] and e != final_wait_eng:
                    continue
                deco = getattr(block, engobj[e])
                deco(lambda eng, e=e: run_stream(e, eng))
        nc.clear_and_free_semaphores(list(sems.values()))
        nc.all_engine_barrier()
        return len(sems)


T = 2048
D = 1024
N_IN = 10052
EPS = 1e-6
PI = math.pi
SINK = 0.999999

O_AQ, O_AK, O_AV, O_IQ, O_IK, O_IW, O_GLU, O_CC, O_SQ, O_SK, O_SV, O_GATE = (
    0, 512, 1024, 1536, 1792, 1856, 1860, 2884, 4420, 4932, 5444, 5956)
R_QA, R_QI, R_SQ, NR_Q = 0, 512, 768, 1280
R_KI, R_KA, R_SK, NR_K = 0, 64, 576, 1088
R_GLU, R_CC, R_GATE, NR_F32 = 0, 1024, 2560, 6656


def l1_items():
    items = []
    for h in range(4):
        items.append(('qkA', O_AQ + 128 * h, 128, R_QA + 128 * h, 0))
    for h in range(4):
        items.append(('qkA', O_AK + 128 * h, 128, R_KA + 128 * h, 1))
    items.append(('tmv', O_AV, 256, 0, 0))
    items.append(('tmv', O_AV + 256, 256, 256, 0))
    items.append(('qi', O_IQ, 128, R_QI, 0))
    items.append(('qi', O_IQ + 128, 128, R_QI + 128, 0))
    items.append(('ki', O_IK, 64, R_KI, 0))
    items.append(('iw', O_IW, 4, 0, 0))
    for j in range(8):
        items.append(('f32', O_GLU + 128 * j, 128, R_GLU + 128 * j, 0))
    for j in range(12):
        items.append(('f32', O_CC + 128 * j, 128, R_CC + 128 * j, 0))
    for j in range(4):
        items.append(('bf', O_SQ + 128 * j, 128, R_SQ + 128 * j, 0))
    for j in range(4):
        items.append(('bf', O_SK + 128 * j, 128, R_SK + 128 * j, 0))
    items.append(('tmv', O_SV, 256, 512, 0))
    items.append(('tmv', O_SV + 256, 256, 768, 0))
    for j in range(32):
        items.append(('f32', O_GATE + 128 * j, 128, R_GATE + 128 * j, 0))
    groups = []
    cur = []
    for it in items:
        if cur and (it[1] + it[2] - cur[0][1]) > 256:
            groups.append(cur); cur = []
        cur.append(it)
    if cur:
        groups.append(cur)
    return groups


def l1_consts():
    half = 16
    invA = (500000.0 ** (-(np.arange(half, dtype=np.float32) * 2.0) / 32)).astype(np.float32)
    invI = (500000.0 ** (-(np.arange(8, dtype=np.float32) * 2.0) / 16)).astype(np.float32)
    cst = np.zeros((128, 8), np.float32)
    for p in range(32):
        cst[p, 0] = invA[p % 16]
    for p in range(128):
        if p % 64 < 16:
            cst[p, 1] = invI[p % 8]
    cst[:, 3] = 0.0
    cst[:, 4] = 0.5 * PI * SINK
    RmA = np.zeros((128, 128), np.float32)
    for m in range(16):
        RmA[m + 16, m] = -1.0
        RmA[m, m + 16] = 1.0
    RmI = np.zeros((128, 128), np.float32)
    for b in (0, 64):
        for m in range(8):
            RmI[b + m + 8, b + m] = -1.0
            RmI[b + m, b + m + 8] = 1.0
    return cst, RmA, RmI


def build_l1(nc, IO, pfx):
    xT, pos, w, gmix, gq, cst, rmA, rmI = IO['xT'], IO['pos'], IO['w'], IO['gmix'], IO['gq'], IO['cst'], IO['rmA'], IO['rmI']
    o_q, o_k, o_f32, o_v, o_iw, tail1 = IO['o_q'], IO['o_k'], IO['o_f32'], IO['o_v'], IO['o_iw'], IO['tail1']

    P = Prog(nc)
    import contextlib
    es = contextlib.ExitStack()

    def sb(name, shape, dt):
        return es.enter_context(nc.sbuf_tensor(pfx + name, shape, dt))

    def pst(name, shape, dt=F32):
        return es.enter_context(nc.psum_tensor(pfx + name, shape, dt))

    with es:
        hT = sb("hT", [128, 8, T], BF16)
        xs = [sb(f"xs{i}", [128, 8, 256], F32) for i in range(2)]
        sqs = sb("sqs", [128, 8, 256], F32)
        rs = sb("rs", [128, 256], F32)
        gm = sb("gm", [128, 8], F32)
        gqt = sb("gqt", [128, 4], F32)
        cs = sb("cs", [128, 8], F32)
        rA = sb("rA", [128, 128], F32)
        rI = sb("rI", [128, 128], F32)
        ones = sb("ones", [128, 128], F32)
        posi = sb("posi", [128, T], I32)
        posf = sb("posf", [128, T], F32)
        ang = sb("ang", [128, T], F32)
        posf2 = sb("posf2", [128, T], F32)
        posi2 = sb("posi2", [128, T], I32)
        cosA = sb("cosA", [128, T], F32)
        sinA = sb("sinA", [128, T], F32)
        cosI = sb("cosI", [128, T], F32)
        sinI = sb("sinI", [128, T], F32)
        wf = [sb(f"wf{i}", [128, 8, 256], F32) for i in range(2)]
        wb = [sb(f"wb{i}", [128, 8, 256], BF16) for i in range(2)]
        of32 = [sb(f"of{i}", [128, T], F32) for i in range(2)]
        obf = [sb(f"ob{i}", [128, T], BF16) for i in range(2)]
        ov = [sb(f"ov{i}", [128, 16, 256], BF16) for i in range(2)]
        oiw = sb("oiw", [128, 16, 4], F32)
        raw = sb("raw", [128, 512], F32)
        sq = sb("sq", [128, 512], F32)
        rstd = sb("rstd", [128, 512], F32)
        nrm = sb("nrm", [128, 512], F32)
        t1 = sb("t1", [128, 512], F32)
        t2 = sb("t2", [128, 512], F32)
        psm = [pst(f"psm{i}", [128, 512]) for i in range(4)]
        psn = pst("psn", [128, 512])
        psr = pst("psr", [128, 512])

        P.dma('sp', gm[:], gmix, writes=['gm'])
        P.dma('sp', gqt[:], gq, writes=['gqt'])
        P.dma('sp', cs[:], cst, writes=['cs'])
        P.dma('sp', rA[:], rmA, writes=['rA'])
        P.dma('sp', rI[:], rmI, writes=['rI'])
        P.dma('sp', posi[:], pos, writes=['posi'])
        P.op('dve', lambda e: e.memset(ones[:], 1.0), writes=['ones'])
        P.op('dve', lambda e: e.tensor_copy(posf[:], posi[:]), reads=['posi'], writes=['posf'])
        for j, (ct, st) in enumerate(((cosA, sinA), (cosI, sinI))):
            cn, sn = f"cos{j}", f"sin{j}"
            P.op('dve', lambda e, j=j: e.tensor_scalar(ang[:], posf[:], cs[:, j:j + 1], None, op0=ALU.mult),
                 reads=['posf', 'cs'], writes=['ang'])
            for (tt_, tn, shift, bcol) in ((st, sn, 0.0, 3), (ct, cn, 0.5 * PI, 4)):
                P.op('dve', lambda e, shift=shift: e.tensor_scalar(posf2[:], ang[:], 1.0 / (2 * PI), 0.5 + shift / (2 * PI),
                                                                   op0=ALU.mult, op1=ALU.add),
                     reads=['ang'], writes=['posf2'])
                P.op('dve', lambda e: e.tensor_copy(posi2[:], posf2[:]), reads=['posf2'], writes=['posi2'])
                P.op('dve', lambda e: e.tensor_copy(posf2[:], posi2[:]), reads=['posi2'], writes=['posf2'])
                P.op('dve', lambda e, tt_=tt_: e.scalar_tensor_tensor(tt_[:], posf2[:], -2 * PI, ang[:], op0=ALU.mult, op1=ALU.add),
                     reads=['posf2', 'ang'], writes=[tn])
                P.op('dve', lambda e, tt_=tt_, shift=shift: e.tensor_scalar(posf2[:], tt_[:], -PI - shift, 2 * PI,
                                                                          op0=ALU.is_lt, op1=ALU.mult),
                     reads=[tn], writes=['posf2'])
                P.op('dve', lambda e, tt_=tt_: e.tensor_tensor(tt_[:], tt_[:], posf2[:], op=ALU.add),
                     reads=[tn, 'posf2'], writes=[tn])
                P.op('act', lambda e, tt_=tt_, bcol=bcol: e.activation(tt_[:], tt_[:], AF.Sin, bias=cs[:, bcol:bcol + 1], scale=SINK),
                     reads=[tn, 'cs'], writes=[tn])

        xTv = xT.rearrange("(c p) t -> p c t", p=128)
        for tg in range(8):
            xb = xs[tg % 2]
            xk = f"xs{tg % 2}"
            P.dma('sp', xb[:], xTv[:, :, tg * 256:(tg + 1) * 256], writes=[xk])
            P.op('act', lambda e, xb=xb: e.activation(sqs[:], xb[:], AF.Square), reads=[xk], writes=['sqs'])
            for c in range(8):
                P.op('pe', lambda e, c=c: e.matmul(psn[:, 0:256], lhsT=ones[:], rhs=sqs[:, c, :],
                                                   start=(c == 0), stop=(c == 7)),
                     reads=['ones', 'sqs'], writes=['psn'])
            P.op('dve', lambda e: e.tensor_scalar(rs[:], psn[:, 0:256], 1.0 / D, EPS, op0=ALU.mult, op1=ALU.add),
                 reads=['psn'], writes=['rs'])
            P.op('act', lambda e: e.activation(rs[:], rs[:], AF.Sqrt), reads=['rs'], writes=['rs'])
            P.op('dve', lambda e: e.reciprocal(rs[:], rs[:]), reads=['rs'], writes=['rs'])
            for c in range(8):
                P.op('dve', lambda e, c=c, xb=xb, tg=tg: e.scalar_tensor_tensor(
                    hT[:, c, tg * 256:(tg + 1) * 256], xb[:, c, :], gm[:, c:c + 1], rs[:],
                    op0=ALU.mult, op1=ALU.mult),
                     reads=[xk, 'gm', 'rs'], writes=[f'hT{tg}'])
        hkeys = [f'hT{tg}' for tg in range(8)]

        wv = w.rearrange("(c p) n -> p c n", p=128)
        groups = l1_items()
        cnt = dict(ps=0, of=0, ob=0, ov=0)
        for gi, grp in enumerate(groups):
            c0 = grp[0][1]
            c1 = grp[-1][1] + grp[-1][2]
            nco = c1 - c0
            b = gi % 2
            P.dma('sp', wf[b][:, :, 0:nco], wv[:, :, c0:c1], writes=[f'wf{b}'])
            ceng = 'pool' if gi % 2 == 0 else 'dve'
            P.op(ceng, lambda e, b=b, nco=nco: e.tensor_copy(wb[b][:, :, 0:nco], wf[b][:, :, 0:nco]),
                 reads=[f'wf{b}'], writes=[f'wb{b}'])
            for (kind, col, ncol, orow, aux) in grp:
                lo = col - c0
                if kind in ('tmv', 'iw'):
                    if kind == 'tmv':
                        ob_ = ov[cnt['ov'] % 2]; okey = f"ov{cnt['ov'] % 2}"; cnt['ov'] += 1
                    else:
                        ob_ = oiw; okey = 'oiw'
                    for tt in range(16):
                        pi = cnt['ps'] % 4; cnt['ps'] += 1
                        for c in range(8):
                            P.op('pe', lambda e, pi=pi, c=c, tt=tt, lo=lo, ncol=ncol, b=b: e.matmul(
                                psm[pi][:, 0:ncol], lhsT=hT[:, c, tt * 128:(tt + 1) * 128],
                                rhs=wb[b][:, c, lo:lo + ncol], start=(c == 0), stop=(c == 7)),
                                 reads=hkeys + [f'wb{b}'], writes=[f'psm{pi}'])
                        ee = 'act' if tt % 2 == 0 else 'dve'
                        if ee == 'act':
                            P.op('act', lambda e, pi=pi, tt=tt, ncol=ncol, ob_=ob_: e.copy(ob_[:, tt, 0:ncol], psm[pi][:, 0:ncol]),
                                 reads=[f'psm{pi}'], writes=[okey])
                        else:
                            P.op('dve', lambda e, pi=pi, tt=tt, ncol=ncol, ob_=ob_: e.tensor_copy(ob_[:, tt, 0:ncol], psm[pi][:, 0:ncol]),
                                 reads=[f'psm{pi}'], writes=[okey])
                    if kind == 'tmv':
                        for q in range(8):
                            P.dma('act', o_v[q].rearrange("(tt p) c -> p tt c", p=128)[:, :, orow:orow + 256], ob_[:, 2 * q:2 * q + 2, :], reads=[okey])
                    else:
                        P.dma('act', o_iw.rearrange("(tt p) c -> p tt c", p=128), ob_[:], reads=[okey])
                    continue
                if kind == 'f32':
                    ob_ = of32[cnt['of'] % 2]; okey = f"of{cnt['of'] % 2}"; cnt['of'] += 1
                else:
                    ob_ = obf[cnt['ob'] % 2]; okey = f"ob{cnt['ob'] % 2}"; cnt['ob'] += 1
                for g in range(4):
                    pi = cnt['ps'] % 4; cnt['ps'] += 1
                    ts_ = slice(g * 512, (g + 1) * 512)
                    for c in range(8):
                        P.op('pe', lambda e, pi=pi, c=c, lo=lo, ncol=ncol, b=b, ts_=ts_: e.matmul(
                            psm[pi][0:ncol, :], lhsT=wb[b][:, c, lo:lo + ncol], rhs=hT[:, c, ts_],
                            start=(c == 0), stop=(c == 7)),
                             reads=hkeys + [f'wb{b}'], writes=[f'psm{pi}'])
                    pk = f'psm{pi}'
                    if kind in ('f32', 'bf'):
                        if g % 2 == 0:
                            P.op('act', lambda e, pi=pi, ts_=ts_, ob_=ob_: e.copy(ob_[:, ts_], psm[pi][:]),
                                 reads=[pk], writes=[okey])
                        else:
                            P.op('dve', lambda e, pi=pi, ts_=ts_, ob_=ob_: e.tensor_copy(ob_[:, ts_], psm[pi][:]),
                                 reads=[pk], writes=[okey])
                        continue
                    np_ = ncol
                    if kind in ('qkA', 'ki'):
                        gcol = aux if kind == 'qkA' else 2
                        P.op('act', lambda e, pi=pi, np_=np_: e.copy(raw[0:np_, :], psm[pi][0:np_, :]), reads=[pk], writes=['raw'])
                        P.op('act', lambda e, pi=pi, np_=np_: e.activation(sq[0:np_, :], psm[pi][0:np_, :], AF.Square),
                             reads=[pk], writes=['sq'])
                        P.op('pe', lambda e, np_=np_: e.matmul(psn[0:np_, :], lhsT=ones[0:np_, 0:np_], rhs=sq[0:np_, :],
                                                              start=True, stop=True),
                             reads=['ones', 'sq'], writes=['psn'])
                        P.op('dve', lambda e, np_=np_: e.tensor_scalar(rstd[0:np_, :], psn[0:np_, :], 1.0 / np_, EPS,
                                                                      op0=ALU.mult, op1=ALU.add),
                             reads=['psn'], writes=['rstd'])
                        P.op('act', lambda e, np_=np_: e.activation(rstd[0:np_, :], rstd[0:np_, :], AF.Sqrt),
                             reads=['rstd'], writes=['rstd'])
                        P.op('dve', lambda e, np_=np_: e.reciprocal(rstd[0:np_, :], rstd[0:np_, :]),
                             reads=['rstd'], writes=['rstd'])
                        P.op('dve', lambda e, np_=np_, gcol=gcol: e.scalar_tensor_tensor(
                            nrm[0:np_, :], raw[0:np_, :], gqt[0:np_, gcol:gcol + 1], rstd[0:np_, :],
                            op0=ALU.mult, op1=ALU.mult), reads=['raw', 'gqt', 'rstd'], writes=['nrm'])
                        src = nrm
                    else:
                        P.op('act', lambda e, pi=pi: e.copy(nrm[:], psm[pi][:]), reads=[pk], writes=['nrm'])
                        src = nrm
                    if kind == 'qkA':
                        rp = 32; R = rA; ct, st, cn, sn = cosA, sinA, 'cos0', 'sin0'
                    else:
                        rp = np_; R = rI; ct, st, cn, sn = cosI, sinI, 'cos1', 'sin1'
                    P.op('pe', lambda e, rp=rp, R=R: e.matmul(psr[0:rp, :], lhsT=R[0:rp, 0:rp], rhs=nrm[0:rp, :],
                                                             start=True, stop=True),
                         reads=['nrm', 'rA', 'rI'], writes=['psr'])
                    P.op('pool', lambda e, rp=rp, ct=ct, ts_=ts_: e.tensor_tensor(t1[0:rp, :], nrm[0:rp, :], ct[0:rp, ts_], op=ALU.mult),
                         reads=['nrm', cn], writes=['t1'])
                    P.op('dve', lambda e, rp=rp, st=st, ts_=ts_: e.tensor_tensor(t2[0:rp, :], psr[0:rp, :], st[0:rp, ts_], op=ALU.mult),
                         reads=['psr', sn], writes=['t2'])
                    P.op('pool', lambda e, rp=rp, ob_=ob_, ts_=ts_: e.tensor_tensor(ob_[0:rp, ts_], t1[0:rp, :], t2[0:rp, :], op=ALU.add),
                         reads=['t1', 't2'], writes=[okey])
                    if rp < np_:
                        P.op('act', lambda e, ob_=ob_, ts_=ts_: e.copy(ob_[32:64, ts_], nrm[32:64, :]),
                             reads=['nrm'], writes=[okey])
                        P.op('act', lambda e, ob_=ob_, ts_=ts_: e.copy(ob_[64:128, ts_], nrm[64:128, :]),
                             reads=['nrm'], writes=[okey])
                oq_ = 'act'
                if kind == 'f32':
                    P.dma(oq_, o_f32[orow:orow + ncol, :], ob_[0:ncol, :], reads=[okey], writes=['o_f32'])
                else:
                    if (kind == 'ki' or (kind == 'qkA' and aux == 1) or (kind == 'bf' and col >= O_SK)):
                        P.dma('act', o_k[orow][0:ncol, :], ob_[0:ncol, :], reads=[okey])
                    else:
                        P.dma('act', o_q[orow:orow + ncol, :], ob_[0:ncol, :], reads=[okey])
        for (r0, t0) in ((R_GLU, 0), (R_CC + 512, 1024)):
            for q in range(4):
                P.dma('sp', tail1[(t0 + q * 256) // 256].rearrange("r (b k) -> r b k", k=32),
                      o_f32[r0 + q * 256:r0 + (q + 1) * 256, :].rearrange("r (b t) -> r b t", t=128)[:, :, 96:128],
                      reads=['o_f32'])
        ns = P.emit()
        print("L1: instrs", len(P.ins), "sems", ns, "waits", P.n_waits)


S = 8192
NQ = 16
NIT = 18
NEG = -1.0e30
MNEG = -30000.0
SCALE = 128 ** -0.5


def l2_masks(j):
    neg = np.zeros((128, 4, 128), np.float32)
    md = np.zeros((128, 4, 4, 128), np.float32)
    tp = np.arange(128)[:, None]
    sp = np.arange(128)[None, :]
    for m in range(4):
        if m < j:
            neg[:, m, :] = 0.0
            md[:, m, :, :] = 1.0
        elif m == j:
            neg[:, m, :] = np.where(sp <= tp, 0.0, NEG)
            md[:, m, :, :] = (np.arange(128)[:, None] < np.arange(128)[None, :]).astype(np.float32)[:, None, :]
        else:
            neg[:, m, :] = NEG
            md[:, m, :, :] = 0.0
    return neg.reshape(128, 512), md


def l2_consts():
    su = (np.arange(128)[:, None] > np.arange(128)[None, :]).astype(np.float32)
    ident = np.eye(128, dtype=np.float32)
    pw = (2.0 ** -(np.arange(NIT, dtype=np.float32) + 1.0))[None, :].repeat(128, 0).astype(np.float32)
    return su, ident, pw


def build_l2(nc, IO, pfx, do_a=True, do_d=True, nq=NQ):
    G_k, G_v, o_q, o_iw = IO['G_k'], IO['G_v'], IO['o_q'], IO['o_iw']
    negm, mdm, su_d, id_d, pw_d = IO['negm'], IO['mdm'], IO['su'], IO['ident'], IO['pw']
    yaT, ydT = IO['yaT'], IO['ydT']
    Gv4 = [g.rearrange("(r i p) n -> p r i n", r=4, p=128) for g in G_v]

    def load_nat(P, eng, dst2d, row0, nrows, key):
        dv = dst2d.rearrange("p (i r t) -> p i r t", r=4, t=128)
        for r in range(4):
            P.dma(eng if r % 2 == 0 else ('pool' if eng == 'sp' else 'sp'), dv[:, :, r, :],
                  G_k[row0][r * nrows:(r + 1) * nrows, :].rearrange("p (i t) -> p i t", t=128), reads=[f'Gk{row0}'], writes=[key])

    P = Prog(nc)
    es = contextlib.ExitStack()

    def sb(name, shape, dt):
        return es.enter_context(nc.sbuf_tensor(pfx + name, shape, dt))

    def pst(name, shape, dt=F32):
        return es.enter_context(nc.psum_tensor(pfx + name, shape, dt))

    with es:
        KT = sb("KT", [128, 4, S], BF16)
        kit = sb("kit", [64, S], BF16)
        score = sb("score", [128, S], F32)
        mneg = sb("mneg", [128, S], BF16)
        junk = sb("junk", [128, S], BF16)
        vt = [sb(f"vt{i}", [128, 4, 512], BF16) for i in range(3)]
        qblk = [sb(f"qblk{i}", [128, 4, 128], BF16) for i in range(2)]
        qiblk = [sb(f"qiblk{i}", [64, 4, 128], BF16) for i in range(2)]
        iwt = sb("iwt", [128, NQ, 4], F32)
        absw = sb("absw", [128, NQ, 4], F32)
        sgnw = sb("sgnw", [128, NQ, 4], F32)
        negt = sb("negt", [128, 512], F32)
        mdt = sb("mdt", [128, 4, 4, 128], F32)
        sut = sb("sut", [128, 128], F32)
        idf = sb("idf", [128, 128], F32)
        idb = sb("idb", [128, 128], BF16)
        pwt = sb("pwt", [128, NIT], F32)
        ones_f = sb("ones_f", [128, 128], F32)
        ones_b = sb("ones_b", [128, 128], BF16)
        rh = [sb(f"rh{i}", [128, 512], F32) for i in range(2)]
        st = sb("st", [128, 16], F32)
        wk = sb("wk", [128, NIT], F32)
        PT = [sb(f"PT{i}", [128, 512], BF16) for i in range(2)]
        rec = sb("rec", [128, 512], F32)
        oat = [sb(f"oat{i}", [128, 512], F32) for i in range(2)]
        e_t = [sb(f"e_t{i}", [128, 512], F32) for i in range(2)]
        sp_t = [sb(f"sp_t{i}", [128, 512], F32) for i in range(3)]
        u_t = [sb(f"u_t{i}", [128, 512], F32) for i in range(2)]
        R_t = [sb(f"R_t{i}", [128, 512], F32) for i in range(2)]
        aT = [sb(f"aT{i}", [128, 512], BF16) for i in range(2)]
        ps = [pst(f"ps{i}", [128, 512]) for i in range(8)]

        for (src_t, dst_t, key) in IO['cc_l2']:
            P.cc('pool', src_t, dst_t, writes=[key])
        P.dma('sp', iwt[:], o_iw.rearrange("(i t) h -> t i h", t=128), writes=['iwt'])
        P.dma('sp', negt[:], negm, writes=['negt'])
        P.dma('sp', mdt[:], mdm, writes=['mdt'])
        P.dma('sp', sut[:], su_d, writes=['sut'])
        P.dma('sp', idf[:], id_d, writes=['idf'])
        P.dma('sp', pwt[:], pw_d, writes=['pwt'])
        P.op('dve', lambda e: e.memset(ones_f[:], 1.0), writes=['ones_f'])
        P.op('dve', lambda e: e.memset(ones_b[:], 1.0), writes=['ones_b'])
        P.op('dve', lambda e: e.tensor_copy(idb[:], idf[:]), reads=['idf'], writes=['idb'])
        P.op('act', lambda e: e.activation(absw[:], iwt[:], AF.Abs), reads=['iwt'], writes=['absw'])
        P.op('act', lambda e: e.activation(sgnw[:], iwt[:], AF.Sign), reads=['iwt'], writes=['sgnw'])

        hsl = [slice(h * 128, (h + 1) * 128) for h in range(4)]

        if do_d:
            for h in range(4):
                load_nat(P, 'sp', KT[:, h, :], 576 + h * 128, 128, f'KT{h}')
            vcnt = 0
            for i in range(nq):
                NK = 4 * i + 4
                NG = i + 1
                qb_ = qblk[i % 2]; qbk = f'qblk{i % 2}'
                P.dma('sp', qb_[:], o_q[768:1280, i * 128:(i + 1) * 128].rearrange("(h d) t -> d h t", d=128), writes=[qbk])
                chs = list(range(NK - 1, -1, -1))
                vbs = {}

                def s1(k, ch):
                    nonlocal vcnt
                    kg, cc = ch // 4, ch % 4
                    if cc == 3:
                        vb = vt[vcnt % 3]; vk = f'vt{vcnt % 3}'; vcnt += 1
                        P.dma('sp', vb[:], Gv4[kg // 2][:, :, kg % 2, 512:1024], reads=[f'Gv{kg // 2}'], writes=[vk])
                        vbs[kg] = (vb, vk)
                    cs_ = slice(ch * 128, (ch + 1) * 128)
                    pz = ps[k % 2]; pzk = f'ps{k % 2}'
                    et = e_t[k % 2]; ek = f'e_t{k % 2}'
                    spt = sp_t[k % 3]; spk = f'sp{k % 3}'
                    for h in range(4):
                        P.op('pe', lambda e, pz=pz, h=h, cs_=cs_, qb_=qb_: e.matmul(pz[:, hsl[h]], lhsT=KT[:, h, cs_], rhs=qb_[:, h, :], start=True, stop=True),
                             reads=[f'KT{h}', qbk], writes=[pzk])
                    P.op('act', lambda e, pz=pz, et=et: e.activation(et[:], pz[:], AF.Exp, scale=SCALE), reads=[pzk], writes=[ek])
                    P.op('act', lambda e, spt=spt, et=et: e.activation(spt[:], et[:], AF.Ln, bias=ones_f[:, 0:1]), reads=[ek, 'ones_f'], writes=[spk])
                    if kg == NG - 1:
                        P.op('dve', lambda e, spt=spt, cc=cc: e.tensor_tensor(
                            spt[:], spt[:], mdt[:, cc, :, :].rearrange("p h t -> p (h t)"), op=ALU.mult), reads=[spk, 'mdt'], writes=[spk])

                def s2(k, ch):
                    first = (k == 0)
                    pz = ps[k % 2]; pzk = f'ps{k % 2}'
                    pB = ps[2 + k % 2]; pBk = f'ps{2 + k % 2}'
                    spt = sp_t[k % 3]; spk = f'sp{k % 3}'
                    ut = u_t[k % 2]; uk = f'u_t{k % 2}'
                    Rp, Rpk = R_t[k % 2], f'R_t{k % 2}'
                    Rn, Rnk = R_t[(k + 1) % 2], f'R_t{(k + 1) % 2}'
                    P.op('pe', lambda e, pB=pB, spt=spt, first=first: e.matmul(pB[:], lhsT=sut[:], rhs=spt[:], start=True, stop=first),
                         reads=['sut', spk], writes=[pBk])
                    if not first:
                        P.op('pe', lambda e, pB=pB, Rp=Rp: e.matmul(pB[:], lhsT=ones_f[:], rhs=Rp[:], start=False, stop=True),
                             reads=['ones_f', Rpk], writes=[pBk])
                    if first:
                        P.op('dve', lambda e, spt=spt, Rn=Rn: e.tensor_copy(Rn[:], spt[:]), reads=[spk], writes=[Rnk])
                    elif k + 1 < NK:
                        P.op('dve', lambda e, spt=spt, Rn=Rn, Rp=Rp: e.tensor_tensor(Rn[:], Rp[:], spt[:], op=ALU.add), reads=[spk, Rpk], writes=[Rnk])
                    P.op('dve', lambda e, pz=pz, spt=spt, ut=ut: e.scalar_tensor_tensor(ut[:], pz[:], SCALE, spt[:], op0=ALU.mult, op1=ALU.subtract),
                         reads=[pzk, spk], writes=[uk])
                    P.op('dve', lambda e, pB=pB, ut=ut: e.tensor_tensor(ut[:], ut[:], pB[:], op=ALU.subtract), reads=[uk, pBk], writes=[uk])

                def s3(k, ch):
                    first = (k == 0)
                    kg, cc = ch // 4, ch % 4
                    ut = u_t[k % 2]; uk = f'u_t{k % 2}'
                    at = aT[k % 2]; atk = f'aT{k % 2}'
                    vb, vk = vbs[kg]
                    P.op('act', lambda e, at=at, ut=ut: e.activation(at[:], ut[:], AF.Exp), reads=[uk], writes=[atk])
                    if kg == NG - 1:
                        P.op('dve', lambda e, at=at, cc=cc: e.tensor_tensor(
                            at[:], at[:], mdt[:, cc, :, :].rearrange("p h t -> p (h t)"), op=ALU.mult), reads=[atk, 'mdt'], writes=[atk])
                    for h in range(4):
                        P.op('pe', lambda e, at=at, h=h, vb=vb, cc=cc, first=first, ch=ch: e.matmul(
                            ps[4 + h][:, 0:128], lhsT=vb[:, cc, hsl[h]], rhs=at[:, hsl[h]], start=first, stop=(ch == 0)),
                             reads=[atk, vk], writes=[f'ps{4 + h}'])

                for n in range(NK + 2):
                    if n < NK:
                        s1(n, chs[n])
                    if 0 <= n - 1 < NK:
                        s2(n - 1, chs[n - 1])
                    if 0 <= n - 2 < NK:
                        s3(n - 2, chs[n - 2])
                ot = oat[i % 2]; otk = f'oat{i % 2}'
                for h in range(4):
                    if h % 2 == 0:
                        P.op('dve', lambda e, h=h, ot=ot: e.tensor_copy(ot[:, hsl[h]], ps[4 + h][:, 0:128]), reads=[f'ps{4 + h}'], writes=[otk])
                    else:
                        P.op('act', lambda e, h=h, ot=ot: e.copy(ot[:, hsl[h]], ps[4 + h][:, 0:128]), reads=[f'ps{4 + h}'], writes=[otk])
                P.dma('sp', ydT[:, i * 128:(i + 1) * 128].rearrange("(h d) t -> d h t", d=128), ot[:].rearrange("p (h t) -> p h t", t=128), reads=[otk])
        if do_a:
            load_nat(P, 'sp', kit[:, :], 0, 64, 'kit')
            for h in range(4):
                load_nat(P, 'sp', KT[:, h, :], 64 + h * 128, 128, f'KT{h}')
            vstate = dict(cnt=0)

            def a_score(i):
                NG = i + 1
                Sc = 512 * NG
                qb_ = qblk[i % 2]; qbk = f'qblk{i % 2}'
                qib = qiblk[i % 2]; qik = f'qiblk{i % 2}'
                P.dma('sp', qb_[:], o_q[0:512, i * 128:(i + 1) * 128].rearrange("(h d) t -> d h t", d=128), writes=[qbk])
                P.dma('sp', qib[:], o_q[512:768, i * 128:(i + 1) * 128].rearrange("(h d) t -> d h t", d=64), writes=[qik])
                for kg in range(NG):
                    ks = slice(kg * 512, (kg + 1) * 512)
                    for h in range(4):
                        pb = ps[h % 2]; pk = f'ps{h % 2}'
                        P.op('pe', lambda e, pb=pb, h=h, ks=ks, qib=qib: e.matmul(pb[:], lhsT=qib[:, h, :], rhs=kit[:, ks], start=True, stop=True),
                             reads=[qik, 'kit'], writes=[pk])
                        r_ = rh[h % 2]; rk = f'rh{h % 2}'
                        P.op('act', lambda e, pb=pb, r_=r_, i=i, h=h: e.activation(r_[:], pb[:], AF.Relu, scale=absw[:, i, h:h + 1]),
                             reads=[pk, 'absw'], writes=[rk])
                        if h == 0:
                            P.op('dve', lambda e, r_=r_, ks=ks, i=i, h=h: e.tensor_scalar(score[:, ks], r_[:], sgnw[:, i, h:h + 1], None, op0=ALU.mult),
                                 reads=[rk, 'sgnw'], writes=['score'])
                        else:
                            P.op('dve', lambda e, r_=r_, ks=ks, i=i, h=h: e.scalar_tensor_tensor(
                                score[:, ks], r_[:], sgnw[:, i, h:h + 1], score[:, ks], op0=ALU.mult, op1=ALU.add),
                                 reads=[rk, 'sgnw', 'score'], writes=['score'])
                P.op('dve', lambda e, Sc=Sc: e.tensor_reduce(st[:, 1:2], score[:, 0:Sc], axis=AX.X, op=ALU.max), reads=['score'], writes=['st_hi'])
                P.op('dve', lambda e, Sc=Sc: e.tensor_reduce(st[:, 0:1], score[:, 0:Sc], axis=AX.X, op=ALU.min), reads=['score'], writes=['st_lo'])
                P.op('pool', lambda e, Sc=Sc: e.tensor_tensor(score[:, Sc - 512:Sc], score[:, Sc - 512:Sc], negt[:], op=ALU.add),
                     reads=['score', 'negt'], writes=['score'])

            def a_bisect(i):
                Sc = 512 * (i + 1)
                P.op('dve', lambda e: e.tensor_tensor(st[:, 2:3], st[:, 1:2], st[:, 0:1], op=ALU.subtract), reads=['st_hi', 'st_lo'], writes=['st_W'])
                P.op('dve', lambda e: e.tensor_scalar(wk[:], pwt[:], st[:, 2:3], None, op0=ALU.mult), reads=['pwt', 'st_W'], writes=['wk'])
                P.op('dve', lambda e: e.tensor_tensor(st[:, 3:4], st[:, 0:1], wk[:, 0:1], op=ALU.add), reads=['st_lo', 'wk'], writes=['st_mid'])
                yield
                Sa = max(64, int(round(0.56 * Sc / 64.0)) * 64)
                for k in range(NIT):
                    P.op('act', lambda e, Sa=Sa: e.activation(junk[:, 0:Sa], score[:, 0:Sa], AF.Sign, bias=st[:, 3:4], scale=-1.0,
                                                              accum_out=st[:, 4:5]),
                         reads=['score', 'st_mid'], writes=['junkA', 'st_cntA'])
                    P.op('dve', lambda e, Sa=Sa, Sc=Sc: e.tensor_scalar(junk[:, Sa:Sc], score[:, Sa:Sc], st[:, 3:4], None,
                                                                        op0=ALU.is_ge, op1=ALU.add, accum_out=st[:, 6:7]),
                         reads=['score', 'st_mid'], writes=['junkD', 'st_cntD'])
                    P.op('dve', lambda e: e.scalar_tensor_tensor(st[:, 7:8], st[:, 4:5], -0.5, st[:, 6:7], op0=ALU.mult, op1=ALU.add),
                         reads=['st_cntA', 'st_cntD'], writes=['st_tmp1'])
                    P.op('dve', lambda e, k=k, Sa=Sa: e.scalar_tensor_tensor(st[:, 5:6], st[:, 7:8], 255.5 - 0.5 * Sa, wk[:, k:k + 1],
                                                                             op0=ALU.is_ge, op1=ALU.mult),
                         reads=['st_tmp1', 'wk'], writes=['st_tmp'])
                    P.op('dve', lambda e: e.tensor_tensor(st[:, 0:1], st[:, 0:1], st[:, 5:6], op=ALU.add), reads=['st_lo', 'st_tmp'], writes=['st_lo'])
                    if k + 1 < NIT:
                        P.op('dve', lambda e, k=k: e.tensor_tensor(st[:, 3:4], st[:, 0:1], wk[:, k + 1:k + 2], op=ALU.add),
                             reads=['st_lo', 'wk'], writes=['st_mid'])
                    yield

            def a_mask(i):
                Sc = 512 * (i + 1)
                P.op('dve', lambda e, Sc=Sc: e.tensor_scalar(mneg[:, 0:Sc], score[:, 0:Sc], st[:, 0:1], MNEG, op0=ALU.is_lt, op1=ALU.mult),
                     reads=['score', 'st_lo'], writes=['mneg'])

            def a_attn(i):
                NK = 4 * i + 4
                qb_ = qblk[i % 2]; qbk = f'qblk{i % 2}'
                vbs = {}

                def s1(ch):
                    if ch % 4 == 0:
                        kg = ch // 4
                        vb = vt[vstate['cnt'] % 3]; vk = f"vt{vstate['cnt'] % 3}"; vstate['cnt'] += 1
                        P.dma('pool', vb[:], Gv4[kg // 2][:, :, kg % 2, 0:512], reads=[f'Gv{kg // 2}'], writes=[vk])
                        vbs[kg] = (vb, vk)
                    cs_ = slice(ch * 128, (ch + 1) * 128)
                    pl = ps[ch % 2]; plk = f'ps{ch % 2}'
                    for h in range(4):
                        P.op('pe', lambda e, pl=pl, h=h, cs_=cs_: e.matmul(pl[:, hsl[h]], lhsT=KT[:, h, cs_], rhs=qb_[:, h, :], start=True, stop=False),
                             reads=[f'KT{h}', qbk], writes=[plk])
                        P.op('pe', lambda e, pl=pl, h=h, cs_=cs_: e.matmul(pl[:, hsl[h]], lhsT=mneg[:, cs_], rhs=idb[:], start=False, stop=True),
                             reads=['mneg', 'idb'], writes=[plk])

                def s2(ch):
                    pl = ps[ch % 2]; plk = f'ps{ch % 2}'
                    pt = PT[ch % 2]; ptk = f'PT{ch % 2}'
                    vb, vk = vbs[ch // 4]
                    cc = ch % 4
                    P.op('act', lambda e, pl=pl, pt=pt: e.activation(pt[:], pl[:], AF.Exp, scale=SCALE), reads=[plk], writes=[ptk])
                    P.op('pe', lambda e, pt=pt, ch=ch: e.matmul(ps[2][:, :], lhsT=ones_b[:, :], rhs=pt[:], start=(ch == 0), stop=(ch == NK - 1)),
                         reads=[ptk, 'ones_b'], writes=['ps2'])
                    for h in range(4):
                        P.op('pe', lambda e, pt=pt, h=h, ch=ch, vb=vb, cc=cc: e.matmul(
                            ps[3 + h][:, 0:128], lhsT=vb[:, cc, hsl[h]], rhs=pt[:, hsl[h]], start=(ch == 0), stop=(ch == NK - 1)),
                             reads=[ptk, vk], writes=[f'ps{3 + h}'])

                for n in range(NK + 1):
                    if n < NK:
                        s1(n)
                    if n >= 1:
                        s2(n - 1)
                    yield
                P.op('dve', lambda e: e.reciprocal(rec[:], ps[2][:, :]), reads=['ps2'], writes=['rec'])
                ot = oat[i % 2]; otk = f'oat{i % 2}'
                for h in range(4):
                    P.op('dve', lambda e, h=h, ot=ot: e.tensor_tensor(ot[:, hsl[h]], ps[3 + h][:, 0:128], rec[:, hsl[h]], op=ALU.mult),
                         reads=[f'ps{3 + h}', 'rec'], writes=[otk])
                P.dma('sp', yaT[:, i * 128:(i + 1) * 128].rearrange("(h d) t -> d h t", d=128), ot[:].rearrange("p (h t) -> p h t", t=128), reads=[otk])

            a_score(0)
            for _ in a_bisect(0):
                pass
            a_mask(0)
            for i in range(nq):
                ga = a_attn(i)
                n_att = 4 * i + 5
                if i + 1 < nq:
                    a_score(i + 1)
                    gb = a_bisect(i + 1)
                    n_bis = NIT + 1
                    done_b = 0
                    for s_ in range(n_att):
                        next(ga)
                        tgt = ((s_ + 1) * n_bis) // n_att
                        while done_b < tgt:
                            next(gb); done_b += 1
                    for _ in gb:
                        pass
                for _ in ga:
                    pass
                if i + 1 < nq:
                    a_mask(i + 1)

        ns = P.emit()
        print("L2: instrs", len(P.ins), "sems", ns, "waits", P.n_waits)

T = 2048
HB = 32
EPS = 1e-6
DFF = 2816
NFF = 22


def _blend(P, out_ap, cands, ckeys, selt, okey):
    P.op('dve', lambda e: e.tensor_scalar(out_ap, cands[0], selt[:, 0:1], None, op0=ALU.mult),
         reads=[ckeys[0], 'selt'], writes=[okey])
    for c in range(1, 4):
        P.op('dve', lambda e, c=c: e.scalar_tensor_tensor(out_ap, cands[c], selt[:, c:c + 1], out_ap, op0=ALU.mult, op1=ALU.add),
             reads=[ckeys[c], 'selt', okey], writes=[okey])


def build_l3a(nc, IO, pfx):
    o_f32, GT1, sel_d, yaT, ydT, xT = IO['o_f32'], IO['GT1'], IO['sel'], IO['yaT'], IO['ydT'], IO['xT']
    wo, wm, sm, xmT, tail2 = IO['wo'], IO['wm'], IO['sm3a'], IO['xm'], IO['tail2']
    gateT = o_f32[2560:6656, :]
    P = Prog(nc)
    es = contextlib.ExitStack()
    sb = lambda name, shape, dt: es.enter_context(nc.sbuf_tensor(pfx + name, shape, dt))
    pst = lambda name, shape, dt=F32: es.enter_context(nc.psum_tensor(pfx + name, shape, dt))
    W = 512
    with es:
        wob = [sb(f"wob{i}", [128, 4, 1024], BF16) for i in range(4)]
        wmb = sb("wmb", [128, 8, 1024], BF16)
        wst = sb("wst", [128, 4, 1024], F32)
        smt = sb("smt", [128, 192], F32)
        selt = sb("selt", [128, 4], F32)
        ones = sb("ones", [128, 128], F32)
        hh = [sb(f"hh{i}", [128, 4, 4, 160], F32) for i in range(2)]
        cand = [sb(f"cand{i}", [128, 4, 4, 32], F32) for i in range(4)]
        acc = sb("acc", [128, 4, W], F32)
        xc = sb("xc", [128, 4, W], F32)
        sq = sb("sq", [128, 4, W], F32)
        gbb = sb("gbb", [128, 4, W], F32)
        brs = [[sb(f"br{i}_{p}", [128, 4, W], BF16) for i in range(4)] for p in range(2)]
        rln = sb("rln", [128, W], F32)
        gt_ = [sb(f"gt{i}", [128, W], F32) for i in range(2)]
        mg = sb("mg", [128, W], F32)
        mgb = sb("mgb", [128, 8, W], BF16)
        xt = sb("xt", [128, 8, W], F32)
        ot = sb("ot", [128, 8, W], F32)
        ps = [pst(f"ps{i}", [128, 512]) for i in range(8)]

        P.dma('sp', smt[:], sm, writes=['smt'])
        P.dma('sp', selt[:], sel_d, writes=['selt'])
        P.op('dve', lambda e: e.memset(ones[:], 1.0 / 512), writes=['ones'])
        for i in range(4):
            P.dma('sp', wst[:], wo[i].rearrange("(c p) n -> p c n", p=128), writes=['wst'])
            P.op('pool' if i % 2 else 'dve', lambda e, i=i: e.tensor_copy(wob[i][:], wst[:]), reads=['wst'], writes=[f'wob{i}'])
        for hf in range(2):
            P.dma('sp', wst[:], wm[hf * 512:(hf + 1) * 512, :].rearrange("(c p) n -> p c n", p=128), writes=['wst'])
            P.op('pool' if hf else 'dve', lambda e, hf=hf: e.tensor_copy(wmb[:, hf * 4:(hf + 1) * 4, :], wst[:]), reads=['wst'], writes=['wmb'])

        def load_haloed(dst, dkey, row_main, row_tail, tl):
            u0 = tl * W
            for ch in range(4):
                P.dma('sp' if ch % 2 == 0 else 'act', dst[:, ch, :, 32:160],
                      o_f32[row_main + ch * 128:row_main + (ch + 1) * 128, u0:u0 + W].rearrange("p (b t) -> p b t", t=128),
                      writes=[dkey])
            q0 = row_tail // 256
            for r in range(4):
                if r == 3 and tl == 0:
                    P.op('pool', lambda e: e.memset(cand[3][:], 0.0), writes=['cand3'])
                for hf in range(2):
                    src = GT1[q0 + hf][r * 256:(r + 1) * 256, :]
                    if r < 3:
                        P.dma('sp' if (r + hf) % 2 == 0 else 'act', cand[r][:, 2 * hf:2 * hf + 2, :, :],
                              src[:, tl * 128:(tl + 1) * 128].rearrange("(c p) (b k) -> p c b k", p=128, k=32), writes=[f'cand{r}'])
                    elif tl == 0:
                        P.dma('act', cand[3][:, 2 * hf:2 * hf + 2, 1:4, :],
                              src[:, 0:96].rearrange("(c p) (b k) -> p c b k", p=128, k=32), writes=['cand3'])
                    else:
                        P.dma('act', cand[3][:, 2 * hf:2 * hf + 2, :, :],
                              src[:, tl * 128 - 32:tl * 128 + 96].rearrange("(c p) (b k) -> p c b k", p=128, k=32), writes=['cand3'])
            _blend(P, dst[:].rearrange("p c b k -> p (c b) k")[:, :, 0:32], [c_[:].rearrange("p c b k -> p (c b) k") for c_ in cand],
                   [f'cand{r}' for r in range(4)], selt, dkey)

        def stage_x(tl):
            u0 = tl * W
            br = brs[tl % 2]
            bk = [f'br{i}_{tl % 2}' for i in range(4)]
            ga, gb_ = hh[0], hh[1]
            load_haloed(ga, 'hh0', 0, 0, tl)
            load_haloed(gb_, 'hh1', 512, 512, tl)
            P.op('act', lambda e: e.activation(gb_[:], gb_[:], AF.Sigmoid), reads=['hh1'], writes=['hh1'])
            P.op('pool', lambda e: e.tensor_tensor(ga[:], ga[:], gb_[:], op=ALU.mult), reads=['hh0', 'hh1'], writes=['hh0'])
            for ch in range(4):
                ak = f'acc{ch}'
                av_ = acc[:, ch, :].rearrange("p (b t) -> p b t", t=128)
                P.op('dve', lambda e, ch=ch, av_=av_: e.tensor_scalar(av_, ga[:, ch, :, 2:130], smt[:, 32 + ch * 31:33 + ch * 31],
                                                                    smt[:, 156 + ch:157 + ch], op0=ALU.mult, op1=ALU.add),
                     reads=['hh0', 'smt'], writes=[ak])
                for k in range(1, 31):
                    P.op('dve', lambda e, ch=ch, k=k, av_=av_: e.scalar_tensor_tensor(
                        av_, ga[:, ch, :, 2 + k:130 + k], smt[:, 32 + ch * 31 + k:33 + ch * 31 + k], av_,
                        op0=ALU.mult, op1=ALU.add), reads=['hh0', 'smt', ak], writes=[ak])
            aks = [f'acc{ch}' for ch in range(4)]
            for ch in range(4):
                P.op('pe', lambda e, ch=ch: e.matmul(ps[0][:], lhsT=ones[:], rhs=acc[:, ch, :], start=(ch == 0), stop=(ch == 3)),
                     reads=['ones'] + aks, writes=['ps0'])
            for ch in range(4):
                P.op('dve', lambda e, ch=ch: e.tensor_tensor(xc[:, ch, :], acc[:, ch, :], ps[0][:], op=ALU.subtract),
                     reads=aks + ['ps0'], writes=['xc'])
            P.op('act', lambda e: e.activation(sq[:], xc[:], AF.Square), reads=['xc'], writes=['sq'])
            for ch in range(4):
                P.op('pe', lambda e, ch=ch: e.matmul(ps[1][:], lhsT=ones[:], rhs=sq[:, ch, :], start=(ch == 0), stop=(ch == 3)),
                     reads=['ones', 'sq'], writes=['ps1'])
            P.op('dve', lambda e: e.tensor_scalar(rln[:], ps[1][:], 1.0, EPS, op0=ALU.mult, op1=ALU.add), reads=['ps1'], writes=['rln'])
            P.op('act', lambda e: e.activation(rln[:], rln[:], AF.Sqrt), reads=['rln'], writes=['rln'])
            P.op('dve', lambda e: e.reciprocal(rln[:], rln[:]), reads=['rln'], writes=['rln'])
            for ch in range(4):
                P.op('dve', lambda e, ch=ch: e.tensor_tensor(xc[:, ch, :], xc[:, ch, :], rln[:], op=ALU.mult),
                     reads=['xc', 'rln'], writes=['xc'])
                P.op('dve', lambda e, ch=ch: e.tensor_scalar(xc[:, ch, :], xc[:, ch, :], smt[:, 160 + ch:161 + ch],
                                                             smt[:, 164 + ch:165 + ch], op0=ALU.mult, op1=ALU.add),
                     reads=['xc', 'smt'], writes=['xc'])
            P.op('act', lambda e: e.activation(br[1][:], xc[:], AF.Silu), reads=['xc'], writes=[bk[1]])
            gc, xcc = hh[0], hh[1]
            load_haloed(gc, 'hh0', 1024 + 512, 1024, tl)
            load_haloed(xcc, 'hh1', 1024 + 1024, 1536, tl)
            P.dma('sp', gbb[:], o_f32[1024:1536, u0:u0 + W].rearrange("(c p) t -> p c t", p=128), writes=['gbb'])
            P.op('pool', lambda e: e.tensor_tensor(gc[:], gc[:], xcc[:], op=ALU.mult), reads=['hh0', 'hh1'], writes=['hh0'])
            for ch in range(4):
                ak = f'acc{ch}'
                av_ = acc[:, ch, :].rearrange("p (b t) -> p b t", t=128)
                P.op('dve', lambda e, ch=ch, av_=av_: e.tensor_scalar(av_, gc[:, ch, :, 30:158], smt[:, 168 + ch * 3:169 + ch * 3],
                                                                    None, op0=ALU.mult), reads=['hh0', 'smt'], writes=[ak])
                for k in (1, 2):
                    P.op('dve', lambda e, ch=ch, k=k, av_=av_: e.scalar_tensor_tensor(
                        av_, gc[:, ch, :, 30 + k:158 + k], smt[:, 168 + ch * 3 + k:169 + ch * 3 + k], av_,
                        op0=ALU.mult, op1=ALU.add), reads=['hh0', 'smt', ak], writes=[ak])
                P.op('pool', lambda e, ch=ch: e.tensor_tensor(br[2][:, ch, :], acc[:, ch, :], gbb[:, ch, :], op=ALU.mult),
                     reads=[ak, 'gbb'], writes=[bk[2]])
            P.dma('sp', xc[:], yaT[:, u0:u0 + W].rearrange("(c p) t -> p c t", p=128), writes=['xc'])
            P.op('act', lambda e: e.copy(br[0][:], xc[:]), reads=['xc'], writes=[bk[0]])
            P.dma('act', sq[:], ydT[:, u0:u0 + W].rearrange("(c p) t -> p c t", p=128), writes=['sq'])
            P.op('act', lambda e: e.copy(br[3][:], sq[:]), reads=['sq'], writes=[bk[3]])
        def stage_y(tl):
            u0 = tl * W
            br = brs[tl % 2]
            bk = [f'br{i}_{tl % 2}' for i in range(4)]
            P.dma('sp', xt[:], xT[:, u0:u0 + W].rearrange("(c p) t -> p c t", p=128), writes=['xt'])
            gi = 0
            for oc in range(8):
                ocs = slice(oc * 128, (oc + 1) * 128)
                for i in range(4):
                    pb = ps[2 + (gi % 4)]; pk = f'ps{2 + gi % 4}'
                    g_ = gt_[gi % 2]; gk = f'gt{gi % 2}'; gi += 1
                    P.dma('act' if gi % 2 else 'sp', g_[:], gateT[i * 1024 + oc * 128:i * 1024 + (oc + 1) * 128, u0:u0 + W], writes=[gk])
                    P.op('act', lambda e, g_=g_, i=i, oc=oc: e.activation(g_[:], g_[:], AF.Sigmoid, bias=smt[:, i * 8 + oc:i * 8 + oc + 1]),
                         reads=[gk, 'smt'], writes=[gk])
                    for kc in range(4):
                        P.op('pe', lambda e, pb=pb, i=i, kc=kc, ocs=ocs: e.matmul(pb[:], lhsT=wob[i][:, kc, ocs], rhs=br[i][:, kc, :],
                                                                                 start=(kc == 0), stop=(kc == 3)),
                             reads=[f'wob{i}', bk[i]], writes=[pk])
                    if i == 0:
                        P.op('dve', lambda e, pb=pb, g_=g_: e.tensor_tensor(mg[:], pb[:], g_[:], op=ALU.mult), reads=[pk, gk], writes=['mg'])
                    else:
                        P.op('dve', lambda e, pb=pb, g_=g_: e.tensor_tensor(g_[:], pb[:], g_[:], op=ALU.mult), reads=[pk, gk], writes=[gk])
                        if i < 3:
                            P.op('pool', lambda e, g_=g_: e.tensor_tensor(mg[:], mg[:], g_[:], op=ALU.add), reads=['mg', gk], writes=['mg'])
                        else:
                            P.op('pool', lambda e, g_=g_, oc=oc: e.tensor_tensor(mgb[:, oc, :], mg[:], g_[:], op=ALU.add),
                                 reads=['mg', gk], writes=[f'mgb{oc}'])
            mks = [f'mgb{oc}' for oc in range(8)]
            for oc in range(8):
                ocs = slice(oc * 128, (oc + 1) * 128)
                pb = ps[6 + oc % 2]; pk = f'ps{6 + oc % 2}'
                for kc in range(8):
                    P.op('pe', lambda e, pb=pb, kc=kc, ocs=ocs: e.matmul(pb[:], lhsT=wmb[:, kc, ocs], rhs=mgb[:, kc, :],
                                                                       start=(kc == 0), stop=(kc == 7)),
                         reads=['wmb'] + mks, writes=[pk])
                P.op('dve', lambda e, pb=pb, oc=oc: e.tensor_tensor(ot[:, oc, :], pb[:], xt[:, oc, :], op=ALU.add),
                     reads=[pk, 'xt'], writes=['ot'])
            P.dma('sp', xmT[:, u0:u0 + W].rearrange("(c p) t -> p c t", p=128), ot[:], reads=['ot'], writes=['xm'])
        stage_x(0)
        for tl in range(4):
            if tl + 1 < 4:
                stage_x(tl + 1)
            stage_y(tl)
        for q in range(4):
            P.dma('sp', tail2[q * 256:(q + 1) * 256, :].rearrange("r (b k) -> r b k", k=2),
                  xmT[q * 256:(q + 1) * 256, :].rearrange("r (b t) -> r b t", t=128)[:, :, 126:128], reads=['xm'])
        ns = P.emit()
        print("L3a: instrs", len(P.ins), "sems", ns, "waits", P.n_waits)


def build_l3b(nc, IO, pfx):
    xmT, GT2, sel_d, wg, wu, wd, sm, xoT = IO['xm'], IO['GT2'], IO['sel'], IO['wg'], IO['wu'], IO['wd'], IO['sm3b'], IO['xo']
    TT = 1024
    NB = 8
    NC_ = NB * 130
    P = Prog(nc)
    es = contextlib.ExitStack()
    sb = lambda name, shape, dt: es.enter_context(nc.sbuf_tensor(pfx + name, shape, dt))
    pst = lambda name, shape, dt=F32: es.enter_context(nc.psum_tensor(pfx + name, shape, dt))
    with es:
        smt = sb("smt", [128, 80], F32)
        selt = sb("selt", [128, 4], F32)
        ones = sb("ones", [128, 128], F32)
        xm = sb("xm", [128, 8, NB, 130], F32)
        cand = [sb(f"cand{i}", [128, 8, NB, 2], F32) for i in range(4)]
        sqt = sb("sqt", [128, NC_], F32)
        rs = sb("rs", [128, NC_], F32)
        h2 = sb("h2", [128, 8, NB, 130], BF16)
        prod = sb("prod", [128, NFF, TT], BF16)
        wgs = [sb(f"wgs{i}", [128, 8, 128], F32) for i in range(2)]
        wus = [sb(f"wus{i}", [128, 8, 128], F32) for i in range(2)]
        wgb = [sb(f"wgb{i}", [128, 8, 128], BF16) for i in range(2)]
        wub = [sb(f"wub{i}", [128, 8, 128], BF16) for i in range(2)]
        gtl = [sb(f"gtl{i}", [128, NB, 130], F32) for i in range(2)]
        av = [sb(f"av{i}", [128, TT], F32) for i in range(2)]
        wds = [sb(f"wds{i}", [128, 512], F32) for i in range(2)]
        wdb = [sb(f"wdb{i}", [128, 512], BF16) for i in range(2)]
        ot = [sb(f"ot{i}", [128, 512], F32) for i in range(2)]
        ps = [pst(f"ps{i}", [128, 512]) for i in range(8)]
        P.cc('pool', IO['cc_l3b'][0], IO['cc_l3b'][1], writes=['GT2'])
        P.dma('sp', smt[:], sm, writes=['smt'])
        P.dma('sp', selt[:], sel_d, writes=['selt'])
        P.op('dve', lambda e: e.memset(ones[:], 1.0 / 1024), writes=['ones'])
        wgv = wg.rearrange("(c p) n -> p c n", p=128)
        wuv = wu.rearrange("(c p) n -> p c n", p=128)
        xmf = xm[:].rearrange("p c b k -> p c (b k)")
        h2f = h2[:].rearrange("p c b k -> p c (b k)")
        for tl in range(2):
            u0 = tl * TT
            for c in range(8):
                P.dma('sp' if c % 2 == 0 else 'act', xm[:, c, :, 2:130],
                      xmT[c * 128:(c + 1) * 128, u0:u0 + TT].rearrange("p (b t) -> p b t", t=128), writes=['xm'])
            for r in range(3):
                P.dma('sp', cand[r][:], GT2[r * 1024:(r + 1) * 1024, tl * 16:(tl + 1) * 16].rearrange("(c p) (b k) -> p c b k", p=128, k=2),
                      reads=['GT2'], writes=[f'cand{r}'])
            if tl == 0:
                P.op('pool', lambda e: e.memset(cand[3][:], 0.0), writes=['cand3'])
                P.dma('act', cand[3][:, :, 1:8, :], GT2[3 * 1024:4 * 1024, 0:14].rearrange("(c p) (b k) -> p c b k", p=128, k=2), reads=['GT2'], writes=['cand3'])
            else:
                P.dma('act', cand[3][:], GT2[3 * 1024:4 * 1024, 14:30].rearrange("(c p) (b k) -> p c b k", p=128, k=2), reads=['GT2'], writes=['cand3'])
            _blend(P, xm[:].rearrange("p c b k -> p (c b) k")[:, :, 0:2], [c_[:].rearrange("p c b k -> p (c b) k") for c_ in cand],
                   [f'cand{r}' for r in range(4)], selt, 'xm')
            col_groups = [(0, 512), (512, 1024), (1024, NC_)]
            for (a, b) in col_groups:
                n = b - a
                for c in range(8):
                    P.op('act', lambda e, c=c, a=a, b=b: e.activation(sqt[:, a:b], xmf[:, c, a:b], AF.Square), reads=['xm'], writes=['sqt'])
                    P.op('pe', lambda e, c=c, a=a, b=b, n=n: e.matmul(ps[0][:, 0:n], lhsT=ones[:], rhs=sqt[:, a:b], start=(c == 0), stop=(c == 7)),
                         reads=['ones', 'sqt'], writes=['ps0'])
                P.op('dve', lambda e, a=a, b=b, n=n: e.tensor_scalar(rs[:, a:b], ps[0][:, 0:n], 1.0, EPS, op0=ALU.mult, op1=ALU.add),
                     reads=['ps0'], writes=['rs'])
            P.op('act', lambda e: e.activation(rs[:], rs[:], AF.Sqrt), reads=['rs'], writes=['rs'])
            P.op('dve', lambda e: e.reciprocal(rs[:], rs[:]), reads=['rs'], writes=['rs'])
            for c in range(8):
                P.op('dve', lambda e, c=c: e.scalar_tensor_tensor(h2f[:, c, :], xmf[:, c, :], smt[:, c:c + 1], rs[:],
                                                                 op0=ALU.mult, op1=ALU.mult),
                     reads=['xm', 'smt', 'rs'], writes=[f'h2_{c}'])
            hk = [f'h2_{c}' for c in range(8)]
            for f in range(NFF):
                b = f % 2
                fs = slice(f * 128, (f + 1) * 128)
                P.dma('sp', wgs[b][:], wgv[:, :, fs], writes=[f'wgs{b}'])
                P.dma('act', wus[b][:], wuv[:, :, fs], writes=[f'wus{b}'])
                P.op('dve', lambda e, b=b: e.tensor_copy(wgb[b][:], wgs[b][:]), reads=[f'wgs{b}'], writes=[f'wgb{b}'])
                P.op('pool', lambda e, b=b: e.tensor_copy(wub[b][:], wus[b][:]), reads=[f'wus{b}'], writes=[f'wub{b}'])
                g_ = gtl[b]; gk = f'gtl{b}'
                gf = g_[:].rearrange("p b k -> p (b k)")
                a_ = av[b]; ak = f'av{b}'
                a3 = a_[:].rearrange("p (b t) -> p b t", t=128)
                for gi_, (a, bb) in enumerate(col_groups):
                    n = bb - a
                    pb = ps[1 + gi_]; pk = f'ps{1 + gi_}'
                    for c in range(8):
                        P.op('pe', lambda e, pb=pb, c=c, a=a, bb=bb, n=n, b=b: e.matmul(pb[:, 0:n], lhsT=wgb[b][:, c, :], rhs=h2f[:, c, a:bb],
                                                                                     start=(c == 0), stop=(c == 7)),
                             reads=[f'wgb{b}'] + hk, writes=[pk])
                    P.op('act', lambda e, pb=pb, a=a, bb=bb, n=n, gf=gf: e.copy(gf[:, a:bb], pb[:, 0:n]), reads=[pk], writes=[gk])
                P.op('dve', lambda e, f=f, g_=g_, a3=a3: e.tensor_scalar(a3, g_[:, :, 0:128], smt[:, 8 + 3 * f:9 + 3 * f], None, op0=ALU.mult),
                     reads=[gk, 'smt'], writes=[ak])
                for k in (1, 2):
                    P.op('dve', lambda e, f=f, g_=g_, a3=a3, k=k: e.scalar_tensor_tensor(
                        a3, g_[:, :, k:k + 128], smt[:, 8 + 3 * f + k:9 + 3 * f + k], a3, op0=ALU.mult, op1=ALU.add),
                         reads=[gk, 'smt', ak], writes=[ak])
                P.op('act', lambda e, a_=a_: e.activation(a_[:], a_[:], AF.Silu), reads=[ak], writes=[ak])
                for gi_ in range(2):
                    pb = ps[4 + gi_]; pk = f'ps{4 + gi_}'
                    for c in range(8):
                        P.op('pe', lambda e, pb=pb, c=c, gi_=gi_, b=b: e.matmul(pb[:], lhsT=wub[b][:, c, :], rhs=h2[:, c, gi_ * 4:(gi_ + 1) * 4, 2:130],
                                                                              start=(c == 0), stop=(c == 7)),
                             reads=[f'wub{b}'] + hk, writes=[pk])
                    P.op('dve', lambda e, pb=pb, gi_=gi_, f=f, a_=a_: e.tensor_tensor(
                        prod[:, f, gi_ * 512:(gi_ + 1) * 512], a_[:, gi_ * 512:(gi_ + 1) * 512], pb[:], op=ALU.mult), reads=[pk, ak], writes=[f'prod{f}'])
            pks = [f'prod{f}' for f in range(NFF)]
            di = 0
            for og in range(2):
                for f in range(NFF):
                    b = di % 2; di += 1
                    P.dma('sp' if di % 2 else 'act', wds[b][:], wd[f * 128:(f + 1) * 128, og * 512:(og + 1) * 512], writes=[f'wds{b}'])
                    P.op('pool' if di % 2 else 'dve', lambda e, b=b: e.tensor_copy(wdb[b][:], wds[b][:]), reads=[f'wds{b}'], writes=[f'wdb{b}'])
                    for th in range(2):
                        tsl = slice(th * 512, (th + 1) * 512)
                        for o4 in range(4):
                            pi = th * 4 + o4
                            P.op('pe', lambda e, o4=o4, pi=pi, b=b, f=f, tsl=tsl: e.matmul(
                                ps[pi][:], lhsT=wdb[b][:, o4 * 128:(o4 + 1) * 128], rhs=prod[:, f, tsl],
                                start=(f == 0), stop=(f == NFF - 1)),
                                 reads=[f'wdb{b}'] + pks, writes=[f'ps{pi}'])
                for th in range(2):
                    for o4 in range(4):
                        pi = th * 4 + o4
                        oc = og * 4 + o4
                        o_ = ot[o4 % 2]; ok = f'ot{o4 % 2}'
                        P.op('dve', lambda e, pi=pi, oc=oc, o_=o_, th=th: e.tensor_tensor(
                            o_[:].rearrange("p (b t) -> p b t", t=128), ps[pi][:].rearrange("p (b t) -> p b t", t=128),
                            xm[:, oc, th * 4:(th + 1) * 4, 2:130], op=ALU.add),
                             reads=[f'ps{pi}', 'xm'], writes=[ok])
                        P.dma('sp' if o4 % 2 == 0 else 'act', xoT[oc * 128:(oc + 1) * 128, u0 + th * 512:u0 + (th + 1) * 512], o_[:], reads=[ok])
        ns = P.emit()
        print("L3b: instrs", len(P.ins), "sems", ns, "waits", P.n_waits)


RG = [[0, 1, 2, 3], [4, 5, 6, 7]]


def _allgather(nc, pairs):
    s = nc.alloc_semaphore(name=nc.make_name("cc_sem", True))
    with nc.Block() as block:
        @block.gpsimd
        def _(g):
            for (src, dst) in pairs:
                g.collective_compute("AllGather", mybir.AluOpType.bypass, replica_groups=RG,
                                     ins=[src.ap().opt()], outs=[dst.ap().opt()]).then_inc(s)
            g.wait_ge(s, len(pairs))
    nc.clear_and_free_semaphores([s])
    nc.all_engine_barrier()


def build_fused(nlayers=2, debug=False):
    nc = bass.Bass("TRN2", target_bir_lowering=False)
    ext = lambda name, shape, dt: nc.dram_tensor(name, shape, dt, kind="ExternalInput").ap()
    xT = ext("xT", [1024, 2048], F32)
    pos = ext("pos", [128, 2048], I32)
    w_in = ext("w_in", [2, 1024, 10052], F32)
    gmix = ext("gmix", [2, 128, 8], F32)
    gq = ext("gq", [2, 128, 4], F32)
    cst = ext("cst", [128, 8], F32)
    rmA = ext("rmA", [128, 128], F32)
    rmI = ext("rmI", [128, 128], F32)
    negm = ext("negm", [128, 512], F32)
    mdm = ext("mdm", [128, 4, 4, 128], F32)
    su = ext("su", [128, 128], F32)
    ident = ext("ident", [128, 128], F32)
    pw = ext("pw", [128, NIT], F32)
    sel = ext("sel", [128, 4], F32)
    wo = ext("wo", [2, 4, 512, 1024], F32)
    wm = ext("wm", [2, 1024, 1024], F32)
    sm3a = ext("sm3a", [2, 128, 192], F32)
    wg = ext("wg", [2, 1024, 2816], F32)
    wu = ext("wu", [2, 1024, 2816], F32)
    wd = ext("wd", [2, 2816, 1024], F32)
    sm3b = ext("sm3b", [2, 128, 80], F32)
    out = nc.dram_tensor("out", [1024, 2048], F32, kind="ExternalOutput").ap()
    dk = dict(kind="ExternalOutput") if debug else {}
    o_q = nc.dram_tensor("o_q", [1280, 2048], BF16, **dk)
    o_f32 = nc.dram_tensor("o_f32", [6656, 2048], F32, **dk)
    o_iw = nc.dram_tensor("o_iw", [2048, 4], F32, **dk)
    krows = [0] + [64 + 128 * h for h in range(4)] + [576 + 128 * h for h in range(4)]
    o_k = {r0: nc.dram_tensor(f"o_k{r0}", [64 if r0 == 0 else 128, 2048], BF16) for r0 in krows}
    G_k = {r0: nc.dram_tensor(f"G_k{r0}", [4 * (64 if r0 == 0 else 128), 2048], BF16) for r0 in krows}
    o_v = [nc.dram_tensor(f"o_v{q}", [256, 1024], BF16) for q in range(8)]
    G_v = [nc.dram_tensor(f"G_v{q}", [4 * 256, 1024], BF16) for q in range(8)]
    tail1 = [nc.dram_tensor(f"tail1_{q}", [256, 512], F32) for q in range(8)]
    GT1 = [nc.dram_tensor(f"GT1_{q}", [4 * 256, 512], F32) for q in range(8)]
    yaT = nc.dram_tensor("yaT", [512, 2048], F32, **dk)
    ydT = nc.dram_tensor("ydT", [512, 2048], F32, **dk)
    xm = nc.dram_tensor("xm", [1024, 2048], F32, **dk)
    tail2 = nc.dram_tensor("tail2", [1024, 32], F32)
    GT2 = nc.dram_tensor("GT2", [4 * 1024, 32], F32)
    xo0 = nc.dram_tensor("xo0", [1024, 2048], F32)
    for l in range(nlayers):
        x_in = xT if l == 0 else xo0.ap()
        IOD = dict(xT=x_in, pos=pos, w=w_in[l], gmix=gmix[l], gq=gq[l], cst=cst, rmA=rmA, rmI=rmI,
                 o_q=o_q.ap(), o_k={k: v.ap() for k, v in o_k.items()}, o_f32=o_f32.ap(), o_v=[v.ap() for v in o_v], o_iw=o_iw.ap(), tail1=[v.ap() for v in tail1],
                 G_k={k: v.ap() for k, v in G_k.items()}, G_v=[v.ap() for v in G_v], GT1=[v.ap() for v in GT1], negm=negm, mdm=mdm, su=su, ident=ident, pw=pw, sel=sel,
                 yaT=yaT.ap(), ydT=ydT.ap(), wo=wo[l], wm=wm[l], sm3a=sm3a[l], xm=xm.ap(), tail2=tail2.ap(), GT2=GT2.ap(),
                 wg=wg[l], wu=wu[l], wd=wd[l], sm3b=sm3b[l], xo=(xo0.ap() if l < nlayers - 1 else out))
        IOD['cc_l2'] = ([(o_k[r0], G_k[r0], f'Gk{r0}') for r0 in krows[5:]] + [(o_v[q], G_v[q], f'Gv{q}') for q in range(8)]
                        + [(o_k[r0], G_k[r0], f'Gk{r0}') for r0 in krows[:5]] + [(tail1[q], GT1[q], f'GT1_{q}') for q in range(8)])
        IOD['cc_l3b'] = (tail2, GT2)
        build_l1(nc, IOD, f"a{l}_")
        build_l2(nc, IOD, f"b{l}_")
        build_l3a(nc, IOD, f"c{l}_")
        build_l3b(nc, IOD, f"d{l}_")
    return nc


_NC = {}


def _stripe(a, j):
    return a.reshape((64, 128) + a.shape[1:])[j::4].reshape((2048,) + a.shape[1:])


def kernel(_nlayers=2, _debug=False, **inputs):
    inp = {k: np.asarray(v) for k, v in inputs.items()}
    if 'nc' not in _NC:
        _NC['nc'] = build_fused(_nlayers, _debug)
    nc = _NC['nc']
    cst, RmA, RmI = l1_consts()
    su, ident, pw = l2_consts()
    gq = np.zeros((2, 128, 4), np.float32)
    gq[:, :, 0] = inp['g_qa']; gq[:, :, 1] = inp['g_ka']; gq[:, :64, 2] = inp['g_kidx']
    gmix = np.ascontiguousarray(inp['g_mix'].reshape(2, 8, 128).transpose(0, 2, 1))
    sm3a = np.zeros((2, 128, 192), np.float32)
    sm3b = np.zeros((2, 128, 80), np.float32)
    for l in range(2):
        sm3a[l, :, 0:32] = inp['b_gate'][l].reshape(32, 128).T
        sm3a[l, :, 32:156] = inp['cb_conv_w'][l].T.reshape(4, 128, 31).transpose(1, 0, 2).reshape(128, 124)
        sm3a[l, :, 156:160] = inp['cb_conv_b'][l].reshape(4, 128).T
        sm3a[l, :, 160:164] = inp['cb_ln_g'][l].reshape(4, 128).T
        sm3a[l, :, 164:168] = inp['cb_ln_b'][l].reshape(4, 128).T
        sm3a[l, :, 168:180] = inp['cc_conv_w'][l].T.reshape(4, 128, 3).transpose(1, 0, 2).reshape(128, 12)
        sm3b[l, :, 0:8] = inp['g_ffn'][l].reshape(8, 128).T
        sm3b[l, :, 8:74] = inp['ffn_conv_w'][l].T.reshape(22, 128, 3).transpose(1, 0, 2).reshape(128, 66)
    wo = np.ascontiguousarray(np.stack([inp['w_oa'], inp['w_ob'], inp['w_oc'], inp['w_od']], axis=1))
    shared = dict(w_in=np.ascontiguousarray(inp['w_in']), gmix=gmix, gq=gq, cst=cst, rmA=RmA, rmI=RmI, su=su, ident=ident, pw=pw,
                  wo=wo, wm=np.ascontiguousarray(inp['w_merge']), sm3a=sm3a, wg=np.ascontiguousarray(inp['w_ffn_gate']),
                  wu=np.ascontiguousarray(inp['w_ffn_up']), wd=np.ascontiguousarray(inp['w_ffn_down']), sm3b=sm3b)
    maps = []
    for c in range(8):
        b, j = c // 4, c % 4
        neg, md = l2_masks(j)
        sel = np.zeros((128, 4), np.float32)
        sel[:, (j - 1) % 4] = 1.0
        m = dict(shared)
        m.update(xT=np.ascontiguousarray(_stripe(inp['x'][b], j).T),
                 pos=np.ascontiguousarray(np.broadcast_to(_stripe(inp['positions'][b], j)[None, :], (128, 2048)).astype(np.int32)),
                 negm=neg, mdm=md, sel=sel)
        maps.append(m)
    res = run_bass_kernel_spmd(nc, maps, core_ids=list(range(8)))
    if _debug:
        _NC['res'] = res.results
    out = np.zeros((2, 64, 128, 1024), np.float32)
    for c in range(8):
        b, j = c // 4, c % 4
        out[b, j::4] = np.asarray(res.results[c]['out']).T.reshape(16, 128, 1024)
    return out.reshape(2, 8192, 1024)
```

```python
import math
import contextlib
import numpy as np
from concourse.bass_utils import run_bass_kernel_spmd
import numpy as np
import concourse.bass as bass
import concourse.mybir as mybir

F32 = mybir.dt.float32
BF16 = mybir.dt.bfloat16
I32 = mybir.dt.int32
ALU = mybir.AluOpType
AF = mybir.ActivationFunctionType
AX = mybir.AxisListType

SEM_EPOCH = 30000


class Prog:
    ENGS = ('pe', 'act', 'dve', 'pool', 'sp')

    def __init__(self, nc, n_dma_sems=6):
        self.nc = nc
        self.ins = []
        self.stream = {e: [] for e in self.ENGS}
        self.last_w = {}
        self.readers = {}
        self.n_dma_sems = n_dma_sems
        self.dma_rr = {e: 0 for e in self.ENGS}
        self.dma_slot_last = {}

    def _deps(self, eng, reads, writes, is_dma):
        deps = set()
        for k in reads:
            w = self.last_w.get(k)
            if w is not None:
                deps.add(w)
        for k in writes:
            w = self.last_w.get(k)
            if w is not None:
                deps.add(w)
            for r in self.readers.get(k, ()):
                deps.add(r)
        out = []
        for d in deps:
            di = self.ins[d]
            if (not is_dma) and (not di['dma']) and di['eng'] == eng:
                if eng == 'pe':
                    continue
            out.append(d)
        return out

    def _commit(self, iid, reads, writes):
        for k in reads:
            self.readers.setdefault(k, []).append(iid)
        for k in writes:
            self.last_w[k] = iid
            self.readers[k] = []

    def op(self, eng, fn, reads=(), writes=()):
        reads = list(reads); writes = list(writes)
        deps = self._deps(eng, reads, writes, False)
        iid = len(self.ins)
        self.ins.append(dict(eng=eng, fn=fn, deps=deps, dma=False, signal=False))
        self.stream[eng].append(iid)
        self._commit(iid, reads, writes)
        return iid

    def dma(self, eng, out, in_, reads=(), writes=(), **kw):
        reads = list(reads); writes = list(writes)
        deps = self._deps(eng, reads, writes, True)
        slot = self.dma_rr[eng] % self.n_dma_sems
        self.dma_rr[eng] += 1
        prev = self.dma_slot_last.get((eng, slot))
        if prev is not None and prev not in deps:
            deps.append(prev)
        iid = len(self.ins)
        self.ins.append(dict(eng=eng, fn=None, deps=deps, dma=True, signal=True,
                             slot=slot, out=out, in_=in_, kw=kw))
        self.dma_slot_last[(eng, slot)] = iid
        self.stream[eng].append(iid)
        self._commit(iid, reads, writes)
        return iid

    def cc(self, eng, src, dst, reads=(), writes=()):
        reads = list(reads); writes = list(writes)
        deps = self._deps(eng, reads, writes, True)
        n = self.dma_rr.get((eng, 'cc'), 0)
        self.dma_rr[(eng, 'cc')] = n + 1
        slot = 100 + n % 4
        prev = self.dma_slot_last.get((eng, slot))
        if prev is not None and prev not in deps:
            deps.append(prev)
        iid = len(self.ins)
        self.ins.append(dict(eng=eng, fn=None, deps=deps, dma=True, signal=True, slot=slot, cc=(src, dst), inc=1))
        self.dma_slot_last[(eng, slot)] = iid
        self.stream[eng].append(iid)
        self._commit(iid, reads, writes)
        return iid

    def emit(self, final_wait_eng='sp'):
        nc = self.nc
        ins = self.ins
        pos = {}
        for e in self.ENGS:
            for p, iid in enumerate(self.stream[e]):
                pos[iid] = p
        def chan(i):
            d = ins[i]
            return ('d', d['eng'], d['slot']) if d['dma'] else ('c', d['eng'])
        need = {}
        for e in self.ENGS:
            waited = {}
            for iid in self.stream[e]:
                lst = []
                for d in sorted(ins[iid]['deps'], key=lambda x: -pos[x]):
                    c = chan(d)
                    if waited.get(c, -1) >= pos[d]:
                        continue
                    waited[c] = pos[d]
                    ins[d]['signal'] = True
                    lst.append(d)
                need[iid] = lst
        final = list(self.dma_slot_last.values())
        semcount = {}
        sems = {}
        stack = []

        def get_sem(key):
            if key not in sems:
                s = nc.alloc_semaphore(name=nc.make_name("s_" + "_".join(str(k) for k in key), True))
                sems[key] = s
            return sems[key]

        for e in self.ENGS:
            ccount = 0
            dcount = {}
            for iid in self.stream[e]:
                d = ins[iid]
                if d['dma']:
                    sl = d['slot']
                    dcount[sl] = dcount.get(sl, 0) + 16
                    d['semkey'] = ('d', e, sl, dcount[sl] // (SEM_EPOCH * 16 + 16))
                    d['semval'] = dcount[sl] - d['semkey'][3] * (SEM_EPOCH * 16 + 16) if False else None
                elif d['signal']:
                    ccount += 1
                    ep = (ccount - 1) // SEM_EPOCH
                    d['semkey'] = ('c', e, ep)
                    d['semval'] = ccount - ep * SEM_EPOCH
            dc2 = {}
            for iid in self.stream[e]:
                d = ins[iid]
                if d['dma']:
                    sl = d['slot']
                    n = dc2.get(sl, 0) + 1
                    dc2[sl] = n
                    ep = (n - 1) // 2000
                    d['semkey'] = ('d', e, sl, ep)
                    d['semval'] = d.get('inc', 16) * (n - ep * 2000)
        engobj = {'pe': 'tensor', 'act': 'scalar', 'dve': 'vector', 'pool': 'gpsimd', 'sp': 'sync'}
        self.n_waits = 0

        def run_stream(e, eng):
            for iid in self.stream[e]:
                d = ins[iid]
                for dep in need[iid]:
                    dd = ins[dep]
                    eng.wait_ge(get_sem(dd['semkey']), dd['semval'])
                    self.n_waits += 1
                if d['dma'] and 'cc' in d:
                    eng.collective_compute("AllGather", mybir.AluOpType.bypass, replica_groups=[[0, 1, 2, 3], [4, 5, 6, 7]],
                                           ins=[d['cc'][0].ap().opt()], outs=[d['cc'][1].ap().opt()]).then_inc(get_sem(d['semkey']))
                elif d['dma']:
                    eng.dma_start(out=d['out'], in_=d['in_'], **d['kw']).then_inc(
                        get_sem(d['semkey']), 16)
                else:
                    r = d['fn'](eng)
                    if d['signal']:
                        r.then_inc(get_sem(d['semkey']), 1)
            if e == final_wait_eng:
                for f in final:
                    dd = ins[f]
                    eng.wait_ge(get_sem(dd['semkey']), dd['semval'])

        for e in self.ENGS:
            for iid in self.stream[e]:
                d = ins[iid]
                if d['dma'] or d['signal']:
                    get_sem(d['semkey'])
        with nc.Block() as block:
            for e in self.ENGS:
                if not self.stream[e] and e != final_wait_eng:
                    continue
                deco = getattr(block, engobj[e])
                deco(lambda eng, e=e: run_stream(e, eng))
        nc.clear_and_free_semaphores(list(sems.values()))
        nc.all_engine_barrier()
        return len(sems)


T = 2048
D = 1024
N_IN = 10052
EPS = 1e-6
PI = math.pi
SINK = 0.999999

O_AQ, O_AK, O_AV, O_IQ, O_IK, O_IW, O_GLU, O_CC, O_SQ, O_SK, O_SV, O_GATE = (
    0, 512, 1024, 1536, 1792, 1856, 1860, 2884, 4420, 4932, 5444, 5956)
R_QA, R_QI, R_SQ, NR_Q = 0, 512, 768, 1280
R_KI, R_KA, R_SK, NR_K = 0, 64, 576, 1088
R_GLU, R_CC, R_GATE, NR_F32 = 0, 1024, 2560, 6656


def l1_items():
    items = []
    for h in range(4):
        items.append(('qkA', O_AQ + 128 * h, 128, R_QA + 128 * h, 0))
    for h in range(4):
        items.append(('qkA', O_AK + 128 * h, 128, R_KA + 128 * h, 1))
    items.append(('tmv', O_AV, 256, 0, 0))
    items.append(('tmv', O_AV + 256, 256, 256, 0))
    items.append(('qi', O_IQ, 128, R_QI, 0))
    items.append(('qi', O_IQ + 128, 128, R_QI + 128, 0))
    items.append(('ki', O_IK, 64, R_KI, 0))
    items.append(('iw', O_IW, 4, 0, 0))
    for j in range(8):
        items.append(('f32', O_GLU + 128 * j, 128, R_GLU + 128 * j, 0))
    for j in range(12):
        items.append(('f32', O_CC + 128 * j, 128, R_CC + 128 * j, 0))
    for j in range(4):
        items.append(('bf', O_SQ + 128 * j, 128, R_SQ + 128 * j, 0))
    for j in range(4):
        items.append(('bf', O_SK + 128 * j, 128, R_SK + 128 * j, 0))
    items.append(('tmv', O_SV, 256, 512, 0))
    items.append(('tmv', O_SV + 256, 256, 768, 0))
    for j in range(32):
        items.append(('f32', O_GATE + 128 * j, 128, R_GATE + 128 * j, 0))
    groups = []
    cur = []
    for it in items:
        if cur and (it[1] + it[2] - cur[0][1]) > 256:
            groups.append(cur); cur = []
        cur.append(it)
    if cur:
        groups.append(cur)
    return groups


def l1_consts():
    half = 16
    invA = (500000.0 ** (-(np.arange(half, dtype=np.float32) * 2.0) / 32)).astype(np.float32)
    invI = (500000.0 ** (-(np.arange(8, dtype=np.float32) * 2.0) / 16)).astype(np.float32)
    cst = np.zeros((128, 8), np.float32)
    for p in range(32):
        cst[p, 0] = invA[p % 16]
    for p in range(128):
        if p % 64 < 16:
            cst[p, 1] = invI[p % 8]
    cst[:, 3] = 0.0
    cst[:, 4] = 0.5 * PI * SINK
    RmA = np.zeros((128, 128), np.float32)
    for m in range(16):
        RmA[m + 16, m] = -1.0
        RmA[m, m + 16] = 1.0
    RmI = np.zeros((128, 128), np.float32)
    for b in (0, 64):
        for m in range(8):
            RmI[b + m + 8, b + m] = -1.0
            RmI[b + m, b + m + 8] = 1.0
    return cst, RmA, RmI


def build_l1(nc, IO, pfx):
    xT, pos, w, gmix, gq, cst, rmA, rmI = IO['xT'], IO['pos'], IO['w'], IO['gmix'], IO['gq'], IO['cst'], IO['rmA'], IO['rmI']
    o_q, o_k, o_f32, o_v, o_iw, tail1 = IO['o_q'], IO['o_k'], IO['o_f32'], IO['o_v'], IO['o_iw'], IO['tail1']

    P = Prog(nc)
    import contextlib
    es = contextlib.ExitStack()

    def sb(name, shape, dt):
        return es.enter_context(nc.sbuf_tensor(pfx + name, shape, dt))

    def pst(name, shape, dt=F32):
        return es.enter_context(nc.psum_tensor(pfx + name, shape, dt))

    with es:
        hT = sb("hT", [128, 8, T], BF16)
        xs = [sb(f"xs{i}", [128, 8, 256], F32) for i in range(2)]
        sqs = sb("sqs", [128, 8, 256], F32)
        rs = sb("rs", [128, 256], F32)
        gm = sb("gm", [128, 8], F32)
        gqt = sb("gqt", [128, 4], F32)
        cs = sb("cs", [128, 8], F32)
        rA = sb("rA", [128, 128], F32)
        rI = sb("rI", [128, 128], F32)
        ones = sb("ones", [128, 128], F32)
        posi = sb("posi", [128, T], I32)
        posf = sb("posf", [128, T], F32)
        ang = sb("ang", [128, T], F32)
        posf2 = sb("posf2", [128, T], F32)
        posi2 = sb("posi2", [128, T], I32)
        cosA = sb("cosA", [128, T], F32)
        sinA = sb("sinA", [128, T], F32)
        cosI = sb("cosI", [128, T], F32)
        sinI = sb("sinI", [128, T], F32)
        wf = [sb(f"wf{i}", [128, 8, 256], F32) for i in range(2)]
        wb = [sb(f"wb{i}", [128, 8, 256], BF16) for i in range(2)]
        of32 = [sb(f"of{i}", [128, T], F32) for i in range(2)]
        obf = [sb(f"ob{i}", [128, T], BF16) for i in range(2)]
        ov = [sb(f"ov{i}", [128, 16, 256], BF16) for i in range(2)]
        oiw = sb("oiw", [128, 16, 4], F32)
        raw = sb("raw", [128, 512], F32)
        sq = sb("sq", [128, 512], F32)
        rstd = sb("rstd", [128, 512], F32)
        nrm = sb("nrm", [128, 512], F32)
        t1 = sb("t1", [128, 512], F32)
        t2 = sb("t2", [128, 512], F32)
        psm = [pst(f"psm{i}", [128, 512]) for i in range(4)]
        psn = pst("psn", [128, 512])
        psr = pst("psr", [128, 512])

        P.dma('sp', gm[:], gmix, writes=['gm'])
        P.dma('sp', gqt[:], gq, writes=['gqt'])
        P.dma('sp', cs[:], cst, writes=['cs'])
        P.dma('sp', rA[:], rmA, writes=['rA'])
        P.dma('sp', rI[:], rmI, writes=['rI'])
        P.dma('sp', posi[:], pos, writes=['posi'])
        P.op('dve', lambda e: e.memset(ones[:], 1.0), writes=['ones'])
        P.op('dve', lambda e: e.tensor_copy(posf[:], posi[:]), reads=['posi'], writes=['posf'])
        for j, (ct, st) in enumerate(((cosA, sinA), (cosI, sinI))):
            cn, sn = f"cos{j}", f"sin{j}"
            P.op('dve', lambda e, j=j: e.tensor_scalar(ang[:], posf[:], cs[:, j:j + 1], None, op0=ALU.mult),
                 reads=['posf', 'cs'], writes=['ang'])
            for (tt_, tn, shift, bcol) in ((st, sn, 0.0, 3), (ct, cn, 0.5 * PI, 4)):
                P.op('dve', lambda e, shift=shift: e.tensor_scalar(posf2[:], ang[:], 1.0 / (2 * PI), 0.5 + shift / (2 * PI),
                                                                   op0=ALU.mult, op1=ALU.add),
                     reads=['ang'], writes=['posf2'])
                P.op('dve', lambda e: e.tensor_copy(posi2[:], posf2[:]), reads=['posf2'], writes=['posi2'])
                P.op('dve', lambda e: e.tensor_copy(posf2[:], posi2[:]), reads=['posi2'], writes=['posf2'])
                P.op('dve', lambda e, tt_=tt_: e.scalar_tensor_tensor(tt_[:], posf2[:], -2 * PI, ang[:], op0=ALU.mult, op1=ALU.add),
                     reads=['posf2', 'ang'], writes=[tn])
                P.op('dve', lambda e, tt_=tt_, shift=shift: e.tensor_scalar(posf2[:], tt_[:], -PI - shift, 2 * PI,
                                                                          op0=ALU.is_lt, op1=ALU.mult),
                     reads=[tn], writes=['posf2'])
                P.op('dve', lambda e, tt_=tt_: e.tensor_tensor(tt_[:], tt_[:], posf2[:], op=ALU.add),
                     reads=[tn, 'posf2'], writes=[tn])
                P.op('act', lambda e, tt_=tt_, bcol=bcol: e.activation(tt_[:], tt_[:], AF.Sin, bias=cs[:, bcol:bcol + 1], scale=SINK),
                     reads=[tn, 'cs'], writes=[tn])

        xTv = xT.rearrange("(c p) t -> p c t", p=128)
        for tg in range(8):
            xb = xs[tg % 2]
            xk = f"xs{tg % 2}"
            P.dma('sp', xb[:], xTv[:, :, tg * 256:(tg + 1) * 256], writes=[xk])
            P.op('act', lambda e, xb=xb: e.activation(sqs[:], xb[:], AF.Square), reads=[xk], writes=['sqs'])
            for c in range(8):
                P.op('pe', lambda e, c=c: e.matmul(psn[:, 0:256], lhsT=ones[:], rhs=sqs[:, c, :],
                                                   start=(c == 0), stop=(c == 7)),
                     reads=['ones', 'sqs'], writes=['psn'])
            P.op('dve', lambda e: e.tensor_scalar(rs[:], psn[:, 0:256], 1.0 / D, EPS, op0=ALU.mult, op1=ALU.add),
                 reads=['psn'], writes=['rs'])
            P.op('act', lambda e: e.activation(rs[:], rs[:], AF.Sqrt), reads=['rs'], writes=['rs'])
            P.op('dve', lambda e: e.reciprocal(rs[:], rs[:]), reads=['rs'], writes=['rs'])
            for c in range(8):
                P.op('dve', lambda e, c=c, xb=xb, tg=tg: e.scalar_tensor_tensor(
                    hT[:, c, tg * 256:(tg + 1) * 256], xb[:, c, :], gm[:, c:c + 1], rs[:],
                    op0=ALU.mult, op1=ALU.mult),
                     reads=[xk, 'gm', 'rs'], writes=[f'hT{tg}'])
        hkeys = [f'hT{tg}' for tg in range(8)]

        wv = w.rearrange("(c p) n -> p c n", p=128)
        groups = l1_items()
        cnt = dict(ps=0, of=0, ob=0, ov=0)
        for gi, grp in enumerate(groups):
            c0 = grp[0][1]
            c1 = grp[-1][1] + grp[-1][2]
            nco = c1 - c0
            b = gi % 2
            P.dma('sp', wf[b][:, :, 0:nco], wv[:, :, c0:c1], writes=[f'wf{b}'])
            ceng = 'pool' if gi % 2 == 0 else 'dve'
            P.op(ceng, lambda e, b=b, nco=nco: e.tensor_copy(wb[b][:, :, 0:nco], wf[b][:, :, 0:nco]),
                 reads=[f'wf{b}'], writes=[f'wb{b}'])
            for (kind, col, ncol, orow, aux) in grp:
                lo = col - c0
                if kind in ('tmv', 'iw'):
                    if kind == 'tmv':
                        ob_ = ov[cnt['ov'] % 2]; okey = f"ov{cnt['ov'] % 2}"; cnt['ov'] += 1
                    else:
                        ob_ = oiw; okey = 'oiw'
                    for tt in range(16):
                        pi = cnt['ps'] % 4; cnt['ps'] += 1
                        for c in range(8):
                            P.op('pe', lambda e, pi=pi, c=c, tt=tt, lo=lo, ncol=ncol, b=b: e.matmul(
                                psm[pi][:, 0:ncol], lhsT=hT[:, c, tt * 128:(tt + 1) * 128],
                                rhs=wb[b][:, c, lo:lo + ncol], start=(c == 0), stop=(c == 7)),
                                 reads=hkeys + [f'wb{b}'], writes=[f'psm{pi}'])
                        ee = 'act' if tt % 2 == 0 else 'dve'
                        if ee == 'act':
                            P.op('act', lambda e, pi=pi, tt=tt, ncol=ncol, ob_=ob_: e.copy(ob_[:, tt, 0:ncol], psm[pi][:, 0:ncol]),
                                 reads=[f'psm{pi}'], writes=[okey])
                        else:
                            P.op('dve', lambda e, pi=pi, tt=tt, ncol=ncol, ob_=ob_: e.tensor_copy(ob_[:, tt, 0:ncol], psm[pi][:, 0:ncol]),
                                 reads=[f'psm{pi}'], writes=[okey])
                    if kind == 'tmv':
                        for q in range(8):
                            P.dma('act', o_v[q].rearrange("(tt p) c -> p tt c", p=128)[:, :, orow:orow + 256], ob_[:, 2 * q:2 * q + 2, :], reads=[okey])
                    else:
                        P.dma('act', o_iw.rearrange("(tt p) c -> p tt c", p=128), ob_[:], reads=[okey])
                    continue
                if kind == 'f32':
                    ob_ = of32[cnt['of'] % 2]; okey = f"of{cnt['of'] % 2}"; cnt['of'] += 1
                else:
                    ob_ = obf[cnt['ob'] % 2]; okey = f"ob{cnt['ob'] % 2}"; cnt['ob'] += 1
                for g in range(4):
                    pi = cnt['ps'] % 4; cnt['ps'] += 1
                    ts_ = slice(g * 512, (g + 1) * 512)
                    for c in range(8):
                        P.op('pe', lambda e, pi=pi, c=c, lo=lo, ncol=ncol, b=b, ts_=ts_: e.matmul(
                            psm[pi][0:ncol, :], lhsT=wb[b][:, c, lo:lo + ncol], rhs=hT[:, c, ts_],
                            start=(c == 0), stop=(c == 7)),
                             reads=hkeys + [f'wb{b}'], writes=[f'psm{pi}'])
                    pk = f'psm{pi}'
                    if kind in ('f32', 'bf'):
                        if g % 2 == 0:
                            P.op('act', lambda e, pi=pi, ts_=ts_, ob_=ob_: e.copy(ob_[:, ts_], psm[pi][:]),
                                 reads=[pk], writes=[okey])
                        else:
                            P.op('dve', lambda e, pi=pi, ts_=ts_, ob_=ob_: e.tensor_copy(ob_[:, ts_], psm[pi][:]),
                                 reads=[pk], writes=[okey])
                        continue
                    np_ = ncol
                    if kind in ('qkA', 'ki'):
                        gcol = aux if kind == 'qkA' else 2
                        P.op('act', lambda e, pi=pi, np_=np_: e.copy(raw[0:np_, :], psm[pi][0:np_, :]), reads=[pk], writes=['raw'])
                        P.op('act', lambda e, pi=pi, np_=np_: e.activation(sq[0:np_, :], psm[pi][0:np_, :], AF.Square),
                             reads=[pk], writes=['sq'])
                        P.op('pe', lambda e, np_=np_: e.matmul(psn[0:np_, :], lhsT=ones[0:np_, 0:np_], rhs=sq[0:np_, :],
                                                              start=True, stop=True),
                             reads=['ones', 'sq'], writes=['psn'])
                        P.op('dve', lambda e, np_=np_: e.tensor_scalar(rstd[0:np_, :], psn[0:np_, :], 1.0 / np_, EPS,
                                                                      op0=ALU.mult, op1=ALU.add),
                             reads=['psn'], writes=['rstd'])
                        P.op('act', lambda e, np_=np_: e.activation(rstd[0:np_, :], rstd[0:np_, :], AF.Sqrt),
                             reads=['rstd'], writes=['rstd'])
                        P.op('dve', lambda e, np_=np_: e.reciprocal(rstd[0:np_, :], rstd[0:np_, :]),
                             reads=['rstd'], writes=['rstd'])
                        P.op('dve', lambda e, np_=np_, gcol=gcol: e.scalar_tensor_tensor(
                            nrm[0:np_, :], raw[0:np_, :], gqt[0:np_, gcol:gcol + 1], rstd[0:np_, :],
                            op0=ALU.mult, op1=ALU.mult), reads=['raw', 'gqt', 'rstd'], writes=['nrm'])
                        src = nrm
                    else:
                        P.op('act', lambda e, pi=pi: e.copy(nrm[:], psm[pi][:]), reads=[pk], writes=['nrm'])
                        src = nrm
                    if kind == 'qkA':
                        rp = 32; R = rA; ct, st, cn, sn = cosA, sinA, 'cos0', 'sin0'
                    else:
                        rp = np_; R = rI; ct, st, cn, sn = cosI, sinI, 'cos1', 'sin1'
                    P.op('pe', lambda e, rp=rp, R=R: e.matmul(psr[0:rp, :], lhsT=R[0:rp, 0:rp], rhs=nrm[0:rp, :],
                                                             start=True, stop=True),
                         reads=['nrm', 'rA', 'rI'], writes=['psr'])
                    P.op('pool', lambda e, rp=rp, ct=ct, ts_=ts_: e.tensor_tensor(t1[0:rp, :], nrm[0:rp, :], ct[0:rp, ts_], op=ALU.mult),
                         reads=['nrm', cn], writes=['t1'])
                    P.op('dve', lambda e, rp=rp, st=st, ts_=ts_: e.tensor_tensor(t2[0:rp, :], psr[0:rp, :], st[0:rp, ts_], op=ALU.mult),
                         reads=['psr', sn], writes=['t2'])
                    P.op('pool', lambda e, rp=rp, ob_=ob_, ts_=ts_: e.tensor_tensor(ob_[0:rp, ts_], t1[0:rp, :], t2[0:rp, :], op=ALU.add),
                         reads=['t1', 't2'], writes=[okey])
                    if rp < np_:
                        P.op('act', lambda e, ob_=ob_, ts_=ts_: e.copy(ob_[32:64, ts_], nrm[32:64, :]),
                             reads=['nrm'], writes=[okey])
                        P.op('act', lambda e, ob_=ob_, ts_=ts_: e.copy(ob_[64:128, ts_], nrm[64:128, :]),
                             reads=['nrm'], writes=[okey])
                oq_ = 'act'
                if kind == 'f32':
                    P.dma(oq_, o_f32[orow:orow + ncol, :], ob_[0:ncol, :], reads=[okey], writes=['o_f32'])
                else:
                    if (kind == 'ki' or (kind == 'qkA' and aux == 1) or (kind == 'bf' and col >= O_SK)):
                        P.dma('act', o_k[orow][0:ncol, :], ob_[0:ncol, :], reads=[okey])
                    else:
                        P.dma('act', o_q[orow:orow + ncol, :], ob_[0:ncol, :], reads=[okey])
        for (r0, t0) in ((R_GLU, 0), (R_CC + 512, 1024)):
            for q in range(4):
                P.dma('sp', tail1[(t0 + q * 256) // 256].rearrange("r (b k) -> r b k", k=32),
                      o_f32[r0 + q * 256:r0 + (q + 1) * 256, :].rearrange("r (b t) -> r b t", t=128)[:, :, 96:128],
                      reads=['o_f32'])
        ns = P.emit()
        print("L1: instrs", len(P.ins), "sems", ns, "waits", P.n_waits)


S = 8192
NQ = 16
NIT = 18
NEG = -1.0e30
MNEG = -30000.0
SCALE = 128 ** -0.5


def l2_masks(j):
    neg = np.zeros((128, 4, 128), np.float32)
    md = np.zeros((128, 4, 4, 128), np.float32)
    tp = np.arange(128)[:, None]
    sp = np.arange(128)[None, :]
    for m in range(4):
        if m < j:
            neg[:, m, :] = 0.0
            md[:, m, :, :] = 1.0
        elif m == j:
            neg[:, m, :] = np.where(sp <= tp, 0.0, NEG)
            md[:, m, :, :] = (np.arange(128)[:, None] < np.arange(128)[None, :]).astype(np.float32)[:, None, :]
        else:
            neg[:, m, :] = NEG
            md[:, m, :, :] = 0.0
    return neg.reshape(128, 512), md


def l2_consts():
    su = (np.arange(128)[:, None] > np.arange(128)[None, :]).astype(np.float32)
    ident = np.eye(128, dtype=np.float32)
    pw = (2.0 ** -(np.arange(NIT, dtype=np.float32) + 1.0))[None, :].repeat(128, 0).astype(np.float32)
    return su, ident, pw


def build_l2(nc, IO, pfx, do_a=True, do_d=True, nq=NQ):
    G_k, G_v, o_q, o_iw = IO['G_k'], IO['G_v'], IO['o_q'], IO['o_iw']
    negm, mdm, su_d, id_d, pw_d = IO['negm'], IO['mdm'], IO['su'], IO['ident'], IO['pw']
    yaT, ydT = IO['yaT'], IO['ydT']
    Gv4 = [g.rearrange("(r i p) n -> p r i n", r=4, p=128) for g in G_v]

    def load_nat(P, eng, dst2d, row0, nrows, key):
        dv = dst2d.rearrange("p (i r t) -> p i r t", r=4, t=128)
        for r in range(4):
            P.dma(eng if r % 2 == 0 else ('pool' if eng == 'sp' else 'sp'), dv[:, :, r, :],
                  G_k[row0][r * nrows:(r + 1) * nrows, :].rearrange("p (i t) -> p i t", t=128), reads=[f'Gk{row0}'], writes=[key])

    P = Prog(nc)
    es = contextlib.ExitStack()

    def sb(name, shape, dt):
        return es.enter_context(nc.sbuf_tensor(pfx + name, shape, dt))

    def pst(name, shape, dt=F32):
        return es.enter_context(nc.psum_tensor(pfx + name, shape, dt))

    with es:
        KT = sb("KT", [128, 4, S], BF16)
        kit = sb("kit", [64, S], BF16)
        score = sb("score", [128, S], F32)
        mneg = sb("mneg", [128, S], BF16)
        junk = sb("junk", [128, S], BF16)
        vt = [sb(f"vt{i}", [128, 4, 512], BF16) for i in range(3)]
        qblk = [sb(f"qblk{i}", [128, 4, 128], BF16) for i in range(2)]
        qiblk = [sb(f"qiblk{i}", [64, 4, 128], BF16) for i in range(2)]
        iwt = sb("iwt", [128, NQ, 4], F32)
        absw = sb("absw", [128, NQ, 4], F32)
        sgnw = sb("sgnw", [128, NQ, 4], F32)
        negt = sb("negt", [128, 512], F32)
        mdt = sb("mdt", [128, 4, 4, 128], F32)
        sut = sb("sut", [128, 128], F32)
        idf = sb("idf", [128, 128], F32)
        idb = sb("idb", [128, 128], BF16)
        pwt = sb("pwt", [128, NIT], F32)
        ones_f = sb("ones_f", [128, 128], F32)
        ones_b = sb("ones_b", [128, 128], BF16)
        rh = [sb(f"rh{i}", [128, 512], F32) for i in range(2)]
        st = sb("st", [128, 16], F32)
        wk = sb("wk", [128, NIT], F32)
        PT = [sb(f"PT{i}", [128, 512], BF16) for i in range(2)]
        rec = sb("rec", [128, 512], F32)
        oat = [sb(f"oat{i}", [128, 512], F32) for i in range(2)]
        e_t = [sb(f"e_t{i}", [128, 512], F32) for i in range(2)]
        sp_t = [sb(f"sp_t{i}", [128, 512], F32) for i in range(3)]
        u_t = [sb(f"u_t{i}", [128, 512], F32) for i in range(2)]
        R_t = [sb(f"R_t{i}", [128, 512], F32) for i in range(2)]
        aT = [sb(f"aT{i}", [128, 512], BF16) for i in range(2)]
        ps = [pst(f"ps{i}", [128, 512]) for i in range(8)]

        for (src_t, dst_t, key) in IO['cc_l2']:
            P.cc('pool', src_t, dst_t, writes=[key])
        P.dma('sp', iwt[:], o_iw.rearrange("(i t) h -> t i h", t=128), writes=['iwt'])
        P.dma('sp', negt[:], negm, writes=['negt'])
        P.dma('sp', mdt[:], mdm, writes=['mdt'])
        P.dma('sp', sut[:], su_d, writes=['sut'])
        P.dma('sp', idf[:], id_d, writes=['idf'])
        P.dma('sp', pwt[:], pw_d, writes=['pwt'])
        P.op('dve', lambda e: e.memset(ones_f[:], 1.0), writes=['ones_f'])
        P.op('dve', lambda e: e.memset(ones_b[:], 1.0), writes=['ones_b'])
        P.op('dve', lambda e: e.tensor_copy(idb[:], idf[:]), reads=['idf'], writes=['idb'])
        P.op('act', lambda e: e.activation(absw[:], iwt[:], AF.Abs), reads=['iwt'], writes=['absw'])
        P.op('act', lambda e: e.activation(sgnw[:], iwt[:], AF.Sign), reads=['iwt'], writes=['sgnw'])

        hsl = [slice(h * 128, (h + 1) * 128) for h in range(4)]

        if do_d:
            for h in range(4):
                load_nat(P, 'sp', KT[:, h, :], 576 + h * 128, 128, f'KT{h}')
            vcnt = 0
            for i in range(nq):
                NK = 4 * i + 4
                NG = i + 1
                qb_ = qblk[i % 2]; qbk = f'qblk{i % 2}'
                P.dma('sp', qb_[:], o_q[768:1280, i * 128:(i + 1) * 128].rearrange("(h d) t -> d h t", d=128), writes=[qbk])
                chs = list(range(NK - 1, -1, -1))
                vbs = {}

                def s1(k, ch):
                    nonlocal vcnt
                    kg, cc = ch // 4, ch % 4
                    if cc == 3:
                        vb = vt[vcnt % 3]; vk = f'vt{vcnt % 3}'; vcnt += 1
                        P.dma('sp', vb[:], Gv4[kg // 2][:, :, kg % 2, 512:1024], reads=[f'Gv{kg // 2}'], writes=[vk])
                        vbs[kg] = (vb, vk)
                    cs_ = slice(ch * 128, (ch + 1) * 128)
                    pz = ps[k % 2]; pzk = f'ps{k % 2}'
                    et = e_t[k % 2]; ek = f'e_t{k % 2}'
                    spt = sp_t[k % 3]; spk = f'sp{k % 3}'
                    for h in range(4):
                        P.op('pe', lambda e, pz=pz, h=h, cs_=cs_, qb_=qb_: e.matmul(pz[:, hsl[h]], lhsT=KT[:, h, cs_], rhs=qb_[:, h, :], start=True, stop=True),
                             reads=[f'KT{h}', qbk], writes=[pzk])
                    P.op('act', lambda e, pz=pz, et=et: e.activation(et[:], pz[:], AF.Exp, scale=SCALE), reads=[pzk], writes=[ek])
                    P.op('act', lambda e, spt=spt, et=et: e.activation(spt[:], et[:], AF.Ln, bias=ones_f[:, 0:1]), reads=[ek, 'ones_f'], writes=[spk])
                    if kg == NG - 1:
                        P.op('dve', lambda e, spt=spt, cc=cc: e.tensor_tensor(
                            spt[:], spt[:], mdt[:, cc, :, :].rearrange("p h t -> p (h t)"), op=ALU.mult), reads=[spk, 'mdt'], writes=[spk])

                def s2(k, ch):
                    first = (k == 0)
                    pz = ps[k % 2]; pzk = f'ps{k % 2}'
                    pB = ps[2 + k % 2]; pBk = f'ps{2 + k % 2}'
                    spt = sp_t[k % 3]; spk = f'sp{k % 3}'
                    ut = u_t[k % 2]; uk = f'u_t{k % 2}'
                    Rp, Rpk = R_t[k % 2], f'R_t{k % 2}'
                    Rn, Rnk = R_t[(k + 1) % 2], f'R_t{(k + 1) % 2}'
                    P.op('pe', lambda e, pB=pB, spt=spt, first=first: e.matmul(pB[:], lhsT=sut[:], rhs=spt[:], start=True, stop=first),
                         reads=['sut', spk], writes=[pBk])
                    if not first:
                        P.op('pe', lambda e, pB=pB, Rp=Rp: e.matmul(pB[:], lhsT=ones_f[:], rhs=Rp[:], start=False, stop=True),
                             reads=['ones_f', Rpk], writes=[pBk])
                    if first:
                        P.op('dve', lambda e, spt=spt, Rn=Rn: e.tensor_copy(Rn[:], spt[:]), reads=[spk], writes=[Rnk])
                    elif k + 1 < NK:
                        P.op('dve', lambda e, spt=spt, Rn=Rn, Rp=Rp: e.tensor_tensor(Rn[:], Rp[:], spt[:], op=ALU.add), reads=[spk, Rpk], writes=[Rnk])
                    P.op('dve', lambda e, pz=pz, spt=spt, ut=ut: e.scalar_tensor_tensor(ut[:], pz[:], SCALE, spt[:], op0=ALU.mult, op1=ALU.subtract),
                         reads=[pzk, spk], writes=[uk])
                    P.op('dve', lambda e, pB=pB, ut=ut: e.tensor_tensor(ut[:], ut[:], pB[:], op=ALU.subtract), reads=[uk, pBk], writes=[uk])

                def s3(k, ch):
                    first = (k == 0)
                    kg, cc = ch // 4, ch % 4
                    ut = u_t[k % 2]; uk = f'u_t{k % 2}'
                    at = aT[k % 2]; atk = f'aT{k % 2}'
                    vb, vk = vbs[kg]
                    P.op('act', lambda e, at=at, ut=ut: e.activation(at[:], ut[:], AF.Exp), reads=[uk], writes=[atk])
                    if kg == NG - 1:
                        P.op('dve', lambda e, at=at, cc=cc: e.tensor_tensor(
                            at[:], at[:], mdt[:, cc, :, :].rearrange("p h t -> p (h t)"), op=ALU.mult), reads=[atk, 'mdt'], writes=[atk])
                    for h in range(4):
                        P.op('pe', lambda e, at=at, h=h, vb=vb, cc=cc, first=first, ch=ch: e.matmul(
                            ps[4 + h][:, 0:128], lhsT=vb[:, cc, hsl[h]], rhs=at[:, hsl[h]], start=first, stop=(ch == 0)),
                             reads=[atk, vk], writes=[f'ps{4 + h}'])

                for n in range(NK + 2):
                    if n < NK:
                        s1(n, chs[n])
                    if 0 <= n - 1 < NK:
                        s2(n - 1, chs[n - 1])
                    if 0 <= n - 2 < NK:
                        s3(n - 2, chs[n - 2])
                ot = oat[i % 2]; otk = f'oat{i % 2}'
                for h in range(4):
                    if h % 2 == 0:
                        P.op('dve', lambda e, h=h, ot=ot: e.tensor_copy(ot[:, hsl[h]], ps[4 + h][:, 0:128]), reads=[f'ps{4 + h}'], writes=[otk])
                    else:
                        P.op('act', lambda e, h=h, ot=ot: e.copy(ot[:, hsl[h]], ps[4 + h][:, 0:128]), reads=[f'ps{4 + h}'], writes=[otk])
                P.dma('sp', ydT[:, i * 128:(i + 1) * 128].rearrange("(h d) t -> d h t", d=128), ot[:].rearrange("p (h t) -> p h t", t=128), reads=[otk])
        if do_a:
            load_nat(P, 'sp', kit[:, :], 0, 64, 'kit')
            for h in range(4):
                load_nat(P, 'sp', KT[:, h, :], 64 + h * 128, 128, f'KT{h}')
            vstate = dict(cnt=0)

            def a_score(i):
                NG = i + 1
                Sc = 512 * NG
                qb_ = qblk[i % 2]; qbk = f'qblk{i % 2}'
                qib = qiblk[i % 2]; qik = f'qiblk{i % 2}'
                P.dma('sp', qb_[:], o_q[0:512, i * 128:(i + 1) * 128].rearrange("(h d) t -> d h t", d=128), writes=[qbk])
                P.dma('sp', qib[:], o_q[512:768, i * 128:(i + 1) * 128].rearrange("(h d) t -> d h t", d=64), writes=[qik])
                for kg in range(NG):
                    ks = slice(kg * 512, (kg + 1) * 512)
                    for h in range(4):
                        pb = ps[h % 2]; pk = f'ps{h % 2}'
                        P.op('pe', lambda e, pb=pb, h=h, ks=ks, qib=qib: e.matmul(pb[:], lhsT=qib[:, h, :], rhs=kit[:, ks], start=True, stop=True),
                             reads=[qik, 'kit'], writes=[pk])
                        r_ = rh[h % 2]; rk = f'rh{h % 2}'
                        P.op('act', lambda e, pb=pb, r_=r_, i=i, h=h: e.activation(r_[:], pb[:], AF.Relu, scale=absw[:, i, h:h + 1]),
                             reads=[pk, 'absw'], writes=[rk])
                        if h == 0:
                            P.op('dve', lambda e, r_=r_, ks=ks, i=i, h=h: e.tensor_scalar(score[:, ks], r_[:], sgnw[:, i, h:h + 1], None, op0=ALU.mult),
                                 reads=[rk, 'sgnw'], writes=['score'])
                        else:
                            P.op('dve', lambda e, r_=r_, ks=ks, i=i, h=h: e.scalar_tensor_tensor(
                                score[:, ks], r_[:], sgnw[:, i, h:h + 1], score[:, ks], op0=ALU.mult, op1=ALU.add),
                                 reads=[rk, 'sgnw', 'score'], writes=['score'])
                P.op('dve', lambda e, Sc=Sc: e.tensor_reduce(st[:, 1:2], score[:, 0:Sc], axis=AX.X, op=ALU.max), reads=['score'], writes=['st_hi'])
                P.op('dve', lambda e, Sc=Sc: e.tensor_reduce(st[:, 0:1], score[:, 0:Sc], axis=AX.X, op=ALU.min), reads=['score'], writes=['st_lo'])
                P.op('pool', lambda e, Sc=Sc: e.tensor_tensor(score[:, Sc - 512:Sc], score[:, Sc - 512:Sc], negt[:], op=ALU.add),
                     reads=['score', 'negt'], writes=['score'])

            def a_bisect(i):
                Sc = 512 * (i + 1)
                P.op('dve', lambda e: e.tensor_tensor(st[:, 2:3], st[:, 1:2], st[:, 0:1], op=ALU.subtract), reads=['st_hi', 'st_lo'], writes=['st_W'])
                P.op('dve', lambda e: e.tensor_scalar(wk[:], pwt[:], st[:, 2:3], None, op0=ALU.mult), reads=['pwt', 'st_W'], writes=['wk'])
                P.op('dve', lambda e: e.tensor_tensor(st[:, 3:4], st[:, 0:1], wk[:, 0:1], op=ALU.add), reads=['st_lo', 'wk'], writes=['st_mid'])
                yield
                Sa = max(64, int(round(0.56 * Sc / 64.0)) * 64)
                for k in range(NIT):
                    P.op('act', lambda e, Sa=Sa: e.activation(junk[:, 0:Sa], score[:, 0:Sa], AF.Sign, bias=st[:, 3:4], scale=-1.0,
                                                              accum_out=st[:, 4:5]),
                         reads=['score', 'st_mid'], writes=['junkA', 'st_cntA'])
                    P.op('dve', lambda e, Sa=Sa, Sc=Sc: e.tensor_scalar(junk[:, Sa:Sc], score[:, Sa:Sc], st[:, 3:4], None,
                                                                        op0=ALU.is_ge, op1=ALU.add, accum_out=st[:, 6:7]),
                         reads=['score', 'st_mid'], writes=['junkD', 'st_cntD'])
                    P.op('dve', lambda e: e.scalar_tensor_tensor(st[:, 7:8], st[:, 4:5], -0.5, st[:, 6:7], op0=ALU.mult, op1=ALU.add),
                         reads=['st_cntA', 'st_cntD'], writes=['st_tmp1'])
                    P.op('dve', lambda e, k=k, Sa=Sa: e.scalar_tensor_tensor(st[:, 5:6], st[:, 7:8], 255.5 - 0.5 * Sa, wk[:, k:k + 1],
                                                                             op0=ALU.is_ge, op1=ALU.mult),
                         reads=['st_tmp1', 'wk'], writes=['st_tmp'])
                    P.op('dve', lambda e: e.tensor_tensor(st[:, 0:1], st[:, 0:1], st[:, 5:6], op=ALU.add), reads=['st_lo', 'st_tmp'], writes=['st_lo'])
                    if k + 1 < NIT:
                        P.op('dve', lambda e, k=k: e.tensor_tensor(st[:, 3:4], st[:, 0:1], wk[:, k + 1:k + 2], op=ALU.add),
                             reads=['st_lo', 'wk'], writes=['st_mid'])
                    yield

            def a_mask(i):
                Sc = 512 * (i + 1)
                P.op('dve', lambda e, Sc=Sc: e.tensor_scalar(mneg[:, 0:Sc], score[:, 0:Sc], st[:, 0:1], MNEG, op0=ALU.is_lt, op1=ALU.mult),
                     reads=['score', 'st_lo'], writes=['mneg'])

            def a_attn(i):
                NK = 4 * i + 4
                qb_ = qblk[i % 2]; qbk = f'qblk{i % 2}'
                vbs = {}

                def s1(ch):
                    if ch % 4 == 0:
                        kg = ch // 4
                        vb = vt[vstate['cnt'] % 3]; vk = f"vt{vstate['cnt'] % 3}"; vstate['cnt'] += 1
                        P.dma('sp', vb[:], Gv4[kg // 2][:, :, kg % 2, 0:512], reads=[f'Gv{kg // 2}'], writes=[vk])
                        vbs[kg] = (vb, vk)
                    cs_ = slice(ch * 128, (ch + 1) * 128)
                    pl = ps[ch % 2]; plk = f'ps{ch % 2}'
                    for h in range(4):
                        P.op('pe', lambda e, pl=pl, h=h, cs_=cs_: e.matmul(pl[:, hsl[h]], lhsT=KT[:, h, cs_], rhs=qb_[:, h, :], start=True, stop=False),
                             reads=[f'KT{h}', qbk], writes=[plk])
                        P.op('pe', lambda e, pl=pl, h=h, cs_=cs_: e.matmul(pl[:, hsl[h]], lhsT=mneg[:, cs_], rhs=idb[:], start=False, stop=True),
                             reads=['mneg', 'idb'], writes=[plk])

                def s2(ch):
                    pl = ps[ch % 2]; plk = f'ps{ch % 2}'
                    pt = PT[ch % 2]; ptk = f'PT{ch % 2}'
                    vb, vk = vbs[ch // 4]
                    cc = ch % 4
                    P.op('act', lambda e, pl=pl, pt=pt: e.activation(pt[:], pl[:], AF.Exp, scale=SCALE), reads=[plk], writes=[ptk])
                    P.op('pe', lambda e, pt=pt, ch=ch: e.matmul(ps[2][:, :], lhsT=ones_b[:, :], rhs=pt[:], start=(ch == 0), stop=(ch == NK - 1)),
                         reads=[ptk, 'ones_b'], writes=['ps2'])
                    for h in range(4):
                        P.op('pe', lambda e, pt=pt, h=h, ch=ch, vb=vb, cc=cc: e.matmul(
                            ps[3 + h][:, 0:128], lhsT=vb[:, cc, hsl[h]], rhs=pt[:, hsl[h]], start=(ch == 0), stop=(ch == NK - 1)),
                             reads=[ptk, vk], writes=[f'ps{3 + h}'])

                for n in range(NK + 1):
                    if n < NK:
                        s1(n)
                    if n >= 1:
                        s2(n - 1)
                    yield
                P.op('dve', lambda e: e.reciprocal(rec[:], ps[2][:, :]), reads=['ps2'], writes=['rec'])
                ot = oat[i % 2]; otk = f'oat{i % 2}'
                for h in range(4):
                    P.op('dve', lambda e, h=h, ot=ot: e.tensor_tensor(ot[:, hsl[h]], ps[3 + h][:, 0:128], rec[:, hsl[h]], op=ALU.mult),
                         reads=[f'ps{3 + h}', 'rec'], writes=[otk])
                P.dma('sp', yaT[:, i * 128:(i + 1) * 128].rearrange("(h d) t -> d h t", d=128), ot[:].rearrange("p (h t) -> p h t", t=128), reads=[otk])

            a_score(0)
            for _ in a_bisect(0):
                pass
            a_mask(0)
            for i in range(nq):
                ga = a_attn(i)
                n_att = 4 * i + 5
                if i + 1 < nq:
                    a_score(i + 1)
                    gb = a_bisect(i + 1)
                    n_bis = NIT + 1
                    done_b = 0
                    for s_ in range(n_att):
                        next(ga)
                        tgt = ((s_ + 1) * n_bis) // n_att
                        while done_b < tgt:
                            next(gb); done_b += 1
                    for _ in gb:
                        pass
                for _ in ga:
                    pass
                if i + 1 < nq:
                    a_mask(i + 1)

        ns = P.emit()
        print("L2: instrs", len(P.ins), "sems", ns, "waits", P.n_waits)

T = 2048
HB = 32
EPS = 1e-6
DFF = 2816
NFF = 22


def _blend(P, out_ap, cands, ckeys, selt, okey):
    P.op('dve', lambda e: e.tensor_scalar(out_ap, cands[0], selt[:, 0:1], None, op0=ALU.mult),
         reads=[ckeys[0], 'selt'], writes=[okey])
    for c in range(1, 4):
        P.op('dve', lambda e, c=c: e.scalar_tensor_tensor(out_ap, cands[c], selt[:, c:c + 1], out_ap, op0=ALU.mult, op1=ALU.add),
             reads=[ckeys[c], 'selt', okey], writes=[okey])


def build_l3a(nc, IO, pfx):
    o_f32, GT1, sel_d, yaT, ydT, xT = IO['o_f32'], IO['GT1'], IO['sel'], IO['yaT'], IO['ydT'], IO['xT']
    wo, wm, sm, xmT, tail2 = IO['wo'], IO['wm'], IO['sm3a'], IO['xm'], IO['tail2']
    gateT = o_f32[2560:6656, :]
    P = Prog(nc)
    es = contextlib.ExitStack()
    sb = lambda name, shape, dt: es.enter_context(nc.sbuf_tensor(pfx + name, shape, dt))
    pst = lambda name, shape, dt=F32: es.enter_context(nc.psum_tensor(pfx + name, shape, dt))
    W = 512
    with es:
        wob = [sb(f"wob{i}", [128, 4, 1024], BF16) for i in range(4)]
        wmb = sb("wmb", [128, 8, 1024], BF16)
        wst = sb("wst", [128, 4, 1024], F32)
        smt = sb("smt", [128, 192], F32)
        selt = sb("selt", [128, 4], F32)
        ones = sb("ones", [128, 128], F32)
        hh = [sb(f"hh{i}", [128, 4, 4, 160], F32) for i in range(2)]
        cand = [sb(f"cand{i}", [128, 4, 4, 32], F32) for i in range(4)]
        acc = sb("acc", [128, 4, W], F32)
        xc = sb("xc", [128, 4, W], F32)
        sq = sb("sq", [128, 4, W], F32)
        gbb = sb("gbb", [128, 4, W], F32)
        brs = [[sb(f"br{i}_{p}", [128, 4, W], BF16) for i in range(4)] for p in range(2)]
        rln = sb("rln", [128, W], F32)
        gt_ = [sb(f"gt{i}", [128, W], F32) for i in range(2)]
        mg = sb("mg", [128, W], F32)
        mgb = sb("mgb", [128, 8, W], BF16)
        xt = sb("xt", [128, 8, W], F32)
        ot = sb("ot", [128, 8, W], F32)
        ps = [pst(f"ps{i}", [128, 512]) for i in range(8)]

        P.dma('sp', smt[:], sm, writes=['smt'])
        P.dma('sp', selt[:], sel_d, writes=['selt'])
        P.op('dve', lambda e: e.memset(ones[:], 1.0 / 512), writes=['ones'])
        for i in range(4):
            P.dma('sp', wst[:], wo[i].rearrange("(c p) n -> p c n", p=128), writes=['wst'])
            P.op('pool' if i % 2 else 'dve', lambda e, i=i: e.tensor_copy(wob[i][:], wst[:]), reads=['wst'], writes=[f'wob{i}'])
        for hf in range(2):
            P.dma('sp', wst[:], wm[hf * 512:(hf + 1) * 512, :].rearrange("(c p) n -> p c n", p=128), writes=['wst'])
            P.op('pool' if hf else 'dve', lambda e, hf=hf: e.tensor_copy(wmb[:, hf * 4:(hf + 1) * 4, :], wst[:]), reads=['wst'], writes=['wmb'])

        def load_haloed(dst, dkey, row_main, row_tail, tl):
            u0 = tl * W
            for ch in range(4):
                P.dma('sp' if ch % 2 == 0 else 'act', dst[:, ch, :, 32:160],
                      o_f32[row_main + ch * 128:row_main + (ch + 1) * 128, u0:u0 + W].rearrange("p (b t) -> p b t", t=128),
                      writes=[dkey])
            q0 = row_tail // 256
            for r in range(4):
                if r == 3 and tl == 0:
                    P.op('pool', lambda e: e.memset(cand[3][:], 0.0), writes=['cand3'])
                for hf in range(2):
                    src = GT1[q0 + hf][r * 256:(r + 1) * 256, :]
                    if r < 3:
                        P.dma('sp' if (r + hf) % 2 == 0 else 'act', cand[r][:, 2 * hf:2 * hf + 2, :, :],
                              src[:, tl * 128:(tl + 1) * 128].rearrange("(c p) (b k) -> p c b k", p=128, k=32), writes=[f'cand{r}'])
                    elif tl == 0:
                        P.dma('act', cand[3][:, 2 * hf:2 * hf + 2, 1:4, :],
                              src[:, 0:96].rearrange("(c p) (b k) -> p c b k", p=128, k=32), writes=['cand3'])
                    else:
                        P.dma('act', cand[3][:, 2 * hf:2 * hf + 2, :, :],
                              src[:, tl * 128 - 32:tl * 128 + 96].rearrange("(c p) (b k) -> p c b k", p=128, k=32), writes=['cand3'])
            _blend(P, dst[:].rearrange("p c b k -> p (c b) k")[:, :, 0:32], [c_[:].rearrange("p c b k -> p (c b) k") for c_ in cand],
                   [f'cand{r}' for r in range(4)], selt, dkey)

        def stage_x(tl):
            u0 = tl * W
            br = brs[tl % 2]
            bk = [f'br{i}_{tl % 2}' for i in range(4)]
            ga, gb_ = hh[0], hh[1]
            load_haloed(ga, 'hh0', 0, 0, tl)
            load_haloed(gb_, 'hh1', 512, 512, tl)
            P.op('act', lambda e: e.activation(gb_[:], gb_[:], AF.Sigmoid), reads=['hh1'], writes=['hh1'])
            P.op('pool', lambda e: e.tensor_tensor(ga[:], ga[:], gb_[:], op=ALU.mult), reads=['hh0', 'hh1'], writes=['hh0'])
            for ch in range(4):
                ak = f'acc{ch}'
                av_ = acc[:, ch, :].rearrange("p (b t) -> p b t", t=128)
                P.op('dve', lambda e, ch=ch, av_=av_: e.tensor_scalar(av_, ga[:, ch, :, 2:130], smt[:, 32 + ch * 31:33 + ch * 31],
                                                                    smt[:, 156 + ch:157 + ch], op0=ALU.mult, op1=ALU.add),
                     reads=['hh0', 'smt'], writes=[ak])
                for k in range(1, 31):
                    P.op('dve', lambda e, ch=ch, k=k, av_=av_: e.scalar_tensor_tensor(
                        av_, ga[:, ch, :, 2 + k:130 + k], smt[:, 32 + ch * 31 + k:33 + ch * 31 + k], av_,
                        op0=ALU.mult, op1=ALU.add), reads=['hh0', 'smt', ak], writes=[ak])
            aks = [f'acc{ch}' for ch in range(4)]
            for ch in range(4):
                P.op('pe', lambda e, ch=ch: e.matmul(ps[0][:], lhsT=ones[:], rhs=acc[:, ch, :], start=(ch == 0), stop=(ch == 3)),
                     reads=['ones'] + aks, writes=['ps0'])
            for ch in range(4):
                P.op('dve', lambda e, ch=ch: e.tensor_tensor(xc[:, ch, :], acc[:, ch, :], ps[0][:], op=ALU.subtract),
                     reads=aks + ['ps0'], writes=['xc'])
            P.op('act', lambda e: e.activation(sq[:], xc[:], AF.Square), reads=['xc'], writes=['sq'])
            for ch in range(4):
                P.op('pe', lambda e, ch=ch: e.matmul(ps[1][:], lhsT=ones[:], rhs=sq[:, ch, :], start=(ch == 0), stop=(ch == 3)),
                     reads=['ones', 'sq'], writes=['ps1'])
            P.op('dve', lambda e: e.tensor_scalar(rln[:], ps[1][:], 1.0, EPS, op0=ALU.mult, op1=ALU.add), reads=['ps1'], writes=['rln'])
            P.op('act', lambda e: e.activation(rln[:], rln[:], AF.Sqrt), reads=['rln'], writes=['rln'])
            P.op('dve', lambda e: e.reciprocal(rln[:], rln[:]), reads=['rln'], writes=['rln'])
            for ch in range(4):
                P.op('dve', lambda e, ch=ch: e.tensor_tensor(xc[:, ch, :], xc[:, ch, :], rln[:], op=ALU.mult),
                     reads=['xc', 'rln'], writes=['xc'])
                P.op('dve', lambda e, ch=ch: e.tensor_scalar(xc[:, ch, :], xc[:, ch, :], smt[:, 160 + ch:161 + ch],
                                                             smt[:, 164 + ch:165 + ch], op0=ALU.mult, op1=ALU.add),
                     reads=['xc', 'smt'], writes=['xc'])
            P.op('act', lambda e: e.activation(br[1][:], xc[:], AF.Silu), reads=['xc'], writes=[bk[1]])
            gc, xcc = hh[0], hh[1]
            load_haloed(gc, 'hh0', 1024 + 512, 1024, tl)
            load_haloed(xcc, 'hh1', 1024 + 1024, 1536, tl)
            P.dma('sp', gbb[:], o_f32[1024:1536, u0:u0 + W].rearrange("(c p) t -> p c t", p=128), writes=['gbb'])
            P.op('pool', lambda e: e.tensor_tensor(gc[:], gc[:], xcc[:], op=ALU.mult), reads=['hh0', 'hh1'], writes=['hh0'])
            for ch in range(4):
                ak = f'acc{ch}'
                av_ = acc[:, ch, :].rearrange("p (b t) -> p b t", t=128)
                P.op('dve', lambda e, ch=ch, av_=av_: e.tensor_scalar(av_, gc[:, ch, :, 30:158], smt[:, 168 + ch * 3:169 + ch * 3],
                                                                    None, op0=ALU.mult), reads=['hh0', 'smt'], writes=[ak])
                for k in (1, 2):
                    P.op('dve', lambda e, ch=ch, k=k, av_=av_: e.scalar_tensor_tensor(
                        av_, gc[:, ch, :, 30 + k:158 + k], smt[:, 168 + ch * 3 + k:169 + ch * 3 + k], av_,
                        op0=ALU.mult, op1=ALU.add), reads=['hh0', 'smt', ak], writes=[ak])
                P.op('pool', lambda e, ch=ch: e.tensor_tensor(br[2][:, ch, :], acc[:, ch, :], gbb[:, ch, :], op=ALU.mult),
                     reads=[ak, 'gbb'], writes=[bk[2]])
            P.dma('sp', xc[:], yaT[:, u0:u0 + W].rearrange("(c p) t -> p c t", p=128), writes=['xc'])
            P.op('act', lambda e: e.copy(br[0][:], xc[:]), reads=['xc'], writes=[bk[0]])
            P.dma('act', sq[:], ydT[:, u0:u0 + W].rearrange("(c p) t -> p c t", p=128), writes=['sq'])
            P.op('act', lambda e: e.copy(br[3][:], sq[:]), reads=['sq'], writes=[bk[3]])
        def stage_y(tl):
            u0 = tl * W
            br = brs[tl % 2]
            bk = [f'br{i}_{tl % 2}' for i in range(4)]
            P.dma('sp', xt[:], xT[:, u0:u0 + W].rearrange("(c p) t -> p c t", p=128), writes=['xt'])
            gi = 0
            for oc in range(8):
                ocs = slice(oc * 128, (oc + 1) * 128)
                for i in range(4):
                    pb = ps[2 + (gi % 4)]; pk = f'ps{2 + gi % 4}'
                    g_ = gt_[gi % 2]; gk = f'gt{gi % 2}'; gi += 1
                    P.dma('act' if gi % 2 else 'sp', g_[:], gateT[i * 1024 + oc * 128:i * 1024 + (oc + 1) * 128, u0:u0 + W], writes=[gk])
                    P.op('act', lambda e, g_=g_, i=i, oc=oc: e.activation(g_[:], g_[:], AF.Sigmoid, bias=smt[:, i * 8 + oc:i * 8 + oc + 1]),
                         reads=[gk, 'smt'], writes=[gk])
                    for kc in range(4):
                        P.op('pe', lambda e, pb=pb, i=i, kc=kc, ocs=ocs: e.matmul(pb[:], lhsT=wob[i][:, kc, ocs], rhs=br[i][:, kc, :],
                                                                                 start=(kc == 0), stop=(kc == 3)),
                             reads=[f'wob{i}', bk[i]], writes=[pk])
                    if i == 0:
                        P.op('dve', lambda e, pb=pb, g_=g_: e.tensor_tensor(mg[:], pb[:], g_[:], op=ALU.mult), reads=[pk, gk], writes=['mg'])
                    else:
                        P.op('dve', lambda e, pb=pb, g_=g_: e.tensor_tensor(g_[:], pb[:], g_[:], op=ALU.mult), reads=[pk, gk], writes=[gk])
                        if i < 3:
                            P.op('pool', lambda e, g_=g_: e.tensor_tensor(mg[:], mg[:], g_[:], op=ALU.add), reads=['mg', gk], writes=['mg'])
                        else:
                            P.op('pool', lambda e, g_=g_, oc=oc: e.tensor_tensor(mgb[:, oc, :], mg[:], g_[:], op=ALU.add),
                                 reads=['mg', gk], writes=[f'mgb{oc}'])
            mks = [f'mgb{oc}' for oc in range(8)]
            for oc in range(8):
                ocs = slice(oc * 128, (oc + 1) * 128)
                pb = ps[6 + oc % 2]; pk = f'ps{6 + oc % 2}'
                for kc in range(8):
                    P.op('pe', lambda e, pb=pb, kc=kc, ocs=ocs: e.matmul(pb[:], lhsT=wmb[:, kc, ocs], rhs=mgb[:, kc, :],
                                                                       start=(kc == 0), stop=(kc == 7)),
                         reads=['wmb'] + mks, writes=[pk])
                P.op('dve', lambda e, pb=pb, oc=oc: e.tensor_tensor(ot[:, oc, :], pb[:], xt[:, oc, :], op=ALU.add),
                     reads=[pk, 'xt'], writes=['ot'])
            P.dma('sp', xmT[:, u0:u0 + W].rearrange("(c p) t -> p c t", p=128), ot[:], reads=['ot'], writes=['xm'])
        stage_x(0)
        for tl in range(4):
            if tl + 1 < 4:
                stage_x(tl + 1)
            stage_y(tl)
        for q in range(4):
            P.dma('sp', tail2[q * 256:(q + 1) * 256, :].rearrange("r (b k) -> r b k", k=2),
                  xmT[q * 256:(q + 1) * 256, :].rearrange("r (b t) -> r b t", t=128)[:, :, 126:128], reads=['xm'])
        ns = P.emit()
        print("L3a: instrs", len(P.ins), "sems", ns, "waits", P.n_waits)


def build_l3b(nc, IO, pfx):
    xmT, GT2, sel_d, wg, wu, wd, sm, xoT = IO['xm'], IO['GT2'], IO['sel'], IO['wg'], IO['wu'], IO['wd'], IO['sm3b'], IO['xo']
    TT = 1024
    NB = 8
    NC_ = NB * 130
    P = Prog(nc)
    es = contextlib.ExitStack()
    sb = lambda name, shape, dt: es.enter_context(nc.sbuf_tensor(pfx + name, shape, dt))
    pst = lambda name, shape, dt=F32: es.enter_context(nc.psum_tensor(pfx + name, shape, dt))
    with es:
        smt = sb("smt", [128, 80], F32)
        selt = sb("selt", [128, 4], F32)
        ones = sb("ones", [128, 128], F32)
        xm = sb("xm", [128, 8, NB, 130], F32)
        cand = [sb(f"cand{i}", [128, 8, NB, 2], F32) for i in range(4)]
        sqt = sb("sqt", [128, NC_], F32)
        rs = sb("rs", [128, NC_], F32)
        h2 = sb("h2", [128, 8, NB, 130], BF16)
        prod = sb("prod", [128, NFF, TT], BF16)
        wgs = [sb(f"wgs{i}", [128, 8, 128], F32) for i in range(2)]
        wus = [sb(f"wus{i}", [128, 8, 128], F32) for i in range(2)]
        wgb = [sb(f"wgb{i}", [128, 8, 128], BF16) for i in range(2)]
        wub = [sb(f"wub{i}", [128, 8, 128], BF16) for i in range(2)]
        gtl = [sb(f"gtl{i}", [128, NB, 130], F32) for i in range(2)]
        av = [sb(f"av{i}", [128, TT], F32) for i in range(2)]
        wds = [sb(f"wds{i}", [128, 512], F32) for i in range(2)]
        wdb = [sb(f"wdb{i}", [128, 512], BF16) for i in range(2)]
        ot = [sb(f"ot{i}", [128, 512], F32) for i in range(2)]
        ps = [pst(f"ps{i}", [128, 512]) for i in range(8)]
        P.cc('pool', IO['cc_l3b'][0], IO['cc_l3b'][1], writes=['GT2'])
        P.dma('sp', smt[:], sm, writes=['smt'])
        P.dma('sp', selt[:], sel_d, writes=['selt'])
        P.op('dve', lambda e: e.memset(ones[:], 1.0 / 1024), writes=['ones'])
        wgv = wg.rearrange("(c p) n -> p c n", p=128)
        wuv = wu.rearrange("(c p) n -> p c n", p=128)
        xmf = xm[:].rearrange("p c b k -> p c (b k)")
        h2f = h2[:].rearrange("p c b k -> p c (b k)")
        for tl in range(2):
            u0 = tl * TT
            for c in range(8):
                P.dma('sp' if c % 2 == 0 else 'act', xm[:, c, :, 2:130],
                      xmT[c * 128:(c + 1) * 128, u0:u0 + TT].rearrange("p (b t) -> p b t", t=128), writes=['xm'])
            for r in range(3):
                P.dma('sp', cand[r][:], GT2[r * 1024:(r + 1) * 1024, tl * 16:(tl + 1) * 16].rearrange("(c p) (b k) -> p c b k", p=128, k=2),
                      reads=['GT2'], writes=[f'cand{r}'])
            if tl == 0:
                P.op('pool', lambda e: e.memset(cand[3][:], 0.0), writes=['cand3'])
                P.dma('act', cand[3][:, :, 1:8, :], GT2[3 * 1024:4 * 1024, 0:14].rearrange("(c p) (b k) -> p c b k", p=128, k=2), reads=['GT2'], writes=['cand3'])
            else:
                P.dma('act', cand[3][:], GT2[3 * 1024:4 * 1024, 14:30].rearrange("(c p) (b k) -> p c b k", p=128, k=2), reads=['GT2'], writes=['cand3'])
            _blend(P, xm[:].rearrange("p c b k -> p (c b) k")[:, :, 0:2], [c_[:].rearrange("p c b k -> p (c b) k") for c_ in cand],
                   [f'cand{r}' for r in range(4)], selt, 'xm')
            col_groups = [(0, 512), (512, 1024), (1024, NC_)]
            for (a, b) in col_groups:
                n = b - a
                for c in range(8):
                    P.op('act', lambda e, c=c, a=a, b=b: e.activation(sqt[:, a:b], xmf[:, c, a:b], AF.Square), reads=['xm'], writes=['sqt'])
                    P.op('pe', lambda e, c=c, a=a, b=b, n=n: e.matmul(ps[0][:, 0:n], lhsT=ones[:], rhs=sqt[:, a:b], start=(c == 0), stop=(c == 7)),
                         reads=['ones', 'sqt'], writes=['ps0'])
                P.op('dve', lambda e, a=a, b=b, n=n: e.tensor_scalar(rs[:, a:b], ps[0][:, 0:n], 1.0, EPS, op0=ALU.mult, op1=ALU.add),
                     reads=['ps0'], writes=['rs'])
            P.op('act', lambda e: e.activation(rs[:], rs[:], AF.Sqrt), reads=['rs'], writes=['rs'])
            P.op('dve', lambda e: e.reciprocal(rs[:], rs[:]), reads=['rs'], writes=['rs'])
            for c in range(8):
                P.op('dve', lambda e, c=c: e.scalar_tensor_tensor(h2f[:, c, :], xmf[:, c, :], smt[:, c:c + 1], rs[:],
                                                                 op0=ALU.mult, op1=ALU.mult),
                     reads=['xm', 'smt', 'rs'], writes=[f'h2_{c}'])
            hk = [f'h2_{c}' for c in range(8)]
            for f in range(NFF):
                b = f % 2
                fs = slice(f * 128, (f + 1) * 128)
                P.dma('sp', wgs[b][:], wgv[:, :, fs], writes=[f'wgs{b}'])
                P.dma('act', wus[b][:], wuv[:, :, fs], writes=[f'wus{b}'])
                P.op('dve', lambda e, b=b: e.tensor_copy(wgb[b][:], wgs[b][:]), reads=[f'wgs{b}'], writes=[f'wgb{b}'])
                P.op('pool', lambda e, b=b: e.tensor_copy(wub[b][:], wus[b][:]), reads=[f'wus{b}'], writes=[f'wub{b}'])
                g_ = gtl[b]; gk = f'gtl{b}'
                gf = g_[:].rearrange("p b k -> p (b k)")
                a_ = av[b]; ak = f'av{b}'
                a3 = a_[:].rearrange("p (b t) -> p b t", t=128)
                for gi_, (a, bb) in enumerate(col_groups):
                    n = bb - a
                    pb = ps[1 + gi_]; pk = f'ps{1 + gi_}'
                    for c in range(8):
                        P.op('pe', lambda e, pb=pb, c=c, a=a, bb=bb, n=n, b=b: e.matmul(pb[:, 0:n], lhsT=wgb[b][:, c, :], rhs=h2f[:, c, a:bb],
                                                                                     start=(c == 0), stop=(c == 7)),
                             reads=[f'wgb{b}'] + hk, writes=[pk])
                    P.op('act', lambda e, pb=pb, a=a, bb=bb, n=n, gf=gf: e.copy(gf[:, a:bb], pb[:, 0:n]), reads=[pk], writes=[gk])
                P.op('dve', lambda e, f=f, g_=g_, a3=a3: e.tensor_scalar(a3, g_[:, :, 0:128], smt[:, 8 + 3 * f:9 + 3 * f], None, op0=ALU.mult),
                     reads=[gk, 'smt'], writes=[ak])
                for k in (1, 2):
                    P.op('dve', lambda e, f=f, g_=g_, a3=a3, k=k: e.scalar_tensor_tensor(
                        a3, g_[:, :, k:k + 128], smt[:, 8 + 3 * f + k:9 + 3 * f + k], a3, op0=ALU.mult, op1=ALU.add),
                         reads=[gk, 'smt', ak], writes=[ak])
                P.op('act', lambda e, a_=a_: e.activation(a_[:], a_[:], AF.Silu), reads=[ak], writes=[ak])
                for gi_ in range(2):
                    pb = ps[4 + gi_]; pk = f'ps{4 + gi_}'
                    for c in range(8):
                        P.op('pe', lambda e, pb=pb, c=c, gi_=gi_, b=b: e.matmul(pb[:], lhsT=wub[b][:, c, :], rhs=h2[:, c, gi_ * 4:(gi_ + 1) * 4, 2:130],
                                                                              start=(c == 0), stop=(c == 7)),
                             reads=[f'wub{b}'] + hk, writes=[pk])
                    P.op('dve', lambda e, pb=pb, gi_=gi_, f=f, a_=a_: e.tensor_tensor(
                        prod[:, f, gi_ * 512:(gi_ + 1) * 512], a_[:, gi_ * 512:(gi_ + 1) * 512], pb[:], op=ALU.mult), reads=[pk, ak], writes=[f'prod{f}'])
            pks = [f'prod{f}' for f in range(NFF)]
            di = 0
            for og in range(2):
                for f in range(NFF):
                    b = di % 2; di += 1
                    P.dma('sp' if di % 2 else 'act', wds[b][:], wd[f * 128:(f + 1) * 128, og * 512:(og + 1) * 512], writes=[f'wds{b}'])
                    P.op('pool' if di % 2 else 'dve', lambda e, b=b: e.tensor_copy(wdb[b][:], wds[b][:]), reads=[f'wds{b}'], writes=[f'wdb{b}'])
                    for th in range(2):
                        tsl = slice(th * 512, (th + 1) * 512)
                        for o4 in range(4):
                            pi = th * 4 + o4
                            P.op('pe', lambda e, o4=o4, pi=pi, b=b, f=f, tsl=tsl: e.matmul(
                                ps[pi][:], lhsT=wdb[b][:, o4 * 128:(o4 + 1) * 128], rhs=prod[:, f, tsl],
                                start=(f == 0), stop=(f == NFF - 1)),
                                 reads=[f'wdb{b}'] + pks, writes=[f'ps{pi}'])
                for th in range(2):
                    for o4 in range(4):
                        pi = th * 4 + o4
                        oc = og * 4 + o4
                        o_ = ot[o4 % 2]; ok = f'ot{o4 % 2}'
                        P.op('dve', lambda e, pi=pi, oc=oc, o_=o_, th=th: e.tensor_tensor(
                            o_[:].rearrange("p (b t) -> p b t", t=128), ps[pi][:].rearrange("p (b t) -> p b t", t=128),
                            xm[:, oc, th * 4:(th + 1) * 4, 2:130], op=ALU.add),
                             reads=[f'ps{pi}', 'xm'], writes=[ok])
                        P.dma('sp' if o4 % 2 == 0 else 'act', xoT[oc * 128:(oc + 1) * 128, u0 + th * 512:u0 + (th + 1) * 512], o_[:], reads=[ok])
        ns = P.emit()
        print("L3b: instrs", len(P.ins), "sems", ns, "waits", P.n_waits)


RG = [[0, 1, 2, 3], [4, 5, 6, 7]]


def _allgather(nc, pairs):
    s = nc.alloc_semaphore(name=nc.make_name("cc_sem", True))
    with nc.Block() as block:
        @block.gpsimd
        def _(g):
            for (src, dst) in pairs:
                g.collective_compute("AllGather", mybir.AluOpType.bypass, replica_groups=RG,
                                     ins=[src.ap().opt()], outs=[dst.ap().opt()]).then_inc(s)
            g.wait_ge(s, len(pairs))
    nc.clear_and_free_semaphores([s])
    nc.all_engine_barrier()


def build_fused(nlayers=2, debug=False):
    nc = bass.Bass("TRN2", target_bir_lowering=False)
    ext = lambda name, shape, dt: nc.dram_tensor(name, shape, dt, kind="ExternalInput").ap()
    xT = ext("xT", [1024, 2048], F32)
    pos = ext("pos", [128, 2048], I32)
    w_in = ext("w_in", [2, 1024, 10052], F32)
    gmix = ext("gmix", [2, 128, 8], F32)
    gq = ext("gq", [2, 128, 4], F32)
    cst = ext("cst", [128, 8], F32)
    rmA = ext("rmA", [128, 128], F32)
    rmI = ext("rmI", [128, 128], F32)
    negm = ext("negm", [128, 512], F32)
    mdm = ext("mdm", [128, 4, 4, 128], F32)
    su = ext("su", [128, 128], F32)
    ident = ext("ident", [128, 128], F32)
    pw = ext("pw", [128, NIT], F32)
    sel = ext("sel", [128, 4], F32)
    wo = ext("wo", [2, 4, 512, 1024], F32)
    wm = ext("wm", [2, 1024, 1024], F32)
    sm3a = ext("sm3a", [2, 128, 192], F32)
    wg = ext("wg", [2, 1024, 2816], F32)
    wu = ext("wu", [2, 1024, 2816], F32)
    wd = ext("wd", [2, 2816, 1024], F32)
    sm3b = ext("sm3b", [2, 128, 80], F32)
    out = nc.dram_tensor("out", [1024, 2048], F32, kind="ExternalOutput").ap()
    dk = dict(kind="ExternalOutput") if debug else {}
    o_q = nc.dram_tensor("o_q", [1280, 2048], BF16, **dk)
    o_f32 = nc.dram_tensor("o_f32", [6656, 2048], F32, **dk)
    o_iw = nc.dram_tensor("o_iw", [2048, 4], F32, **dk)
    krows = [0] + [64 + 128 * h for h in range(4)] + [576 + 128 * h for h in range(4)]
    o_k = {r0: nc.dram_tensor(f"o_k{r0}", [64 if r0 == 0 else 128, 2048], BF16) for r0 in krows}
    G_k = {r0: nc.dram_tensor(f"G_k{r0}", [4 * (64 if r0 == 0 else 128), 2048], BF16) for r0 in krows}
    o_v = [nc.dram_tensor(f"o_v{q}", [256, 1024], BF16) for q in range(8)]
    G_v = [nc.dram_tensor(f"G_v{q}", [4 * 256, 1024], BF16) for q in range(8)]
    tail1 = [nc.dram_tensor(f"tail1_{q}", [256, 512], F32) for q in range(8)]
    GT1 = [nc.dram_tensor(f"GT1_{q}", [4 * 256, 512], F32) for q in range(8)]
    yaT = nc.dram_tensor("yaT", [512, 2048], F32, **dk)
    ydT = nc.dram_tensor("ydT", [512, 2048], F32, **dk)
    xm = nc.dram_tensor("xm", [1024, 2048], F32, **dk)
    tail2 = nc.dram_tensor("tail2", [1024, 32], F32)
    GT2 = nc.dram_tensor("GT2", [4 * 1024, 32], F32)
    xo0 = nc.dram_tensor("xo0", [1024, 2048], F32)
    for l in range(nlayers):
        x_in = xT if l == 0 else xo0.ap()
        IOD = dict(xT=x_in, pos=pos, w=w_in[l], gmix=gmix[l], gq=gq[l], cst=cst, rmA=rmA, rmI=rmI,
                 o_q=o_q.ap(), o_k={k: v.ap() for k, v in o_k.items()}, o_f32=o_f32.ap(), o_v=[v.ap() for v in o_v], o_iw=o_iw.ap(), tail1=[v.ap() for v in tail1],
                 G_k={k: v.ap() for k, v in G_k.items()}, G_v=[v.ap() for v in G_v], GT1=[v.ap() for v in GT1], negm=negm, mdm=mdm, su=su, ident=ident, pw=pw, sel=sel,
                 yaT=yaT.ap(), ydT=ydT.ap(), wo=wo[l], wm=wm[l], sm3a=sm3a[l], xm=xm.ap(), tail2=tail2.ap(), GT2=GT2.ap(),
                 wg=wg[l], wu=wu[l], wd=wd[l], sm3b=sm3b[l], xo=(xo0.ap() if l < nlayers - 1 else out))
        IOD['cc_l2'] = ([(o_k[r0], G_k[r0], f'Gk{r0}') for r0 in krows[5:]] + [(o_v[q], G_v[q], f'Gv{q}') for q in range(8)]
                        + [(o_k[r0], G_k[r0], f'Gk{r0}') for r0 in krows[:5]] + [(tail1[q], GT1[q], f'GT1_{q}') for q in range(8)])
        IOD['cc_l3b'] = (tail2, GT2)
        build_l1(nc, IOD, f"a{l}_")
        build_l2(nc, IOD, f"b{l}_")
        build_l3a(nc, IOD, f"c{l}_")
        build_l3b(nc, IOD, f"d{l}_")
    return nc


_NC = {}


def _stripe(a, j):
    return a.reshape((64, 128) + a.shape[1:])[j::4].reshape((2048,) + a.shape[1:])


def kernel(_nlayers=2, _debug=False, **inputs):
    inp = {k: np.asarray(v) for k, v in inputs.items()}
    if 'nc' not in _NC:
        _NC['nc'] = build_fused(_nlayers, _debug)
    nc = _NC['nc']
    cst, RmA, RmI = l1_consts()
    su, ident, pw = l2_consts()
    gq = np.zeros((2, 128, 4), np.float32)
    gq[:, :, 0] = inp['g_qa']; gq[:, :, 1] = inp['g_ka']; gq[:, :64, 2] = inp['g_kidx']
    gmix = np.ascontiguousarray(inp['g_mix'].reshape(2, 8, 128).transpose(0, 2, 1))
    sm3a = np.zeros((2, 128, 192), np.float32)
    sm3b = np.zeros((2, 128, 80), np.float32)
    for l in range(2):
        sm3a[l, :, 0:32] = inp['b_gate'][l].reshape(32, 128).T
        sm3a[l, :, 32:156] = inp['cb_conv_w'][l].T.reshape(4, 128, 31).transpose(1, 0, 2).reshape(128, 124)
        sm3a[l, :, 156:160] = inp['cb_conv_b'][l].reshape(4, 128).T
        sm3a[l, :, 160:164] = inp['cb_ln_g'][l].reshape(4, 128).T
        sm3a[l, :, 164:168] = inp['cb_ln_b'][l].reshape(4, 128).T
        sm3a[l, :, 168:180] = inp['cc_conv_w'][l].T.reshape(4, 128, 3).transpose(1, 0, 2).reshape(128, 12)
        sm3b[l, :, 0:8] = inp['g_ffn'][l].reshape(8, 128).T
        sm3b[l, :, 8:74] = inp['ffn_conv_w'][l].T.reshape(22, 128, 3).transpose(1, 0, 2).reshape(128, 66)
    wo = np.ascontiguousarray(np.stack([inp['w_oa'], inp['w_ob'], inp['w_oc'], inp['w_od']], axis=1))
    shared = dict(w_in=np.ascontiguousarray(inp['w_in']), gmix=gmix, gq=gq, cst=cst, rmA=RmA, rmI=RmI, su=su, ident=ident, pw=pw,
                  wo=wo, wm=np.ascontiguousarray(inp['w_merge']), sm3a=sm3a, wg=np.ascontiguousarray(inp['w_ffn_gate']),
                  wu=np.ascontiguousarray(inp['w_ffn_up']), wd=np.ascontiguousarray(inp['w_ffn_down']), sm3b=sm3b)
    maps = []
    for c in range(8):
        b, j = c // 4, c % 4
        neg, md = l2_masks(j)
        sel = np.zeros((128, 4), np.float32)
        sel[:, (j - 1) % 4] = 1.0
        m = dict(shared)
        m.update(xT=np.ascontiguousarray(_stripe(inp['x'][b], j).T),
                 pos=np.ascontiguousarray(np.broadcast_to(_stripe(inp['positions'][b], j)[None, :], (128, 2048)).astype(np.int32)),
                 negm=neg, mdm=md, sel=sel)
        maps.append(m)
    res = run_bass_kernel_spmd(nc, maps, core_ids=list(range(8)))
    if _debug:
        _NC['res'] = res.results
    out = np.zeros((2, 64, 128, 1024), np.float32)
    for c in range(8):
        b, j = c // 4, c % 4
        out[b, j::4] = np.asarray(res.results[c]['out']).T.reshape(16, 128, 1024)
    return out.reshape(2, 8192, 1024)
```

```python
import math
import contextlib
import numpy as np
from concourse.bass_utils import run_bass_kernel_spmd
import numpy as np
import concourse.bass as bass
import concourse.mybir as mybir

F32 = mybir.dt.float32
BF16 = mybir.dt.bfloat16
I32 = mybir.dt.int32
ALU = mybir.AluOpType
AF = mybir.ActivationFunctionType
AX = mybir.AxisListType

SEM_EPOCH = 30000


class Prog:
    ENGS = ('pe', 'act', 'dve', 'pool', 'sp')

    def __init__(self, nc, n_dma_sems=6):
        self.nc = nc
        self.ins = []
        self.stream = {e: [] for e in self.ENGS}
        self.last_w = {}
        self.readers = {}
        self.n_dma_sems = n_dma_sems
        self.dma_rr = {e: 0 for e in self.ENGS}
        self.dma_slot_last = {}

    def _deps(self, eng, reads, writes, is_dma):
        deps = set()
        for k in reads:
            w = self.last_w.get(k)
            if w is not None:
                deps.add(w)
        for k in writes:
            w = self.last_w.get(k)
            if w is not None:
                deps.add(w)
            for r in self.readers.get(k, ()):
                deps.add(r)
        out = []
        for d in deps:
            di = self.ins[d]
            if (not is_dma) and (not di['dma']) and di['eng'] == eng:
                if eng == 'pe':
                    continue
            out.append(d)
        return out

    def _commit(self, iid, reads, writes):
        for k in reads:
            self.readers.setdefault(k, []).append(iid)
        for k in writes:
            self.last_w[k] = iid
            self.readers[k] = []

    def op(self, eng, fn, reads=(), writes=()):
        reads = list(reads); writes = list(writes)
        deps = self._deps(eng, reads, writes, False)
        iid = len(self.ins)
        self.ins.append(dict(eng=eng, fn=fn, deps=deps, dma=False, signal=False))
        self.stream[eng].append(iid)
        self._commit(iid, reads, writes)
        return iid

    def dma(self, eng, out, in_, reads=(), writes=(), **kw):
        reads = list(reads); writes = list(writes)
        deps = self._deps(eng, reads, writes, True)
        slot = self.dma_rr[eng] % self.n_dma_sems
        self.dma_rr[eng] += 1
        prev = self.dma_slot_last.get((eng, slot))
        if prev is not None and prev not in deps:
            deps.append(prev)
        iid = len(self.ins)
        self.ins.append(dict(eng=eng, fn=None, deps=deps, dma=True, signal=True,
                             slot=slot, out=out, in_=in_, kw=kw))
        self.dma_slot_last[(eng, slot)] = iid
        self.stream[eng].append(iid)
        self._commit(iid, reads, writes)
        return iid

    def cc(self, eng, src, dst, reads=(), writes=()):
        reads = list(reads); writes = list(writes)
        deps = self._deps(eng, reads, writes, True)
        n = self.dma_rr.get((eng, 'cc'), 0)
        self.dma_rr[(eng, 'cc')] = n + 1
        slot = 100 + n % 4
        prev = self.dma_slot_last.get((eng, slot))
        if prev is not None and prev not in deps:
            deps.append(prev)
        iid = len(self.ins)
        self.ins.append(dict(eng=eng, fn=None, deps=deps, dma=True, signal=True, slot=slot, cc=(src, dst), inc=1))
        self.dma_slot_last[(eng, slot)] = iid
        self.stream[eng].append(iid)
        self._commit(iid, reads, writes)
        return iid

    def emit(self, final_wait_eng='sp'):
        nc = self.nc
        ins = self.ins
        pos = {}
        for e in self.ENGS:
            for p, iid in enumerate(self.stream[e]):
                pos[iid] = p
        def chan(i):
            d = ins[i]
            return ('d', d['eng'], d['slot']) if d['dma'] else ('c', d['eng'])
        need = {}
        for e in self.ENGS:
            waited = {}
            for iid in self.stream[e]:
                lst = []
                for d in sorted(ins[iid]['deps'], key=lambda x: -pos[x]):
                    c = chan(d)
                    if waited.get(c, -1) >= pos[d]:
                        continue
                    waited[c] = pos[d]
                    ins[d]['signal'] = True
                    lst.append(d)
                need[iid] = lst
        final = list(self.dma_slot_last.values())
        semcount = {}
        sems = {}
        stack = []

        def get_sem(key):
            if key not in sems:
                s = nc.alloc_semaphore(name=nc.make_name("s_" + "_".join(str(k) for k in key), True))
                sems[key] = s
            return sems[key]

        for e in self.ENGS:
            ccount = 0
            dcount = {}
            for iid in self.stream[e]:
                d = ins[iid]
                if d['dma']:
                    sl = d['slot']
                    dcount[sl] = dcount.get(sl, 0) + 16
                    d['semkey'] = ('d', e, sl, dcount[sl] // (SEM_EPOCH * 16 + 16))
                    d['semval'] = dcount[sl] - d['semkey'][3] * (SEM_EPOCH * 16 + 16) if False else None
                elif d['signal']:
                    ccount += 1
                    ep = (ccount - 1) // SEM_EPOCH
                    d['semkey'] = ('c', e, ep)
                    d['semval'] = ccount - ep * SEM_EPOCH
            dc2 = {}
            for iid in self.stream[e]:
                d = ins[iid]
                if d['dma']:
                    sl = d['slot']
                    n = dc2.get(sl, 0) + 1
                    dc2[sl] = n
                    ep = (n - 1) // 2000
                    d['semkey'] = ('d', e, sl, ep)
                    d['semval'] = d.get('inc', 16) * (n - ep * 2000)
        engobj = {'pe': 'tensor', 'act': 'scalar', 'dve': 'vector', 'pool': 'gpsimd', 'sp': 'sync'}
        self.n_waits = 0

        def run_stream(e, eng):
            for iid in self.stream[e]:
                d = ins[iid]
                for dep in need[iid]:
                    dd = ins[dep]
                    eng.wait_ge(get_sem(dd['semkey']), dd['semval'])
                    self.n_waits += 1
                if d['dma'] and 'cc' in d:
                    eng.collective_compute("AllGather", mybir.AluOpType.bypass, replica_groups=[[0, 1, 2, 3], [4, 5, 6, 7]],
                                           ins=[d['cc'][0].ap().opt()], outs=[d['cc'][1].ap().opt()]).then_inc(get_sem(d['semkey']))
                elif d['dma']:
                    eng.dma_start(out=d['out'], in_=d['in_'], **d['kw']).then_inc(
                        get_sem(d['semkey']), 16)
                else:
                    r = d['fn'](eng)
                    if d['signal']:
                        r.then_inc(get_sem(d['semkey']), 1)
            if e == final_wait_eng:
                for f in final:
                    dd = ins[f]
                    eng.wait_ge(get_sem(dd['semkey']), dd['semval'])

        for e in self.ENGS:
            for iid in self.stream[e]:
                d = ins[iid]
                if d['dma'] or d['signal']:
                    get_sem(d['semkey'])
        with nc.Block() as block:
            for e in self.ENGS:
                if not self.stream[e] and e != final_wait_eng:
                    continue
                deco = getattr(block, engobj[e])
                deco(lambda eng, e=e: run_stream(e, eng))
        nc.clear_and_free_semaphores(list(sems.values()))
        nc.all_engine_barrier()
        return len(sems)


T = 2048
D = 1024
N_IN = 10052
EPS = 1e-6
PI = math.pi
SINK = 0.999999

O_AQ, O_AK, O_AV, O_IQ, O_IK, O_IW, O_GLU, O_CC, O_SQ, O_SK, O_SV, O_GATE = (
    0, 512, 1024, 1536, 1792, 1856, 1860, 2884, 4420, 4932, 5444, 5956)
R_QA, R_QI, R_SQ, NR_Q = 0, 512, 768, 1280
R_KI, R_KA, R_SK, NR_K = 0, 64, 576, 1088
R_GLU, R_CC, R_GATE, NR_F32 = 0, 1024, 2560, 6656


def l1_items():
    items = []
    for h in range(4):
        items.append(('qkA', O_AQ + 128 * h, 128, R_QA + 128 * h, 0))
    for h in range(4):
        items.append(('qkA', O_AK + 128 * h, 128, R_KA + 128 * h, 1))
    items.append(('tmv', O_AV, 256, 0, 0))
    items.append(('tmv', O_AV + 256, 256, 256, 0))
    items.append(('qi', O_IQ, 128, R_QI, 0))
    items.append(('qi', O_IQ + 128, 128, R_QI + 128, 0))
    items.append(('ki', O_IK, 64, R_KI, 0))
    items.append(('iw', O_IW, 4, 0, 0))
    for j in range(8):
        items.append(('f32', O_GLU + 128 * j, 128, R_GLU + 128 * j, 0))
    for j in range(12):
        items.append(('f32', O_CC + 128 * j, 128, R_CC + 128 * j, 0))
    for j in range(4):
        items.append(('bf', O_SQ + 128 * j, 128, R_SQ + 128 * j, 0))
    for j in range(4):
        items.append(('bf', O_SK + 128 * j, 128, R_SK + 128 * j, 0))
    items.append(('tmv', O_SV, 256, 512, 0))
    items.append(('tmv', O_SV + 256, 256, 768, 0))
    for j in range(32):
        items.append(('f32', O_GATE + 128 * j, 128, R_GATE + 128 * j, 0))
    groups = []
    cur = []
    for it in items:
        if cur and (it[1] + it[2] - cur[0][1]) > 256:
            groups.append(cur); cur = []
        cur.append(it)
    if cur:
        groups.append(cur)
    return groups


def l1_consts():
    half = 16
    invA = (500000.0 ** (-(np.arange(half, dtype=np.float32) * 2.0) / 32)).astype(np.float32)
    invI = (500000.0 ** (-(np.arange(8, dtype=np.float32) * 2.0) / 16)).astype(np.float32)
    cst = np.zeros((128, 8), np.float32)
    for p in range(32):
        cst[p, 0] = invA[p % 16]
    for p in range(128):
        if p % 64 < 16:
            cst[p, 1] = invI[p % 8]
    cst[:, 3] = 0.0
    cst[:, 4] = 0.5 * PI * SINK
    RmA = np.zeros((128, 128), np.float32)
    for m in range(16):
        RmA[m + 16, m] = -1.0
        RmA[m, m + 16] = 1.0
    RmI = np.zeros((128, 128), np.float32)
    for b in (0, 64):
        for m in range(8):
            RmI[b + m + 8, b + m] = -1.0
            RmI[b + m, b + m + 8] = 1.0
    return cst, RmA, RmI


def build_l1(nc, IO, pfx):
    xT, pos, w, gmix, gq, cst, rmA, rmI = IO['xT'], IO['pos'], IO['w'], IO['gmix'], IO['gq'], IO['cst'], IO['rmA'], IO['rmI']
    o_q, o_k, o_f32, o_v, o_iw, tail1 = IO['o_q'], IO['o_k'], IO['o_f32'], IO['o_v'], IO['o_iw'], IO['tail1']

    P = Prog(nc)
    import contextlib
    es = contextlib.ExitStack()

    def sb(name, shape, dt):
        return es.enter_context(nc.sbuf_tensor(pfx + name, shape, dt))

    def pst(name, shape, dt=F32):
        return es.enter_context(nc.psum_tensor(pfx + name, shape, dt))

    with es:
        hT = sb("hT", [128, 8, T], BF16)
        xs = [sb(f"xs{i}", [128, 8, 256], F32) for i in range(2)]
        sqs = sb("sqs", [128, 8, 256], F32)
        rs = sb("rs", [128, 256], F32)
        gm = sb("gm", [128, 8], F32)
        gqt = sb("gqt", [128, 4], F32)
        cs = sb("cs", [128, 8], F32)
        rA = sb("rA", [128, 128], F32)
        rI = sb("rI", [128, 128], F32)
        ones = sb("ones", [128, 128], F32)
        posi = sb("posi", [128, T], I32)
        posf = sb("posf", [128, T], F32)
        ang = sb("ang", [128, T], F32)
        posf2 = sb("posf2", [128, T], F32)
        posi2 = sb("posi2", [128, T], I32)
        cosA = sb("cosA", [128, T], F32)
        sinA = sb("sinA", [128, T], F32)
        cosI = sb("cosI", [128, T], F32)
        sinI = sb("sinI", [128, T], F32)
        wf = [sb(f"wf{i}", [128, 8, 256], F32) for i in range(2)]
        wb = [sb(f"wb{i}", [128, 8, 256], BF16) for i in range(2)]
        of32 = [sb(f"of{i}", [128, T], F32) for i in range(2)]
        obf = [sb(f"ob{i}", [128, T], BF16) for i in range(2)]
        ov = [sb(f"ov{i}", [128, 16, 256], BF16) for i in range(2)]
        oiw = sb("oiw", [128, 16, 4], F32)
        raw = sb("raw", [128, 512], F32)
        sq = sb("sq", [128, 512], F32)
        rstd = sb("rstd", [128, 512], F32)
        nrm = sb("nrm", [128, 512], F32)
        t1 = sb("t1", [128, 512], F32)
        t2 = sb("t2", [128, 512], F32)
        psm = [pst(f"psm{i}", [128, 512]) for i in range(4)]
        psn = pst("psn", [128, 512])
        psr = pst("psr", [128, 512])

        P.dma('sp', gm[:], gmix, writes=['gm'])
        P.dma('sp', gqt[:], gq, writes=['gqt'])
        P.dma('sp', cs[:], cst, writes=['cs'])
        P.dma('sp', rA[:], rmA, writes=['rA'])
        P.dma('sp', rI[:], rmI, writes=['rI'])
        P.dma('sp', posi[:], pos, writes=['posi'])
        P.op('dve', lambda e: e.memset(ones[:], 1.0), writes=['ones'])
        P.op('dve', lambda e: e.tensor_copy(posf[:], posi[:]), reads=['posi'], writes=['posf'])
        for j, (ct, st) in enumerate(((cosA, sinA), (cosI, sinI))):
            cn, sn = f"cos{j}", f"sin{j}"
            P.op('dve', lambda e, j=j: e.tensor_scalar(ang[:], posf[:], cs[:, j:j + 1], None, op0=ALU.mult),
                 reads=['posf', 'cs'], writes=['ang'])
            for (tt_, tn, shift, bcol) in ((st, sn, 0.0, 3), (ct, cn, 0.5 * PI, 4)):
                P.op('dve', lambda e, shift=shift: e.tensor_scalar(posf2[:], ang[:], 1.0 / (2 * PI), 0.5 + shift / (2 * PI),
                                                                   op0=ALU.mult, op1=ALU.add),
                     reads=['ang'], writes=['posf2'])
                P.op('dve', lambda e: e.tensor_copy(posi2[:], posf2[:]), reads=['posf2'], writes=['posi2'])
                P.op('dve', lambda e: e.tensor_copy(posf2[:], posi2[:]), reads=['posi2'], writes=['posf2'])
                P.op('dve', lambda e, tt_=tt_: e.scalar_tensor_tensor(tt_[:], posf2[:], -2 * PI, ang[:], op0=ALU.mult, op1=ALU.add),
                     reads=['posf2', 'ang'], writes=[tn])
                P.op('dve', lambda e, tt_=tt_, shift=shift: e.tensor_scalar(posf2[:], tt_[:], -PI - shift, 2 * PI,
                                                                          op0=ALU.is_lt, op1=ALU.mult),
                     reads=[tn], writes=['posf2'])
                P.op('dve', lambda e, tt_=tt_: e.tensor_tensor(tt_[:], tt_[:], posf2[:], op=ALU.add),
                     reads=[tn, 'posf2'], writes=[tn])
                P.op('act', lambda e, tt_=tt_, bcol=bcol: e.activation(tt_[:], tt_[:], AF.Sin, bias=cs[:, bcol:bcol + 1], scale=SINK),
                     reads=[tn, 'cs'], writes=[tn])

        xTv = xT.rearrange("(c p) t -> p c t", p=128)
        for tg in range(8):
            xb = xs[tg % 2]
            xk = f"xs{tg % 2}"
            P.dma('sp', xb[:], xTv[:, :, tg * 256:(tg + 1) * 256], writes=[xk])
            P.op('act', lambda e, xb=xb: e.activation(sqs[:], xb[:], AF.Square), reads=[xk], writes=['sqs'])
            for c in range(8):
                P.op('pe', lambda e, c=c: e.matmul(psn[:, 0:256], lhsT=ones[:], rhs=sqs[:, c, :],
                                                   start=(c == 0), stop=(c == 7)),
                     reads=['ones', 'sqs'], writes=['psn'])
            P.op('dve', lambda e: e.tensor_scalar(rs[:], psn[:, 0:256], 1.0 / D, EPS, op0=ALU.mult, op1=ALU.add),
                 reads=['psn'], writes=['rs'])
            P.op('act', lambda e: e.activation(rs[:], rs[:], AF.Sqrt), reads=['rs'], writes=['rs'])
            P.op('dve', lambda e: e.reciprocal(rs[:], rs[:]), reads=['rs'], writes=['rs'])
            for c in range(8):
                P.op('dve', lambda e, c=c, xb=xb, tg=tg: e.scalar_tensor_tensor(
                    hT[:, c, tg * 256:(tg + 1) * 256], xb[:, c, :], gm[:, c:c + 1], rs[:],
                    op0=ALU.mult, op1=ALU.mult),
                     reads=[xk, 'gm', 'rs'], writes=[f'hT{tg}'])
        hkeys = [f'hT{tg}' for tg in range(8)]

        wv = w.rearrange("(c p) n -> p c n", p=128)
        groups = l1_items()
        cnt = dict(ps=0, of=0, ob=0, ov=0)
        for gi, grp in enumerate(groups):
            c0 = grp[0][1]
            c1 = grp[-1][1] + grp[-1][2]
            nco = c1 - c0
            b = gi % 2
            P.dma('sp', wf[b][:, :, 0:nco], wv[:, :, c0:c1], writes=[f'wf{b}'])
            ceng = 'pool' if gi % 2 == 0 else 'dve'
            P.op(ceng, lambda e, b=b, nco=nco: e.tensor_copy(wb[b][:, :, 0:nco], wf[b][:, :, 0:nco]),
                 reads=[f'wf{b}'], writes=[f'wb{b}'])
            for (kind, col, ncol, orow, aux) in grp:
                lo = col - c0
                if kind in ('tmv', 'iw'):
                    if kind == 'tmv':
                        ob_ = ov[cnt['ov'] % 2]; okey = f"ov{cnt['ov'] % 2}"; cnt['ov'] += 1
                    else:
                        ob_ = oiw; okey = 'oiw'
                    for tt in range(16):
                        pi = cnt['ps'] % 4; cnt['ps'] += 1
                        for c in range(8):
                            P.op('pe', lambda e, pi=pi, c=c, tt=tt, lo=lo, ncol=ncol, b=b: e.matmul(
                                psm[pi][:, 0:ncol], lhsT=hT[:, c, tt * 128:(tt + 1) * 128],
                                rhs=wb[b][:, c, lo:lo + ncol], start=(c == 0), stop=(c == 7)),
                                 reads=hkeys + [f'wb{b}'], writes=[f'psm{pi}'])
                        ee = 'act' if tt % 2 == 0 else 'dve'
                        if ee == 'act':
                            P.op('act', lambda e, pi=pi, tt=tt, ncol=ncol, ob_=ob_: e.copy(ob_[:, tt, 0:ncol], psm[pi][:, 0:ncol]),
                                 reads=[f'psm{pi}'], writes=[okey])
                        else:
                            P.op('dve', lambda e, pi=pi, tt=tt, ncol=ncol, ob_=ob_: e.tensor_copy(ob_[:, tt, 0:ncol], psm[pi][:, 0:ncol]),
                                 reads=[f'psm{pi}'], writes=[okey])
                    if kind == 'tmv':
                        for q in range(8):
                            P.dma('act', o_v[q].rearrange("(tt p) c -> p tt c", p=128)[:, :, orow:orow + 256], ob_[:, 2 * q:2 * q + 2, :], reads=[okey])
                    else:
                        P.dma('act', o_iw.rearrange("(tt p) c -> p tt c", p=128), ob_[:], reads=[okey])
                    continue
                if kind == 'f32':
                    ob_ = of32[cnt['of'] % 2]; okey = f"of{cnt['of'] % 2}"; cnt['of'] += 1
                else:
                    ob_ = obf[cnt['ob'] % 2]; okey = f"ob{cnt['ob'] % 2}"; cnt['ob'] += 1
                for g in range(4):
                    pi = cnt['ps'] % 4; cnt['ps'] += 1
                    ts_ = slice(g * 512, (g + 1) * 512)
                    for c in range(8):
                        P.op('pe', lambda e, pi=pi, c=c, lo=lo, ncol=ncol, b=b, ts_=ts_: e.matmul(
                            psm[pi][0:ncol, :], lhsT=wb[b][:, c, lo:lo + ncol], rhs=hT[:, c, ts_],
                            start=(c == 0), stop=(c == 7)),
                             reads=hkeys + [f'wb{b}'], writes=[f'psm{pi}'])
                    pk = f'psm{pi}'
                    if kind in ('f32', 'bf'):
                        if g % 2 == 0:
                            P.op('act', lambda e, pi=pi, ts_=ts_, ob_=ob_: e.copy(ob_[:, ts_], psm[pi][:]),
                                 reads=[pk], writes=[okey])
                        else:
                            P.op('dve', lambda e, pi=pi, ts_=ts_, ob_=ob_: e.tensor_copy(ob_[:, ts_], psm[pi][:]),
                                 reads=[pk], writes=[okey])
                        continue
                    np_ = ncol
                    if kind in ('qkA', 'ki'):
                        gcol = aux if kind == 'qkA' else 2
                        P.op('act', lambda e, pi=pi, np_=np_: e.copy(raw[0:np_, :], psm[pi][0:np_, :]), reads=[pk], writes=['raw'])
                        P.op('act', lambda e, pi=pi, np_=np_: e.activation(sq[0:np_, :], psm[pi][0:np_, :], AF.Square),
                             reads=[pk], writes=['sq'])
                        P.op('pe', lambda e, np_=np_: e.matmul(psn[0:np_, :], lhsT=ones[0:np_, 0:np_], rhs=sq[0:np_, :],
                                                              start=True, stop=True),
                             reads=['ones', 'sq'], writes=['psn'])
                        P.op('dve', lambda e, np_=np_: e.tensor_scalar(rstd[0:np_, :], psn[0:np_, :], 1.0 / np_, EPS,
                                                                      op0=ALU.mult, op1=ALU.add),
                             reads=['psn'], writes=['rstd'])
                        P.op('act', lambda e, np_=np_: e.activation(rstd[0:np_, :], rstd[0:np_, :], AF.Sqrt),
                             reads=['rstd'], writes=['rstd'])
                        P.op('dve', lambda e, np_=np_: e.reciprocal(rstd[0:np_, :], rstd[0:np_, :]),
                             reads=['rstd'], writes=['rstd'])
                        P.op('dve', lambda e, np_=np_, gcol=gcol: e.scalar_tensor_tensor(
                            nrm[0:np_, :], raw[0:np_, :], gqt[0:np_, gcol:gcol + 1], rstd[0:np_, :],
                            op0=ALU.mult, op1=ALU.mult), reads=['raw', 'gqt', 'rstd'], writes=['nrm'])
                        src = nrm
                    else:
                        P.op('act', lambda e, pi=pi: e.copy(nrm[:], psm[pi][:]), reads=[pk], writes=['nrm'])
                        src = nrm
                    if kind == 'qkA':
                        rp = 32; R = rA; ct, st, cn, sn = cosA, sinA, 'cos0', 'sin0'
                    else:
                        rp = np_; R = rI; ct, st, cn, sn = cosI, sinI, 'cos1', 'sin1'
                    P.op('pe', lambda e, rp=rp, R=R: e.matmul(psr[0:rp, :], lhsT=R[0:rp, 0:rp], rhs=nrm[0:rp, :],
                                                             start=True, stop=True),
                         reads=['nrm', 'rA', 'rI'], writes=['psr'])
                    P.op('pool', lambda e, rp=rp, ct=ct, ts_=ts_: e.tensor_tensor(t1[0:rp, :], nrm[0:rp, :], ct[0:rp, ts_], op=ALU.mult),
                         reads=['nrm', cn], writes=['t1'])
                    P.op('dve', lambda e, rp=rp, st=st, ts_=ts_: e.tensor_tensor(t2[0:rp, :], psr[0:rp, :], st[0:rp, ts_], op=ALU.mult),
                         reads=['psr', sn], writes=['t2'])
                    P.op('pool', lambda e, rp=rp, ob_=ob_, ts_=ts_: e.tensor_tensor(ob_[0:rp, ts_], t1[0:rp, :], t2[0:rp, :], op=ALU.add),
                         reads=['t1', 't2'], writes=[okey])
                    if rp < np_:
                        P.op('act', lambda e, ob_=ob_, ts_=ts_: e.copy(ob_[32:64, ts_], nrm[32:64, :]),
                             reads=['nrm'], writes=[okey])
                        P.op('act', lambda e, ob_=ob_, ts_=ts_: e.copy(ob_[64:128, ts_], nrm[64:128, :]),
                             reads=['nrm'], writes=[okey])
                oq_ = 'act'
                if kind == 'f32':
                    P.dma(oq_, o_f32[orow:orow + ncol, :], ob_[0:ncol, :], reads=[okey], writes=['o_f32'])
                else:
                    if (kind == 'ki' or (kind == 'qkA' and aux == 1) or (kind == 'bf' and col >= O_SK)):
                        P.dma('act', o_k[orow][0:ncol, :], ob_[0:ncol, :], reads=[okey])
                    else:
                        P.dma('act', o_q[orow:orow + ncol, :], ob_[0:ncol, :], reads=[okey])
        for (r0, t0) in ((R_GLU, 0), (R_CC + 512, 1024)):
            for q in range(4):
                P.dma('sp', tail1[(t0 + q * 256) // 256].rearrange("r (b k) -> r b k", k=32),
                      o_f32[r0 + q * 256:r0 + (q + 1) * 256, :].rearrange("r (b t) -> r b t", t=128)[:, :, 96:128],
                      reads=['o_f32'])
        ns = P.emit()
        print("L1: instrs", len(P.ins), "sems", ns, "waits", P.n_waits)


S = 8192
NQ = 16
NIT = 18
NEG = -1.0e30
MNEG = -30000.0
SCALE = 128 ** -0.5


def l2_masks(j):
    neg = np.zeros((128, 4, 128), np.float32)
    md = np.zeros((128, 4, 4, 128), np.float32)
    tp = np.arange(128)[:, None]
    sp = np.arange(128)[None, :]
    for m in range(4):
        if m < j:
            neg[:, m, :] = 0.0
            md[:, m, :, :] = 1.0
        elif m == j:
            neg[:, m, :] = np.where(sp <= tp, 0.0, NEG)
            md[:, m, :, :] = (np.arange(128)[:, None] < np.arange(128)[None, :]).astype(np.float32)[:, None, :]
        else:
            neg[:, m, :] = NEG
            md[:, m, :, :] = 0.0
    return neg.reshape(128, 512), md


def l2_consts():
    su = (np.arange(128)[:, None] > np.arange(128)[None, :]).astype(np.float32)
    ident = np.eye(128, dtype=np.float32)
    pw = (2.0 ** -(np.arange(NIT, dtype=np.float32) + 1.0))[None, :].repeat(128, 0).astype(np.float32)
    return su, ident, pw


def build_l2(nc, IO, pfx, do_a=True, do_d=True, nq=NQ):
    G_k, G_v, o_q, o_iw = IO['G_k'], IO['G_v'], IO['o_q'], IO['o_iw']
    negm, mdm, su_d, id_d, pw_d = IO['negm'], IO['mdm'], IO['su'], IO['ident'], IO['pw']
    yaT, ydT = IO['yaT'], IO['ydT']
    Gv4 = [g.rearrange("(r i p) n -> p r i n", r=4, p=128) for g in G_v]

    def load_nat(P, eng, dst2d, row0, nrows, key):
        dv = dst2d.rearrange("p (i r t) -> p i r t", r=4, t=128)
        for r in range(4):
            P.dma('sp', dv[:, :, r, :],
                  G_k[row0][r * nrows:(r + 1) * nrows, :].rearrange("p (i t) -> p i t", t=128), reads=[f'Gk{row0}'], writes=[key])

    P = Prog(nc)
    es = contextlib.ExitStack()

    def sb(name, shape, dt):
        return es.enter_context(nc.sbuf_tensor(pfx + name, shape, dt))

    def pst(name, shape, dt=F32):
        return es.enter_context(nc.psum_tensor(pfx + name, shape, dt))

    with es:
        KT = sb("KT", [128, 4, S], BF16)
        kit = sb("kit", [64, S], BF16)
        score = sb("score", [128, S], F32)
        mneg = sb("mneg", [128, S], BF16)
        junk = sb("junk", [128, S], BF16)
        vt = [sb(f"vt{i}", [128, 4, 512], BF16) for i in range(3)]
        qblk = [sb(f"qblk{i}", [128, 4, 128], BF16) for i in range(2)]
        qiblk = [sb(f"qiblk{i}", [64, 4, 128], BF16) for i in range(2)]
        iwt = sb("iwt", [128, NQ, 4], F32)
        absw = sb("absw", [128, NQ, 4], F32)
        sgnw = sb("sgnw", [128, NQ, 4], F32)
        negt = sb("negt", [128, 512], F32)
        mdt = sb("mdt", [128, 4, 4, 128], F32)
        sut = sb("sut", [128, 128], F32)
        idf = sb("idf", [128, 128], F32)
        idb = sb("idb", [128, 128], BF16)
        pwt = sb("pwt", [128, NIT], F32)
        ones_f = sb("ones_f", [128, 128], F32)
        ones_b = sb("ones_b", [128, 128], BF16)
        rh = [sb(f"rh{i}", [128, 512], F32) for i in range(2)]
        st = sb("st", [128, 16], F32)
        wk = sb("wk", [128, NIT], F32)
        PT = [sb(f"PT{i}", [128, 512], BF16) for i in range(2)]
        rec = sb("rec", [128, 512], F32)
        oat = [sb(f"oat{i}", [128, 512], F32) for i in range(2)]
        e_t = [sb(f"e_t{i}", [128, 512], F32) for i in range(2)]
        sp_t = [sb(f"sp_t{i}", [128, 512], F32) for i in range(3)]
        u_t = [sb(f"u_t{i}", [128, 512], F32) for i in range(2)]
        R_t = [sb(f"R_t{i}", [128, 512], F32) for i in range(2)]
        aT = [sb(f"aT{i}", [128, 512], BF16) for i in range(2)]
        ps = [pst(f"ps{i}", [128, 512]) for i in range(8)]

        for (src_t, dst_t, key) in IO['cc_l2']:
            P.cc('pool', src_t, dst_t, writes=[key])
        P.dma('sp', iwt[:], o_iw.rearrange("(i t) h -> t i h", t=128), writes=['iwt'])
        P.dma('sp', negt[:], negm, writes=['negt'])
        P.dma('sp', mdt[:], mdm, writes=['mdt'])
        P.dma('sp', sut[:], su_d, writes=['sut'])
        P.dma('sp', idf[:], id_d, writes=['idf'])
        P.dma('sp', pwt[:], pw_d, writes=['pwt'])
        P.op('dve', lambda e: e.memset(ones_f[:], 1.0), writes=['ones_f'])
        P.op('dve', lambda e: e.memset(ones_b[:], 1.0), writes=['ones_b'])
        P.op('dve', lambda e: e.tensor_copy(idb[:], idf[:]), reads=['idf'], writes=['idb'])
        P.op('act', lambda e: e.activation(absw[:], iwt[:], AF.Abs), reads=['iwt'], writes=['absw'])
        P.op('act', lambda e: e.activation(sgnw[:], iwt[:], AF.Sign), reads=['iwt'], writes=['sgnw'])

        hsl = [slice(h * 128, (h + 1) * 128) for h in range(4)]

        if do_d:
            for h in range(4):
                load_nat(P, 'sp', KT[:, h, :], 576 + h * 128, 128, f'KT{h}')
            vcnt = 0
            for i in range(nq):
                NK = 4 * i + 4
                NG = i + 1
                qb_ = qblk[i % 2]; qbk = f'qblk{i % 2}'
                P.dma('sp', qb_[:], o_q[768:1280, i * 128:(i + 1) * 128].rearrange("(h d) t -> d h t", d=128), writes=[qbk])
                chs = list(range(NK - 1, -1, -1))
                vbs = {}

                def s1(k, ch):
                    nonlocal vcnt
                    kg, cc = ch // 4, ch % 4
                    if cc == 3:
                        vb = vt[vcnt % 3]; vk = f'vt{vcnt % 3}'; vcnt += 1
                        P.dma('sp', vb[:], Gv4[kg // 2][:, :, kg % 2, 512:1024], reads=[f'Gv{kg // 2}'], writes=[vk])
                        vbs[kg] = (vb, vk)
                    cs_ = slice(ch * 128, (ch + 1) * 128)
                    pz = ps[k % 2]; pzk = f'ps{k % 2}'
                    et = e_t[k % 2]; ek = f'e_t{k % 2}'
                    spt = sp_t[k % 3]; spk = f'sp{k % 3}'
                    for h in range(4):
                        P.op('pe', lambda e, pz=pz, h=h, cs_=cs_, qb_=qb_: e.matmul(pz[:, hsl[h]], lhsT=KT[:, h, cs_], rhs=qb_[:, h, :], start=True, stop=True),
                             reads=[f'KT{h}', qbk], writes=[pzk])
                    P.op('act', lambda e, pz=pz, et=et: e.activation(et[:], pz[:], AF.Exp, scale=SCALE), reads=[pzk], writes=[ek])
                    P.op('act', lambda e, spt=spt, et=et: e.activation(spt[:], et[:], AF.Ln, bias=ones_f[:, 0:1]), reads=[ek, 'ones_f'], writes=[spk])
                    if kg == NG - 1:
                        P.op('dve', lambda e, spt=spt, cc=cc: e.tensor_tensor(
                            spt[:], spt[:], mdt[:, cc, :, :].rearrange("p h t -> p (h t)"), op=ALU.mult), reads=[spk, 'mdt'], writes=[spk])

                def s2(k, ch):
                    first = (k == 0)
                    pz = ps[k % 2]; pzk = f'ps{k % 2}'
                    pB = ps[2 + k % 2]; pBk = f'ps{2 + k % 2}'
                    spt = sp_t[k % 3]; spk = f'sp{k % 3}'
                    ut = u_t[k % 2]; uk = f'u_t{k % 2}'
                    Rp, Rpk = R_t[k % 2], f'R_t{k % 2}'
                    Rn, Rnk = R_t[(k + 1) % 2], f'R_t{(k + 1) % 2}'
                    P.op('pe', lambda e, pB=pB, spt=spt, first=first: e.matmul(pB[:], lhsT=sut[:], rhs=spt[:], start=True, stop=first),
                         reads=['sut', spk], writes=[pBk])
                    if not first:
                        P.op('pe', lambda e, pB=pB, Rp=Rp: e.matmul(pB[:], lhsT=ones_f[:], rhs=Rp[:], start=False, stop=True),
                             reads=['ones_f', Rpk], writes=[pBk])
                    if first:
                        P.op('dve', lambda e, spt=spt, Rn=Rn: e.tensor_copy(Rn[:], spt[:]), reads=[spk], writes=[Rnk])
                    elif k + 1 < NK:
                        P.op('dve', lambda e, spt=spt, Rn=Rn, Rp=Rp: e.tensor_tensor(Rn[:], Rp[:], spt[:], op=ALU.add), reads=[spk, Rpk], writes=[Rnk])
                    P.op('dve', lambda e, pz=pz, spt=spt, ut=ut: e.scalar_tensor_tensor(ut[:], pz[:], SCALE, spt[:], op0=ALU.mult, op1=ALU.subtract),
                         reads=[pzk, spk], writes=[uk])
                    P.op('dve', lambda e, pB=pB, ut=ut: e.tensor_tensor(ut[:], ut[:], pB[:], op=ALU.subtract), reads=[uk, pBk], writes=[uk])

                def s3(k, ch):
                    first = (k == 0)
                    kg, cc = ch // 4, ch % 4
                    ut = u_t[k % 2]; uk = f'u_t{k % 2}'
                    at = aT[k % 2]; atk = f'aT{k % 2}'
                    vb, vk = vbs[kg]
                    P.op('act', lambda e, at=at, ut=ut: e.activation(at[:], ut[:], AF.Exp), reads=[uk], writes=[atk])
                    if kg == NG - 1:
                        P.op('dve', lambda e, at=at, cc=cc: e.tensor_tensor(
                            at[:], at[:], mdt[:, cc, :, :].rearrange("p h t -> p (h t)"), op=ALU.mult), reads=[atk, 'mdt'], writes=[atk])
                    for h in range(4):
                        P.op('pe', lambda e, at=at, h=h, vb=vb, cc=cc, first=first, ch=ch: e.matmul(
                            ps[4 + h][:, 0:128], lhsT=vb[:, cc, hsl[h]], rhs=at[:, hsl[h]], start=first, stop=(ch == 0)),
                             reads=[atk, vk], writes=[f'ps{4 + h}'])

                for n in range(NK + 2):
                    if n < NK:
                        s1(n, chs[n])
                    if 0 <= n - 1 < NK:
                        s2(n - 1, chs[n - 1])
                    if 0 <= n - 2 < NK:
                        s3(n - 2, chs[n - 2])
                ot = oat[i % 2]; otk = f'oat{i % 2}'
                for h in range(4):
                    if h % 2 == 0:
                        P.op('dve', lambda e, h=h, ot=ot: e.tensor_copy(ot[:, hsl[h]], ps[4 + h][:, 0:128]), reads=[f'ps{4 + h}'], writes=[otk])
                    else:
                        P.op('act', lambda e, h=h, ot=ot: e.copy(ot[:, hsl[h]], ps[4 + h][:, 0:128]), reads=[f'ps{4 + h}'], writes=[otk])
                P.dma('sp', ydT[:, i * 128:(i + 1) * 128].rearrange("(h d) t -> d h t", d=128), ot[:].rearrange("p (h t) -> p h t", t=128), reads=[otk])
        if do_a:
            load_nat(P, 'sp', kit[:, :], 0, 64, 'kit')
            for h in range(4):
                load_nat(P, 'sp', KT[:, h, :], 64 + h * 128, 128, f'KT{h}')
            vstate = dict(cnt=0)

            def a_score(i):
                NG = i + 1
                Sc = 512 * NG
                qb_ = qblk[i % 2]; qbk = f'qblk{i % 2}'
                qib = qiblk[i % 2]; qik = f'qiblk{i % 2}'
                P.dma('sp', qb_[:], o_q[0:512, i * 128:(i + 1) * 128].rearrange("(h d) t -> d h t", d=128), writes=[qbk])
                P.dma('sp', qib[:], o_q[512:768, i * 128:(i + 1) * 128].rearrange("(h d) t -> d h t", d=64), writes=[qik])
                for kg in range(NG):
                    ks = slice(kg * 512, (kg + 1) * 512)
                    for h in range(4):
                        pb = ps[h % 2]; pk = f'ps{h % 2}'
                        P.op('pe', lambda e, pb=pb, h=h, ks=ks, qib=qib: e.matmul(pb[:], lhsT=qib[:, h, :], rhs=kit[:, ks], start=True, stop=True),
                             reads=[qik, 'kit'], writes=[pk])
                        r_ = rh[h % 2]; rk = f'rh{h % 2}'
                        P.op('act', lambda e, pb=pb, r_=r_, i=i, h=h: e.activation(r_[:], pb[:], AF.Relu, scale=absw[:, i, h:h + 1]),
                             reads=[pk, 'absw'], writes=[rk])
                        if h == 0:
                            P.op('dve', lambda e, r_=r_, ks=ks, i=i, h=h: e.tensor_scalar(score[:, ks], r_[:], sgnw[:, i, h:h + 1], None, op0=ALU.mult),
                                 reads=[rk, 'sgnw'], writes=['score'])
                        else:
                            P.op('dve', lambda e, r_=r_, ks=ks, i=i, h=h: e.scalar_tensor_tensor(
                                score[:, ks], r_[:], sgnw[:, i, h:h + 1], score[:, ks], op0=ALU.mult, op1=ALU.add),
                                 reads=[rk, 'sgnw', 'score'], writes=['score'])
                P.op('dve', lambda e, Sc=Sc: e.tensor_reduce(st[:, 1:2], score[:, 0:Sc], axis=AX.X, op=ALU.max), reads=['score'], writes=['st_hi'])
                P.op('dve', lambda e, Sc=Sc: e.tensor_reduce(st[:, 0:1], score[:, 0:Sc], axis=AX.X, op=ALU.min), reads=['score'], writes=['st_lo'])
                P.op('pool', lambda e, Sc=Sc: e.tensor_tensor(score[:, Sc - 512:Sc], score[:, Sc - 512:Sc], negt[:], op=ALU.add),
                     reads=['score', 'negt'], writes=['score'])

            def a_bisect(i):
                Sc = 512 * (i + 1)
                P.op('dve', lambda e: e.tensor_tensor(st[:, 2:3], st[:, 1:2], st[:, 0:1], op=ALU.subtract), reads=['st_hi', 'st_lo'], writes=['st_W'])
                P.op('dve', lambda e: e.tensor_scalar(wk[:], pwt[:], st[:, 2:3], None, op0=ALU.mult), reads=['pwt', 'st_W'], writes=['wk'])
                P.op('dve', lambda e: e.tensor_tensor(st[:, 3:4], st[:, 0:1], wk[:, 0:1], op=ALU.add), reads=['st_lo', 'wk'], writes=['st_mid'])
                yield
                Sa = max(64, int(round(0.56 * Sc / 64.0)) * 64)
                for k in range(NIT):
                    P.op('act', lambda e, Sa=Sa: e.activation(junk[:, 0:Sa], score[:, 0:Sa], AF.Sign, bias=st[:, 3:4], scale=-1.0,
                                                              accum_out=st[:, 4:5]),
                         reads=['score', 'st_mid'], writes=['junkA', 'st_cntA'])
                    P.op('dve', lambda e, Sa=Sa, Sc=Sc: e.tensor_scalar(junk[:, Sa:Sc], score[:, Sa:Sc], st[:, 3:4], None,
                                                                        op0=ALU.is_ge, op1=ALU.add, accum_out=st[:, 6:7]),
                         reads=['score', 'st_mid'], writes=['junkD', 'st_cntD'])
                    P.op('dve', lambda e: e.scalar_tensor_tensor(st[:, 7:8], st[:, 4:5], -0.5, st[:, 6:7], op0=ALU.mult, op1=ALU.add),
                         reads=['st_cntA', 'st_cntD'], writes=['st_tmp1'])
                    P.op('dve', lambda e, k=k, Sa=Sa: e.scalar_tensor_tensor(st[:, 5:6], st[:, 7:8], 255.5 - 0.5 * Sa, wk[:, k:k + 1],
                                                                             op0=ALU.is_ge, op1=ALU.mult),
                         reads=['st_tmp1', 'wk'], writes=['st_tmp'])
                    P.op('dve', lambda e: e.tensor_tensor(st[:, 0:1], st[:, 0:1], st[:, 5:6], op=ALU.add), reads=['st_lo', 'st_tmp'], writes=['st_lo'])
                    if k + 1 < NIT:
                        P.op('dve', lambda e, k=k: e.tensor_tensor(st[:, 3:4], st[:, 0:1], wk[:, k + 1:k + 2], op=ALU.add),
                             reads=['st_lo', 'wk'], writes=['st_mid'])
                    yield

            def a_mask(i):
                Sc = 512 * (i + 1)
                P.op('dve', lambda e, Sc=Sc: e.tensor_scalar(mneg[:, 0:Sc], score[:, 0:Sc], st[:, 0:1], MNEG, op0=ALU.is_lt, op1=ALU.mult),
                     reads=['score', 'st_lo'], writes=['mneg'])

            def a_attn(i):
                NK = 4 * i + 4
                qb_ = qblk[i % 2]; qbk = f'qblk{i % 2}'
                vbs = {}

                def s1(ch):
                    if ch % 4 == 0:
                        kg = ch // 4
                        vb = vt[vstate['cnt'] % 3]; vk = f"vt{vstate['cnt'] % 3}"; vstate['cnt'] += 1
                        P.dma('sp', vb[:], Gv4[kg // 2][:, :, kg % 2, 0:512], reads=[f'Gv{kg // 2}'], writes=[vk])
                        vbs[kg] = (vb, vk)
                    cs_ = slice(ch * 128, (ch + 1) * 128)
                    pl = ps[ch % 2]; plk = f'ps{ch % 2}'
                    for h in range(4):
                        P.op('pe', lambda e, pl=pl, h=h, cs_=cs_: e.matmul(pl[:, hsl[h]], lhsT=KT[:, h, cs_], rhs=qb_[:, h, :], start=True, stop=False),
                             reads=[f'KT{h}', qbk], writes=[plk])
                        P.op('pe', lambda e, pl=pl, h=h, cs_=cs_: e.matmul(pl[:, hsl[h]], lhsT=mneg[:, cs_], rhs=idb[:], start=False, stop=True),
                             reads=['mneg', 'idb'], writes=[plk])

                def s2(ch):
                    pl = ps[ch % 2]; plk = f'ps{ch % 2}'
                    pt = PT[ch % 2]; ptk = f'PT{ch % 2}'
                    vb, vk = vbs[ch // 4]
                    cc = ch % 4
                    P.op('act', lambda e, pl=pl, pt=pt: e.activation(pt[:], pl[:], AF.Exp, scale=SCALE), reads=[plk], writes=[ptk])
                    P.op('pe', lambda e, pt=pt, ch=ch: e.matmul(ps[2][:, :], lhsT=ones_b[:, :], rhs=pt[:], start=(ch == 0), stop=(ch == NK - 1)),
                         reads=[ptk, 'ones_b'], writes=['ps2'])
                    for h in range(4):
                        P.op('pe', lambda e, pt=pt, h=h, ch=ch, vb=vb, cc=cc: e.matmul(
                            ps[3 + h][:, 0:128], lhsT=vb[:, cc, hsl[h]], rhs=pt[:, hsl[h]], start=(ch == 0), stop=(ch == NK - 1)),
                             reads=[ptk, vk], writes=[f'ps{3 + h}'])

                for n in range(NK + 1):
                    if n < NK:
                        s1(n)
                    if n >= 1:
                        s2(n - 1)
                    yield
                P.op('dve', lambda e: e.reciprocal(rec[:], ps[2][:, :]), reads=['ps2'], writes=['rec'])
                ot = oat[i % 2]; otk = f'oat{i % 2}'
                for h in range(4):
                    P.op('dve', lambda e, h=h, ot=ot: e.tensor_tensor(ot[:, hsl[h]], ps[3 + h][:, 0:128], rec[:, hsl[h]], op=ALU.mult),
                         reads=[f'ps{3 + h}', 'rec'], writes=[otk])
                P.dma('sp', yaT[:, i * 128:(i + 1) * 128].rearrange("(h d) t -> d h t", d=128), ot[:].rearrange("p (h t) -> p h t", t=128), reads=[otk])

            a_score(0)
            for _ in a_bisect(0):
                pass
            a_mask(0)
            for i in range(nq):
                ga = a_attn(i)
                n_att = 4 * i + 5
                if i + 1 < nq:
                    a_score(i + 1)
                    gb = a_bisect(i + 1)
                    n_bis = NIT + 1
                    done_b = 0
                    for s_ in range(n_att):
                        next(ga)
                        tgt = ((s_ + 1) * n_bis) // n_att
                        while done_b < tgt:
                            next(gb); done_b += 1
                    for _ in gb:
                        pass
                for _ in ga:
                    pass
                if i + 1 < nq:
                    a_mask(i + 1)

        ns = P.emit()
        print("L2: instrs", len(P.ins), "sems", ns, "waits", P.n_waits)

T = 2048
HB = 32
EPS = 1e-6
DFF = 2816
NFF = 22


def _blend(P, out_ap, cands, ckeys, selt, okey):
    P.op('dve', lambda e: e.tensor_scalar(out_ap, cands[0], selt[:, 0:1], None, op0=ALU.mult),
         reads=[ckeys[0], 'selt'], writes=[okey])
    for c in range(1, 4):
        P.op('dve', lambda e, c=c: e.scalar_tensor_tensor(out_ap, cands[c], selt[:, c:c + 1], out_ap, op0=ALU.mult, op1=ALU.add),
             reads=[ckeys[c], 'selt', okey], writes=[okey])


def build_l3a(nc, IO, pfx):
    o_f32, GT1, sel_d, yaT, ydT, xT = IO['o_f32'], IO['GT1'], IO['sel'], IO['yaT'], IO['ydT'], IO['xT']
    wo, wm, sm, xmT, tail2 = IO['wo'], IO['wm'], IO['sm3a'], IO['xm'], IO['tail2']
    gateT = o_f32[2560:6656, :]
    P = Prog(nc)
    es = contextlib.ExitStack()
    sb = lambda name, shape, dt: es.enter_context(nc.sbuf_tensor(pfx + name, shape, dt))
    pst = lambda name, shape, dt=F32: es.enter_context(nc.psum_tensor(pfx + name, shape, dt))
    W = 512
    with es:
        wob = [sb(f"wob{i}", [128, 4, 1024], BF16) for i in range(4)]
        wmb = sb("wmb", [128, 8, 1024], BF16)
        wst = sb("wst", [128, 4, 1024], F32)
        smt = sb("smt", [128, 192], F32)
        selt = sb("selt", [128, 4], F32)
        ones = sb("ones", [128, 128], F32)
        hh = [sb(f"hh{i}", [128, 4, 4, 160], F32) for i in range(2)]
        cand = [sb(f"cand{i}", [128, 4, 4, 32], F32) for i in range(4)]
        acc = sb("acc", [128, 4, W], F32)
        xc = sb("xc", [128, 4, W], F32)
        sq = sb("sq", [128, 4, W], F32)
        gbb = sb("gbb", [128, 4, W], F32)
        brs = [[sb(f"br{i}_{p}", [128, 4, W], BF16) for i in range(4)] for p in range(2)]
        rln = sb("rln", [128, W], F32)
        gt_ = [sb(f"gt{i}", [128, W], F32) for i in range(2)]
        mg = sb("mg", [128, W], F32)
        mgb = sb("mgb", [128, 8, W], BF16)
        xt = sb("xt", [128, 8, W], F32)
        ot = sb("ot", [128, 8, W], F32)
        ps = [pst(f"ps{i}", [128, 512]) for i in range(8)]

        P.dma('sp', smt[:], sm, writes=['smt'])
        P.dma('sp', selt[:], sel_d, writes=['selt'])
        P.op('dve', lambda e: e.memset(ones[:], 1.0 / 512), writes=['ones'])
        for i in range(4):
            P.dma('sp', wst[:], wo[i].rearrange("(c p) n -> p c n", p=128), writes=['wst'])
            P.op('pool' if i % 2 else 'dve', lambda e, i=i: e.tensor_copy(wob[i][:], wst[:]), reads=['wst'], writes=[f'wob{i}'])
        for hf in range(2):
            P.dma('sp', wst[:], wm[hf * 512:(hf + 1) * 512, :].rearrange("(c p) n -> p c n", p=128), writes=['wst'])
            P.op('pool' if hf else 'dve', lambda e, hf=hf: e.tensor_copy(wmb[:, hf * 4:(hf + 1) * 4, :], wst[:]), reads=['wst'], writes=['wmb'])

        def load_haloed(dst, dkey, row_main, row_tail, tl):
            u0 = tl * W
            for ch in range(4):
                P.dma('sp' if ch % 2 == 0 else 'act', dst[:, ch, :, 32:160],
                      o_f32[row_main + ch * 128:row_main + (ch + 1) * 128, u0:u0 + W].rearrange("p (b t) -> p b t", t=128),
                      writes=[dkey])
            q0 = row_tail // 256
            for r in range(4):
                if r == 3 and tl == 0:
                    P.op('pool', lambda e: e.memset(cand[3][:], 0.0), writes=['cand3'])
                for hf in range(2):
                    src = GT1[q0 + hf][r * 256:(r + 1) * 256, :]
                    if r < 3:
                        P.dma('sp' if (r + hf) % 2 == 0 else 'act', cand[r][:, 2 * hf:2 * hf + 2, :, :],
                              src[:, tl * 128:(tl + 1) * 128].rearrange("(c p) (b k) -> p c b k", p=128, k=32), writes=[f'cand{r}'])
                    elif tl == 0:
                        P.dma('act', cand[3][:, 2 * hf:2 * hf + 2, 1:4, :],
                              src[:, 0:96].rearrange("(c p) (b k) -> p c b k", p=128, k=32), writes=['cand3'])
                    else:
                        P.dma('act', cand[3][:, 2 * hf:2 * hf + 2, :, :],
                              src[:, tl * 128 - 32:tl * 128 + 96].rearrange("(c p) (b k) -> p c b k", p=128, k=32), writes=['cand3'])
            _blend(P, dst[:].rearrange("p c b k -> p (c b) k")[:, :, 0:32], [c_[:].rearrange("p c b k -> p (c b) k") for c_ in cand],
                   [f'cand{r}' for r in range(4)], selt, dkey)

        def stage_x(tl):
            u0 = tl * W
            br = brs[tl % 2]
            bk = [f'br{i}_{tl % 2}' for i in range(4)]
            ga, gb_ = hh[0], hh[1]
            load_haloed(ga, 'hh0', 0, 0, tl)
            load_haloed(gb_, 'hh1', 512, 512, tl)
            P.op('act', lambda e: e.activation(gb_[:], gb_[:], AF.Sigmoid), reads=['hh1'], writes=['hh1'])
            P.op('pool', lambda e: e.tensor_tensor(ga[:], ga[:], gb_[:], op=ALU.mult), reads=['hh0', 'hh1'], writes=['hh0'])
            for ch in range(4):
                ak = f'acc{ch}'
                av_ = acc[:, ch, :].rearrange("p (b t) -> p b t", t=128)
                P.op('dve', lambda e, ch=ch, av_=av_: e.tensor_scalar(av_, ga[:, ch, :, 2:130], smt[:, 32 + ch * 31:33 + ch * 31],
                                                                    smt[:, 156 + ch:157 + ch], op0=ALU.mult, op1=ALU.add),
                     reads=['hh0', 'smt'], writes=[ak])
                for k in range(1, 31):
                    P.op('dve', lambda e, ch=ch, k=k, av_=av_: e.scalar_tensor_tensor(
                        av_, ga[:, ch, :, 2 + k:130 + k], smt[:, 32 + ch * 31 + k:33 + ch * 31 + k], av_,
                        op0=ALU.mult, op1=ALU.add), reads=['hh0', 'smt', ak], writes=[ak])
            aks = [f'acc{ch}' for ch in range(4)]
            for ch in range(4):
                P.op('pe', lambda e, ch=ch: e.matmul(ps[0][:], lhsT=ones[:], rhs=acc[:, ch, :], start=(ch == 0), stop=(ch == 3)),
                     reads=['ones'] + aks, writes=['ps0'])
            for ch in range(4):
                P.op('dve', lambda e, ch=ch: e.tensor_tensor(xc[:, ch, :], acc[:, ch, :], ps[0][:], op=ALU.subtract),
                     reads=aks + ['ps0'], writes=['xc'])
            P.op('act', lambda e: e.activation(sq[:], xc[:], AF.Square), reads=['xc'], writes=['sq'])
            for ch in range(4):
                P.op('pe', lambda e, ch=ch: e.matmul(ps[1][:], lhsT=ones[:], rhs=sq[:, ch, :], start=(ch == 0), stop=(ch == 3)),
                     reads=['ones', 'sq'], writes=['ps1'])
            P.op('dve', lambda e: e.tensor_scalar(rln[:], ps[1][:], 1.0, EPS, op0=ALU.mult, op1=ALU.add), reads=['ps1'], writes=['rln'])
            P.op('act', lambda e: e.activation(rln[:], rln[:], AF.Sqrt), reads=['rln'], writes=['rln'])
            P.op('dve', lambda e: e.reciprocal(rln[:], rln[:]), reads=['rln'], writes=['rln'])
            for ch in range(4):
                P.op('dve', lambda e, ch=ch: e.tensor_tensor(xc[:, ch, :], xc[:, ch, :], rln[:], op=ALU.mult),
                     reads=['xc', 'rln'], writes=['xc'])
                P.op('dve', lambda e, ch=ch: e.tensor_scalar(xc[:, ch, :], xc[:, ch, :], smt[:, 160 + ch:161 + ch],
                                                             smt[:, 164 + ch:165 + ch], op0=ALU.mult, op1=ALU.add),
                     reads=['xc', 'smt'], writes=['xc'])
            P.op('act', lambda e: e.activation(br[1][:], xc[:], AF.Silu), reads=['xc'], writes=[bk[1]])
            gc, xcc = hh[0], hh[1]
            load_haloed(gc, 'hh0', 1024 + 512, 1024, tl)
            load_haloed(xcc, 'hh1', 1024 + 1024, 1536, tl)
            P.dma('sp', gbb[:], o_f32[1024:1536, u0:u0 + W].rearrange("(c p) t -> p c t", p=128), writes=['gbb'])
            P.op('pool', lambda e: e.tensor_tensor(gc[:], gc[:], xcc[:], op=ALU.mult), reads=['hh0', 'hh1'], writes=['hh0'])
            for ch in range(4):
                ak = f'acc{ch}'
                av_ = acc[:, ch, :].rearrange("p (b t) -> p b t", t=128)
                P.op('dve', lambda e, ch=ch, av_=av_: e.tensor_scalar(av_, gc[:, ch, :, 30:158], smt[:, 168 + ch * 3:169 + ch * 3],
                                                                    None, op0=ALU.mult), reads=['hh0', 'smt'], writes=[ak])
                for k in (1, 2):
                    P.op('dve', lambda e, ch=ch, k=k, av_=av_: e.scalar_tensor_tensor(
                        av_, gc[:, ch, :, 30 + k:158 + k], smt[:, 168 + ch * 3 + k:169 + ch * 3 + k], av_,
                        op0=ALU.mult, op1=ALU.add), reads=['hh0', 'smt', ak], writes=[ak])
                P.op('pool', lambda e, ch=ch: e.tensor_tensor(br[2][:, ch, :], acc[:, ch, :], gbb[:, ch, :], op=ALU.mult),
                     reads=[ak, 'gbb'], writes=[bk[2]])
            P.dma('sp', xc[:], yaT[:, u0:u0 + W].rearrange("(c p) t -> p c t", p=128), writes=['xc'])
            P.op('act', lambda e: e.copy(br[0][:], xc[:]), reads=['xc'], writes=[bk[0]])
            P.dma('act', sq[:], ydT[:, u0:u0 + W].rearrange("(c p) t -> p c t", p=128), writes=['sq'])
            P.op('act', lambda e: e.copy(br[3][:], sq[:]), reads=['sq'], writes=[bk[3]])
        def stage_y(tl):
            u0 = tl * W
            br = brs[tl % 2]
            bk = [f'br{i}_{tl % 2}' for i in range(4)]
            P.dma('sp', xt[:], xT[:, u0:u0 + W].rearrange("(c p) t -> p c t", p=128), writes=['xt'])
            gi = 0
            for oc in range(8):
                ocs = slice(oc * 128, (oc + 1) * 128)
                for i in range(4):
                    pb = ps[2 + (gi % 4)]; pk = f'ps{2 + gi % 4}'
                    g_ = gt_[gi % 2]; gk = f'gt{gi % 2}'; gi += 1
                    P.dma('act' if gi % 2 else 'sp', g_[:], gateT[i * 1024 + oc * 128:i * 1024 + (oc + 1) * 128, u0:u0 + W], writes=[gk])
                    P.op('act', lambda e, g_=g_, i=i, oc=oc: e.activation(g_[:], g_[:], AF.Sigmoid, bias=smt[:, i * 8 + oc:i * 8 + oc + 1]),
                         reads=[gk, 'smt'], writes=[gk])
                    for kc in range(4):
                        P.op('pe', lambda e, pb=pb, i=i, kc=kc, ocs=ocs: e.matmul(pb[:], lhsT=wob[i][:, kc, ocs], rhs=br[i][:, kc, :],
                                                                                 start=(kc == 0), stop=(kc == 3)),
                             reads=[f'wob{i}', bk[i]], writes=[pk])
                    if i == 0:
                        P.op('dve', lambda e, pb=pb, g_=g_: e.tensor_tensor(mg[:], pb[:], g_[:], op=ALU.mult), reads=[pk, gk], writes=['mg'])
                    else:
                        P.op('dve', lambda e, pb=pb, g_=g_: e.tensor_tensor(g_[:], pb[:], g_[:], op=ALU.mult), reads=[pk, gk], writes=[gk])
                        if i < 3:
                            P.op('pool', lambda e, g_=g_: e.tensor_tensor(mg[:], mg[:], g_[:], op=ALU.add), reads=['mg', gk], writes=['mg'])
                        else:
                            P.op('pool', lambda e, g_=g_, oc=oc: e.tensor_tensor(mgb[:, oc, :], mg[:], g_[:], op=ALU.add),
                                 reads=['mg', gk], writes=[f'mgb{oc}'])
            mks = [f'mgb{oc}' for oc in range(8)]
            for oc in range(8):
                ocs = slice(oc * 128, (oc + 1) * 128)
                pb = ps[6 + oc % 2]; pk = f'ps{6 + oc % 2}'
                for kc in range(8):
                    P.op('pe', lambda e, pb=pb, kc=kc, ocs=ocs: e.matmul(pb[:], lhsT=wmb[:, kc, ocs], rhs=mgb[:, kc, :],
                                                                       start=(kc == 0), stop=(kc == 7)),
                         reads=['wmb'] + mks, writes=[pk])
                P.op('dve', lambda e, pb=pb, oc=oc: e.tensor_tensor(ot[:, oc, :], pb[:], xt[:, oc, :], op=ALU.add),
                     reads=[pk, 'xt'], writes=['ot'])
            P.dma('sp', xmT[:, u0:u0 + W].rearrange("(c p) t -> p c t", p=128), ot[:], reads=['ot'], writes=['xm'])
        stage_x(0)
        for tl in range(4):
            if tl + 1 < 4:
                stage_x(tl + 1)
            stage_y(tl)
        for q in range(4):
            P.dma('sp', tail2[q * 256:(q + 1) * 256, :].rearrange("r (b k) -> r b k", k=2),
                  xmT[q * 256:(q + 1) * 256, :].rearrange("r (b t) -> r b t", t=128)[:, :, 126:128], reads=['xm'])
        ns = P.emit()
        print("L3a: instrs", len(P.ins), "sems", ns, "waits", P.n_waits)


def build_l3b(nc, IO, pfx):
    xmT, GT2, sel_d, wg, wu, wd, sm, xoT = IO['xm'], IO['GT2'], IO['sel'], IO['wg'], IO['wu'], IO['wd'], IO['sm3b'], IO['xo']
    TT = 1024
    NB = 8
    NC_ = NB * 130
    P = Prog(nc)
    es = contextlib.ExitStack()
    sb = lambda name, shape, dt: es.enter_context(nc.sbuf_tensor(pfx + name, shape, dt))
    pst = lambda name, shape, dt=F32: es.enter_context(nc.psum_tensor(pfx + name, shape, dt))
    with es:
        smt = sb("smt", [128, 80], F32)
        selt = sb("selt", [128, 4], F32)
        ones = sb("ones", [128, 128], F32)
        xm = sb("xm", [128, 8, NB, 130], F32)
        cand = [sb(f"cand{i}", [128, 8, NB, 2], F32) for i in range(4)]
        sqt = sb("sqt", [128, NC_], F32)
        rs = sb("rs", [128, NC_], F32)
        h2 = sb("h2", [128, 8, NB, 130], BF16)
        prod = sb("prod", [128, NFF, TT], BF16)
        wgs = [sb(f"wgs{i}", [128, 8, 128], F32) for i in range(2)]
        wus = [sb(f"wus{i}", [128, 8, 128], F32) for i in range(2)]
        wgb = [sb(f"wgb{i}", [128, 8, 128], BF16) for i in range(2)]
        wub = [sb(f"wub{i}", [128, 8, 128], BF16) for i in range(2)]
        gtl = [sb(f"gtl{i}", [128, NB, 130], F32) for i in range(2)]
        av = [sb(f"av{i}", [128, TT], F32) for i in range(2)]
        wds = [sb(f"wds{i}", [128, 512], F32) for i in range(2)]
        wdb = [sb(f"wdb{i}", [128, 512], BF16) for i in range(2)]
        ot = [sb(f"ot{i}", [128, 512], F32) for i in range(2)]
        ps = [pst(f"ps{i}", [128, 512]) for i in range(8)]
        P.cc('pool', IO['cc_l3b'][0], IO['cc_l3b'][1], writes=['GT2'])
        P.dma('sp', smt[:], sm, writes=['smt'])
        P.dma('sp', selt[:], sel_d, writes=['selt'])
        P.op('dve', lambda e: e.memset(ones[:], 1.0 / 1024), writes=['ones'])
        wgv = wg.rearrange("(c p) n -> p c n", p=128)
        wuv = wu.rearrange("(c p) n -> p c n", p=128)
        xmf = xm[:].rearrange("p c b k -> p c (b k)")
        h2f = h2[:].rearrange("p c b k -> p c (b k)")
        for tl in range(2):
            u0 = tl * TT
            for c in range(8):
                P.dma('sp' if c % 2 == 0 else 'act', xm[:, c, :, 2:130],
                      xmT[c * 128:(c + 1) * 128, u0:u0 + TT].rearrange("p (b t) -> p b t", t=128), writes=['xm'])
            for r in range(3):
                P.dma('sp', cand[r][:], GT2[r * 1024:(r + 1) * 1024, tl * 16:(tl + 1) * 16].rearrange("(c p) (b k) -> p c b k", p=128, k=2),
                      reads=['GT2'], writes=[f'cand{r}'])
            if tl == 0:
                P.op('pool', lambda e: e.memset(cand[3][:], 0.0), writes=['cand3'])
                P.dma('act', cand[3][:, :, 1:8, :], GT2[3 * 1024:4 * 1024, 0:14].rearrange("(c p) (b k) -> p c b k", p=128, k=2), reads=['GT2'], writes=['cand3'])
            else:
                P.dma('act', cand[3][:], GT2[3 * 1024:4 * 1024, 14:30].rearrange("(c p) (b k) -> p c b k", p=128, k=2), reads=['GT2'], writes=['cand3'])
            _blend(P, xm[:].rearrange("p c b k -> p (c b) k")[:, :, 0:2], [c_[:].rearrange("p c b k -> p (c b) k") for c_ in cand],
                   [f'cand{r}' for r in range(4)], selt, 'xm')
            col_groups = [(0, 512), (512, 1024), (1024, NC_)]
            for (a, b) in col_groups:
                n = b - a
                for c in range(8):
                    P.op('act', lambda e, c=c, a=a, b=b: e.activation(sqt[:, a:b], xmf[:, c, a:b], AF.Square), reads=['xm'], writes=['sqt'])
                    P.op('pe', lambda e, c=c, a=a, b=b, n=n: e.matmul(ps[0][:, 0:n], lhsT=ones[:], rhs=sqt[:, a:b], start=(c == 0), stop=(c == 7)),
                         reads=['ones', 'sqt'], writes=['ps0'])
                P.op('dve', lambda e, a=a, b=b, n=n: e.tensor_scalar(rs[:, a:b], ps[0][:, 0:n], 1.0, EPS, op0=ALU.mult, op1=ALU.add),
                     reads=['ps0'], writes=['rs'])
            P.op('act', lambda e: e.activation(rs[:], rs[:], AF.Sqrt), reads=['rs'], writes=['rs'])
            P.op('dve', lambda e: e.reciprocal(rs[:], rs[:]), reads=['rs'], writes=['rs'])
            for c in range(8):
                P.op('dve', lambda e, c=c: e.scalar_tensor_tensor(h2f[:, c, :], xmf[:, c, :], smt[:, c:c + 1], rs[:],
                                                                 op0=ALU.mult, op1=ALU.mult),
                     reads=['xm', 'smt', 'rs'], writes=[f'h2_{c}'])
            hk = [f'h2_{c}' for c in range(8)]
            for f in range(NFF):
                b = f % 2
                fs = slice(f * 128, (f + 1) * 128)
                P.dma('sp', wgs[b][:], wgv[:, :, fs], writes=[f'wgs{b}'])
                P.dma('act', wus[b][:], wuv[:, :, fs], writes=[f'wus{b}'])
                P.op('dve', lambda e, b=b: e.tensor_copy(wgb[b][:], wgs[b][:]), reads=[f'wgs{b}'], writes=[f'wgb{b}'])
                P.op('pool', lambda e, b=b: e.tensor_copy(wub[b][:], wus[b][:]), reads=[f'wus{b}'], writes=[f'wub{b}'])
                g_ = gtl[b]; gk = f'gtl{b}'
                gf = g_[:].rearrange("p b k -> p (b k)")
                a_ = av[b]; ak = f'av{b}'
                a3 = a_[:].rearrange("p (b t) -> p b t", t=128)
                for gi_, (a, bb) in enumerate(col_groups):
                    n = bb - a
                    pb = ps[1 + gi_]; pk = f'ps{1 + gi_}'
                    for c in range(8):
                        P.op('pe', lambda e, pb=pb, c=c, a=a, bb=bb, n=n, b=b: e.matmul(pb[:, 0:n], lhsT=wgb[b][:, c, :], rhs=h2f[:, c, a:bb],
                                                                                     start=(c == 0), stop=(c == 7)),
                             reads=[f'wgb{b}'] + hk, writes=[pk])
                    P.op('act', lambda e, pb=pb, a=a, bb=bb, n=n, gf=gf: e.copy(gf[:, a:bb], pb[:, 0:n]), reads=[pk], writes=[gk])
                P.op('dve', lambda e, f=f, g_=g_, a3=a3: e.tensor_scalar(a3, g_[:, :, 0:128], smt[:, 8 + 3 * f:9 + 3 * f], None, op0=ALU.mult),
                     reads=[gk, 'smt'], writes=[ak])
                for k in (1, 2):
                    P.op('dve', lambda e, f=f, g_=g_, a3=a3, k=k: e.scalar_tensor_tensor(
                        a3, g_[:, :, k:k + 128], smt[:, 8 + 3 * f + k:9 + 3 * f + k], a3, op0=ALU.mult, op1=ALU.add),
                         reads=[gk, 'smt', ak], writes=[ak])
                P.op('act', lambda e, a_=a_: e.activation(a_[:], a_[:], AF.Silu), reads=[ak], writes=[ak])
                for gi_ in range(2):
                    pb = ps[4 + gi_]; pk = f'ps{4 + gi_}'
                    for c in range(8):
                        P.op('pe', lambda e, pb=pb, c=c, gi_=gi_, b=b: e.matmul(pb[:], lhsT=wub[b][:, c, :], rhs=h2[:, c, gi_ * 4:(gi_ + 1) * 4, 2:130],
                                                                              start=(c == 0), stop=(c == 7)),
                             reads=[f'wub{b}'] + hk, writes=[pk])
                    P.op('dve', lambda e, pb=pb, gi_=gi_, f=f, a_=a_: e.tensor_tensor(
                        prod[:, f, gi_ * 512:(gi_ + 1) * 512], a_[:, gi_ * 512:(gi_ + 1) * 512], pb[:], op=ALU.mult), reads=[pk, ak], writes=[f'prod{f}'])
            pks = [f'prod{f}' for f in range(NFF)]
            di = 0
            for og in range(2):
                for f in range(NFF):
                    b = di % 2; di += 1
                    P.dma('sp' if di % 2 else 'act', wds[b][:], wd[f * 128:(f + 1) * 128, og * 512:(og + 1) * 512], writes=[f'wds{b}'])
                    P.op('pool' if di % 2 else 'dve', lambda e, b=b: e.tensor_copy(wdb[b][:], wds[b][:]), reads=[f'wds{b}'], writes=[f'wdb{b}'])
                    for th in range(2):
                        tsl = slice(th * 512, (th + 1) * 512)
                        for o4 in range(4):
                            pi = th * 4 + o4
                            P.op('pe', lambda e, o4=o4, pi=pi, b=b, f=f, tsl=tsl: e.matmul(
                                ps[pi][:], lhsT=wdb[b][:, o4 * 128:(o4 + 1) * 128], rhs=prod[:, f, tsl],
                                start=(f == 0), stop=(f == NFF - 1)),
                                 reads=[f'wdb{b}'] + pks, writes=[f'ps{pi}'])
                for th in range(2):
                    for o4 in range(4):
                        pi = th * 4 + o4
                        oc = og * 4 + o4
                        o_ = ot[o4 % 2]; ok = f'ot{o4 % 2}'
                        P.op('dve', lambda e, pi=pi, oc=oc, o_=o_, th=th: e.tensor_tensor(
                            o_[:].rearrange("p (b t) -> p b t", t=128), ps[pi][:].rearrange("p (b t) -> p b t", t=128),
                            xm[:, oc, th * 4:(th + 1) * 4, 2:130], op=ALU.add),
                             reads=[f'ps{pi}', 'xm'], writes=[ok])
                        P.dma('sp' if o4 % 2 == 0 else 'act', xoT[oc * 128:(oc + 1) * 128, u0 + th * 512:u0 + (th + 1) * 512], o_[:], reads=[ok])
        ns = P.emit()
        print("L3b: instrs", len(P.ins), "sems", ns, "waits", P.n_waits)


RG = [[0, 1, 2, 3], [4, 5, 6, 7]]


def _allgather(nc, pairs):
    s = nc.alloc_semaphore(name=nc.make_name("cc_sem", True))
    with nc.Block() as block:
        @block.gpsimd
        def _(g):
            for (src, dst) in pairs:
                g.collective_compute("AllGather", mybir.AluOpType.bypass, replica_groups=RG,
                                     ins=[src.ap().opt()], outs=[dst.ap().opt()]).then_inc(s)
            g.wait_ge(s, len(pairs))
    nc.clear_and_free_semaphores([s])
    nc.all_engine_barrier()


def build_fused(nlayers=2, debug=False):
    nc = bass.Bass("TRN2", target_bir_lowering=False)
    ext = lambda name, shape, dt: nc.dram_tensor(name, shape, dt, kind="ExternalInput").ap()
    xT = ext("xT", [1024, 2048], F32)
    pos = ext("pos", [128, 2048], I32)
    w_in = ext("w_in", [2, 1024, 10052], F32)
    gmix = ext("gmix", [2, 128, 8], F32)
    gq = ext("gq", [2, 128, 4], F32)
    cst = ext("cst", [128, 8], F32)
    rmA = ext("rmA", [128, 128], F32)
    rmI = ext("rmI", [128, 128], F32)
    negm = ext("negm", [128, 512], F32)
    mdm = ext("mdm", [128, 4, 4, 128], F32)
    su = ext("su", [128, 128], F32)
    ident = ext("ident", [128, 128], F32)
    pw = ext("pw", [128, NIT], F32)
    sel = ext("sel", [128, 4], F32)
    wo = ext("wo", [2, 4, 512, 1024], F32)
    wm = ext("wm", [2, 1024, 1024], F32)
    sm3a = ext("sm3a", [2, 128, 192], F32)
    wg = ext("wg", [2, 1024, 2816], F32)
    wu = ext("wu", [2, 1024, 2816], F32)
    wd = ext("wd", [2, 2816, 1024], F32)
    sm3b = ext("sm3b", [2, 128, 80], F32)
    out = nc.dram_tensor("out", [1024, 2048], F32, kind="ExternalOutput").ap()
    dk = dict(kind="ExternalOutput") if debug else {}
    o_q = nc.dram_tensor("o_q", [1280, 2048], BF16, **dk)
    o_f32 = nc.dram_tensor("o_f32", [6656, 2048], F32, **dk)
    o_iw = nc.dram_tensor("o_iw", [2048, 4], F32, **dk)
    krows = [0] + [64 + 128 * h for h in range(4)] + [576 + 128 * h for h in range(4)]
    o_k = {r0: nc.dram_tensor(f"o_k{r0}", [64 if r0 == 0 else 128, 2048], BF16) for r0 in krows}
    G_k = {r0: nc.dram_tensor(f"G_k{r0}", [4 * (64 if r0 == 0 else 128), 2048], BF16) for r0 in krows}
    o_v = [nc.dram_tensor(f"o_v{q}", [256, 1024], BF16) for q in range(8)]
    G_v = [nc.dram_tensor(f"G_v{q}", [4 * 256, 1024], BF16) for q in range(8)]
    tail1 = [nc.dram_tensor(f"tail1_{q}", [256, 512], F32) for q in range(8)]
    GT1 = [nc.dram_tensor(f"GT1_{q}", [4 * 256, 512], F32) for q in range(8)]
    yaT = nc.dram_tensor("yaT", [512, 2048], F32, **dk)
    ydT = nc.dram_tensor("ydT", [512, 2048], F32, **dk)
    xm = nc.dram_tensor("xm", [1024, 2048], F32, **dk)
    tail2 = nc.dram_tensor("tail2", [1024, 32], F32)
    GT2 = nc.dram_tensor("GT2", [4 * 1024, 32], F32)
    xo0 = nc.dram_tensor("xo0", [1024, 2048], F32)
    for l in range(nlayers):
        x_in = xT if l == 0 else xo0.ap()
        IOD = dict(xT=x_in, pos=pos, w=w_in[l], gmix=gmix[l], gq=gq[l], cst=cst, rmA=rmA, rmI=rmI,
                 o_q=o_q.ap(), o_k={k: v.ap() for k, v in o_k.items()}, o_f32=o_f32.ap(), o_v=[v.ap() for v in o_v], o_iw=o_iw.ap(), tail1=[v.ap() for v in tail1],
                 G_k={k: v.ap() for k, v in G_k.items()}, G_v=[v.ap() for v in G_v], GT1=[v.ap() for v in GT1], negm=negm, mdm=mdm, su=su, ident=ident, pw=pw, sel=sel,
                 yaT=yaT.ap(), ydT=ydT.ap(), wo=wo[l], wm=wm[l], sm3a=sm3a[l], xm=xm.ap(), tail2=tail2.ap(), GT2=GT2.ap(),
                 wg=wg[l], wu=wu[l], wd=wd[l], sm3b=sm3b[l], xo=(xo0.ap() if l < nlayers - 1 else out))
        IOD['cc_l2'] = ([(o_k[r0], G_k[r0], f'Gk{r0}') for r0 in krows[5:]] + [(o_v[q], G_v[q], f'Gv{q}') for q in range(8)]
                        + [(o_k[r0], G_k[r0], f'Gk{r0}') for r0 in krows[:5]] + [(tail1[q], GT1[q], f'GT1_{q}') for q in range(8)])
        IOD['cc_l3b'] = (tail2, GT2)
        build_l1(nc, IOD, f"a{l}_")
        build_l2(nc, IOD, f"b{l}_")
        build_l3a(nc, IOD, f"c{l}_")
        build_l3b(nc, IOD, f"d{l}_")
    return nc


_NC = {}


def _stripe(a, j):
    return a.reshape((64, 128) + a.shape[1:])[j::4].reshape((2048,) + a.shape[1:])


def kernel(_nlayers=2, _debug=False, **inputs):
    inp = {k: np.asarray(v) for k, v in inputs.items()}
    if 'nc' not in _NC:
        _NC['nc'] = build_fused(_nlayers, _debug)
    nc = _NC['nc']
    cst, RmA, RmI = l1_consts()
    su, ident, pw = l2_consts()
    gq = np.zeros((2, 128, 4), np.float32)
    gq[:, :, 0] = inp['g_qa']; gq[:, :, 1] = inp['g_ka']; gq[:, :64, 2] = inp['g_kidx']
    gmix = np.ascontiguousarray(inp['g_mix'].reshape(2, 8, 128).transpose(0, 2, 1))
    sm3a = np.zeros((2, 128, 192), np.float32)
    sm3b = np.zeros((2, 128, 80), np.float32)
    for l in range(2):
        sm3a[l, :, 0:32] = inp['b_gate'][l].reshape(32, 128).T
        sm3a[l, :, 32:156] = inp['cb_conv_w'][l].T.reshape(4, 128, 31).transpose(1, 0, 2).reshape(128, 124)
        sm3a[l, :, 156:160] = inp['cb_conv_b'][l].reshape(4, 128).T
        sm3a[l, :, 160:164] = inp['cb_ln_g'][l].reshape(4, 128).T
        sm3a[l, :, 164:168] = inp['cb_ln_b'][l].reshape(4, 128).T
        sm3a[l, :, 168:180] = inp['cc_conv_w'][l].T.reshape(4, 128, 3).transpose(1, 0, 2).reshape(128, 12)
        sm3b[l, :, 0:8] = inp['g_ffn'][l].reshape(8, 128).T
        sm3b[l, :, 8:74] = inp['ffn_conv_w'][l].T.reshape(22, 128, 3).transpose(1, 0, 2).reshape(128, 66)
    wo = np.ascontiguousarray(np.stack([inp['w_oa'], inp['w_ob'], inp['w_oc'], inp['w_od']], axis=1))
    shared = dict(w_in=np.ascontiguousarray(inp['w_in']), gmix=gmix, gq=gq, cst=cst, rmA=RmA, rmI=RmI, su=su, ident=ident, pw=pw,
                  wo=wo, wm=np.ascontiguousarray(inp['w_merge']), sm3a=sm3a, wg=np.ascontiguousarray(inp['w_ffn_gate']),
                  wu=np.ascontiguousarray(inp['w_ffn_up']), wd=np.ascontiguousarray(inp['w_ffn_down']), sm3b=sm3b)
    maps = []
    for c in range(8):
        b, j = c // 4, c % 4
        neg, md = l2_masks(j)
        sel = np.zeros((128, 4), np.float32)
        sel[:, (j - 1) % 4] = 1.0
        m = dict(shared)
        m.update(xT=np.ascontiguousarray(_stripe(inp['x'][b], j).T),
                 pos=np.ascontiguousarray(np.broadcast_to(_stripe(inp['positions'][b], j)[None, :], (128, 2048)).astype(np.int32)),
                 negm=neg, mdm=md, sel=sel)
        maps.append(m)
    res = run_bass_kernel_spmd(nc, maps, core_ids=list(range(8)))
    if _debug:
        _NC['res'] = res.results
    out = np.zeros((2, 64, 128, 1024), np.float32)
    for c in range(8):
        b, j = c // 4, c % 4
        out[b, j::4] = np.asarray(res.results[c]['out']).T.reshape(16, 128, 1024)
    return out.reshape(2, 8192, 1024)
```
